# Optimizing a Trainium2 kernel written in Bass

```python
import jax, jax.numpy as jnp
from jax import lax
import numpy as np

D_MODEL = 1024
BATCH = 8
SEQ = 4096
DEPTH = 1

HEAD_DIM = 64
SB_HEADS = 6
NSA_HEADS = 6
NSA_KV_GROUPS = 2
NSA_GROUP = NSA_HEADS // NSA_KV_GROUPS
MEM_HEADS = 4
MEM_TOKENS = 256
N_BRANCHES = 3
SB_W = SB_HEADS * HEAD_DIM
NSA_W = NSA_HEADS * HEAD_DIM
NSA_KV_W = NSA_KV_GROUPS * HEAD_DIM
MEM_W = MEM_HEADS * HEAD_DIM
IN_WIDTHS = (SB_W, SB_W, SB_W, NSA_W, 6 * NSA_KV_W, NSA_HEADS * 3, MEM_W, N_BRANCHES * D_MODEL)
IN_DIM = sum(IN_WIDTHS)
SB_Q_BLOCK = 128
NSA_Q_BLOCK = 64
CMP_LEN = 32
CMP_STRIDE = 16
SEL_BLOCK = 64
N_SELECT = 16
WINDOW = 512
FORCED_SCORE = 1e4
PEER_HEADS = 8
PEER_N_KEYS = 128
PEER_N_EXPERTS = PEER_N_KEYS * PEER_N_KEYS
PEER_QUERY_DIM = 256
PEER_TOPK = 16
PEER_T_BLOCK = 32
RMS_EPS = 1e-6
NEG_INF = -1e30

kernel_name = 'hybrid_sb_nsa_mem_peer_block'


def rmsnorm(x, g):
    xf = x.astype(jnp.float32)
    y = xf * lax.rsqrt(jnp.mean(xf * xf, axis=-1, keepdims=True) + RMS_EPS)
    return (y * g.astype(jnp.float32)).astype(x.dtype)


def alibi_slopes(n):
    return jnp.asarray([2.0 ** (-8.0 * (h + 1) / n) for h in range(n)], dtype=jnp.float32)


def split_offsets():
    return [int(o) for o in np.cumsum(IN_WIDTHS)[:-1]]


def stick_breaking_attention(q, k, v):
    B, H, T, dh = q.shape
    nblk = T // SB_Q_BLOCK
    scale = dh ** -0.5
    qb = q.reshape(B, H, nblk, SB_Q_BLOCK, dh).transpose(2, 0, 1, 3, 4)
    kpos = jnp.arange(T)

    def block(args):
        i, qi = args
        qpos = i * SB_Q_BLOCK + jnp.arange(SB_Q_BLOCK)
        z = jnp.einsum('bhqd,bhkd->bhqk', qi, k).astype(jnp.float32) * scale
        before = kpos[None, :] < qpos[:, None]
        log_keep = jnp.where(before, jax.nn.log_sigmoid(-z), 0.0)
        rev = lax.cumsum(log_keep, axis=3, reverse=True)
        excl = jnp.concatenate([rev[..., 1:], jnp.zeros_like(rev[..., :1])], axis=-1)
        a = jnp.where(before, jnp.exp(jax.nn.log_sigmoid(z) + excl), 0.0)
        return jnp.einsum('bhqk,bhkd->bhqd', a.astype(v.dtype), v)

    out = lax.map(block, (jnp.arange(nblk), qb))
    return out.transpose(1, 0, 3, 2, 4).reshape(B, T, H * dh)


def nsa_attention(q, k_cmp, v_cmp, k_slc, v_slc, k_win, v_win, gates, pe_k, pe_v, w_cmp_k, w_cmp_v, slopes):
    B, T, _, dh = q.shape
    G, R = NSA_KV_GROUPS, NSA_GROUP
    scale = dh ** -0.5

    def compress(z, pe, w):
        chunks = z.reshape(B, T // CMP_STRIDE, CMP_STRIDE, G, dh)
        blocks = jnp.concatenate([chunks[:, :-1], chunks[:, 1:]], axis=2)
        blocks = blocks + pe[None, None, :, None, :]
        return jnp.einsum('bnlgd,lde->bgne', blocks, w)

    kc = compress(k_cmp, pe_k, w_cmp_k)
    vc = compress(v_cmp, pe_v, w_cmp_v)
    n_cmp = T // CMP_STRIDE - 1
    cmp_end = jnp.arange(n_cmp) * CMP_STRIDE + CMP_LEN - 1
    n_blk = T // SEL_BLOCK
    n_sel = min(N_SELECT, n_blk)
    ks = k_slc.reshape(B, n_blk, SEL_BLOCK, G, dh).transpose(0, 3, 1, 2, 4)
    vs = v_slc.reshape(B, n_blk, SEL_BLOCK, G, dh).transpose(0, 3, 1, 2, 4)
    kw = jnp.pad(k_win, ((0, 0), (WINDOW, 0), (0, 0), (0, 0))).transpose(0, 2, 1, 3)
    vw = jnp.pad(v_win, ((0, 0), (WINDOW, 0), (0, 0), (0, 0))).transpose(0, 2, 1, 3)
    nq = T // NSA_Q_BLOCK
    qg = q.reshape(B, nq, NSA_Q_BLOCK, G, R, dh).transpose(1, 0, 3, 4, 2, 5)
    gb = gates.reshape(B, nq, NSA_Q_BLOCK, G, R, 3).transpose(1, 0, 3, 4, 2, 5)
    slope = slopes.reshape(G, R)[None, :, :, None, None]
    b_idx = jnp.arange(B)[:, None, None, None]
    g_idx = jnp.arange(G)[None, :, None, None]
    blk = jnp.arange(n_blk)
    offs = jnp.arange(SEL_BLOCK)

    def block(args):
        i, qi, gi = args
        t = i * NSA_Q_BLOCK + jnp.arange(NSA_Q_BLOCK)
        s_c = jnp.einsum('bgrqd,bgnd->bgrqn', qi, kc).astype(jnp.float32) * scale
        dist_c = (t[:, None] - cmp_end[None, :]).astype(jnp.float32)
        valid_c = dist_c >= 0
        s_c = jnp.where(valid_c, s_c - slope * dist_c, NEG_INF)
        p_c = jax.nn.softmax(s_c, axis=-1)
        p_c = jnp.where(valid_c.any(-1)[:, None], p_c, 0.0)
        o_c = jnp.einsum('bgrqn,bgnd->bgrqd', p_c.astype(vc.dtype), vc)
        imp = jnp.pad(p_c.sum(axis=2), ((0, 0), (0, 0), (0, 0), (0, 1)))
        imp = imp.reshape(B, G, NSA_Q_BLOCK, n_blk, SEL_BLOCK // CMP_STRIDE).sum(-1)
        cur = t // SEL_BLOCK
        forced = (blk[None, :] == 0) | (blk[None, :] == cur[:, None]) | (blk[None, :] == cur[:, None] - 1)
        imp = jnp.where(forced, FORCED_SCORE, jnp.where(blk[None, :] <= cur[:, None], imp, -1.0))
        _, idx = lax.top_k(imp, n_sel)
        k_g = ks[b_idx, g_idx, idx]
        v_g = vs[b_idx, g_idx, idx]
        pos = idx[..., None] * SEL_BLOCK + offs
        s_s = jnp.einsum('bgrqd,bgqnld->bgrqnl', qi, k_g).astype(jnp.float32) * scale
        dist_s = (t[:, None, None] - pos).astype(jnp.float32)[:, :, None]
        s_s = jnp.where(dist_s >= 0, s_s - slope[..., None] * dist_s, NEG_INF)
        p_s = jax.nn.softmax(s_s.reshape(B, G, R, NSA_Q_BLOCK, n_sel * SEL_BLOCK), axis=-1)
        p_s = p_s.reshape(B, G, R, NSA_Q_BLOCK, n_sel, SEL_BLOCK)
        o_s = jnp.einsum('bgrqnl,bgqnld->bgrqd', p_s.astype(v_g.dtype), v_g)
        kwi = lax.dynamic_slice_in_dim(kw, i * NSA_Q_BLOCK, WINDOW + NSA_Q_BLOCK, axis=2)
        vwi = lax.dynamic_slice_in_dim(vw, i * NSA_Q_BLOCK, WINDOW + NSA_Q_BLOCK, axis=2)
        kpos = i * NSA_Q_BLOCK - WINDOW + jnp.arange(WINDOW + NSA_Q_BLOCK)
        dist_w = t[:, None] - kpos[None, :]
        valid_w = (dist_w >= 0) & (dist_w < WINDOW) & (kpos[None, :] >= 0)
        s_w = jnp.einsum('bgrqd,bgkd->bgrqk', qi, kwi).astype(jnp.float32) * scale
        s_w = jnp.where(valid_w, s_w - slope * dist_w.astype(jnp.float32), NEG_INF)
        p_w = jax.nn.softmax(s_w, axis=-1)
        o_w = jnp.einsum('bgrqk,bgkd->bgrqd', p_w.astype(vwi.dtype), vwi)
        return gi[..., 0:1] * o_c + gi[..., 1:2] * o_s + gi[..., 2:3] * o_w

    out = lax.map(block, (jnp.arange(nq), qg, gb))
    return out.transpose(1, 0, 4, 2, 3, 5).reshape(B, T, NSA_HEADS * dh)


def memory_attention(q, mk, mv):
    B, T, H, dh = q.shape
    s = jnp.einsum('bthd,bmhd->bhtm', q, mk).astype(jnp.float32) * (dh ** -0.5)
    p = jax.nn.softmax(s, axis=-1)
    o = jnp.einsum('bhtm,bmhd->bthd', p.astype(mv.dtype), mv)
    return o.reshape(B, T, H * dh)


def peer_ffn(h, w_q, subkeys, u_tab, v_tab):
    B, T, D = h.shape
    nb = T // PEER_T_BLOCK
    half = PEER_QUERY_DIM // 2
    K = PEER_TOPK
    hb = h.reshape(B, nb, PEER_T_BLOCK, D).transpose(1, 0, 2, 3)

    def block(hi):
        q = (hi @ w_q).reshape(B, PEER_T_BLOCK, PEER_HEADS, 2, half)
        s = jnp.einsum('bthpd,hpkd->bthpk', q, subkeys).astype(jnp.float32)
        s_top, i_top = lax.top_k(s, K)
        cand = s_top[..., 0, :, None] + s_top[..., 1, None, :]
        cand_idx = i_top[..., 0, :, None] * PEER_N_KEYS + i_top[..., 1, None, :]
        best, pos = lax.top_k(cand.reshape(B, PEER_T_BLOCK, PEER_HEADS, K * K), K)
        expert = jnp.take_along_axis(cand_idx.reshape(B, PEER_T_BLOCK, PEER_HEADS, K * K), pos, axis=-1)
        g = jax.nn.softmax(best, axis=-1)
        a = jax.nn.gelu(jnp.einsum('btd,bthkd->bthk', hi, u_tab[expert]).astype(jnp.float32), approximate=False)
        w = (g * a).astype(v_tab.dtype)
        return jnp.einsum('bthk,bthkd->btd', w, v_tab[expert])

    out = lax.map(block, hb)
    return out.transpose(1, 0, 2, 3).reshape(B, T, D)


def setup_inputs(seed: int = 0) -> dict:
    key = jax.random.key(seed)
    ks = jax.random.split(key, 24)
    f32 = jnp.float32
    L, D = DEPTH, D_MODEL
    half = PEER_QUERY_DIM // 2

    def nrm(k, shape, scale):
        return jax.random.normal(k, shape, f32) * scale

    return {
        'x': nrm(ks[0], (BATCH, SEQ, D), 1.0),
        'mem': nrm(ks[1], (BATCH, MEM_TOKENS, D), 1.0),
        'mix_norm_g': 1.0 + nrm(ks[2], (L, D), 0.02),
        'mem_norm_g': 1.0 + nrm(ks[3], (L, D), 0.02),
        'w_in': nrm(ks[4], (L, D, IN_DIM), D ** -0.5),
        'b_merge': nrm(ks[5], (L, N_BRANCHES * D), 0.02),
        'cmp_pe_k': nrm(ks[6], (L, CMP_LEN, HEAD_DIM), 0.02),
        'cmp_pe_v': nrm(ks[7], (L, CMP_LEN, HEAD_DIM), 0.02),
        'cmp_w_k': nrm(ks[8], (L, CMP_LEN, HEAD_DIM, HEAD_DIM), (CMP_LEN * HEAD_DIM) ** -0.5),
        'cmp_w_v': nrm(ks[9], (L, CMP_LEN, HEAD_DIM, HEAD_DIM), (CMP_LEN * HEAD_DIM) ** -0.5),
        'w_mem_kv': nrm(ks[10], (L, D, 2 * MEM_W), D ** -0.5),
        'w_sb_br': nrm(ks[11], (L, SB_W, D), SB_W ** -0.5),
        'w_nsa_br': nrm(ks[12], (L, NSA_W, D), NSA_W ** -0.5),
        'w_mem_br': nrm(ks[13], (L, MEM_W, D), MEM_W ** -0.5),
        'w_out': nrm(ks[14], (L, D, D), D ** -0.5),
        'ffn_norm_g': 1.0 + nrm(ks[15], (L, D), 0.02),
        'peer_w_q': nrm(ks[16], (L, D, PEER_HEADS * PEER_QUERY_DIM), D ** -0.5),
        'peer_subkeys': nrm(ks[17], (L, PEER_HEADS, 2, PEER_N_KEYS, half), half ** -0.5),
        'peer_u': nrm(ks[18], (L, PEER_N_EXPERTS, D), D ** -0.5),
        'peer_v': nrm(ks[19], (L, PEER_N_EXPERTS, D), 0.3),
        'final_norm_g': 1.0 + nrm(ks[20], (D,), 0.02),
    }


def reference(x, mem, mix_norm_g, mem_norm_g, w_in, b_merge, cmp_pe_k, cmp_pe_v, cmp_w_k, cmp_w_v, w_mem_kv, w_sb_br, w_nsa_br, w_mem_br, w_out, ffn_norm_g, peer_w_q, peer_subkeys, peer_u, peer_v, final_norm_g):
    B, T, D = x.shape
    M = mem.shape[1]
    G = NSA_KV_GROUPS
    slopes = alibi_slopes(NSA_HEADS)
    offsets = split_offsets()
    for l in range(DEPTH):
        h = rmsnorm(x, mix_norm_g[l])
        proj = h @ w_in[l]
        sb_q, sb_k, sb_v, nsa_q, nsa_kv, nsa_g, mem_q, merge_g = jnp.split(proj, offsets, axis=-1)
        sbh = lambda z: z.reshape(B, T, SB_HEADS, HEAD_DIM).transpose(0, 2, 1, 3)
        sb_out = stick_breaking_attention(sbh(sb_q), sbh(sb_k), sbh(sb_v))
        kv = nsa_kv.reshape(B, T, 6, G, HEAD_DIM)
        nsa_out = nsa_attention(nsa_q.reshape(B, T, NSA_HEADS, HEAD_DIM), kv[:, :, 0], kv[:, :, 1], kv[:, :, 2], kv[:, :, 3], kv[:, :, 4], kv[:, :, 5], jax.nn.sigmoid(nsa_g.reshape(B, T, NSA_HEADS, 3)), cmp_pe_k[l], cmp_pe_v[l], cmp_w_k[l], cmp_w_v[l], slopes)
        mkv = (rmsnorm(mem, mem_norm_g[l]) @ w_mem_kv[l]).reshape(B, M, 2, MEM_HEADS, HEAD_DIM)
        mem_out = memory_attention(mem_q.reshape(B, T, MEM_HEADS, HEAD_DIM), mkv[:, :, 0], mkv[:, :, 1])
        gates = jax.nn.sigmoid(merge_g + b_merge[l]).reshape(B, T, N_BRANCHES, D)
        merged = gates[:, :, 0] * (sb_out @ w_sb_br[l]) + gates[:, :, 1] * (nsa_out @ w_nsa_br[l]) + gates[:, :, 2] * (mem_out @ w_mem_br[l])
        x = x + merged @ w_out[l]
        x = x + peer_ffn(rmsnorm(x, ffn_norm_g[l]), peer_w_q[l], peer_subkeys[l], peer_u[l], peer_v[l])
    return rmsnorm(x, final_norm_g)
```

```python
import os
import sys
import numpy as np
from contextlib import ExitStack
import concourse.bass as bass
import concourse.mybir as mybir
from concourse.bass_utils import run_bass_kernel_spmd

F32 = mybir.dt.float32
BF16 = mybir.dt.bfloat16
I32 = mybir.dt.int32
U32 = mybir.dt.uint32
AF = mybir.ActivationFunctionType
ALU = mybir.AluOpType
AX = mybir.AxisListType

T = 4096
D = 1024
NT = T // 128
IN_DIM = 5650
EPS = 1e-6
SLOPES = [2.0 ** (-8.0 * (h + 1) / 6) for h in range(6)]
BIG = 30000.0

ENGS = ("pe", "act", "dve", "pool", "sp")
DMA_RING = 8


class Prog:
    def __init__(self, nc, same_engine_sync=True):
        self.nc = nc
        self.ops = {e: [] for e in ENGS}
        self.cnt = {e: 0 for e in ENGS}
        self.dma_n = {e: 0 for e in ENGS}
        self.last_w = {}
        self.readers = {}
        self.waited = {}
        self.same_engine_sync = same_engine_sync
        self.fill_vals = set()
        self.fill_regs = {}

    def _deps(self, eng, reads, writes):
        need = {}

        def add(tok):
            if tok is None:
                return
            sk, val, teng = tok
            if teng == eng and sk[0] == "c":
                if not self.same_engine_sync or eng == "pe":
                    return
            if need.get(sk, 0) < val:
                need[sk] = val

        for r in reads:
            add(self.last_w.get(r))
        for w in writes:
            add(self.last_w.get(w))
            for t in self.readers.get(w, ()):
                add(t)
        out = []
        for sk, val in need.items():
            if self.waited.get((eng, sk), 0) >= val:
                continue
            self.waited[(eng, sk)] = val
            out.append((sk, val))
        return out

    def _commit(self, tok, reads, writes):
        for r in reads:
            self.readers.setdefault(r, []).append(tok)
        for w in writes:
            self.last_w[w] = tok
            self.readers[w] = []

    def op(self, eng, fn, reads=(), writes=()):
        reads = tuple(reads)
        writes = tuple(writes)
        waits = self._deps(eng, reads, writes)
        self.cnt[eng] += 1
        tok = (("c", eng), self.cnt[eng], eng)
        fr = sys._getframe(1)
        self.ops[eng].append(dict(fn=fn, waits=waits, inc=(("c", eng), 1),
                                  where=(fr.f_lineno, fr.f_back.f_lineno if fr.f_back else 0)))
        self._commit(tok, reads, writes)
        return tok

    def dma(self, eng, fn, reads=(), writes=()):
        reads = tuple(reads)
        writes = tuple(writes)
        n = self.dma_n[eng]
        self.dma_n[eng] += 1
        sk = ("d", eng, n % DMA_RING)
        val = 16 * (n // DMA_RING + 1)
        waits = self._deps(eng, reads, writes)
        if val > 16 and self.waited.get((eng, sk), 0) < val - 16:
            self.waited[(eng, sk)] = val - 16
            waits.append((sk, val - 16))
        tok = (sk, val, eng)
        self.ops[eng].append(dict(fn=fn, waits=waits, inc=(sk, 16)))
        self._commit(tok, reads, writes)
        return tok

    def finish(self, eng, toks):
        self.ops[eng].append(dict(fn=None, waits=[(sk, val) for sk, val, _ in toks], inc=None))

    def end_phase(self):
        targets = []
        for e in ENGS:
            if self.cnt[e] > 0:
                targets.append((("c", e), self.cnt[e]))
            n = self.dma_n[e]
            for r in range(min(n, DMA_RING)):
                last = ((n - 1 - r) // DMA_RING) * DMA_RING + r
                targets.append((("d", e, r), 16 * (last // DMA_RING + 1)))
        for e in ENGS:
            waits = []
            for sk, val in targets:
                if self.waited.get((e, sk), 0) >= val:
                    continue
                self.waited[(e, sk)] = val
                waits.append((sk, val))
            self.ops[e].append(dict(fn=None, waits=waits, inc=None))
        self.last_w = {}
        self.readers = {}

    def sem_keys(self):
        keys = set()
        for e in ENGS:
            for o in self.ops[e]:
                if o["inc"]:
                    keys.add(o["inc"][0])
                for sk, _ in o["waits"]:
                    keys.add(sk)
        return sorted(keys)

    def alloc_sems(self, es, sems):
        for k in self.sem_keys():
            if k not in sems:
                sems[k] = es.enter_context(self.nc.semaphore("s_" + "_".join(map(str, k))))

    def emit(self, block, sems):
        engobj = {"pe": "tensor", "act": "scalar", "dve": "vector", "pool": "gpsimd", "sp": "sync"}

        def make(e):
            ops = self.ops[e]

            def body(eng):
                if e == "pool":
                    self.fill_regs = {v: eng.to_reg(v) for v in sorted(self.fill_vals)}
                for o in ops:
                    for sk, val in o["waits"]:
                        eng.wait_ge(sems[sk], val)
                    if o["fn"] is not None:
                        try:
                            ins = o["fn"](eng)
                        except Exception:
                            print("EMIT FAILED at lines", o.get("where"))
                            raise
                        if o["inc"]:
                            ins.then_inc(sems[o["inc"][0]], o["inc"][1])
            return body

        for e in ENGS:
            if self.ops[e]:
                getattr(block, engobj[e])(make(e))
        self.ops = {e: [] for e in ENGS}


class Ctx:
    pass


class Rot:
    def __init__(self, tiles, name):
        self.tiles = tiles
        self.name = name
        self.i = 0

    def next(self):
        j = self.i % len(self.tiles)
        self.i += 1
        return self.tiles[j], (self.name, j)


def MM(P, out, lhsT, rhs, start, stop, r, w):
    return P.op("pe", lambda e: e.matmul(out, lhsT=lhsT, rhs=rhs, start=start, stop=stop), r, w)


def TR(P, out, in_, ident, r, w):
    return P.op("pe", lambda e: e.transpose(out, in_, ident), r, w)


def ACT(P, out, in_, func, r, w, scale=None, bias=None, accum=None):
    kw = {}
    if scale is not None:
        kw["scale"] = scale
    if bias is not None:
        kw["bias"] = bias
    if accum is not None:
        kw["accum_out"] = accum
    return P.op("act", lambda e: e.activation(out=out, in_=in_, func=func, **kw), r, w)


def TS(P, eng, out, in0, s1, s2, op0, op1, r, w):
    if op1 is None:
        return P.op(eng, lambda e: e.tensor_scalar(out=out, in0=in0, scalar1=s1, scalar2=None, op0=op0), r, w)
    return P.op(eng, lambda e: e.tensor_scalar(out=out, in0=in0, scalar1=s1, scalar2=s2, op0=op0, op1=op1), r, w)


def TT(P, eng, out, in0, in1, op, r, w):
    return P.op(eng, lambda e: e.tensor_tensor(out=out, in0=in0, in1=in1, op=op), r, w)


def STT(P, out, in0, scalar, in1, op0, op1, r, w):
    return P.op("dve", lambda e: e.scalar_tensor_tensor(out=out, in0=in0, scalar=scalar, in1=in1, op0=op0, op1=op1), r, w)


def CP(P, eng, out, in_, r, w):
    if eng == "act":
        return P.op("act", lambda e: e.copy(out=out, in_=in_), r, w)
    return P.op(eng, lambda e: e.tensor_copy(out=out, in_=in_), r, w)


def DMA(P, eng, out, in_, r, w):
    return P.dma(eng, lambda e: e.dma_start(out=out, in_=in_), r, w)


def MEMSET(P, eng, ap, val, r, w):
    return P.op(eng, lambda e: e.memset(ap, val), r, w)


def ASEL(P, out, in_, pattern, cmp, fill, base, cm, r, w):
    P.fill_vals.add(float(fill))
    return P.op("pool", lambda e: e.affine_select(out=out, in_=in_, pattern=pattern, compare_op=cmp,
                                                  fill=P.fill_regs[float(fill)], base=base, channel_multiplier=cm), r, w)


def IOTA(P, out, pattern, base, cm, r, w):
    return P.op("pool", lambda e: e.iota(out, pattern=pattern, base=base, channel_multiplier=cm), r, w)


def RECIP(P, out, in_, r, w):
    return P.op("dve", lambda e: e.reciprocal(out=out, in_=in_), r, w)


def rstd_chain(P, ss, key):
    TS(P, "dve", ss[:, 1:2], ss[:, 0:1], 1.0 / D, EPS, ALU.mult, ALU.add, [key], [key])
    P.op("act", lambda e: e.sqrt(out=ss[:, 2:3], in_=ss[:, 1:2]), [key], [key])
    RECIP(P, ss[:, 3:4], ss[:, 2:3], [key], [key])


FM_QSB, FM_KSB, FM_QNSA, FM_KCMP, FM_VCMP, FM_KSLC, FM_KWIN, FM_QMEM = 0, 384, 768, 1152, 1280, 1408, 1536, 1664
FM_ROWS = 1920
FM_COLMAP = [(0, 0, 768), (768, 1152, 384), (1152, 1536, 128), (1280, 1664, 128), (1408, 1792, 128),
             (1536, 2048, 128), (1664, 2322, 256)]
TM_COLMAP = [(0, 768, 384), (384, 1920, 128), (512, 2176, 128), (640, 2304, 18)]
TM_COLS = 658
Q_CHUNKS = {0, 1, 2, 6, 7, 8, 13, 14}


def phase_A(c):
    nc, P = c.nc, c.P
    with ExitStack() as es:
        sb = lambda name, shape, dt: es.enter_context(nc.sbuf_tensor(name, shape, dt))
        Wfm = sb("Wfm", [128, 8, FM_ROWS], BF16)
        Wtm = sb("Wtm", [128, 8, TM_COLS], BF16)
        wst = Rot([sb(f"wst{i}", [128, 2578], F32) for i in range(2)], "wst")
        gcol = sb("gcolA", [128, 8], F32)
        xbuf = Rot([sb(f"xt{i}", [128, D], F32) for i in range(2)], "xt")
        xsbuf = Rot([sb(f"xs{i}", [128, D], F32) for i in range(2)], "xs")
        junk = sb("junkA", [128, D], F32)
        ssbuf = Rot([sb(f"ss{i}", [128, 4], F32) for i in range(4)], "ss")
        hTg = Rot([sb(f"hTg{i}", [128, 8, 512], BF16) for i in range(2)], "hTg")
        FMst = Rot([sb(f"FMst{i}", [128, 15, 512], BF16) for i in range(2)], "FMst")
        TMst = Rot([sb(f"TMst{i}", [128, 640], BF16) for i in range(3)], "TMst")
        gst = Rot([sb(f"gst{i}", [128, 18], F32) for i in range(3)], "gst")
        pstr = Rot(c.ps[0:2], "ps_tr")
        psfm = Rot(c.ps[2:5], "ps_fm")
        pstm = Rot(c.ps[5:8], "ps_tm")
        uvst = Rot([sb(f"uvst{i}", [128, 2 * D], BF16) for i in range(4)], "uvst")

        def convert_chunk(ch):
            t_, tk = uvst.next()
            P.dma("pool", lambda e, o=t_[:], i_=c.inp["peer_uv"][ch * 128:(ch + 1) * 128, :]: e.dma_start(out=o, in_=i_), [], [tk])
            DMA(P, "sp", c.UVB[ch * 128:(ch + 1) * 128, :], t_[:], [tk], ["UVB"])

        DMA(P, "sp", gcol[:], c.inp["mix_g"], [], ["gcol"])
        for kc in range(8):
            st, sk = wst.next()
            DMA(P, "sp", st[:], c.inp["w_in"][kc * 128:(kc + 1) * 128, 0:2578], [], [sk])
            n = 0
            for (dst, cm) in ((Wfm, FM_COLMAP), (Wtm, TM_COLMAP)):
                for (dc, sc, w) in cm:
                    if n % 2 == 0:
                        TS(P, "dve", dst[:, kc, dc:dc + w], st[:, sc:sc + w], gcol[:, kc:kc + 1], None, ALU.mult, None,
                           [sk, "gcol"], [("W", kc)])
                    else:
                        ACT(P, dst[:, kc, dc:dc + w], st[:, sc:sc + w], AF.Copy, [sk, "gcol"], [("W", kc)],
                            scale=gcol[:, kc:kc + 1])
                    n += 1
        Wkeys = [("W", kc) for kc in range(8)]

        for tg in range(8):
            hT, hk = hTg.next()
            hpieces = [(hk, s, half) for s in range(4) for half in range(2)]
            for s in range(4):
                i = tg * 4 + s
                xt, xk = xbuf.next()
                DMA(P, "sp", xt[:], c.inp["x"][i * 128:(i + 1) * 128, :], [], [xk])
                for cv in range(4):
                    convert_chunk(i * 4 + cv)
                ss, ssk = ssbuf.next()
                ACT(P, junk[:], xt[:], AF.Square, [xk], ["junkA", ssk], accum=ss[:, 0:1])
                rstd_chain(P, ss, ssk)
                xs, xsk = xsbuf.next()
                TS(P, "dve", xs[:], xt[:], ss[:, 3:4], None, ALU.mult, None, [xk, ssk], [xsk])
                for half in range(2):
                    pt, ptk = pstr.next()
                    for j in range(4):
                        cc = half * 4 + j
                        TR(P, pt[:, j * 128:(j + 1) * 128], xs[:, cc * 128:(cc + 1) * 128], c.ident[:], [xsk, "ident"], [ptk])
                    dst = hT[:, half * 4:(half + 1) * 4, s * 128:(s + 1) * 128]
                    src = pt[:].rearrange("p (j t) -> p j t", j=4)
                    CP(P, "act" if half == 0 else "dve", dst, src, [ptk], [(hk, s, half)])
            fst, fsk = FMst.next()
            for ch in range(15):
                pf, pfk = psfm.next()
                for kc in range(8):
                    MM(P, pf[:, :], Wfm[:, kc, ch * 128:(ch + 1) * 128], hT[:, kc, :], kc == 0, kc == 7,
                       hpieces + [("W", kc)], [pfk])
                sc = 0.125 if ch in Q_CHUNKS else 1.0
                if ch % 2 == 0:
                    ACT(P, fst[:, ch, :], pf[:, :], AF.Copy, [pfk], [(fsk, ch)], scale=sc)
                else:
                    TS(P, "dve", fst[:, ch, :], pf[:, :], sc, None, ALU.mult, None, [pfk], [(fsk, ch)])
            DMA(P, "sp", c.FM.rearrange("(c p) t -> p c t", p=128)[:, :, tg * 512:(tg + 1) * 512], fst[:],
                [(fsk, ch) for ch in range(15)], ["FM"])
            DMA(P, "sp", c.HT.rearrange("(c p) t -> p c t", p=128)[:, :, tg * 512:(tg + 1) * 512], hT[:],
                hpieces, ["HT"])
            for s in range(4):
                i = tg * 4 + s
                pa, pak = pstm.next()
                for kc in range(8):
                    MM(P, pa[:, 0:512], hT[:, kc, s * 128:(s + 1) * 128], Wtm[:, kc, 0:512], kc == 0, kc == 7,
                       hpieces + [("W", kc)], [pak])
                tst, tsk = TMst.next()
                CP(P, "dve", tst[:, 0:512], pa[:, 0:512], [pak], [tsk])
                pb, pbk = pstm.next()
                for kc in range(8):
                    MM(P, pb[:, 0:146], hT[:, kc, s * 128:(s + 1) * 128], Wtm[:, kc, 512:658], kc == 0, kc == 7,
                       hpieces + [("W", kc)], [pbk])
                CP(P, "dve", tst[:, 512:640], pb[:, 0:128], [pbk], [tsk])
                gs, gsk = gst.next()
                ACT(P, gs[:], pb[:, 128:146], AF.Sigmoid, [pbk], [gsk])
                DMA(P, "sp", c.TMV[i * 128:(i + 1) * 128, :], tst[:], [tsk], ["TMV"])
                DMA(P, "sp", c.GATES[i * 128:(i + 1) * 128, :], gs[:], [gsk], ["GATES"])
        P.end_phase()
        P.alloc_sems(c.es0, c.sems)
        with nc.Block() as block:
            P.emit(block, c.sems)


def phase_B(c):
    nc, P = c.nc, c.P
    with ExitStack() as es:
        sb = lambda name, shape, dt: es.enter_context(nc.sbuf_tensor(name, shape, dt))
        qT = [sb(f"qTb{j}", [128, T], BF16) for j in range(3)]
        kT = [sb(f"kTb{j}", [128, T], BF16) for j in range(3)]
        V = sb("Vsb", [128, NT, 384], BF16)
        ntri = sb("ntri", [128, 128], BF16)
        nones = sb("nones", [128, 128], BF16)
        Ebuf = Rot([sb(f"E{i}", [128, 512], F32) for i in range(3)], "E")
        SPbuf = Rot([sb(f"SP{i}", [128, 512], BF16) for i in range(4)], "SP")
        Sbuf = Rot([sb(f"Ssum{i}", [128, 512], BF16) for i in range(3)], "Ssum")
        abuf = Rot([sb(f"aT{i}", [128, 512], BF16) for i in range(3)], "aT")
        obuf = Rot([sb(f"sbo{i}", [64, 512], BF16) for i in range(2)], "sbo")
        psA = Rot(c.ps[0:3], "psA")
        psB = Rot(c.ps[3:6], "psB")
        psO = Rot(c.ps[6:8], "psO")
        for j in range(3):
            DMA(P, "sp", qT[j][:], c.FM[FM_QSB + 128 * j:FM_QSB + 128 * (j + 1), :], ["FM"], [("qT", j)])
            DMA(P, "sp", kT[j][:], c.FM[FM_KSB + 128 * j:FM_KSB + 128 * (j + 1), :], ["FM"], [("kT", j)])
        DMA(P, "sp", V[:], c.TMV.rearrange("(c p) f -> p c f", p=128)[:, :, 0:384], ["TMV"], ["V"])
        MEMSET(P, "pool", nones[:], -1.0, [], ["nones"])
        MEMSET(P, "pool", ntri[:], -1.0, [], ["ntri"])
        ASEL(P, ntri[:], ntri[:], [[-1, 128]], ALU.is_ge, 0.0, 0, 1, ["ntri"], ["ntri"])
        steps = []
        for h in range(6):
            for g in range(8):
                nch = 4 * g + 4
                for idx_, cch in enumerate(range(nch - 1, -1, -1)):
                    steps.append(dict(h=h, g=g, cch=cch, first=idx_ == 0, last=cch == 0))
        N = len(steps)
        cur = dict(po=None, pok=None, ssum=None, ssumk=None)

        def S12(st):
            h, g, cch = st["h"], st["g"], st["cch"]
            j, half = h // 2, h % 2
            pr = slice(64 * half, 64 * half + 64)
            st["qs"] = qT[j][pr, g * 512:(g + 1) * 512]
            st["ks"] = kT[j][pr, cch * 128:(cch + 1) * 128]
            st["rk"] = [("kT", j), ("qT", j)]
            st["m"] = cch - 4 * g
            if st["first"]:
                cur["po"], cur["pok"] = psO.next()
                cur["ssum"] = cur["ssumk"] = None
            st["po"], st["pok"] = cur["po"], cur["pok"]
            pa, pak = psA.next()
            MM(P, pa[:, :], st["ks"], st["qs"], True, True, st["rk"], [pak])
            E, Ek = Ebuf.next()
            ACT(P, E[:], pa[:, :], AF.Exp, [pak], [Ek])
            SP, SPk = SPbuf.next()
            ACT(P, SP[:], E[:], AF.Ln, [Ek], [SPk], bias=1.0)
            if st["m"] >= 0:
                ASEL(P, SP[:], SP[:], [[1, 512]], ALU.is_gt, 0.0, -128 * st["m"], -1, [SPk], [SPk])
            st["SP"], st["SPk"] = SP, SPk
            st["ssum_prev"], st["ssum_prevk"] = cur["ssum"], cur["ssumk"]
            if not st["last"]:
                if st["first"]:
                    cur["ssum"], cur["ssumk"] = SP, SPk
                else:
                    sn, snk = Sbuf.next()
                    TT(P, "pool", sn[:], cur["ssum"][:], SP[:], ALU.add, [cur["ssumk"], SPk], [snk])
                    cur["ssum"], cur["ssumk"] = sn, snk

        def S34(st):
            pb, pbk = psB.next()
            MM(P, pb[:, :], ntri[:], st["SP"][:], True, False, ["ntri", st["SPk"]], [pbk])
            if not st["first"]:
                MM(P, pb[:, :], nones[:], st["ssum_prev"][:], False, False, ["nones", st["ssum_prevk"]], [pbk])
            MM(P, pb[:, :], st["ks"], st["qs"], False, True, st["rk"], [pbk])
            aT, aTk = abuf.next()
            ACT(P, aT[:], pb[:, :], AF.Exp, [pbk], [aTk])
            if st["m"] >= 0:
                ASEL(P, aT[:], aT[:], [[1, 512]], ALU.is_gt, 0.0, -128 * st["m"], -1, [aTk], [aTk])
            st["aT"], st["aTk"] = aT, aTk

        def S5(st):
            h, g, cch = st["h"], st["g"], st["cch"]
            MM(P, st["po"][0:64, :], V[:, cch, 64 * h:64 * h + 64], st["aT"][:], st["first"], st["last"], ["V", st["aTk"]], [st["pok"]])
            if st["last"]:
                ob, obk = obuf.next()
                CP(P, "dve", ob[:], st["po"][0:64, :], [st["pok"]], [obk])
                DMA(P, "sp", c.SBOT[64 * h:64 * h + 64, g * 512:(g + 1) * 512], ob[:], [obk], ["SBOT"])

        for n in range(N + 2):
            if n < N:
                S12(steps[n])
            if 0 <= n - 1 < N:
                S34(steps[n - 1])
            if 0 <= n - 2 < N:
                S5(steps[n - 2])
                steps[n - 2].clear()
        P.end_phase()
        P.alloc_sems(c.es0, c.sems)
        with nc.Block() as block:
            P.emit(block, c.sems)


def phase_C(c):
    nc, P = c.nc, c.P
    with ExitStack() as es:
        sb = lambda name, shape, dt: es.enter_context(nc.sbuf_tensor(name, shape, dt))
        qaug = [sb(f"qaug{h}", [128, T], BF16) for h in range(6)]
        kslc = [sb(f"kslc{g}", [128, T], BF16) for g in range(2)]
        kwin = [sb(f"kwin{g}", [64, T], BF16) for g in range(2)]
        kcmp = [sb(f"kcmp{g}", [64, T], BF16) for g in range(2)]
        vcmp = [sb(f"vcmp{g}", [64, T], BF16) for g in range(2)]
        kcT = [sb(f"kcT{g}", [64, 256], BF16) for g in range(2)]
        vcaug = [sb(f"vcaug{g}", [128, 2, 129], BF16) for g in range(2)]
        vwin = sb("vwin", [128, NT, 2, 65], BF16)
        vslc = sb("vslc", [128, NT, 2, 65], BF16)
        vstage = sb("vstage", [128, NT, 256], BF16)
        gates = sb("gatesC", [128, NT, 18], F32)
        pe = [sb("pek_sb", [64, 32], F32), sb("pev_sb", [64, 32], F32)]
        cwst = sb("cwst", [64, 2048], F32)
        cw = [sb("cwk", [64, 32, 64], BF16), sb("cwv", [64, 32, 64], BF16)]
        tmpb = Rot([sb(f"ctmp{i}", [64, 256], BF16) for i in range(3)], "ctmp")
        itmp = sb("itmp", [128, 64], I32)
        ftmp = sb("ftmp", [128, 64], F32)
        bias_sel = sb("bias_sel", [128, 6], F32)
        bias_win = sb("bias_win", [128, 6, 5], F32)
        bias_cmp = sb("bias_cmp", [128, 6, 64], F32)
        IOTi = sb("IOTi", [128, 128], I32)
        IOT = sb("IOT", [128, 128], F32)
        e32b = Rot([sb(f"e32_{i}", [128, 128], F32) for i in range(4)], "e32")
        ebb = Rot([sb(f"eb{i}", [128, 128], BF16) for i in range(7)], "eb")
        obuf = Rot([sb(f"oC{i}", [128, 384], F32) for i in range(2)], "oC")
        impb = Rot([sb(f"imp{i}", [128, 64], F32) for i in range(2)], "imp")
        wb = Rot([sb(f"wC{i}", [128, 4], F32) for i in range(4)], "wC")
        m8b = Rot([sb(f"m8_{i}", [128, 16], F32) for i in range(2)], "m8")
        repb = Rot([sb(f"rep{i}", [128, 64], F32) for i in range(2)], "rep")
        selb = Rot([sb(f"selp{i}", [128, 128], F32) for i in range(2)], "selp")
        Ctb = Rot([sb(f"Ct{i}", [128, 128], F32) for i in range(2)], "Ct")
        ostb = Rot([sb(f"ostC{i}", [128, 3, 128], BF16) for i in range(2)], "ostC")
        pss = Rot(c.ps[0:3], "pss")
        psacc = Rot(c.ps[3:6], "psacc")
        pstr = Rot(c.ps[6:8], "pstrC")
        pk = c.ps[6]

        for h in range(6):
            DMA(P, "sp", qaug[h][0:64, :], c.FM[FM_QNSA + 64 * h:FM_QNSA + 64 * (h + 1), :], ["FM"], [("q", h)])
        for g in range(2):
            DMA(P, "sp", kslc[g][0:64, :], c.FM[FM_KSLC + 64 * g:FM_KSLC + 64 * (g + 1), :], ["FM"], [("kslc", g)])
            DMA(P, "sp", kwin[g][:], c.FM[FM_KWIN + 64 * g:FM_KWIN + 64 * (g + 1), :], ["FM"], [("kwin", g)])
            DMA(P, "sp", kcmp[g][:], c.FM[FM_KCMP + 64 * g:FM_KCMP + 64 * (g + 1), :], ["FM"], [("kcmp", g)])
            DMA(P, "sp", vcmp[g][:], c.FM[FM_VCMP + 64 * g:FM_VCMP + 64 * (g + 1), :], ["FM"], [("vcmp", g)])
        DMA(P, "sp", vstage[:], c.TMV.rearrange("(c p) f -> p c f", p=128)[:, :, 384:640], ["TMV"], ["vstage"])
        DMA(P, "sp", gates[:], c.GATES.rearrange("(c p) f -> p c f", p=128), ["GATES"], ["gates"])
        DMA(P, "sp", pe[0][:], c.inp["pe_k"], [], ["pe0"])
        DMA(P, "sp", pe[1][:], c.inp["pe_v"], [], ["pe1"])
        for kv, nm in ((0, "cw_k"), (1, "cw_v")):
            DMA(P, "sp", cwst[:], c.inp[nm].rearrange("d l e -> d (l e)"), [], ["cwst"])
            CP(P, "dve", cw[kv][:].rearrange("d l e -> d (l e)"), cwst[:], ["cwst"], [("cw", kv)])
        CP(P, "dve", vslc[:, :, :, 0:64], vstage[:, :, 0:128].rearrange("p c (g d) -> p c g d", g=2), ["vstage"], ["vslc"])
        CP(P, "pool", vwin[:, :, :, 0:64], vstage[:, :, 128:256].rearrange("p c (g d) -> p c g d", g=2), ["vstage"], ["vwin"])
        MEMSET(P, "dve", vslc[:, :, :, 64:65], 1.0, ["vslc"], ["vslc"])
        MEMSET(P, "pool", vwin[:, :, :, 64:65], 1.0, ["vwin"], ["vwin"])
        for g in range(2):
            MEMSET(P, "pool", kslc[g][64:128, :], 1.0, [], [("kx", g)])
            ASEL(P, kslc[g][64:128, :], kslc[g][64:128, :], [[1, T]], ALU.is_ge, 0.0, 0, -64, [("kx", g)], [("kx", g)])
            ASEL(P, kslc[g][64:128, :], kslc[g][64:128, :], [[-1, T]], ALU.is_ge, 0.0, 63, 64, [("kx", g)], [("kx", g)])
        IOTA(P, itmp[0:64, 0:1], [[0, 1]], 0, 1, [], ["itmp"])
        IOTA(P, itmp[64:128, 0:1], [[0, 1]], 0, 1, ["itmp"], ["itmp"])
        CP(P, "dve", ftmp[:, 0:1], itmp[:, 0:1], ["itmp"], ["ftmp"])
        for h in range(6):
            TS(P, "dve", bias_sel[:, h:h + 1], ftmp[:, 0:1], SLOPES[h], None, ALU.mult, None, ["ftmp"], ["bias_sel"])
        IOTA(P, itmp[:, 0:5], [[128, 5]], -512, 1, ["itmp", "ftmp"], ["itmp"])
        CP(P, "dve", ftmp[:, 0:5], itmp[:, 0:5], ["itmp"], ["ftmp"])
        for h in range(6):
            TS(P, "dve", bias_win[:, h, :], ftmp[:, 0:5], SLOPES[h], None, ALU.mult, None, ["ftmp"], ["bias_win"])
        IOTA(P, itmp[:, 0:64], [[2048, 2], [-128, 32]], 31, 16, ["itmp", "ftmp"], ["itmp"])
        CP(P, "dve", ftmp[:, 0:64], itmp[:, 0:64], ["itmp"], ["ftmp"])
        for h in range(6):
            TS(P, "dve", bias_cmp[:, h, :], ftmp[:, 0:64], SLOPES[h], None, ALU.mult, None, ["ftmp"], ["bias_cmp"])
        IOTA(P, IOTi[64:128, :], [[-1, 128]], 0, 64, [], ["IOTi"])
        CP(P, "dve", IOT[64:128, :], IOTi[64:128, :], ["IOTi"], ["IOT"])
        for g in range(2):
            MEMSET(P, "pool", vcaug[g][:], 1.0, [], [("vcaug", g)])
            for ci in range(2):
                ASEL(P, vcaug[g][:, ci, 65:129], vcaug[g][:, ci, 65:129], [[-4, 64]], ALU.is_ge, 0.0, 128 * ci, 1,
                     [("vcaug", g)], [("vcaug", g)])
                ASEL(P, vcaug[g][:, ci, 65:129], vcaug[g][:, ci, 65:129], [[4, 64]], ALU.is_ge, 0.0, 3 - 128 * ci, -1,
                     [("vcaug", g)], [("vcaug", g)])
        for s_ in selb.tiles:
            pass
        for j in range(2):
            MEMSET(P, "dve", selb.tiles[j][:, 0:64], 0.0, [], [("selp", j)])
        for g in range(2):
            kview = kcmp[g][:].rearrange("p (n s) -> p n s", s=16)
            vview = vcmp[g][:].rearrange("p (n s) -> p n s", s=16)
            for l in range(32):
                tmp, tk = tmpb.next()
                TS(P, "dve" if l % 2 == 0 else "pool", tmp[:, 0:255], kview[:, l // 16:l // 16 + 255, l % 16],
                   pe[0][:, l:l + 1], None, ALU.add, None, [("kcmp", g), "pe0"], [tk])
                MM(P, pk[0:64, 0:255], cw[0][:, l, :], tmp[:, 0:255], l == 0, l == 31, [("cw", 0), tk], [("pstrC", 0)])
            CP(P, "dve", kcT[g][:, 0:255], pk[0:64, 0:255], [("pstrC", 0)], [("kcT", g)])
            for ci in range(2):
                rows = 128 if ci == 0 else 127
                for l in range(32):
                    tmp, tk = tmpb.next()
                    n0 = l // 16 + ci * 128
                    TS(P, "dve" if l % 2 == 0 else "pool", tmp[:, 0:rows], vview[:, n0:n0 + rows, l % 16],
                       pe[1][:, l:l + 1], None, ALU.add, None, [("vcmp", g), "pe1"], [tk])
                    MM(P, pk[0:rows, 256:320], tmp[:, 0:rows], cw[1][:, l, :], l == 0, l == 31, [("cw", 1), tk], [("pstrC", 0)])
                CP(P, "dve", vcaug[g][0:rows, ci, 0:64], pk[0:rows, 256:320], [("pstrC", 0)], [("vcaug", g)])

        def consume(pacc, pacck, o, ok, h, i, gcol, first):
            w, wk = wb.next()
            TS(P, "dve", w[:, 0:1], pacc[:, 64:65], 1e-30, None, ALU.max, None, [pacck], [wk])
            RECIP(P, w[:, 1:2], w[:, 0:1], [wk], [wk])
            TT(P, "dve", w[:, 2:3], w[:, 1:2], gates[:, i, gcol:gcol + 1], ALU.mult, [wk, "gates"], [wk])
            if first:
                TS(P, "dve", o[:, 64 * h:64 * h + 64], pacc[:, 0:64], w[:, 2:3], None, ALU.mult, None, [pacck, wk], [(ok, h)])
            else:
                STT(P, o[:, 64 * h:64 * h + 64], pacc[:, 0:64], w[:, 2:3], o[:, 64 * h:64 * h + 64], ALU.mult, ALU.add,
                    [pacck, wk, (ok, h)], [(ok, h)])
            return w, wk

        from collections import deque
        fifo = deque()
        LAGC = 3

        def defer(fn):
            fifo.append(fn)
            while len(fifo) > LAGC:
                fifo.popleft()()

        def flush():
            while fifo:
                fifo.popleft()()

        def consume_cmp(pc, pck, o, ok, h, i, hh, imp, impk):
            w, wk = consume(pc, pck, o, ok, h, i, 3 * h + 0, True)
            if hh == 0:
                TS(P, "dve", imp[:], pc[:, 65:129], w[:, 1:2], None, ALU.mult, None, [pck, wk], [impk])
            else:
                STT(P, imp[:], pc[:, 65:129], w[:, 1:2], imp[:], ALU.mult, ALU.add, [pck, wk, impk], [impk])

        for i in range(NT):
            t0 = 128 * i
            qc = slice(t0, t0 + 128)
            o, ok = obuf.next()
            for g in range(2):
                imp, impk = impb.next()
                for hh in range(3):
                    h = 3 * g + hh
                    nvalid = min(255, (t0 + 96) // 16 + 1)
                    chunks = [(0, min(128, nvalid))] + ([(1, nvalid - 128)] if nvalid > 128 else [])
                    pc, pck = psacc.next()
                    for ni, (ci, rows) in enumerate(chunks):
                        ps_, psk = pss.next()
                        MM(P, ps_[0:rows, 0:128], kcT[g][:, ci * 128:ci * 128 + rows], qaug[h][0:64, qc], True, True,
                           [("kcT", g), ("q", h)], [psk])
                        e32, e32k = e32b.next()
                        ACT(P, e32[0:rows, :], ps_[0:rows, 0:128], AF.Exp, [psk, "bias_cmp"], [e32k],
                            bias=bias_cmp[0:rows, h, ci * 32 + i:ci * 32 + i + 1])
                        eb, ebk = ebb.next()
                        ASEL(P, eb[0:rows, :], e32[0:rows, :], [[1, 128]], ALU.is_ge, 0.0, t0 - 2048 * ci - 31, -16,
                             [e32k], [ebk])
                        defer(lambda pc=pc, pck=pck, eb=eb, ebk=ebk, rows=rows, ci=ci, g=g, st_=(ni == 0), sp_=(ni == len(chunks) - 1):
                              MM(P, pc[:, 0:129], eb[0:rows, :], vcaug[g][0:rows, ci, :], st_, sp_, [ebk, ("vcaug", g)], [pck]))
                    defer(lambda pc=pc, pck=pck, o=o, ok=ok, h=h, i=i, hh=hh, imp=imp, impk=impk:
                          consume_cmp(pc, pck, o, ok, h, i, hh, imp, impk))
                    pw, pwk = psacc.next()
                    cl = list(range(max(0, i - 4), i + 1))
                    for ni, cch in enumerate(cl):
                        dc = cch - i
                        ps_, psk = pss.next()
                        MM(P, ps_[:, 0:128], kwin[g][:, cch * 128:(cch + 1) * 128], qaug[h][0:64, qc], True, True,
                           [("kwin", g), ("q", h)], [psk])
                        eb, ebk = ebb.next()
                        bw = bias_win[:, h, dc + 4:dc + 5]
                        if dc == 0 or dc == -4:
                            e32, e32k = e32b.next()
                            ACT(P, e32[:], ps_[:, 0:128], AF.Exp, [psk, "bias_win"], [e32k], bias=bw)
                            if dc == 0:
                                ASEL(P, eb[:], e32[:], [[1, 128]], ALU.is_ge, 0.0, 0, -1, [e32k], [ebk])
                            else:
                                ASEL(P, eb[:], e32[:], [[-1, 128]], ALU.is_gt, 0.0, 0, 1, [e32k], [ebk])
                        else:
                            ACT(P, eb[:], ps_[:, 0:128], AF.Exp, [psk, "bias_win"], [ebk], bias=bw)
                        defer(lambda pw=pw, pwk=pwk, eb=eb, ebk=ebk, cch=cch, g=g, st_=(ni == 0), sp_=(ni == len(cl) - 1):
                              MM(P, pw[:, 0:65], eb[:], vwin[:, cch, g, :], st_, sp_, [ebk, "vwin"], [pwk]))
                    defer(lambda pw=pw, pwk=pwk, o=o, ok=ok, h=h, i=i: consume(pw, pwk, o, ok, h, i, 3 * h + 2, False))
                flush()
                ASEL(P, imp[:], imp[:], [[-64, 64]], ALU.is_ge, 1e4, t0 - 128, 1, [impk], [impk])
                ASEL(P, imp[:], imp[:], [[-64, 64]], ALU.is_ge, -1.0, t0, 1, [impk], [impk])
                MEMSET(P, "pool", imp[:, 0:1], 1e4, [impk], [impk])
                m8, m8k = m8b.next()
                rep, repk = repb.next()
                P.op("dve", lambda e, m8=m8, imp=imp: e.max(out=m8[:, 0:8], in_=imp[:]), [impk], [m8k])
                P.op("dve", lambda e, m8=m8, imp=imp, rep=rep: e.match_replace(out=rep[:], in_to_replace=m8[:, 0:8],
                                                                                 in_values=imp[:], imm_value=-1e30),
                     [impk, m8k], [repk])
                P.op("dve", lambda e, m8=m8, rep=rep: e.max(out=m8[:, 8:16], in_=rep[:]), [repk, m8k], [m8k])
                selp, selk = selb.next()
                TS(P, "dve", selp[:, 64:128], imp[:], m8[:, 15:16], None, ALU.is_ge, None, [impk, m8k], [selk])
                pt_, ptk = pstr.next()
                TR(P, pt_[:, 0:128], selp[:], c.ident[:], [selk, "ident"], [ptk])
                for hh in range(3):
                    h = 3 * g + hh
                    Ct, Ctk = Ctb.next()
                    TS(P, "pool", Ct[64:128, :], IOT[64:128, :], SLOPES[h], -BIG - SLOPES[h] * t0, ALU.mult, ALU.add,
                       ["IOT"], [Ctk])
                    STT(P, qaug[h][64:128, qc], pt_[64:128, 0:128], BIG, Ct[64:128, :], ALU.mult, ALU.add,
                        [ptk, Ctk], [("qm", h, i)])
                for hh in range(3):
                    h = 3 * g + hh
                    psl, pslk = psacc.next()
                    for cch in range(i + 1):
                        ps_, psk = pss.next()
                        MM(P, ps_[:, 0:128], kslc[g][:, cch * 128:(cch + 1) * 128], qaug[h][:, qc], True, True,
                           [("kslc", g), ("kx", g), ("q", h), ("qm", h, i)], [psk])
                        eb, ebk = ebb.next()
                        if cch == i:
                            e32, e32k = e32b.next()
                            ACT(P, e32[:], ps_[:, 0:128], AF.Exp, [psk, "bias_sel"], [e32k], bias=bias_sel[:, h:h + 1])
                            ASEL(P, eb[:], e32[:], [[1, 128]], ALU.is_ge, 0.0, 0, -1, [e32k], [ebk])
                        else:
                            ACT(P, eb[:], ps_[:, 0:128], AF.Exp, [psk, "bias_sel"], [ebk], bias=bias_sel[:, h:h + 1])
                        defer(lambda psl=psl, pslk=pslk, eb=eb, ebk=ebk, cch=cch, g=g, st_=(cch == 0), sp_=(cch == i):
                              MM(P, psl[:, 0:65], eb[:], vslc[:, cch, g, :], st_, sp_, [ebk, "vslc"], [pslk]))
                    defer(lambda psl=psl, pslk=pslk, o=o, ok=ok, h=h, i=i: consume(psl, pslk, o, ok, h, i, 3 * h + 1, False))

            def finish_tile(o=o, ok=ok, qc=qc):
                pt2, pt2k = pstr.next()
                for j in range(3):
                    TR(P, pt2[:, j * 128:(j + 1) * 128], o[:, j * 128:(j + 1) * 128], c.ident[:],
                       [(ok, 2 * j), (ok, 2 * j + 1), "ident"], [pt2k])
                ost, ostk = ostb.next()
                CP(P, "act", ost[:], pt2[:, 0:384].rearrange("p (j t) -> p j t", j=3), [pt2k], [ostk])
                DMA(P, "sp", c.NSAOT.rearrange("(j p) t -> p j t", p=128)[:, :, qc], ost[:], [ostk], ["NSAOT"])
            defer(finish_tile)
        flush()
        P.end_phase()
        P.alloc_sems(c.es0, c.sems)
        with nc.Block() as block:
            P.emit(block, c.sems)


def phase_D(c):
    nc, P = c.nc, c.P
    with ExitStack() as es:
        sb = lambda name, shape, dt: es.enter_context(nc.sbuf_tensor(name, shape, dt))
        memt = sb("memt", [128, 2, D], F32)
        mems = sb("mems", [128, 2, D], F32)
        junk = sb("junkD", [128, D], F32)
        ssb = [sb(f"ssD{i}", [128, 4], F32) for i in range(2)]
        gcol = sb("gcolD", [128, 8], F32)
        mhT = sb("mhT", [128, 8, 256], BF16)
        wst = Rot([sb(f"wstD{i}", [128, 512], F32) for i in range(2)], "wstD")
        Wkv = sb("Wkv", [128, 8, 512], BF16)
        mkT = [sb(f"mkT{h}", [64, 256], BF16) for h in range(4)]
        mvaug = sb("mvaug", [128, 2, 4, 65], BF16)
        qm = [sb(f"qm{h}", [64, T], BF16) for h in range(4)]
        eb = Rot([sb(f"ebD{i}", [128, 512], BF16) for i in range(4)], "ebD")
        ob = Rot([sb(f"oD{i}", [128, 4, 256], F32) for i in range(2)], "oD")
        wb = Rot([sb(f"wD{i}", [128, 2], F32) for i in range(4)], "wD")
        ostb = Rot([sb(f"ostD{i}", [128, 2, 512], BF16) for i in range(2)], "ostD")
        pss = Rot(c.ps[0:2], "pssD")
        psacc = Rot(c.ps[2:5], "psaccD")
        pstr = Rot(c.ps[5:7], "pstrD")
        pmisc = c.ps[7]

        DMA(P, "sp", gcol[:], c.inp["mem_g"], [], ["gcol"])
        DMA(P, "sp", memt[:], c.inp["mem"].rearrange("(c p) d -> p c d", p=128), [], ["memt"])
        for h in range(4):
            DMA(P, "sp", qm[h][:], c.FM[FM_QMEM + 64 * h:FM_QMEM + 64 * (h + 1), :], ["FM"], [("qm", h)])
        for kc in range(8):
            st, sk = wst.next()
            DMA(P, "sp", st[:], c.inp["w_mem_kv"][kc * 128:(kc + 1) * 128, :], [], [sk])
            TS(P, "dve", Wkv[:, kc, :], st[:], gcol[:, kc:kc + 1], None, ALU.mult, None, [sk, "gcol"], [("Wkv", kc)])
        Wk = [("Wkv", kc) for kc in range(8)]
        for ci in range(2):
            ss = ssb[ci]
            ssk = ("ssD", ci)
            ACT(P, junk[:], memt[:, ci, :], AF.Square, ["memt"], ["junkD", ssk], accum=ss[:, 0:1])
            rstd_chain(P, ss, ssk)
            TS(P, "dve", mems[:, ci, :], memt[:, ci, :], ss[:, 3:4], None, ALU.mult, None, ["memt", ssk], [("mems", ci)])
            for half in range(2):
                pt, ptk = pstr.next()
                for j in range(4):
                    cc = half * 4 + j
                    TR(P, pt[:, j * 128:(j + 1) * 128], mems[:, ci, cc * 128:(cc + 1) * 128], c.ident[:],
                       [("mems", ci), "ident"], [ptk])
                CP(P, "dve", mhT[:, half * 4:(half + 1) * 4, ci * 128:(ci + 1) * 128],
                   pt[:].rearrange("p (j t) -> p j t", j=4), [ptk], [("mhT", ci, half)])
        mh = [("mhT", ci, half) for ci in range(2) for half in range(2)]
        for h in range(4):
            for kc in range(8):
                MM(P, pmisc[0:64, 0:256], Wkv[:, kc, 64 * h:64 * h + 64], mhT[:, kc, :], kc == 0, kc == 7, Wk + mh, ["pmisc"])
            CP(P, "dve", mkT[h][:], pmisc[0:64, 0:256], ["pmisc"], [("mkT", h)])
        for ci in range(2):
            for kc in range(8):
                MM(P, pmisc[:, 256:512], mhT[:, kc, ci * 128:(ci + 1) * 128], Wkv[:, kc, 256:512], kc == 0, kc == 7,
                   Wk + mh, ["pmisc"])
            CP(P, "dve", mvaug[:, ci, :, 0:64], pmisc[:, 256:512].rearrange("p (h d) -> p h d", h=4), ["pmisc"], ["mvaug"])
        MEMSET(P, "dve", mvaug[:, :, :, 64:65], 1.0, ["mvaug"], ["mvaug"])

        for g in range(8):
            o, ok = ob.next()
            for h in range(4):
                es_ = []
                for ci in range(2):
                    ps_, psk = pss.next()
                    MM(P, ps_[:, :], mkT[h][:, ci * 128:(ci + 1) * 128], qm[h][:, g * 512:(g + 1) * 512], True, True,
                       [("mkT", h), ("qm", h)], [psk])
                    e, ek = eb.next()
                    ACT(P, e[:], ps_[:, :], AF.Exp, [psk], [ek])
                    es_.append((e, ek))
                for sub in range(4):
                    pa, pak = psacc.next()
                    for ci in range(2):
                        MM(P, pa[:, 0:65], es_[ci][0][:, sub * 128:(sub + 1) * 128], mvaug[:, ci, h, :], ci == 0, ci == 1,
                           [es_[ci][1], "mvaug"], [pak])
                    w, wk = wb.next()
                    RECIP(P, w[:, 0:1], pa[:, 64:65], [pak], [wk])
                    TS(P, "dve", o[:, sub, 64 * h:64 * h + 64], pa[:, 0:64], w[:, 0:1], None, ALU.mult, None, [pak, wk],
                       [(ok, sub, h)])
            ost, ostk = ostb.next()
            for sub in range(4):
                pt, ptk = pstr.next()
                for j in range(2):
                    TR(P, pt[:, j * 128:(j + 1) * 128], o[:, sub, j * 128:(j + 1) * 128], c.ident[:],
                       [(ok, sub, 2 * j), (ok, sub, 2 * j + 1), "ident"], [ptk])
                CP(P, "act", ost[:, :, sub * 128:(sub + 1) * 128], pt[:, 0:256].rearrange("p (j t) -> p j t", j=2),
                   [ptk], [(ostk, sub)])
            DMA(P, "sp", c.MEMOT.rearrange("(j p) t -> p j t", p=128)[:, :, g * 512:(g + 1) * 512], ost[:],
                [(ostk, sub) for sub in range(4)], ["MEMOT"])
        P.end_phase()
        P.alloc_sems(c.es0, c.sems)
        with nc.Block() as block:
            P.emit(block, c.sems)


def phase_E(c):
    nc, P = c.nc, c.P
    with ExitStack() as es:
        sb = lambda name, shape, dt: es.enter_context(nc.sbuf_tensor(name, shape, dt))
        Wmg = sb("Wmg", [128, 8, 3072], BF16)
        Wbr = [sb("Wsb", [128, 3, D], BF16), sb("Wnsa", [128, 3, D], BF16), sb("Wmem", [128, 2, D], BF16)]
        Wout = sb("Wout", [128, 8, D], BF16)
        wst = Rot([sb(f"wstE{i}", [128, 1024], F32) for i in range(3)], "wstE")
        gcol = sb("gcolE", [128, 8], F32)
        bmg = sb("bmg", [128, 24], F32)
        hTb = Rot([sb(f"hTE{i}", [128, 8, 512], BF16) for i in range(2)], "hTE")
        srcb = [Rot([sb(f"srcE{b}_{i}", [128, 3 if b < 2 else 2, 512], BF16) for i in range(2)], f"srcE{b}") for b in range(3)]
        mgb = Rot([sb(f"mgT{i}", [128, 8, 512], BF16) for i in range(2)], "mgT")
        gateb = Rot([sb(f"gateE{i}", [128, 512], F32) for i in range(3)], "gateE")
        accb = Rot([sb(f"accE{i}", [128, 512], F32) for i in range(2)], "accE")
        tmpb = Rot([sb(f"tmpE{i}", [128, 512], F32) for i in range(2)], "tmpE")
        xb = Rot([sb(f"xE{i}", [128, D], F32) for i in range(2)], "xE")
        x1b = Rot([sb(f"x1E{i}", [128, D], F32) for i in range(2)], "x1E")
        psbr = Rot(c.ps[0:2], "psbr")
        psg = Rot(c.ps[2:5], "psg")
        psy = Rot(c.ps[5:8], "psy")

        DMA(P, "sp", gcol[:], c.inp["mix_g"], [], ["gcol"])
        DMA(P, "sp", bmg[:], c.inp["b_merge"], [], ["bmg"])
        n = 0
        for kc in range(8):
            for j in range(3):
                st, sk = wst.next()
                DMA(P, "sp", st[:], c.inp["w_in"][kc * 128:(kc + 1) * 128, 2578 + 1024 * j:2578 + 1024 * (j + 1)], [], [sk])
                if n % 2 == 0:
                    TS(P, "dve", Wmg[:, kc, 1024 * j:1024 * (j + 1)], st[:], gcol[:, kc:kc + 1], None, ALU.mult, None,
                       [sk, "gcol"], [("Wmg", kc)])
                else:
                    ACT(P, Wmg[:, kc, 1024 * j:1024 * (j + 1)], st[:], AF.Copy, [sk, "gcol"], [("Wmg", kc)],
                        scale=gcol[:, kc:kc + 1])
                n += 1
        for b, (nm, nf) in enumerate((("w_sb_br", 3), ("w_nsa_br", 3), ("w_mem_br", 2))):
            for f in range(nf):
                st, sk = wst.next()
                DMA(P, "sp", st[:], c.inp[nm][f * 128:(f + 1) * 128, :], [], [sk])
                CP(P, "dve" if n % 2 == 0 else "act", Wbr[b][:, f, :], st[:], [sk], [("Wbr", b)])
                n += 1
        for kc in range(8):
            st, sk = wst.next()
            DMA(P, "sp", st[:], c.inp["w_out"][kc * 128:(kc + 1) * 128, :], [], [sk])
            CP(P, "dve" if n % 2 == 0 else "act", Wout[:, kc, :], st[:], [sk], ["Wout"])
            n += 1
        Wmgk = [("Wmg", kc) for kc in range(8)]
        srcs = [(c.SBOT, 3, "SBOT"), (c.NSAOT, 3, "NSAOT"), (c.MEMOT, 2, "MEMOT")]
        for tg in range(8):
            tc_ = slice(tg * 512, (tg + 1) * 512)
            hT, hk = hTb.next()
            DMA(P, "sp", hT[:], c.HT.rearrange("(c p) t -> p c t", p=128)[:, :, tc_], ["HT"], [hk])
            src = []
            for b, (ap, nf, nm) in enumerate(srcs):
                t_, tk = srcb[b].next()
                DMA(P, "sp", t_[:], ap.rearrange("(f p) t -> p f t", p=128)[:, :, tc_], [nm], [tk])
                src.append((t_, tk, nf))
            mg, mgk = mgb.next()
            for dc in range(8):
                acc, acck = accb.next()
                for b in range(3):
                    t_, tk, nf = src[b]
                    pb, pbk = psbr.next()
                    for f in range(nf):
                        MM(P, pb[:, :], Wbr[b][:, f, dc * 128:(dc + 1) * 128], t_[:, f, :], f == 0, f == nf - 1,
                           [("Wbr", b), tk], [pbk])
                    pg, pgk = psg.next()
                    for kc in range(8):
                        MM(P, pg[:, :], Wmg[:, kc, b * 1024 + dc * 128:b * 1024 + (dc + 1) * 128], hT[:, kc, :], kc == 0, kc == 7,
                           Wmgk + [hk], [pgk])
                    gt, gtk = gateb.next()
                    ACT(P, gt[:], pg[:, :], AF.Sigmoid, [pgk, "bmg"], [gtk], bias=bmg[:, b * 8 + dc:b * 8 + dc + 1])
                    if b == 0:
                        TT(P, "dve", acc[:], gt[:], pb[:, :], ALU.mult, [gtk, pbk], [acck])
                    else:
                        tmp, tmpk = tmpb.next()
                        TT(P, "dve", tmp[:], gt[:], pb[:, :], ALU.mult, [gtk, pbk], [tmpk])
                        if b == 1:
                            TT(P, "pool", acc[:], acc[:], tmp[:], ALU.add, [acck, tmpk], [acck])
                        else:
                            TT(P, "pool", mg[:, dc, :], acc[:], tmp[:], ALU.add, [acck, tmpk], [(mgk, dc)])
            mgks = [(mgk, dc) for dc in range(8)]
            for s in range(4):
                i = tg * 4 + s
                xt, xk = xb.next()
                DMA(P, "sp", xt[:], c.inp["x"][i * 128:(i + 1) * 128, :], [], [xk])
                x1, x1k = x1b.next()
                for half in range(2):
                    py, pyk = psy.next()
                    for dc in range(8):
                        MM(P, py[:, :], mg[:, dc, s * 128:(s + 1) * 128], Wout[:, dc, half * 512:(half + 1) * 512], dc == 0, dc == 7,
                           mgks + ["Wout"], [pyk])
                    TT(P, "dve", x1[:, half * 512:(half + 1) * 512], xt[:, half * 512:(half + 1) * 512], py[:, :], ALU.add,
                       [xk, pyk], [(x1k, half)])
                DMA(P, "sp", c.X1[i * 128:(i + 1) * 128, :], x1[:], [(x1k, 0), (x1k, 1)], ["X1"])
        P.end_phase()
        P.alloc_sems(c.es0, c.sems)
        with nc.Block() as block:
            P.emit(block, c.sems)


def phase_F(c):
    nc, P = c.nc, c.P
    GS = 8
    with ExitStack() as es:
        sb = lambda name, shape, dt: es.enter_context(nc.sbuf_tensor(name, shape, dt))
        Wq = sb("Wq", [128, 8, 2048], BF16)
        subk = sb("subk", [128, 16, 128], BF16)
        g2b = sb("g2b", [128, D], F32)
        gFb = sb("gFb", [128, D], F32)
        keyidx = sb("keyidx", [128, 2048], I32)
        posidx = sb("posidx", [128, 2048], I32)
        iotaA = sb("iotaA", [128, 2048], F32)
        cI = sb("cI", [128, 8], I32)
        with ExitStack() as es1:
            sb1 = lambda name, shape, dt: es1.enter_context(nc.sbuf_tensor(name, shape, dt))
            wst = Rot([sb1(f"wstF{i}", [128, 2048], F32) for i in range(2)], "wstF")
            iotaAi = sb1("iotaAi", [128, 2048], I32)
            for kc in range(8):
                st, sk = wst.next()
                DMA(P, "sp", st[:], c.inp["peer_w_q"][kc * 128:(kc + 1) * 128, :], [], [sk])
                CP(P, "dve" if kc % 2 == 0 else "act", Wq[:, kc, :], st[:], [sk], [("Wq", kc)])
            st, sk = wst.next()
            DMA(P, "sp", st[:], c.inp["subkT"].rearrange("d b k -> d (b k)"), [], [sk])
            CP(P, "dve", subk[:].rearrange("d b k -> d (b k)"), st[:], [sk], ["subk"])
            DMA(P, "sp", g2b[:], c.inp["ffn_g"].partition_broadcast(128), [], ["g2b"])
            DMA(P, "sp", gFb[:], c.inp["final_g"].partition_broadcast(128), [], ["gFb"])
            IOTA(P, keyidx[:], [[0, 16], [1, 128]], 0, 0, [], ["keyidx"])
            IOTA(P, posidx[:], [[0, 8], [1, 256]], 0, 0, [], ["posidx"])
            IOTA(P, iotaAi[:], [[0, 128], [1, 16]], 0, 0, [], ["iotaAi"])
            CP(P, "dve", iotaA[:], iotaAi[:], ["iotaAi"], ["iotaA"])
            for j, v in enumerate((-128, -256, 127, 255, 15, 4)):
                IOTA(P, cI[:, j:j + 1], [[0, 1]], v, 0, ["cI"], ["cI"])
            P.end_phase()
            P.alloc_sems(c.es0, c.sems)
            with nc.Block() as block:
                P.emit(block, c.sems)
        x1b = Rot([sb(f"x1F{i}", [128, D], F32) for i in range(2)], "x1F")
        h2b = Rot([sb(f"h2F{i}", [128, D], F32) for i in range(2)], "h2F")
        ssb = Rot([sb(f"ssF{i}", [128, 4], F32) for i in range(4)], "ssF")
        junk = sb("junkF", [128, D], BF16)
        prodb = Rot([sb(f"prodF{i}", [128, D], F32) for i in range(3)], "prodF")
        h2Tb = Rot([sb(f"h2T{i}", [128, 8, 128], BF16) for i in range(2)], "h2T")
        qTb = sb("qTbF", [128, 16, 128], BF16)
        Sc = sb("Sc", [128, 2048], F32)
        rep = sb("repF", [128, 256], F32)
        stop = sb("stop", [128, 16, 16], F32)
        itop_i = sb("itop_i", [128, 256], I32)
        itop_f = sb("itop_f", [128, 16, 16], F32)
        tmpA = sb("tmpA", [128, 2048], F32)
        tmpB = sb("tmpB", [128, 2048], F32)
        best = sb("best", [128, 8, 16], F32)
        pos_i = sb("pos_i", [128, 3, 128], I32)
        ab_f = sb("ab_f", [128, 2, 128], F32)
        sel_f = sb("sel_f", [128, 3, 128], F32)
        idxb = Rot([sb(f"idxF{i}", [128, 128], I32) for i in range(2)], "idxF")
        gwb = Rot([sb(f"gwF{i}", [128, 3, 128], F32) for i in range(2)], "gwF")
        gsum = sb("gsum", [128, 16], F32)
        ab = Rot([sb(f"aF{i}", [128, 2, 128], F32) for i in range(2)], "aF")
        uvb = Rot([sb(f"uvg{i}", [128, 2 * D], BF16) for i in range(12)], "uvg")
        dgb = Rot([sb(f"dg{i}", [128, 128], BF16) for i in range(4)], "dg")
        x2b = Rot([sb(f"x2F{i}", [128, D], F32) for i in range(2)], "x2F")
        ptq = Rot(c.ps[0:2], "ptq")
        psS = Rot(c.ps[2:4], "psS")
        pvb = Rot([(c.ps[4], c.ps[5]), (c.ps[6], c.ps[7])], "pv")
        Wqk = [("Wq", kc) for kc in range(8)]

        def route(i, st):
            x1, x1k = x1b.next()
            DMA(P, "sp", x1[:], c.X1[i * 128:(i + 1) * 128, :], ["X1"], [x1k])
            ss, ssk = ssb.next()
            ACT(P, junk[:], x1[:], AF.Square, [x1k], [ssk], accum=ss[:, 0:1])
            rstd_chain(P, ss, ssk)
            h2, h2k = h2b.next()
            STT(P, h2[:], x1[:], ss[:, 3:4], g2b[:], ALU.mult, ALU.mult, [x1k, ssk, "g2b"], [h2k])
            yield
            h2T, h2Tk = h2Tb.next()
            for half in range(2):
                pt, ptk = ptq.next()
                for j in range(4):
                    cc = half * 4 + j
                    TR(P, pt[:, j * 128:(j + 1) * 128], h2[:, cc * 128:(cc + 1) * 128], c.ident[:], [h2k, "ident"], [ptk])
                CP(P, "act", h2T[:, half * 4:(half + 1) * 4, :], pt[:].rearrange("p (j t) -> p j t", j=4), [ptk], [(h2Tk, half)])
            h2Tks = [(h2Tk, 0), (h2Tk, 1)]
            yield
            for b4 in range(4):
                pq, pqk = ptq.next()
                for j in range(4):
                    blk = b4 * 4 + j
                    for kc in range(8):
                        MM(P, pq[:, j * 128:(j + 1) * 128], Wq[:, kc, blk * 128:(blk + 1) * 128], h2T[:, kc, :], kc == 0, kc == 7,
                           Wqk + h2Tks, [pqk])
                CP(P, "act", qTb[:, b4 * 4:(b4 + 1) * 4, :], pq[:].rearrange("p (j t) -> p j t", j=4), [pqk], [("qTb", b4)])
                yield
            for b4 in range(4):
                pS, pSk = psS.next()
                for j in range(4):
                    blk = b4 * 4 + j
                    MM(P, pS[:, j * 128:(j + 1) * 128], qTb[:, blk, :], subk[:, blk, :], True, True, [("qTb", b4), "subk"], [pSk])
                STT(P, Sc[:, b4 * 512:(b4 + 1) * 512].bitcast(I32), pS[:, :].bitcast(I32), cI[:, 0:1],
                    keyidx[:, b4 * 512:(b4 + 1) * 512], ALU.bitwise_and, ALU.bitwise_or, [pSk, "cI", "keyidx"], [("Sc", b4)])
                yield
            for blk in range(16):
                sblk = Sc[:, blk * 128:(blk + 1) * 128]
                sck = ("Sc", blk // 4)
                P.op("dve", lambda e, o=stop[:, blk, 0:8], s=sblk: e.max(out=o, in_=s), [sck], ["stop"])
                P.op("dve", lambda e, o=rep[:, 0:128], m=stop[:, blk, 0:8], s=sblk: e.match_replace(
                    out=o, in_to_replace=m, in_values=s, imm_value=-1e30), [sck, "stop"], ["repF"])
                P.op("dve", lambda e, o=stop[:, blk, 8:16], s=rep[:, 0:128]: e.max(out=o, in_=s), ["repF"], ["stop"])
                yield
            stop2 = stop[:].rearrange("p b k -> p (b k)")
            TS(P, "dve", itop_i[:], stop2.bitcast(I32), cI[:, 2:3], None, ALU.bitwise_and, None, ["stop", "cI"], ["itop_i"])
            CP(P, "dve", itop_f[:].rearrange("p b k -> p (b k)"), itop_i[:], ["itop_i"], ["itop_f"])
            yield
            sv = stop[:].rearrange("p (h q) k -> p h q k", q=2)
            iv = itop_f[:].rearrange("p (h q) k -> p h q k", q=2)
            cand = tmpA[:].rearrange("p (h a b) -> p h a b", h=8, a=16)
            TT(P, "dve", cand, sv[:, :, 0, :].unsqueeze(3).to_broadcast([128, 8, 16, 16]),
               sv[:, :, 1, :].unsqueeze(2).to_broadcast([128, 8, 16, 16]), ALU.add, ["stop"], ["tmpA"])
            yield
            STT(P, tmpB[:].bitcast(I32), tmpA[:].bitcast(I32), cI[:, 1:2], posidx[:], ALU.bitwise_and, ALU.bitwise_or,
                ["tmpA", "cI", "posidx"], ["tmpB"])
            yield
            for h in range(8):
                sblk = tmpB[:, h * 256:(h + 1) * 256]
                P.op("dve", lambda e, o=best[:, h, 0:8], s=sblk: e.max(out=o, in_=s), ["tmpB"], ["best"])
                P.op("dve", lambda e, o=rep[:], m=best[:, h, 0:8], s=sblk: e.match_replace(
                    out=o, in_to_replace=m, in_values=s, imm_value=-1e30), ["tmpB", "best"], ["repF"])
                P.op("dve", lambda e, o=best[:, h, 8:16], s=rep[:]: e.max(out=o, in_=s), ["repF"], ["best"])
                yield
            best2 = best[:].rearrange("p h k -> p (h k)")
            TS(P, "dve", pos_i[:, 0, :], best2.bitcast(I32), cI[:, 3:4], None, ALU.bitwise_and, None, ["best", "cI"], ["pos_i"])
            TS(P, "dve", pos_i[:, 1, :], pos_i[:, 0, :], cI[:, 5:6], None, ALU.logical_shift_right, None, ["pos_i", "cI"], ["pos_i"])
            TS(P, "dve", pos_i[:, 2, :], pos_i[:, 0, :], cI[:, 4:5], None, ALU.bitwise_and, None, ["pos_i", "cI"], ["pos_i"])
            CP(P, "dve", ab_f[:], pos_i[:, 1:3, :], ["pos_i"], ["ab_f"])
            yield
            for q in range(2):
                akv = ab_f[:, q, :].rearrange("p (h k) -> p h k", h=8)
                eq = tmpA[:].rearrange("p (h k a) -> p h k a", h=8, k=16)
                TT(P, "dve", eq, akv.unsqueeze(3).to_broadcast([128, 8, 16, 16]),
                   iotaA[:].rearrange("p (h k a) -> p h k a", h=8, k=16), ALU.is_equal, ["ab_f", "iotaA"], ["tmpA"])
                yield
                pr = tmpB[:].rearrange("p (h k a) -> p h k a", h=8, k=16)
                TT(P, "dve", pr, eq, iv[:, :, q, :].unsqueeze(2).to_broadcast([128, 8, 16, 16]), ALU.mult,
                   ["tmpA", "itop_f"], ["tmpB"])
                yield
                P.op("dve", lambda e, o=sel_f[:, q, :], s=tmpB[:].rearrange("p (x a) -> p x a", a=16): e.tensor_reduce(
                    out=o, in_=s, axis=AX.X, op=ALU.add), ["tmpB"], ["sel_f"])
                yield
            STT(P, sel_f[:, 2, :], sel_f[:, 0, :], 128.0, sel_f[:, 1, :], ALU.mult, ALU.add, ["sel_f"], ["sel_f"])
            TS(P, "dve", sel_f[:, 2, :], sel_f[:, 2, :], 0.0, 16383.0, ALU.max, ALU.min, ["sel_f"], ["sel_f"])
            idx, idxk = idxb.next()
            CP(P, "dve", idx[:], sel_f[:, 2, :], ["sel_f"], [idxk])
            yield
            gw, gwk = gwb.next()
            v3 = lambda ap: ap.rearrange("p (h k) -> p h k", h=8)
            TT(P, "dve", v3(gw[:, 0, :]), best[:], best[:, :, 0:1].to_broadcast([128, 8, 16]), ALU.subtract, ["best"], [gwk])
            ACT(P, gw[:, 1, :], gw[:, 0, :], AF.Exp, [gwk], [gwk])
            yield
            P.op("dve", lambda e, o=gsum[:, 0:8], s=v3(gw[:, 1, :]): e.tensor_reduce(out=o, in_=s, axis=AX.X, op=ALU.add),
                 [gwk], ["gsum"])
            RECIP(P, gsum[:, 8:16], gsum[:, 0:8], ["gsum"], ["gsum"])
            TT(P, "dve", v3(gw[:, 2, :]), v3(gw[:, 1, :]), gsum[:, 8:16].unsqueeze(2).to_broadcast([128, 8, 16]), ALU.mult,
               [gwk, "gsum"], [gwk])
            st.update(x1=x1, x1k=x1k, h2=h2, h2k=h2k, idx=idx, idxk=idxk, gw=gw, gwk=gwk)
            yield

        def slots(i, st, bg):
            x1, x1k, h2, h2k, idx, idxk, gw, gwk = (st[k_] for k_ in ("x1", "x1k", "h2", "h2k", "idx", "idxk", "gw", "gwk"))
            a, ak = ab.next()
            (pv0, pv1), pvk = pvb.next()
            LAG = 3
            held = {}
            for s in range(128 + LAG):
                if s < 128:
                    uv, uvk = uvb.next()
                    held[s] = (uv, uvk)
                    P.dma("pool", lambda e, o=uv[:], ix=idx[:, s:s + 1]: e.indirect_dma_start(
                        out=o, out_offset=None, in_=c.UVB,
                        in_offset=bass.IndirectOffsetOnAxis(ap=ix.bitcast(U32), axis=0)), [idxk, "UVB"], [uvk])
                    pd, pdk = prodb.next()
                    TT(P, "dve", pd[:], uv[:, 0:D], h2[:], ALU.mult, [uvk, h2k], [pdk])
                    ACT(P, junk[:], pd[:], AF.Copy, [pdk], [(ak, s)], accum=a[:, 0, s:s + 1])
                    ACT(P, a[:, 1, s:s + 1], a[:, 0, s:s + 1], AF.Gelu, [(ak, s)], [(ak, "g", s)])
                r_ = s - LAG
                if r_ >= 0:
                    uv, uvk = held.pop(r_)
                    dg, dgk = dgb.next()
                    TS(P, "dve", dg[:], c.identb[:], a[:, 1, r_:r_ + 1], gw[:, 2, r_:r_ + 1], ALU.mult, ALU.mult,
                       ["identb", (ak, "g", r_), gwk], [dgk])
                    MM(P, pv0[:, :], dg[:], uv[:, D:D + 512], r_ == 0, r_ == 127, [dgk, uvk], [(pvk, 0)])
                    MM(P, pv1[:, :], dg[:], uv[:, D + 512:2 * D], r_ == 0, r_ == 127, [dgk, uvk], [(pvk, 1)])
                if bg is not None:
                    next(bg, None)
            if bg is not None:
                for _ in bg:
                    pass
            x2, x2k = x2b.next()
            TT(P, "dve", x2[:, 0:512], x1[:, 0:512], pv0[:, :], ALU.add, [x1k, (pvk, 0)], [(x2k, 0)])
            TT(P, "dve", x2[:, 512:1024], x1[:, 512:1024], pv1[:, :], ALU.add, [x1k, (pvk, 1)], [(x2k, 1)])
            ss2, ss2k = ssb.next()
            ACT(P, junk[:], x2[:], AF.Square, [(x2k, 0), (x2k, 1)], [ss2k], accum=ss2[:, 0:1])
            rstd_chain(P, ss2, ss2k)
            STT(P, x2[:], x2[:], ss2[:, 3:4], gFb[:], ALU.mult, ALU.mult, [(x2k, 0), (x2k, 1), ss2k, "gFb"], [(x2k, 0), (x2k, 1)])
            DMA(P, "sp", c.out[i * 128:(i + 1) * 128, :], x2[:], [(x2k, 0), (x2k, 1)], ["out"])

        states = [dict() for _ in range(NT)]
        for _ in route(0, states[0]):
            pass
        for i in range(NT):
            bg = route(i + 1, states[i + 1]) if i + 1 < NT else None
            slots(i, states[i], bg)
        P.end_phase()
        P.alloc_sems(c.es0, c.sems)
        with nc.Block() as block:
            P.emit(block, c.sems)


def build(upto="F", debug=False):
    nc = bass.Bass("TRN2", target_bir_lowering=False)
    c = Ctx()
    c.nc = nc
    c.P = Prog(nc, same_engine_sync=os.environ.get('MK_SES', '1') == '1')
    c.sems = {}
    inp = {}

    def din(name, shape, dt=F32):
        inp[name] = nc.dram_tensor(name, list(shape), dt, kind="ExternalInput").ap()

    din("x", [T, D])
    din("mem", [256, D])
    din("mix_g", [128, 8])
    din("mem_g", [128, 8])
    din("w_in", [D, IN_DIM])
    din("b_merge", [128, 24])
    din("pe_k", [64, 32])
    din("pe_v", [64, 32])
    din("cw_k", [64, 32, 64])
    din("cw_v", [64, 32, 64])
    din("w_mem_kv", [D, 512])
    din("w_sb_br", [384, D])
    din("w_nsa_br", [384, D])
    din("w_mem_br", [256, D])
    din("w_out", [D, D])
    din("ffn_g", [D])
    din("peer_w_q", [D, 2048])
    din("subkT", [128, 16, 128])
    din("peer_uv", [16384, 2 * D])
    din("final_g", [D])
    c.inp = inp
    kind = "ExternalOutput" if debug else "Internal"

    def scr(name, shape, dt):
        return nc.dram_tensor(name, list(shape), dt, kind=kind).ap()

    c.FM = scr("FM", [FM_ROWS, T], BF16)
    c.HT = scr("HT", [D, T], BF16)
    c.TMV = scr("TMV", [T, 640], BF16)
    c.GATES = scr("GATES", [T, 18], F32)
    c.SBOT = scr("SBOT", [384, T], BF16)
    c.NSAOT = scr("NSAOT", [384, T], BF16)
    c.MEMOT = scr("MEMOT", [256, T], BF16)
    c.X1 = scr("X1", [T, D], F32)
    c.UVB = nc.dram_tensor("UVB", [16384, 2 * D], BF16, kind="Internal").ap()
    c.out = nc.dram_tensor("out", [T, D], F32, kind="ExternalOutput").ap()

    with ExitStack() as es0:
        c.es0 = es0
        c.ps = [es0.enter_context(nc.psum_tensor(f"ps{i}", [128, 512], F32)) for i in range(8)]
        c.ident = es0.enter_context(nc.sbuf_tensor("ident", [128, 128], F32))
        c.identb = es0.enter_context(nc.sbuf_tensor("identb", [128, 128], BF16))
        P = c.P
        MEMSET(P, "pool", c.ident[:], 1.0, [], ["ident"])
        ASEL(P, c.ident[:], c.ident[:], [[1, 128]], ALU.is_equal, 0.0, 0, -1, ["ident"], ["ident"])
        CP(P, "pool", c.identb[:], c.ident[:], ["ident"], ["identb"])
        phases = [("A", phase_A), ("B", phase_B), ("C", phase_C), ("D", phase_D), ("E", phase_E), ("F", phase_F)]
        for name, fn in phases:
            fn(c)
            if name == upto:
                break
    return nc


def make_inputs(inputs, b):
    f = lambda a: np.ascontiguousarray(a, dtype=np.float32)
    gcol = lambda g: f(np.asarray(g).reshape(8, 128).T)
    m = {
        "x": f(inputs["x"][b]),
        "mem": f(inputs["mem"][b]),
        "mix_g": gcol(inputs["mix_norm_g"][0]),
        "mem_g": gcol(inputs["mem_norm_g"][0]),
        "w_in": f(inputs["w_in"][0]),
        "b_merge": f(np.asarray(inputs["b_merge"][0]).reshape(24, 128).T),
        "pe_k": f(np.asarray(inputs["cmp_pe_k"][0]).T),
        "pe_v": f(np.asarray(inputs["cmp_pe_v"][0]).T),
        "cw_k": f(np.asarray(inputs["cmp_w_k"][0]).transpose(1, 0, 2)),
        "cw_v": f(np.asarray(inputs["cmp_w_v"][0]).transpose(1, 0, 2)),
        "w_mem_kv": f(inputs["w_mem_kv"][0]),
        "w_sb_br": f(inputs["w_sb_br"][0]),
        "w_nsa_br": f(inputs["w_nsa_br"][0]),
        "w_mem_br": f(inputs["w_mem_br"][0]),
        "w_out": f(inputs["w_out"][0]),
        "ffn_g": f(inputs["ffn_norm_g"][0]),
        "peer_w_q": f(inputs["peer_w_q"][0]),
        "subkT": f(np.asarray(inputs["peer_subkeys"][0]).transpose(3, 0, 1, 2).reshape(128, 16, 128)),
        "peer_uv": np.ascontiguousarray(np.concatenate([np.asarray(inputs["peer_u"][0], dtype=np.float32),
                                                        np.asarray(inputs["peer_v"][0], dtype=np.float32)], axis=1)),
        "final_g": f(inputs["final_norm_g"]),
    }
    return m


def kernel(**inputs):
    nc = build()
    shared = None
    in_maps = []
    for b in range(8):
        m = make_inputs(inputs, b)
        if shared is None:
            shared = m
        else:
            for k in m:
                if k not in ("x", "mem"):
                    m[k] = shared[k]
        in_maps.append(m)
    res = run_bass_kernel_spmd(nc, in_maps, core_ids=list(range(8)))
    return np.stack([np.asarray(r["out"]) for r in res.results], axis=0).astype(np.float32)
```

```python
import os
import sys
import numpy as np
from contextlib import ExitStack
import concourse.bass as bass
import concourse.mybir as mybir
from concourse.bass_utils import run_bass_kernel_spmd

F32 = mybir.dt.float32
BF16 = mybir.dt.bfloat16
I32 = mybir.dt.int32
U32 = mybir.dt.uint32
AF = mybir.ActivationFunctionType
ALU = mybir.AluOpType
AX = mybir.AxisListType

T = 4096
D = 1024
NT = T // 128
IN_DIM = 5650
EPS = 1e-6
SLOPES = [2.0 ** (-8.0 * (h + 1) / 6) for h in range(6)]
BIG = 30000.0

ENGS = ("pe", "act", "dve", "pool", "sp")
DMA_RING = 8


class Prog:
    def __init__(self, nc, same_engine_sync=True):
        self.nc = nc
        self.ops = {e: [] for e in ENGS}
        self.cnt = {e: 0 for e in ENGS}
        self.dma_n = {e: 0 for e in ENGS}
        self.last_w = {}
        self.readers = {}
        self.waited = {}
        self.same_engine_sync = same_engine_sync
        self.fill_vals = set()
        self.fill_regs = {}

    def _deps(self, eng, reads, writes):
        need = {}

        def add(tok):
            if tok is None:
                return
            sk, val, teng = tok
            if teng == eng and sk[0] == "c":
                if not self.same_engine_sync or eng == "pe":
                    return
            if need.get(sk, 0) < val:
                need[sk] = val

        for r in reads:
            add(self.last_w.get(r))
        for w in writes:
            add(self.last_w.get(w))
            for t in self.readers.get(w, ()):
                add(t)
        out = []
        for sk, val in need.items():
            if self.waited.get((eng, sk), 0) >= val:
                continue
            self.waited[(eng, sk)] = val
            out.append((sk, val))
        return out

    def _commit(self, tok, reads, writes):
        for r in reads:
            self.readers.setdefault(r, []).append(tok)
        for w in writes:
            self.last_w[w] = tok
            self.readers[w] = []

    def op(self, eng, fn, reads=(), writes=()):
        reads = tuple(reads)
        writes = tuple(writes)
        waits = self._deps(eng, reads, writes)
        self.cnt[eng] += 1
        tok = (("c", eng), self.cnt[eng], eng)
        fr = sys._getframe(1)
        self.ops[eng].append(dict(fn=fn, waits=waits, inc=(("c", eng), 1),
                                  where=(fr.f_lineno, fr.f_back.f_lineno if fr.f_back else 0)))
        self._commit(tok, reads, writes)
        return tok

    def dma(self, eng, fn, reads=(), writes=()):
        reads = tuple(reads)
        writes = tuple(writes)
        n = self.dma_n[eng]
        self.dma_n[eng] += 1
        sk = ("d", eng, n % DMA_RING)
        val = 16 * (n // DMA_RING + 1)
        waits = self._deps(eng, reads, writes)
        if val > 16 and self.waited.get((eng, sk), 0) < val - 16:
            self.waited[(eng, sk)] = val - 16
            waits.append((sk, val - 16))
        tok = (sk, val, eng)
        self.ops[eng].append(dict(fn=fn, waits=waits, inc=(sk, 16)))
        self._commit(tok, reads, writes)
        return tok

    def finish(self, eng, toks):
        self.ops[eng].append(dict(fn=None, waits=[(sk, val) for sk, val, _ in toks], inc=None))

    def end_phase(self):
        targets = []
        for e in ENGS:
            if self.cnt[e] > 0:
                targets.append((("c", e), self.cnt[e]))
            n = self.dma_n[e]
            for r in range(min(n, DMA_RING)):
                last = ((n - 1 - r) // DMA_RING) * DMA_RING + r
                targets.append((("d", e, r), 16 * (last // DMA_RING + 1)))
        for e in ENGS:
            waits = []
            for sk, val in targets:
                if self.waited.get((e, sk), 0) >= val:
                    continue
                self.waited[(e, sk)] = val
                waits.append((sk, val))
            self.ops[e].append(dict(fn=None, waits=waits, inc=None))
        self.last_w = {}
        self.readers = {}

    def sem_keys(self):
        keys = set()
        for e in ENGS:
            for o in self.ops[e]:
                if o["inc"]:
                    keys.add(o["inc"][0])
                for sk, _ in o["waits"]:
                    keys.add(sk)
        return sorted(keys)

    def alloc_sems(self, es, sems):
        for k in self.sem_keys():
            if k not in sems:
                sems[k] = es.enter_context(self.nc.semaphore("s_" + "_".join(map(str, k))))

    def emit(self, block, sems):
        engobj = {"pe": "tensor", "act": "scalar", "dve": "vector", "pool": "gpsimd", "sp": "sync"}

        def make(e):
            ops = self.ops[e]

            def body(eng):
                if e == "pool":
                    self.fill_regs = {v: eng.to_reg(v) for v in sorted(self.fill_vals)}
                for o in ops:
                    for sk, val in o["waits"]:
                        eng.wait_ge(sems[sk], val)
                    if o["fn"] is not None:
                        try:
                            ins = o["fn"](eng)
                        except Exception:
                            print("EMIT FAILED at lines", o.get("where"))
                            raise
                        if o["inc"]:
                            ins.then_inc(sems[o["inc"][0]], o["inc"][1])
            return body

        for e in ENGS:
            if self.ops[e]:
                getattr(block, engobj[e])(make(e))
        self.ops = {e: [] for e in ENGS}


class Ctx:
    pass


class Rot:
    def __init__(self, tiles, name):
        self.tiles = tiles
        self.name = name
        self.i = 0

    def next(self):
        j = self.i % len(self.tiles)
        self.i += 1
        return self.tiles[j], (self.name, j)


def MM(P, out, lhsT, rhs, start, stop, r, w):
    return P.op("pe", lambda e: e.matmul(out, lhsT=lhsT, rhs=rhs, start=start, stop=stop), r, w)


def TR(P, out, in_, ident, r, w):
    return P.op("pe", lambda e: e.transpose(out, in_, ident), r, w)


def ACT(P, out, in_, func, r, w, scale=None, bias=None, accum=None):
    kw = {}
    if scale is not None:
        kw["scale"] = scale
    if bias is not None:
        kw["bias"] = bias
    if accum is not None:
        kw["accum_out"] = accum
    return P.op("act", lambda e: e.activation(out=out, in_=in_, func=func, **kw), r, w)


def TS(P, eng, out, in0, s1, s2, op0, op1, r, w):
    if op1 is None:
        return P.op(eng, lambda e: e.tensor_scalar(out=out, in0=in0, scalar1=s1, scalar2=None, op0=op0), r, w)
    return P.op(eng, lambda e: e.tensor_scalar(out=out, in0=in0, scalar1=s1, scalar2=s2, op0=op0, op1=op1), r, w)


def TT(P, eng, out, in0, in1, op, r, w):
    return P.op(eng, lambda e: e.tensor_tensor(out=out, in0=in0, in1=in1, op=op), r, w)


def STT(P, out, in0, scalar, in1, op0, op1, r, w):
    return P.op("dve", lambda e: e.scalar_tensor_tensor(out=out, in0=in0, scalar=scalar, in1=in1, op0=op0, op1=op1), r, w)


def CP(P, eng, out, in_, r, w):
    if eng == "act":
        return P.op("act", lambda e: e.copy(out=out, in_=in_), r, w)
    return P.op(eng, lambda e: e.tensor_copy(out=out, in_=in_), r, w)


def DMA(P, eng, out, in_, r, w):
    return P.dma(eng, lambda e: e.dma_start(out=out, in_=in_), r, w)


def MEMSET(P, eng, ap, val, r, w):
    return P.op(eng, lambda e: e.memset(ap, val), r, w)


def ASEL(P, out, in_, pattern, cmp, fill, base, cm, r, w):
    P.fill_vals.add(float(fill))
    return P.op("pool", lambda e: e.affine_select(out=out, in_=in_, pattern=pattern, compare_op=cmp,
                                                  fill=P.fill_regs[float(fill)], base=base, channel_multiplier=cm), r, w)


def IOTA(P, out, pattern, base, cm, r, w):
    return P.op("pool", lambda e: e.iota(out, pattern=pattern, base=base, channel_multiplier=cm), r, w)


def RECIP(P, out, in_, r, w):
    return P.op("dve", lambda e: e.reciprocal(out=out, in_=in_), r, w)


def rstd_chain(P, ss, key):
    TS(P, "dve", ss[:, 1:2], ss[:, 0:1], 1.0 / D, EPS, ALU.mult, ALU.add, [key], [key])
    P.op("act", lambda e: e.sqrt(out=ss[:, 2:3], in_=ss[:, 1:2]), [key], [key])
    RECIP(P, ss[:, 3:4], ss[:, 2:3], [key], [key])


FM_QSB, FM_KSB, FM_QNSA, FM_KCMP, FM_VCMP, FM_KSLC, FM_KWIN, FM_QMEM = 0, 384, 768, 1152, 1280, 1408, 1536, 1664
FM_ROWS = 1920
FM_COLMAP = [(0, 0, 768), (768, 1152, 384), (1152, 1536, 128), (1280, 1664, 128), (1408, 1792, 128),
             (1536, 2048, 128), (1664, 2322, 256)]
TM_COLMAP = [(0, 768, 384), (384, 1920, 128), (512, 2176, 128), (640, 2304, 18)]
TM_COLS = 658
Q_CHUNKS = {0, 1, 2, 6, 7, 8, 13, 14}


def phase_A(c):
    nc, P = c.nc, c.P
    with ExitStack() as es:
        sb = lambda name, shape, dt: es.enter_context(nc.sbuf_tensor(name, shape, dt))
        Wfm = sb("Wfm", [128, 8, FM_ROWS], BF16)
        Wtm = sb("Wtm", [128, 8, TM_COLS], BF16)
        wst = Rot([sb(f"wst{i}", [128, 2578], F32) for i in range(2)], "wst")
        gcol = sb("gcolA", [128, 8], F32)
        xbuf = Rot([sb(f"xt{i}", [128, D], F32) for i in range(2)], "xt")
        xsbuf = Rot([sb(f"xs{i}", [128, D], F32) for i in range(2)], "xs")
        junk = sb("junkA", [128, D], F32)
        ssbuf = Rot([sb(f"ss{i}", [128, 4], F32) for i in range(4)], "ss")
        hTg = Rot([sb(f"hTg{i}", [128, 8, 512], BF16) for i in range(2)], "hTg")
        FMst = Rot([sb(f"FMst{i}", [128, 15, 512], BF16) for i in range(2)], "FMst")
        TMst = Rot([sb(f"TMst{i}", [128, 640], BF16) for i in range(3)], "TMst")
        gst = Rot([sb(f"gst{i}", [128, 18], F32) for i in range(3)], "gst")
        pstr = Rot(c.ps[0:2], "ps_tr")
        psfm = Rot(c.ps[2:5], "ps_fm")
        pstm = Rot(c.ps[5:8], "ps_tm")

        DMA(P, "sp", gcol[:], c.inp["mix_g"], [], ["gcol"])
        for kc in range(8):
            st, sk = wst.next()
            DMA(P, "sp", st[:], c.inp["w_in"][kc * 128:(kc + 1) * 128, 0:2578], [], [sk])
            n = 0
            for (dst, cm) in ((Wfm, FM_COLMAP), (Wtm, TM_COLMAP)):
                for (dc, sc, w) in cm:
                    if n % 2 == 0:
                        TS(P, "dve", dst[:, kc, dc:dc + w], st[:, sc:sc + w], gcol[:, kc:kc + 1], None, ALU.mult, None,
                           [sk, "gcol"], [("W", kc)])
                    else:
                        ACT(P, dst[:, kc, dc:dc + w], st[:, sc:sc + w], AF.Copy, [sk, "gcol"], [("W", kc)],
                            scale=gcol[:, kc:kc + 1])
                    n += 1
        Wkeys = [("W", kc) for kc in range(8)]

        for tg in range(8):
            hT, hk = hTg.next()
            hpieces = [(hk, s, half) for s in range(4) for half in range(2)]
            for s in range(4):
                i = tg * 4 + s
                xt, xk = xbuf.next()
                DMA(P, "sp", xt[:], c.inp["x"][i * 128:(i + 1) * 128, :], [], [xk])
                ss, ssk = ssbuf.next()
                ACT(P, junk[:], xt[:], AF.Square, [xk], ["junkA", ssk], accum=ss[:, 0:1])
                rstd_chain(P, ss, ssk)
                xs, xsk = xsbuf.next()
                TS(P, "dve", xs[:], xt[:], ss[:, 3:4], None, ALU.mult, None, [xk, ssk], [xsk])
                for half in range(2):
                    pt, ptk = pstr.next()
                    for j in range(4):
                        cc = half * 4 + j
                        TR(P, pt[:, j * 128:(j + 1) * 128], xs[:, cc * 128:(cc + 1) * 128], c.ident[:], [xsk, "ident"], [ptk])
                    dst = hT[:, half * 4:(half + 1) * 4, s * 128:(s + 1) * 128]
                    src = pt[:].rearrange("p (j t) -> p j t", j=4)
                    CP(P, "act" if half == 0 else "dve", dst, src, [ptk], [(hk, s, half)])
            fst, fsk = FMst.next()
            for ch in range(15):
                pf, pfk = psfm.next()
                for kc in range(8):
                    MM(P, pf[:, :], Wfm[:, kc, ch * 128:(ch + 1) * 128], hT[:, kc, :], kc == 0, kc == 7,
                       hpieces + [("W", kc)], [pfk])
                sc = 0.125 if ch in Q_CHUNKS else 1.0
                if ch % 2 == 0:
                    ACT(P, fst[:, ch, :], pf[:, :], AF.Copy, [pfk], [(fsk, ch)], scale=sc)
                else:
                    TS(P, "dve", fst[:, ch, :], pf[:, :], sc, None, ALU.mult, None, [pfk], [(fsk, ch)])
            DMA(P, "sp", c.FM.rearrange("(c p) t -> p c t", p=128)[:, :, tg * 512:(tg + 1) * 512], fst[:],
                [(fsk, ch) for ch in range(15)], ["FM"])
            DMA(P, "sp", c.HT.rearrange("(c p) t -> p c t", p=128)[:, :, tg * 512:(tg + 1) * 512], hT[:],
                hpieces, ["HT"])
            for s in range(4):
                i = tg * 4 + s
                pa, pak = pstm.next()
                for kc in range(8):
                    MM(P, pa[:, 0:512], hT[:, kc, s * 128:(s + 1) * 128], Wtm[:, kc, 0:512], kc == 0, kc == 7,
                       hpieces + [("W", kc)], [pak])
                tst, tsk = TMst.next()
                CP(P, "dve", tst[:, 0:512], pa[:, 0:512], [pak], [tsk])
                pb, pbk = pstm.next()
                for kc in range(8):
                    MM(P, pb[:, 0:146], hT[:, kc, s * 128:(s + 1) * 128], Wtm[:, kc, 512:658], kc == 0, kc == 7,
                       hpieces + [("W", kc)], [pbk])
                CP(P, "dve", tst[:, 512:640], pb[:, 0:128], [pbk], [tsk])
                gs, gsk = gst.next()
                ACT(P, gs[:], pb[:, 128:146], AF.Sigmoid, [pbk], [gsk])
                DMA(P, "sp", c.TMV[i * 128:(i + 1) * 128, :], tst[:], [tsk], ["TMV"])
                DMA(P, "sp", c.GATES[i * 128:(i + 1) * 128, :], gs[:], [gsk], ["GATES"])
        P.end_phase()
        P.alloc_sems(c.es0, c.sems)
        with nc.Block() as block:
            P.emit(block, c.sems)


def phase_B(c):
    nc, P = c.nc, c.P
    with ExitStack() as es:
        sb = lambda name, shape, dt: es.enter_context(nc.sbuf_tensor(name, shape, dt))
        qT = [sb(f"qTb{j}", [128, T], BF16) for j in range(3)]
        kT = [sb(f"kTb{j}", [128, T], BF16) for j in range(3)]
        V = sb("Vsb", [128, NT, 384], BF16)
        ntri = sb("ntri", [128, 128], BF16)
        nones = sb("nones", [128, 128], BF16)
        Ebuf = Rot([sb(f"E{i}", [128, 512], F32) for i in range(3)], "E")
        SPbuf = Rot([sb(f"SP{i}", [128, 512], BF16) for i in range(4)], "SP")
        Sbuf = Rot([sb(f"Ssum{i}", [128, 512], BF16) for i in range(3)], "Ssum")
        abuf = Rot([sb(f"aT{i}", [128, 512], BF16) for i in range(3)], "aT")
        obuf = Rot([sb(f"sbo{i}", [64, 512], BF16) for i in range(2)], "sbo")
        psA = Rot(c.ps[0:3], "psA")
        psB = Rot(c.ps[3:6], "psB")
        psO = Rot(c.ps[6:8], "psO")
        for j in range(3):
            DMA(P, "sp", qT[j][:], c.FM[FM_QSB + 128 * j:FM_QSB + 128 * (j + 1), :], ["FM"], [("qT", j)])
            DMA(P, "sp", kT[j][:], c.FM[FM_KSB + 128 * j:FM_KSB + 128 * (j + 1), :], ["FM"], [("kT", j)])
        DMA(P, "sp", V[:], c.TMV.rearrange("(c p) f -> p c f", p=128)[:, :, 0:384], ["TMV"], ["V"])
        MEMSET(P, "pool", nones[:], -1.0, [], ["nones"])
        MEMSET(P, "pool", ntri[:], -1.0, [], ["ntri"])
        ASEL(P, ntri[:], ntri[:], [[-1, 128]], ALU.is_ge, 0.0, 0, 1, ["ntri"], ["ntri"])
        uvst = Rot([sb(f"uvst{i}", [128, 2 * D], BF16) for i in range(4)], "uvst")

        def convert_chunk(ch):
            t_, tk = uvst.next()
            P.dma("pool", lambda e, o=t_[:], i_=c.inp["peer_uv"][ch * 128:(ch + 1) * 128, :]: e.dma_start(out=o, in_=i_), [], [tk])
            DMA(P, "sp", c.UVB[ch * 128:(ch + 1) * 128, :], t_[:], [tk], ["UVB"])
        steps = []
        for h in range(6):
            for g in range(8):
                nch = 4 * g + 4
                for idx_, cch in enumerate(range(nch - 1, -1, -1)):
                    steps.append(dict(h=h, g=g, cch=cch, first=idx_ == 0, last=cch == 0))
        N = len(steps)
        cur = dict(po=None, pok=None, ssum=None, ssumk=None)

        def S12(st):
            h, g, cch = st["h"], st["g"], st["cch"]
            j, half = h // 2, h % 2
            pr = slice(64 * half, 64 * half + 64)
            st["qs"] = qT[j][pr, g * 512:(g + 1) * 512]
            st["ks"] = kT[j][pr, cch * 128:(cch + 1) * 128]
            st["rk"] = [("kT", j), ("qT", j)]
            st["m"] = cch - 4 * g
            if st["first"]:
                cur["po"], cur["pok"] = psO.next()
                cur["ssum"] = cur["ssumk"] = None
            st["po"], st["pok"] = cur["po"], cur["pok"]
            pa, pak = psA.next()
            MM(P, pa[:, :], st["ks"], st["qs"], True, True, st["rk"], [pak])
            E, Ek = Ebuf.next()
            ACT(P, E[:], pa[:, :], AF.Exp, [pak], [Ek])
            SP, SPk = SPbuf.next()
            ACT(P, SP[:], E[:], AF.Ln, [Ek], [SPk], bias=1.0)
            if st["m"] >= 0:
                ASEL(P, SP[:], SP[:], [[1, 512]], ALU.is_gt, 0.0, -128 * st["m"], -1, [SPk], [SPk])
            st["SP"], st["SPk"] = SP, SPk
            st["ssum_prev"], st["ssum_prevk"] = cur["ssum"], cur["ssumk"]
            if not st["last"]:
                if st["first"]:
                    cur["ssum"], cur["ssumk"] = SP, SPk
                else:
                    sn, snk = Sbuf.next()
                    TT(P, "pool", sn[:], cur["ssum"][:], SP[:], ALU.add, [cur["ssumk"], SPk], [snk])
                    cur["ssum"], cur["ssumk"] = sn, snk

        def S34(st):
            pb, pbk = psB.next()
            MM(P, pb[:, :], ntri[:], st["SP"][:], True, False, ["ntri", st["SPk"]], [pbk])
            if not st["first"]:
                MM(P, pb[:, :], nones[:], st["ssum_prev"][:], False, False, ["nones", st["ssum_prevk"]], [pbk])
            MM(P, pb[:, :], st["ks"], st["qs"], False, True, st["rk"], [pbk])
            aT, aTk = abuf.next()
            ACT(P, aT[:], pb[:, :], AF.Exp, [pbk], [aTk])
            if st["m"] >= 0:
                ASEL(P, aT[:], aT[:], [[1, 512]], ALU.is_gt, 0.0, -128 * st["m"], -1, [aTk], [aTk])
            st["aT"], st["aTk"] = aT, aTk

        def S5(st):
            h, g, cch = st["h"], st["g"], st["cch"]
            MM(P, st["po"][0:64, :], V[:, cch, 64 * h:64 * h + 64], st["aT"][:], st["first"], st["last"], ["V", st["aTk"]], [st["pok"]])
            if st["last"]:
                ob, obk = obuf.next()
                CP(P, "dve", ob[:], st["po"][0:64, :], [st["pok"]], [obk])
                DMA(P, "sp", c.SBOT[64 * h:64 * h + 64, g * 512:(g + 1) * 512], ob[:], [obk], ["SBOT"])

        for n in range(N + 2):
            if n % 6 == 0 and n // 6 < 128:
                convert_chunk(n // 6)
            if n < N:
                S12(steps[n])
            if 0 <= n - 1 < N:
                S34(steps[n - 1])
            if 0 <= n - 2 < N:
                S5(steps[n - 2])
                steps[n - 2].clear()
        P.end_phase()
        P.alloc_sems(c.es0, c.sems)
        with nc.Block() as block:
            P.emit(block, c.sems)


def phase_C(c):
    nc, P = c.nc, c.P
    with ExitStack() as es:
        sb = lambda name, shape, dt: es.enter_context(nc.sbuf_tensor(name, shape, dt))
        qaug = [sb(f"qaug{h}", [128, T], BF16) for h in range(6)]
        kslc = [sb(f"kslc{g}", [128, T], BF16) for g in range(2)]
        kwin = [sb(f"kwin{g}", [64, T], BF16) for g in range(2)]
        kcmp = [sb(f"kcmp{g}", [64, T], BF16) for g in range(2)]
        vcmp = [sb(f"vcmp{g}", [64, T], BF16) for g in range(2)]
        kcT = [sb(f"kcT{g}", [64, 256], BF16) for g in range(2)]
        vcaug = [sb(f"vcaug{g}", [128, 2, 129], BF16) for g in range(2)]
        vwin = sb("vwin", [128, NT, 2, 65], BF16)
        vslc = sb("vslc", [128, NT, 2, 65], BF16)
        vstage = sb("vstage", [128, NT, 256], BF16)
        gates = sb("gatesC", [128, NT, 18], F32)
        pe = [sb("pek_sb", [64, 32], F32), sb("pev_sb", [64, 32], F32)]
        cwst = sb("cwst", [64, 2048], F32)
        cw = [sb("cwk", [64, 32, 64], BF16), sb("cwv", [64, 32, 64], BF16)]
        tmpb = Rot([sb(f"ctmp{i}", [64, 256], BF16) for i in range(3)], "ctmp")
        itmp = sb("itmp", [128, 64], I32)
        ftmp = sb("ftmp", [128, 64], F32)
        bias_sel = sb("bias_sel", [128, 6], F32)
        bias_win = sb("bias_win", [128, 6, 5], F32)
        bias_cmp = sb("bias_cmp", [128, 6, 64], F32)
        IOTi = sb("IOTi", [128, 128], I32)
        IOT = sb("IOT", [128, 128], F32)
        e32b = Rot([sb(f"e32_{i}", [128, 128], F32) for i in range(4)], "e32")
        ebb = Rot([sb(f"eb{i}", [128, 128], BF16) for i in range(7)], "eb")
        obuf = Rot([sb(f"oC{i}", [128, 384], F32) for i in range(2)], "oC")
        impb = Rot([sb(f"imp{i}", [128, 64], F32) for i in range(2)], "imp")
        wb = Rot([sb(f"wC{i}", [128, 4], F32) for i in range(4)], "wC")
        m8b = Rot([sb(f"m8_{i}", [128, 16], F32) for i in range(2)], "m8")
        repb = Rot([sb(f"rep{i}", [128, 64], F32) for i in range(2)], "rep")
        selb = Rot([sb(f"selp{i}", [128, 128], F32) for i in range(2)], "selp")
        Ctb = Rot([sb(f"Ct{i}", [128, 128], F32) for i in range(2)], "Ct")
        ostb = Rot([sb(f"ostC{i}", [128, 3, 128], BF16) for i in range(2)], "ostC")
        pss = Rot(c.ps[0:3], "pss")
        psacc = Rot(c.ps[3:6], "psacc")
        pstr = Rot(c.ps[6:8], "pstrC")
        pk = c.ps[6]

        for h in range(6):
            DMA(P, "sp", qaug[h][0:64, :], c.FM[FM_QNSA + 64 * h:FM_QNSA + 64 * (h + 1), :], ["FM"], [("q", h)])
        for g in range(2):
            DMA(P, "sp", kslc[g][0:64, :], c.FM[FM_KSLC + 64 * g:FM_KSLC + 64 * (g + 1), :], ["FM"], [("kslc", g)])
            DMA(P, "sp", kwin[g][:], c.FM[FM_KWIN + 64 * g:FM_KWIN + 64 * (g + 1), :], ["FM"], [("kwin", g)])
            DMA(P, "sp", kcmp[g][:], c.FM[FM_KCMP + 64 * g:FM_KCMP + 64 * (g + 1), :], ["FM"], [("kcmp", g)])
            DMA(P, "sp", vcmp[g][:], c.FM[FM_VCMP + 64 * g:FM_VCMP + 64 * (g + 1), :], ["FM"], [("vcmp", g)])
        DMA(P, "sp", vstage[:], c.TMV.rearrange("(c p) f -> p c f", p=128)[:, :, 384:640], ["TMV"], ["vstage"])
        DMA(P, "sp", gates[:], c.GATES.rearrange("(c p) f -> p c f", p=128), ["GATES"], ["gates"])
        DMA(P, "sp", pe[0][:], c.inp["pe_k"], [], ["pe0"])
        DMA(P, "sp", pe[1][:], c.inp["pe_v"], [], ["pe1"])
        for kv, nm in ((0, "cw_k"), (1, "cw_v")):
            DMA(P, "sp", cwst[:], c.inp[nm].rearrange("d l e -> d (l e)"), [], ["cwst"])
            CP(P, "dve", cw[kv][:].rearrange("d l e -> d (l e)"), cwst[:], ["cwst"], [("cw", kv)])
        CP(P, "dve", vslc[:, :, :, 0:64], vstage[:, :, 0:128].rearrange("p c (g d) -> p c g d", g=2), ["vstage"], ["vslc"])
        CP(P, "pool", vwin[:, :, :, 0:64], vstage[:, :, 128:256].rearrange("p c (g d) -> p c g d", g=2), ["vstage"], ["vwin"])
        MEMSET(P, "dve", vslc[:, :, :, 64:65], 1.0, ["vslc"], ["vslc"])
        MEMSET(P, "pool", vwin[:, :, :, 64:65], 1.0, ["vwin"], ["vwin"])
        for g in range(2):
            MEMSET(P, "pool", kslc[g][64:128, :], 1.0, [], [("kx", g)])
            ASEL(P, kslc[g][64:128, :], kslc[g][64:128, :], [[1, T]], ALU.is_ge, 0.0, 0, -64, [("kx", g)], [("kx", g)])
            ASEL(P, kslc[g][64:128, :], kslc[g][64:128, :], [[-1, T]], ALU.is_ge, 0.0, 63, 64, [("kx", g)], [("kx", g)])
        IOTA(P, itmp[0:64, 0:1], [[0, 1]], 0, 1, [], ["itmp"])
        IOTA(P, itmp[64:128, 0:1], [[0, 1]], 0, 1, ["itmp"], ["itmp"])
        CP(P, "dve", ftmp[:, 0:1], itmp[:, 0:1], ["itmp"], ["ftmp"])
        for h in range(6):
            TS(P, "dve", bias_sel[:, h:h + 1], ftmp[:, 0:1], SLOPES[h], None, ALU.mult, None, ["ftmp"], ["bias_sel"])
        IOTA(P, itmp[:, 0:5], [[128, 5]], -512, 1, ["itmp", "ftmp"], ["itmp"])
        CP(P, "dve", ftmp[:, 0:5], itmp[:, 0:5], ["itmp"], ["ftmp"])
        for h in range(6):
            TS(P, "dve", bias_win[:, h, :], ftmp[:, 0:5], SLOPES[h], None, ALU.mult, None, ["ftmp"], ["bias_win"])
        IOTA(P, itmp[:, 0:64], [[2048, 2], [-128, 32]], 31, 16, ["itmp", "ftmp"], ["itmp"])
        CP(P, "dve", ftmp[:, 0:64], itmp[:, 0:64], ["itmp"], ["ftmp"])
        for h in range(6):
            TS(P, "dve", bias_cmp[:, h, :], ftmp[:, 0:64], SLOPES[h], None, ALU.mult, None, ["ftmp"], ["bias_cmp"])
        IOTA(P, IOTi[64:128, :], [[-1, 128]], 0, 64, [], ["IOTi"])
        CP(P, "dve", IOT[64:128, :], IOTi[64:128, :], ["IOTi"], ["IOT"])
        for g in range(2):
            MEMSET(P, "pool", vcaug[g][:], 1.0, [], [("vcaug", g)])
            for ci in range(2):
                ASEL(P, vcaug[g][:, ci, 65:129], vcaug[g][:, ci, 65:129], [[-4, 64]], ALU.is_ge, 0.0, 128 * ci, 1,
                     [("vcaug", g)], [("vcaug", g)])
                ASEL(P, vcaug[g][:, ci, 65:129], vcaug[g][:, ci, 65:129], [[4, 64]], ALU.is_ge, 0.0, 3 - 128 * ci, -1,
                     [("vcaug", g)], [("vcaug", g)])
        for s_ in selb.tiles:
            pass
        for j in range(2):
            MEMSET(P, "dve", selb.tiles[j][:, 0:64], 0.0, [], [("selp", j)])
        for g in range(2):
            kview = kcmp[g][:].rearrange("p (n s) -> p n s", s=16)
            vview = vcmp[g][:].rearrange("p (n s) -> p n s", s=16)
            for l in range(32):
                tmp, tk = tmpb.next()
                TS(P, "dve" if l % 2 == 0 else "pool", tmp[:, 0:255], kview[:, l // 16:l // 16 + 255, l % 16],
                   pe[0][:, l:l + 1], None, ALU.add, None, [("kcmp", g), "pe0"], [tk])
                MM(P, pk[0:64, 0:255], cw[0][:, l, :], tmp[:, 0:255], l == 0, l == 31, [("cw", 0), tk], [("pstrC", 0)])
            CP(P, "dve", kcT[g][:, 0:255], pk[0:64, 0:255], [("pstrC", 0)], [("kcT", g)])
            for ci in range(2):
                rows = 128 if ci == 0 else 127
                for l in range(32):
                    tmp, tk = tmpb.next()
                    n0 = l // 16 + ci * 128
                    TS(P, "dve" if l % 2 == 0 else "pool", tmp[:, 0:rows], vview[:, n0:n0 + rows, l % 16],
                       pe[1][:, l:l + 1], None, ALU.add, None, [("vcmp", g), "pe1"], [tk])
                    MM(P, pk[0:rows, 256:320], tmp[:, 0:rows], cw[1][:, l, :], l == 0, l == 31, [("cw", 1), tk], [("pstrC", 0)])
                CP(P, "dve", vcaug[g][0:rows, ci, 0:64], pk[0:rows, 256:320], [("pstrC", 0)], [("vcaug", g)])

        def consume(pacc, pacck, o, ok, h, i, gcol, first):
            w, wk = wb.next()
            TS(P, "dve", w[:, 0:1], pacc[:, 64:65], 1e-30, None, ALU.max, None, [pacck], [wk])
            RECIP(P, w[:, 1:2], w[:, 0:1], [wk], [wk])
            TT(P, "dve", w[:, 2:3], w[:, 1:2], gates[:, i, gcol:gcol + 1], ALU.mult, [wk, "gates"], [wk])
            if first:
                TS(P, "dve", o[:, 64 * h:64 * h + 64], pacc[:, 0:64], w[:, 2:3], None, ALU.mult, None, [pacck, wk], [(ok, h)])
            else:
                STT(P, o[:, 64 * h:64 * h + 64], pacc[:, 0:64], w[:, 2:3], o[:, 64 * h:64 * h + 64], ALU.mult, ALU.add,
                    [pacck, wk, (ok, h)], [(ok, h)])
            return w, wk

        from collections import deque
        fifo = deque()
        LAGC = 3

        def defer(fn):
            fifo.append(fn)
            while len(fifo) > LAGC:
                fifo.popleft()()

        def flush():
            while fifo:
                fifo.popleft()()

        def consume_cmp(pc, pck, o, ok, h, i, hh, imp, impk):
            w, wk = consume(pc, pck, o, ok, h, i, 3 * h + 0, True)
            if hh == 0:
                TS(P, "dve", imp[:], pc[:, 65:129], w[:, 1:2], None, ALU.mult, None, [pck, wk], [impk])
            else:
                STT(P, imp[:], pc[:, 65:129], w[:, 1:2], imp[:], ALU.mult, ALU.add, [pck, wk, impk], [impk])

        for i in range(NT):
            t0 = 128 * i
            qc = slice(t0, t0 + 128)
            o, ok = obuf.next()
            for g in range(2):
                imp, impk = impb.next()
                for hh in range(3):
                    h = 3 * g + hh
                    nvalid = min(255, (t0 + 96) // 16 + 1)
                    chunks = [(0, min(128, nvalid))] + ([(1, nvalid - 128)] if nvalid > 128 else [])
                    pc, pck = psacc.next()
                    for ni, (ci, rows) in enumerate(chunks):
                        ps_, psk = pss.next()
                        MM(P, ps_[0:rows, 0:128], kcT[g][:, ci * 128:ci * 128 + rows], qaug[h][0:64, qc], True, True,
                           [("kcT", g), ("q", h)], [psk])
                        e32, e32k = e32b.next()
                        ACT(P, e32[0:rows, :], ps_[0:rows, 0:128], AF.Exp, [psk, "bias_cmp"], [e32k],
                            bias=bias_cmp[0:rows, h, ci * 32 + i:ci * 32 + i + 1])
                        eb, ebk = ebb.next()
                        ASEL(P, eb[0:rows, :], e32[0:rows, :], [[1, 128]], ALU.is_ge, 0.0, t0 - 2048 * ci - 31, -16,
                             [e32k], [ebk])
                        defer(lambda pc=pc, pck=pck, eb=eb, ebk=ebk, rows=rows, ci=ci, g=g, st_=(ni == 0), sp_=(ni == len(chunks) - 1):
                              MM(P, pc[:, 0:129], eb[0:rows, :], vcaug[g][0:rows, ci, :], st_, sp_, [ebk, ("vcaug", g)], [pck]))
                    defer(lambda pc=pc, pck=pck, o=o, ok=ok, h=h, i=i, hh=hh, imp=imp, impk=impk:
                          consume_cmp(pc, pck, o, ok, h, i, hh, imp, impk))
                    pw, pwk = psacc.next()
                    cl = list(range(max(0, i - 4), i + 1))
                    for ni, cch in enumerate(cl):
                        dc = cch - i
                        ps_, psk = pss.next()
                        MM(P, ps_[:, 0:128], kwin[g][:, cch * 128:(cch + 1) * 128], qaug[h][0:64, qc], True, True,
                           [("kwin", g), ("q", h)], [psk])
                        eb, ebk = ebb.next()
                        bw = bias_win[:, h, dc + 4:dc + 5]
                        if dc == 0 or dc == -4:
                            e32, e32k = e32b.next()
                            ACT(P, e32[:], ps_[:, 0:128], AF.Exp, [psk, "bias_win"], [e32k], bias=bw)
                            if dc == 0:
                                ASEL(P, eb[:], e32[:], [[1, 128]], ALU.is_ge, 0.0, 0, -1, [e32k], [ebk])
                            else:
                                ASEL(P, eb[:], e32[:], [[-1, 128]], ALU.is_gt, 0.0, 0, 1, [e32k], [ebk])
                        else:
                            ACT(P, eb[:], ps_[:, 0:128], AF.Exp, [psk, "bias_win"], [ebk], bias=bw)
                        defer(lambda pw=pw, pwk=pwk, eb=eb, ebk=ebk, cch=cch, g=g, st_=(ni == 0), sp_=(ni == len(cl) - 1):
                              MM(P, pw[:, 0:65], eb[:], vwin[:, cch, g, :], st_, sp_, [ebk, "vwin"], [pwk]))
                    defer(lambda pw=pw, pwk=pwk, o=o, ok=ok, h=h, i=i: consume(pw, pwk, o, ok, h, i, 3 * h + 2, False))
                flush()
                ASEL(P, imp[:], imp[:], [[-64, 64]], ALU.is_ge, 1e4, t0 - 128, 1, [impk], [impk])
                ASEL(P, imp[:], imp[:], [[-64, 64]], ALU.is_ge, -1.0, t0, 1, [impk], [impk])
                MEMSET(P, "pool", imp[:, 0:1], 1e4, [impk], [impk])
                m8, m8k = m8b.next()
                rep, repk = repb.next()
                P.op("dve", lambda e, m8=m8, imp=imp: e.max(out=m8[:, 0:8], in_=imp[:]), [impk], [m8k])
                P.op("dve", lambda e, m8=m8, imp=imp, rep=rep: e.match_replace(out=rep[:], in_to_replace=m8[:, 0:8],
                                                                                 in_values=imp[:], imm_value=-1e30),
                     [impk, m8k], [repk])
                P.op("dve", lambda e, m8=m8, rep=rep: e.max(out=m8[:, 8:16], in_=rep[:]), [repk, m8k], [m8k])
                selp, selk = selb.next()
                TS(P, "dve", selp[:, 64:128], imp[:], m8[:, 15:16], None, ALU.is_ge, None, [impk, m8k], [selk])
                pt_, ptk = pstr.next()
                TR(P, pt_[:, 0:128], selp[:], c.ident[:], [selk, "ident"], [ptk])
                for hh in range(3):
                    h = 3 * g + hh
                    Ct, Ctk = Ctb.next()
                    TS(P, "pool", Ct[64:128, :], IOT[64:128, :], SLOPES[h], -BIG - SLOPES[h] * t0, ALU.mult, ALU.add,
                       ["IOT"], [Ctk])
                    STT(P, qaug[h][64:128, qc], pt_[64:128, 0:128], BIG, Ct[64:128, :], ALU.mult, ALU.add,
                        [ptk, Ctk], [("qm", h, i)])
                for hh in range(3):
                    h = 3 * g + hh
                    psl, pslk = psacc.next()
                    for cch in range(i + 1):
                        ps_, psk = pss.next()
                        MM(P, ps_[:, 0:128], kslc[g][:, cch * 128:(cch + 1) * 128], qaug[h][:, qc], True, True,
                           [("kslc", g), ("kx", g), ("q", h), ("qm", h, i)], [psk])
                        eb, ebk = ebb.next()
                        if cch == i:
                            e32, e32k = e32b.next()
                            ACT(P, e32[:], ps_[:, 0:128], AF.Exp, [psk, "bias_sel"], [e32k], bias=bias_sel[:, h:h + 1])
                            ASEL(P, eb[:], e32[:], [[1, 128]], ALU.is_ge, 0.0, 0, -1, [e32k], [ebk])
                        else:
                            ACT(P, eb[:], ps_[:, 0:128], AF.Exp, [psk, "bias_sel"], [ebk], bias=bias_sel[:, h:h + 1])
                        defer(lambda psl=psl, pslk=pslk, eb=eb, ebk=ebk, cch=cch, g=g, st_=(cch == 0), sp_=(cch == i):
                              MM(P, psl[:, 0:65], eb[:], vslc[:, cch, g, :], st_, sp_, [ebk, "vslc"], [pslk]))
                    defer(lambda psl=psl, pslk=pslk, o=o, ok=ok, h=h, i=i: consume(psl, pslk, o, ok, h, i, 3 * h + 1, False))

            def finish_tile(o=o, ok=ok, qc=qc):
                pt2, pt2k = pstr.next()
                for j in range(3):
                    TR(P, pt2[:, j * 128:(j + 1) * 128], o[:, j * 128:(j + 1) * 128], c.ident[:],
                       [(ok, 2 * j), (ok, 2 * j + 1), "ident"], [pt2k])
                ost, ostk = ostb.next()
                CP(P, "act", ost[:], pt2[:, 0:384].rearrange("p (j t) -> p j t", j=3), [pt2k], [ostk])
                DMA(P, "sp", c.NSAOT.rearrange("(j p) t -> p j t", p=128)[:, :, qc], ost[:], [ostk], ["NSAOT"])
            defer(finish_tile)
        flush()
        P.end_phase()
        P.alloc_sems(c.es0, c.sems)
        with nc.Block() as block:
            P.emit(block, c.sems)


def phase_D(c):
    nc, P = c.nc, c.P
    with ExitStack() as es:
        sb = lambda name, shape, dt: es.enter_context(nc.sbuf_tensor(name, shape, dt))
        memt = sb("memt", [128, 2, D], F32)
        mems = sb("mems", [128, 2, D], F32)
        junk = sb("junkD", [128, D], F32)
        ssb = [sb(f"ssD{i}", [128, 4], F32) for i in range(2)]
        gcol = sb("gcolD", [128, 8], F32)
        mhT = sb("mhT", [128, 8, 256], BF16)
        wst = Rot([sb(f"wstD{i}", [128, 512], F32) for i in range(2)], "wstD")
        Wkv = sb("Wkv", [128, 8, 512], BF16)
        mkT = [sb(f"mkT{h}", [64, 256], BF16) for h in range(4)]
        mvaug = sb("mvaug", [128, 2, 4, 65], BF16)
        qm = [sb(f"qm{h}", [64, T], BF16) for h in range(4)]
        eb = Rot([sb(f"ebD{i}", [128, 512], BF16) for i in range(4)], "ebD")
        ob = Rot([sb(f"oD{i}", [128, 4, 256], F32) for i in range(2)], "oD")
        wb = Rot([sb(f"wD{i}", [128, 2], F32) for i in range(4)], "wD")
        ostb = Rot([sb(f"ostD{i}", [128, 2, 512], BF16) for i in range(2)], "ostD")
        pss = Rot(c.ps[0:2], "pssD")
        psacc = Rot(c.ps[2:5], "psaccD")
        pstr = Rot(c.ps[5:7], "pstrD")
        pmisc = c.ps[7]

        DMA(P, "sp", gcol[:], c.inp["mem_g"], [], ["gcol"])
        DMA(P, "sp", memt[:], c.inp["mem"].rearrange("(c p) d -> p c d", p=128), [], ["memt"])
        for h in range(4):
            DMA(P, "sp", qm[h][:], c.FM[FM_QMEM + 64 * h:FM_QMEM + 64 * (h + 1), :], ["FM"], [("qm", h)])
        for kc in range(8):
            st, sk = wst.next()
            DMA(P, "sp", st[:], c.inp["w_mem_kv"][kc * 128:(kc + 1) * 128, :], [], [sk])
            TS(P, "dve", Wkv[:, kc, :], st[:], gcol[:, kc:kc + 1], None, ALU.mult, None, [sk, "gcol"], [("Wkv", kc)])
        Wk = [("Wkv", kc) for kc in range(8)]
        for ci in range(2):
            ss = ssb[ci]
            ssk = ("ssD", ci)
            ACT(P, junk[:], memt[:, ci, :], AF.Square, ["memt"], ["junkD", ssk], accum=ss[:, 0:1])
            rstd_chain(P, ss, ssk)
            TS(P, "dve", mems[:, ci, :], memt[:, ci, :], ss[:, 3:4], None, ALU.mult, None, ["memt", ssk], [("mems", ci)])
            for half in range(2):
                pt, ptk = pstr.next()
                for j in range(4):
                    cc = half * 4 + j
                    TR(P, pt[:, j * 128:(j + 1) * 128], mems[:, ci, cc * 128:(cc + 1) * 128], c.ident[:],
                       [("mems", ci), "ident"], [ptk])
                CP(P, "dve", mhT[:, half * 4:(half + 1) * 4, ci * 128:(ci + 1) * 128],
                   pt[:].rearrange("p (j t) -> p j t", j=4), [ptk], [("mhT", ci, half)])
        mh = [("mhT", ci, half) for ci in range(2) for half in range(2)]
        for h in range(4):
            for kc in range(8):
                MM(P, pmisc[0:64, 0:256], Wkv[:, kc, 64 * h:64 * h + 64], mhT[:, kc, :], kc == 0, kc == 7, Wk + mh, ["pmisc"])
            CP(P, "dve", mkT[h][:], pmisc[0:64, 0:256], ["pmisc"], [("mkT", h)])
        for ci in range(2):
            for kc in range(8):
                MM(P, pmisc[:, 256:512], mhT[:, kc, ci * 128:(ci + 1) * 128], Wkv[:, kc, 256:512], kc == 0, kc == 7,
                   Wk + mh, ["pmisc"])
            CP(P, "dve", mvaug[:, ci, :, 0:64], pmisc[:, 256:512].rearrange("p (h d) -> p h d", h=4), ["pmisc"], ["mvaug"])
        MEMSET(P, "dve", mvaug[:, :, :, 64:65], 1.0, ["mvaug"], ["mvaug"])

        for g in range(8):
            o, ok = ob.next()
            for h in range(4):
                es_ = []
                for ci in range(2):
                    ps_, psk = pss.next()
                    MM(P, ps_[:, :], mkT[h][:, ci * 128:(ci + 1) * 128], qm[h][:, g * 512:(g + 1) * 512], True, True,
                       [("mkT", h), ("qm", h)], [psk])
                    e, ek = eb.next()
                    ACT(P, e[:], ps_[:, :], AF.Exp, [psk], [ek])
                    es_.append((e, ek))
                for sub in range(4):
                    pa, pak = psacc.next()
                    for ci in range(2):
                        MM(P, pa[:, 0:65], es_[ci][0][:, sub * 128:(sub + 1) * 128], mvaug[:, ci, h, :], ci == 0, ci == 1,
                           [es_[ci][1], "mvaug"], [pak])
                    w, wk = wb.next()
                    RECIP(P, w[:, 0:1], pa[:, 64:65], [pak], [wk])
                    TS(P, "dve", o[:, sub, 64 * h:64 * h + 64], pa[:, 0:64], w[:, 0:1], None, ALU.mult, None, [pak, wk],
                       [(ok, sub, h)])
            ost, ostk = ostb.next()
            for sub in range(4):
                pt, ptk = pstr.next()
                for j in range(2):
                    TR(P, pt[:, j * 128:(j + 1) * 128], o[:, sub, j * 128:(j + 1) * 128], c.ident[:],
                       [(ok, sub, 2 * j), (ok, sub, 2 * j + 1), "ident"], [ptk])
                CP(P, "act", ost[:, :, sub * 128:(sub + 1) * 128], pt[:, 0:256].rearrange("p (j t) -> p j t", j=2),
                   [ptk], [(ostk, sub)])
            DMA(P, "sp", c.MEMOT.rearrange("(j p) t -> p j t", p=128)[:, :, g * 512:(g + 1) * 512], ost[:],
                [(ostk, sub) for sub in range(4)], ["MEMOT"])
        P.end_phase()
        P.alloc_sems(c.es0, c.sems)
        with nc.Block() as block:
            P.emit(block, c.sems)


def phase_E(c):
    nc, P = c.nc, c.P
    with ExitStack() as es:
        sb = lambda name, shape, dt: es.enter_context(nc.sbuf_tensor(name, shape, dt))
        Wmg = sb("Wmg", [128, 8, 3072], BF16)
        Wbr = [sb("Wsb", [128, 3, D], BF16), sb("Wnsa", [128, 3, D], BF16), sb("Wmem", [128, 2, D], BF16)]
        Wout = sb("Wout", [128, 8, D], BF16)
        wst = Rot([sb(f"wstE{i}", [128, 1024], F32) for i in range(3)], "wstE")
        gcol = sb("gcolE", [128, 8], F32)
        bmg = sb("bmg", [128, 24], F32)
        hTb = Rot([sb(f"hTE{i}", [128, 8, 512], BF16) for i in range(2)], "hTE")
        srcb = [Rot([sb(f"srcE{b}_{i}", [128, 3 if b < 2 else 2, 512], BF16) for i in range(2)], f"srcE{b}") for b in range(3)]
        mgb = Rot([sb(f"mgT{i}", [128, 8, 512], BF16) for i in range(2)], "mgT")
        gateb = Rot([sb(f"gateE{i}", [128, 512], F32) for i in range(3)], "gateE")
        accb = Rot([sb(f"accE{i}", [128, 512], F32) for i in range(2)], "accE")
        tmpb = Rot([sb(f"tmpE{i}", [128, 512], F32) for i in range(2)], "tmpE")
        xb = Rot([sb(f"xE{i}", [128, D], F32) for i in range(2)], "xE")
        x1b = Rot([sb(f"x1E{i}", [128, D], F32) for i in range(2)], "x1E")
        psbr = Rot(c.ps[0:2], "psbr")
        psg = Rot(c.ps[2:5], "psg")
        psy = Rot(c.ps[5:8], "psy")

        DMA(P, "sp", gcol[:], c.inp["mix_g"], [], ["gcol"])
        DMA(P, "sp", bmg[:], c.inp["b_merge"], [], ["bmg"])
        n = 0
        for kc in range(8):
            for j in range(3):
                st, sk = wst.next()
                DMA(P, "sp", st[:], c.inp["w_in"][kc * 128:(kc + 1) * 128, 2578 + 1024 * j:2578 + 1024 * (j + 1)], [], [sk])
                if n % 2 == 0:
                    TS(P, "dve", Wmg[:, kc, 1024 * j:1024 * (j + 1)], st[:], gcol[:, kc:kc + 1], None, ALU.mult, None,
                       [sk, "gcol"], [("Wmg", kc)])
                else:
                    ACT(P, Wmg[:, kc, 1024 * j:1024 * (j + 1)], st[:], AF.Copy, [sk, "gcol"], [("Wmg", kc)],
                        scale=gcol[:, kc:kc + 1])
                n += 1
        for b, (nm, nf) in enumerate((("w_sb_br", 3), ("w_nsa_br", 3), ("w_mem_br", 2))):
            for f in range(nf):
                st, sk = wst.next()
                DMA(P, "sp", st[:], c.inp[nm][f * 128:(f + 1) * 128, :], [], [sk])
                CP(P, "dve" if n % 2 == 0 else "act", Wbr[b][:, f, :], st[:], [sk], [("Wbr", b)])
                n += 1
        for kc in range(8):
            st, sk = wst.next()
            DMA(P, "sp", st[:], c.inp["w_out"][kc * 128:(kc + 1) * 128, :], [], [sk])
            CP(P, "dve" if n % 2 == 0 else "act", Wout[:, kc, :], st[:], [sk], ["Wout"])
            n += 1
        Wmgk = [("Wmg", kc) for kc in range(8)]
        srcs = [(c.SBOT, 3, "SBOT"), (c.NSAOT, 3, "NSAOT"), (c.MEMOT, 2, "MEMOT")]
        for tg in range(8):
            tc_ = slice(tg * 512, (tg + 1) * 512)
            hT, hk = hTb.next()
            DMA(P, "sp", hT[:], c.HT.rearrange("(c p) t -> p c t", p=128)[:, :, tc_], ["HT"], [hk])
            src = []
            for b, (ap, nf, nm) in enumerate(srcs):
                t_, tk = srcb[b].next()
                DMA(P, "sp", t_[:], ap.rearrange("(f p) t -> p f t", p=128)[:, :, tc_], [nm], [tk])
                src.append((t_, tk, nf))
            mg, mgk = mgb.next()
            for dc in range(8):
                acc, acck = accb.next()
                for b in range(3):
                    t_, tk, nf = src[b]
                    pb, pbk = psbr.next()
                    for f in range(nf):
                        MM(P, pb[:, :], Wbr[b][:, f, dc * 128:(dc + 1) * 128], t_[:, f, :], f == 0, f == nf - 1,
                           [("Wbr", b), tk], [pbk])
                    pg, pgk = psg.next()
                    for kc in range(8):
                        MM(P, pg[:, :], Wmg[:, kc, b * 1024 + dc * 128:b * 1024 + (dc + 1) * 128], hT[:, kc, :], kc == 0, kc == 7,
                           Wmgk + [hk], [pgk])
                    gt, gtk = gateb.next()
                    ACT(P, gt[:], pg[:, :], AF.Sigmoid, [pgk, "bmg"], [gtk], bias=bmg[:, b * 8 + dc:b * 8 + dc + 1])
                    if b == 0:
                        TT(P, "dve", acc[:], gt[:], pb[:, :], ALU.mult, [gtk, pbk], [acck])
                    else:
                        tmp, tmpk = tmpb.next()
                        TT(P, "dve", tmp[:], gt[:], pb[:, :], ALU.mult, [gtk, pbk], [tmpk])
                        if b == 1:
                            TT(P, "pool", acc[:], acc[:], tmp[:], ALU.add, [acck, tmpk], [acck])
                        else:
                            TT(P, "pool", mg[:, dc, :], acc[:], tmp[:], ALU.add, [acck, tmpk], [(mgk, dc)])
            mgks = [(mgk, dc) for dc in range(8)]
            for s in range(4):
                i = tg * 4 + s
                xt, xk = xb.next()
                DMA(P, "sp", xt[:], c.inp["x"][i * 128:(i + 1) * 128, :], [], [xk])
                x1, x1k = x1b.next()
                for half in range(2):
                    py, pyk = psy.next()
                    for dc in range(8):
                        MM(P, py[:, :], mg[:, dc, s * 128:(s + 1) * 128], Wout[:, dc, half * 512:(half + 1) * 512], dc == 0, dc == 7,
                           mgks + ["Wout"], [pyk])
                    TT(P, "dve", x1[:, half * 512:(half + 1) * 512], xt[:, half * 512:(half + 1) * 512], py[:, :], ALU.add,
                       [xk, pyk], [(x1k, half)])
                DMA(P, "sp", c.X1[i * 128:(i + 1) * 128, :], x1[:], [(x1k, 0), (x1k, 1)], ["X1"])
        P.end_phase()
        P.alloc_sems(c.es0, c.sems)
        with nc.Block() as block:
            P.emit(block, c.sems)


def phase_F(c):
    nc, P = c.nc, c.P
    GS = 8
    with ExitStack() as es:
        sb = lambda name, shape, dt: es.enter_context(nc.sbuf_tensor(name, shape, dt))
        Wq = sb("Wq", [128, 8, 2048], BF16)
        subk = sb("subk", [128, 16, 128], BF16)
        g2b = sb("g2b", [128, D], F32)
        gFb = sb("gFb", [128, D], F32)
        keyidx = sb("keyidx", [128, 2048], I32)
        posidx = sb("posidx", [128, 2048], I32)
        iotaA = sb("iotaA", [128, 2048], F32)
        cI = sb("cI", [128, 8], I32)
        with ExitStack() as es1:
            sb1 = lambda name, shape, dt: es1.enter_context(nc.sbuf_tensor(name, shape, dt))
            wst = Rot([sb1(f"wstF{i}", [128, 2048], F32) for i in range(2)], "wstF")
            iotaAi = sb1("iotaAi", [128, 2048], I32)
            for kc in range(8):
                st, sk = wst.next()
                DMA(P, "sp", st[:], c.inp["peer_w_q"][kc * 128:(kc + 1) * 128, :], [], [sk])
                CP(P, "dve" if kc % 2 == 0 else "act", Wq[:, kc, :], st[:], [sk], [("Wq", kc)])
            st, sk = wst.next()
            DMA(P, "sp", st[:], c.inp["subkT"].rearrange("d b k -> d (b k)"), [], [sk])
            CP(P, "dve", subk[:].rearrange("d b k -> d (b k)"), st[:], [sk], ["subk"])
            DMA(P, "sp", g2b[:], c.inp["ffn_g"].partition_broadcast(128), [], ["g2b"])
            DMA(P, "sp", gFb[:], c.inp["final_g"].partition_broadcast(128), [], ["gFb"])
            IOTA(P, keyidx[:], [[0, 16], [1, 128]], 0, 0, [], ["keyidx"])
            IOTA(P, posidx[:], [[0, 8], [1, 256]], 0, 0, [], ["posidx"])
            IOTA(P, iotaAi[:], [[0, 128], [1, 16]], 0, 0, [], ["iotaAi"])
            CP(P, "dve", iotaA[:], iotaAi[:], ["iotaAi"], ["iotaA"])
            for j, v in enumerate((-128, -256, 127, 255, 15, 4)):
                IOTA(P, cI[:, j:j + 1], [[0, 1]], v, 0, ["cI"], ["cI"])
            P.end_phase()
            P.alloc_sems(c.es0, c.sems)
            with nc.Block() as block:
                P.emit(block, c.sems)
        x1b = Rot([sb(f"x1F{i}", [128, D], F32) for i in range(2)], "x1F")
        h2b = Rot([sb(f"h2F{i}", [128, D], F32) for i in range(2)], "h2F")
        ssb = Rot([sb(f"ssF{i}", [128, 4], F32) for i in range(4)], "ssF")
        junk = sb("junkF", [128, D], BF16)
        prodb = Rot([sb(f"prodF{i}", [128, D], BF16) for i in range(4)], "prodF")
        h2hb = Rot([sb(f"h2hF{i}", [128, D], BF16) for i in range(2)], "h2hF")
        h2Tb = Rot([sb(f"h2T{i}", [128, 8, 128], BF16) for i in range(2)], "h2T")
        qTb = sb("qTbF", [128, 16, 128], BF16)
        Sc = sb("Sc", [128, 2048], F32)
        rep = sb("repF", [128, 256], F32)
        stop = sb("stop", [128, 16, 16], F32)
        itop_i = sb("itop_i", [128, 256], I32)
        itop_f = sb("itop_f", [128, 16, 16], F32)
        tmpA = sb("tmpA", [128, 2048], F32)
        tmpB = sb("tmpB", [128, 2048], F32)
        best = sb("best", [128, 8, 16], F32)
        pos_i = sb("pos_i", [128, 3, 128], I32)
        ab_f = sb("ab_f", [128, 2, 128], F32)
        sel_f = sb("sel_f", [128, 3, 128], F32)
        idxb = Rot([sb(f"idxF{i}", [128, 128], I32) for i in range(2)], "idxF")
        gwb = Rot([sb(f"gwF{i}", [128, 3, 128], F32) for i in range(2)], "gwF")
        gsum = sb("gsum", [128, 16], F32)
        ab = Rot([sb(f"aF{i}", [128, 2, 128], F32) for i in range(2)], "aF")
        uvb = Rot([sb(f"uvg{i}", [128, 2 * D], BF16) for i in range(12)], "uvg")
        dgb = Rot([sb(f"dg{i}", [128, 128], BF16) for i in range(4)], "dg")
        x2b = Rot([sb(f"x2F{i}", [128, D], F32) for i in range(2)], "x2F")
        ptq = Rot(c.ps[0:2], "ptq")
        psS = Rot(c.ps[2:4], "psS")
        pvb = Rot([(c.ps[4], c.ps[5]), (c.ps[6], c.ps[7])], "pv")
        Wqk = [("Wq", kc) for kc in range(8)]

        def route(i, st):
            x1, x1k = x1b.next()
            DMA(P, "sp", x1[:], c.X1[i * 128:(i + 1) * 128, :], ["X1"], [x1k])
            ss, ssk = ssb.next()
            ACT(P, junk[:], x1[:], AF.Square, [x1k], [ssk], accum=ss[:, 0:1])
            rstd_chain(P, ss, ssk)
            h2, h2k = h2b.next()
            STT(P, h2[:], x1[:], ss[:, 3:4], g2b[:], ALU.mult, ALU.mult, [x1k, ssk, "g2b"], [h2k])
            h2h, h2hk = h2hb.next()
            CP(P, "act", h2h[:], h2[:], [h2k], [h2hk])
            yield
            h2T, h2Tk = h2Tb.next()
            for half in range(2):
                pt, ptk = ptq.next()
                for j in range(4):
                    cc = half * 4 + j
                    TR(P, pt[:, j * 128:(j + 1) * 128], h2[:, cc * 128:(cc + 1) * 128], c.ident[:], [h2k, "ident"], [ptk])
                CP(P, "act", h2T[:, half * 4:(half + 1) * 4, :], pt[:].rearrange("p (j t) -> p j t", j=4), [ptk], [(h2Tk, half)])
            h2Tks = [(h2Tk, 0), (h2Tk, 1)]
            yield
            for b4 in range(4):
                pq, pqk = ptq.next()
                for j in range(4):
                    blk = b4 * 4 + j
                    for kc in range(8):
                        MM(P, pq[:, j * 128:(j + 1) * 128], Wq[:, kc, blk * 128:(blk + 1) * 128], h2T[:, kc, :], kc == 0, kc == 7,
                           Wqk + h2Tks, [pqk])
                CP(P, "act", qTb[:, b4 * 4:(b4 + 1) * 4, :], pq[:].rearrange("p (j t) -> p j t", j=4), [pqk], [("qTb", b4)])
                yield
            for b4 in range(4):
                pS, pSk = psS.next()
                for j in range(4):
                    blk = b4 * 4 + j
                    MM(P, pS[:, j * 128:(j + 1) * 128], qTb[:, blk, :], subk[:, blk, :], True, True, [("qTb", b4), "subk"], [pSk])
                STT(P, Sc[:, b4 * 512:(b4 + 1) * 512].bitcast(I32), pS[:, :].bitcast(I32), cI[:, 0:1],
                    keyidx[:, b4 * 512:(b4 + 1) * 512], ALU.bitwise_and, ALU.bitwise_or, [pSk, "cI", "keyidx"], [("Sc", b4)])
                yield
            for blk in range(16):
                sblk = Sc[:, blk * 128:(blk + 1) * 128]
                sck = ("Sc", blk // 4)
                P.op("dve", lambda e, o=stop[:, blk, 0:8], s=sblk: e.max(out=o, in_=s), [sck], ["stop"])
                P.op("dve", lambda e, o=rep[:, 0:128], m=stop[:, blk, 0:8], s=sblk: e.match_replace(
                    out=o, in_to_replace=m, in_values=s, imm_value=-1e30), [sck, "stop"], ["repF"])
                P.op("dve", lambda e, o=stop[:, blk, 8:16], s=rep[:, 0:128]: e.max(out=o, in_=s), ["repF"], ["stop"])
                yield
            stop2 = stop[:].rearrange("p b k -> p (b k)")
            TS(P, "dve", itop_i[:], stop2.bitcast(I32), cI[:, 2:3], None, ALU.bitwise_and, None, ["stop", "cI"], ["itop_i"])
            CP(P, "dve", itop_f[:].rearrange("p b k -> p (b k)"), itop_i[:], ["itop_i"], ["itop_f"])
            yield
            sv = stop[:].rearrange("p (h q) k -> p h q k", q=2)
            iv = itop_f[:].rearrange("p (h q) k -> p h q k", q=2)
            cand = tmpA[:].rearrange("p (h a b) -> p h a b", h=8, a=16)
            TT(P, "dve", cand, sv[:, :, 0, :].unsqueeze(3).to_broadcast([128, 8, 16, 16]),
               sv[:, :, 1, :].unsqueeze(2).to_broadcast([128, 8, 16, 16]), ALU.add, ["stop"], ["tmpA"])
            yield
            STT(P, tmpB[:].bitcast(I32), tmpA[:].bitcast(I32), cI[:, 1:2], posidx[:], ALU.bitwise_and, ALU.bitwise_or,
                ["tmpA", "cI", "posidx"], ["tmpB"])
            yield
            for h in range(8):
                sblk = tmpB[:, h * 256:(h + 1) * 256]
                P.op("dve", lambda e, o=best[:, h, 0:8], s=sblk: e.max(out=o, in_=s), ["tmpB"], ["best"])
                P.op("dve", lambda e, o=rep[:], m=best[:, h, 0:8], s=sblk: e.match_replace(
                    out=o, in_to_replace=m, in_values=s, imm_value=-1e30), ["tmpB", "best"], ["repF"])
                P.op("dve", lambda e, o=best[:, h, 8:16], s=rep[:]: e.max(out=o, in_=s), ["repF"], ["best"])
                yield
            best2 = best[:].rearrange("p h k -> p (h k)")
            TS(P, "dve", pos_i[:, 0, :], best2.bitcast(I32), cI[:, 3:4], None, ALU.bitwise_and, None, ["best", "cI"], ["pos_i"])
            TS(P, "dve", pos_i[:, 1, :], pos_i[:, 0, :], cI[:, 5:6], None, ALU.logical_shift_right, None, ["pos_i", "cI"], ["pos_i"])
            TS(P, "dve", pos_i[:, 2, :], pos_i[:, 0, :], cI[:, 4:5], None, ALU.bitwise_and, None, ["pos_i", "cI"], ["pos_i"])
            CP(P, "dve", ab_f[:], pos_i[:, 1:3, :], ["pos_i"], ["ab_f"])
            yield
            for q in range(2):
                akv = ab_f[:, q, :].rearrange("p (h k) -> p h k", h=8)
                eq = tmpA[:].rearrange("p (h k a) -> p h k a", h=8, k=16)
                TT(P, "dve", eq, akv.unsqueeze(3).to_broadcast([128, 8, 16, 16]),
                   iotaA[:].rearrange("p (h k a) -> p h k a", h=8, k=16), ALU.is_equal, ["ab_f", "iotaA"], ["tmpA"])
                yield
                pr = tmpB[:].rearrange("p (h k a) -> p h k a", h=8, k=16)
                TT(P, "dve", pr, eq, iv[:, :, q, :].unsqueeze(2).to_broadcast([128, 8, 16, 16]), ALU.mult,
                   ["tmpA", "itop_f"], ["tmpB"])
                yield
                P.op("dve", lambda e, o=sel_f[:, q, :], s=tmpB[:].rearrange("p (x a) -> p x a", a=16): e.tensor_reduce(
                    out=o, in_=s, axis=AX.X, op=ALU.add), ["tmpB"], ["sel_f"])
                yield
            STT(P, sel_f[:, 2, :], sel_f[:, 0, :], 128.0, sel_f[:, 1, :], ALU.mult, ALU.add, ["sel_f"], ["sel_f"])
            TS(P, "dve", sel_f[:, 2, :], sel_f[:, 2, :], 0.0, 16383.0, ALU.max, ALU.min, ["sel_f"], ["sel_f"])
            idx, idxk = idxb.next()
            CP(P, "dve", idx[:], sel_f[:, 2, :], ["sel_f"], [idxk])
            yield
            gw, gwk = gwb.next()
            v3 = lambda ap: ap.rearrange("p (h k) -> p h k", h=8)
            TT(P, "dve", v3(gw[:, 0, :]), best[:], best[:, :, 0:1].to_broadcast([128, 8, 16]), ALU.subtract, ["best"], [gwk])
            ACT(P, gw[:, 1, :], gw[:, 0, :], AF.Exp, [gwk], [gwk])
            yield
            P.op("dve", lambda e, o=gsum[:, 0:8], s=v3(gw[:, 1, :]): e.tensor_reduce(out=o, in_=s, axis=AX.X, op=ALU.add),
                 [gwk], ["gsum"])
            RECIP(P, gsum[:, 8:16], gsum[:, 0:8], ["gsum"], ["gsum"])
            TT(P, "dve", v3(gw[:, 2, :]), v3(gw[:, 1, :]), gsum[:, 8:16].unsqueeze(2).to_broadcast([128, 8, 16]), ALU.mult,
               [gwk, "gsum"], [gwk])
            st.update(x1=x1, x1k=x1k, h2=h2h, h2k=h2hk, idx=idx, idxk=idxk, gw=gw, gwk=gwk)
            yield

        def slots(i, st, bg):
            x1, x1k, h2, h2k, idx, idxk, gw, gwk = (st[k_] for k_ in ("x1", "x1k", "h2", "h2k", "idx", "idxk", "gw", "gwk"))
            a, ak = ab.next()
            (pv0, pv1), pvk = pvb.next()
            LAG = 3
            held = {}
            for s in range(128 + LAG):
                if s < 128:
                    uv, uvk = uvb.next()
                    held[s] = (uv, uvk)
                    P.dma("pool", lambda e, o=uv[:], ix=idx[:, s:s + 1]: e.indirect_dma_start(
                        out=o, out_offset=None, in_=c.UVB,
                        in_offset=bass.IndirectOffsetOnAxis(ap=ix.bitcast(U32), axis=0)), [idxk, "UVB"], [uvk])
                    pd, pdk = prodb.next()
                    TT(P, "dve", pd[:], uv[:, 0:D], h2[:], ALU.mult, [uvk, h2k], [pdk])
                    ACT(P, junk[:], pd[:], AF.Copy, [pdk], [(ak, s)], accum=a[:, 0, s:s + 1])
                    ACT(P, a[:, 1, s:s + 1], a[:, 0, s:s + 1], AF.Gelu, [(ak, s)], [(ak, "g", s)])
                r_ = s - LAG
                if r_ >= 0:
                    uv, uvk = held.pop(r_)
                    dg, dgk = dgb.next()
                    TS(P, "dve", dg[:], c.identb[:], a[:, 1, r_:r_ + 1], gw[:, 2, r_:r_ + 1], ALU.mult, ALU.mult,
                       ["identb", (ak, "g", r_), gwk], [dgk])
                    MM(P, pv0[:, :], dg[:], uv[:, D:D + 512], r_ == 0, r_ == 127, [dgk, uvk], [(pvk, 0)])
                    MM(P, pv1[:, :], dg[:], uv[:, D + 512:2 * D], r_ == 0, r_ == 127, [dgk, uvk], [(pvk, 1)])
                if bg is not None:
                    next(bg, None)
            if bg is not None:
                for _ in bg:
                    pass
            x2, x2k = x2b.next()
            TT(P, "dve", x2[:, 0:512], x1[:, 0:512], pv0[:, :], ALU.add, [x1k, (pvk, 0)], [(x2k, 0)])
            TT(P, "dve", x2[:, 512:1024], x1[:, 512:1024], pv1[:, :], ALU.add, [x1k, (pvk, 1)], [(x2k, 1)])
            ss2, ss2k = ssb.next()
            ACT(P, junk[:], x2[:], AF.Square, [(x2k, 0), (x2k, 1)], [ss2k], accum=ss2[:, 0:1])
            rstd_chain(P, ss2, ss2k)
            STT(P, x2[:], x2[:], ss2[:, 3:4], gFb[:], ALU.mult, ALU.mult, [(x2k, 0), (x2k, 1), ss2k, "gFb"], [(x2k, 0), (x2k, 1)])
            DMA(P, "sp", c.out[i * 128:(i + 1) * 128, :], x2[:], [(x2k, 0), (x2k, 1)], ["out"])

        states = [dict() for _ in range(NT)]
        for _ in route(0, states[0]):
            pass
        for i in range(NT):
            bg = route(i + 1, states[i + 1]) if i + 1 < NT else None
            slots(i, states[i], bg)
        P.end_phase()
        P.alloc_sems(c.es0, c.sems)
        with nc.Block() as block:
            P.emit(block, c.sems)


def build(upto="F", debug=False):
    nc = bass.Bass("TRN2", target_bir_lowering=False)
    c = Ctx()
    c.nc = nc
    c.P = Prog(nc, same_engine_sync=os.environ.get('MK_SES', '1') == '1')
    c.sems = {}
    inp = {}

    def din(name, shape, dt=F32):
        inp[name] = nc.dram_tensor(name, list(shape), dt, kind="ExternalInput").ap()

    din("x", [T, D])
    din("mem", [256, D])
    din("mix_g", [128, 8])
    din("mem_g", [128, 8])
    din("w_in", [D, IN_DIM])
    din("b_merge", [128, 24])
    din("pe_k", [64, 32])
    din("pe_v", [64, 32])
    din("cw_k", [64, 32, 64])
    din("cw_v", [64, 32, 64])
    din("w_mem_kv", [D, 512])
    din("w_sb_br", [384, D])
    din("w_nsa_br", [384, D])
    din("w_mem_br", [256, D])
    din("w_out", [D, D])
    din("ffn_g", [D])
    din("peer_w_q", [D, 2048])
    din("subkT", [128, 16, 128])
    din("peer_uv", [16384, 2 * D])
    din("final_g", [D])
    c.inp = inp
    kind = "ExternalOutput" if debug else "Internal"

    def scr(name, shape, dt):
        return nc.dram_tensor(name, list(shape), dt, kind=kind).ap()

    c.FM = scr("FM", [FM_ROWS, T], BF16)
    c.HT = scr("HT", [D, T], BF16)
    c.TMV = scr("TMV", [T, 640], BF16)
    c.GATES = scr("GATES", [T, 18], F32)
    c.SBOT = scr("SBOT", [384, T], BF16)
    c.NSAOT = scr("NSAOT", [384, T], BF16)
    c.MEMOT = scr("MEMOT", [256, T], BF16)
    c.X1 = scr("X1", [T, D], F32)
    c.UVB = nc.dram_tensor("UVB", [16384, 2 * D], BF16, kind="Internal").ap()
    c.out = nc.dram_tensor("out", [T, D], F32, kind="ExternalOutput").ap()

    with ExitStack() as es0:
        c.es0 = es0
        c.ps = [es0.enter_context(nc.psum_tensor(f"ps{i}", [128, 512], F32)) for i in range(8)]
        c.ident = es0.enter_context(nc.sbuf_tensor("ident", [128, 128], F32))
        c.identb = es0.enter_context(nc.sbuf_tensor("identb", [128, 128], BF16))
        P = c.P
        MEMSET(P, "pool", c.ident[:], 1.0, [], ["ident"])
        ASEL(P, c.ident[:], c.ident[:], [[1, 128]], ALU.is_equal, 0.0, 0, -1, ["ident"], ["ident"])
        CP(P, "pool", c.identb[:], c.ident[:], ["ident"], ["identb"])
        phases = [("A", phase_A), ("B", phase_B), ("C", phase_C), ("D", phase_D), ("E", phase_E), ("F", phase_F)]
        for name, fn in phases:
            fn(c)
            if name == upto:
                break
    return nc


def make_inputs(inputs, b):
    f = lambda a: np.ascontiguousarray(a, dtype=np.float32)
    gcol = lambda g: f(np.asarray(g).reshape(8, 128).T)
    m = {
        "x": f(inputs["x"][b]),
        "mem": f(inputs["mem"][b]),
        "mix_g": gcol(inputs["mix_norm_g"][0]),
        "mem_g": gcol(inputs["mem_norm_g"][0]),
        "w_in": f(inputs["w_in"][0]),
        "b_merge": f(np.asarray(inputs["b_merge"][0]).reshape(24, 128).T),
        "pe_k": f(np.asarray(inputs["cmp_pe_k"][0]).T),
        "pe_v": f(np.asarray(inputs["cmp_pe_v"][0]).T),
        "cw_k": f(np.asarray(inputs["cmp_w_k"][0]).transpose(1, 0, 2)),
        "cw_v": f(np.asarray(inputs["cmp_w_v"][0]).transpose(1, 0, 2)),
        "w_mem_kv": f(inputs["w_mem_kv"][0]),
        "w_sb_br": f(inputs["w_sb_br"][0]),
        "w_nsa_br": f(inputs["w_nsa_br"][0]),
        "w_mem_br": f(inputs["w_mem_br"][0]),
        "w_out": f(inputs["w_out"][0]),
        "ffn_g": f(inputs["ffn_norm_g"][0]),
        "peer_w_q": f(inputs["peer_w_q"][0]),
        "subkT": f(np.asarray(inputs["peer_subkeys"][0]).transpose(3, 0, 1, 2).reshape(128, 16, 128)),
        "peer_uv": np.ascontiguousarray(np.concatenate([np.asarray(inputs["peer_u"][0], dtype=np.float32),
                                                        np.asarray(inputs["peer_v"][0], dtype=np.float32)], axis=1)),
        "final_g": f(inputs["final_norm_g"]),
    }
    return m


def kernel(**inputs):
    nc = build()
    shared = None
    in_maps = []
    for b in range(8):
        m = make_inputs(inputs, b)
        if shared is None:
            shared = m
        else:
            for k in m:
                if k not in ("x", "mem"):
                    m[k] = shared[k]
        in_maps.append(m)
    res = run_bass_kernel_spmd(nc, in_maps, core_ids=list(range(8)))
    return np.stack([np.asarray(r["out"]) for r in res.results], axis=0).astype(np.float32)
```

```python
import os
import sys
import numpy as np
from contextlib import ExitStack
import concourse.bass as bass
import concourse.mybir as mybir
from concourse.bass_utils import run_bass_kernel_spmd

F32 = mybir.dt.float32
BF16 = mybir.dt.bfloat16
I32 = mybir.dt.int32
U32 = mybir.dt.uint32
AF = mybir.ActivationFunctionType
ALU = mybir.AluOpType
AX = mybir.AxisListType

T = 4096
D = 1024
NT = T // 128
IN_DIM = 5650
EPS = 1e-6
SLOPES = [2.0 ** (-8.0 * (h + 1) / 6) for h in range(6)]
BIG = 30000.0

ENGS = ("pe", "act", "dve", "pool", "sp")
DMA_RING = 8


class Prog:
    def __init__(self, nc, same_engine_sync=True):
        self.nc = nc
        self.ops = {e: [] for e in ENGS}
        self.cnt = {e: 0 for e in ENGS}
        self.dma_n = {e: 0 for e in ENGS}
        self.last_w = {}
        self.readers = {}
        self.waited = {}
        self.same_engine_sync = same_engine_sync
        self.fill_vals = set()
        self.fill_regs = {}

    def _deps(self, eng, reads, writes):
        need = {}

        def add(tok):
            if tok is None:
                return
            sk, val, teng = tok
            if teng == eng and sk[0] == "c":
                if not self.same_engine_sync or eng == "pe":
                    return
            if need.get(sk, 0) < val:
                need[sk] = val

        for r in reads:
            add(self.last_w.get(r))
        for w in writes:
            add(self.last_w.get(w))
            for t in self.readers.get(w, ()):
                add(t)
        out = []
        for sk, val in need.items():
            if self.waited.get((eng, sk), 0) >= val:
                continue
            self.waited[(eng, sk)] = val
            out.append((sk, val))
        return out

    def _commit(self, tok, reads, writes):
        for r in reads:
            self.readers.setdefault(r, []).append(tok)
        for w in writes:
            self.last_w[w] = tok
            self.readers[w] = []

    def op(self, eng, fn, reads=(), writes=()):
        reads = tuple(reads)
        writes = tuple(writes)
        waits = self._deps(eng, reads, writes)
        self.cnt[eng] += 1
        tok = (("c", eng), self.cnt[eng], eng)
        fr = sys._getframe(1)
        self.ops[eng].append(dict(fn=fn, waits=waits, inc=(("c", eng), 1),
                                  where=(fr.f_lineno, fr.f_back.f_lineno if fr.f_back else 0)))
        self._commit(tok, reads, writes)
        return tok

    def dma(self, eng, fn, reads=(), writes=()):
        reads = tuple(reads)
        writes = tuple(writes)
        n = self.dma_n[eng]
        self.dma_n[eng] += 1
        sk = ("d", eng, n % DMA_RING)
        val = 16 * (n // DMA_RING + 1)
        waits = self._deps(eng, reads, writes)
        if val > 16 and self.waited.get((eng, sk), 0) < val - 16:
            self.waited[(eng, sk)] = val - 16
            waits.append((sk, val - 16))
        tok = (sk, val, eng)
        self.ops[eng].append(dict(fn=fn, waits=waits, inc=(sk, 16)))
        self._commit(tok, reads, writes)
        return tok

    def finish(self, eng, toks):
        self.ops[eng].append(dict(fn=None, waits=[(sk, val) for sk, val, _ in toks], inc=None))

    def end_phase(self):
        targets = []
        for e in ENGS:
            if self.cnt[e] > 0:
                targets.append((("c", e), self.cnt[e]))
            n = self.dma_n[e]
            for r in range(min(n, DMA_RING)):
                last = ((n - 1 - r) // DMA_RING) * DMA_RING + r
                targets.append((("d", e, r), 16 * (last // DMA_RING + 1)))
        for e in ENGS:
            waits = []
            for sk, val in targets:
                if self.waited.get((e, sk), 0) >= val:
                    continue
                self.waited[(e, sk)] = val
                waits.append((sk, val))
            self.ops[e].append(dict(fn=None, waits=waits, inc=None))
        self.last_w = {}
        self.readers = {}

    def sem_keys(self):
        keys = set()
        for e in ENGS:
            for o in self.ops[e]:
                if o["inc"]:
                    keys.add(o["inc"][0])
                for sk, _ in o["waits"]:
                    keys.add(sk)
        return sorted(keys)

    def alloc_sems(self, es, sems):
        for k in self.sem_keys():
            if k not in sems:
                sems[k] = es.enter_context(self.nc.semaphore("s_" + "_".join(map(str, k))))

    def emit(self, block, sems):
        engobj = {"pe": "tensor", "act": "scalar", "dve": "vector", "pool": "gpsimd", "sp": "sync"}

        def make(e):
            ops = self.ops[e]

            def body(eng):
                if e == "pool":
                    self.fill_regs = {v: eng.to_reg(v) for v in sorted(self.fill_vals)}
                for o in ops:
                    for sk, val in o["waits"]:
                        eng.wait_ge(sems[sk], val)
                    if o["fn"] is not None:
                        try:
                            ins = o["fn"](eng)
                        except Exception:
                            print("EMIT FAILED at lines", o.get("where"))
                            raise
                        if o["inc"]:
                            ins.then_inc(sems[o["inc"][0]], o["inc"][1])
            return body

        for e in ENGS:
            if self.ops[e]:
                getattr(block, engobj[e])(make(e))
        self.ops = {e: [] for e in ENGS}


class Ctx:
    pass


class Rot:
    def __init__(self, tiles, name):
        self.tiles = tiles
        self.name = name
        self.i = 0

    def next(self):
        j = self.i % len(self.tiles)
        self.i += 1
        return self.tiles[j], (self.name, j)


def MM(P, out, lhsT, rhs, start, stop, r, w):
    return P.op("pe", lambda e: e.matmul(out, lhsT=lhsT, rhs=rhs, start=start, stop=stop), r, w)


def TR(P, out, in_, ident, r, w):
    return P.op("pe", lambda e: e.transpose(out, in_, ident), r, w)


def ACT(P, out, in_, func, r, w, scale=None, bias=None, accum=None):
    kw = {}
    if scale is not None:
        kw["scale"] = scale
    if bias is not None:
        kw["bias"] = bias
    if accum is not None:
        kw["accum_out"] = accum
    return P.op("act", lambda e: e.activation(out=out, in_=in_, func=func, **kw), r, w)


def TS(P, eng, out, in0, s1, s2, op0, op1, r, w):
    if op1 is None:
        return P.op(eng, lambda e: e.tensor_scalar(out=out, in0=in0, scalar1=s1, scalar2=None, op0=op0), r, w)
    return P.op(eng, lambda e: e.tensor_scalar(out=out, in0=in0, scalar1=s1, scalar2=s2, op0=op0, op1=op1), r, w)


def TT(P, eng, out, in0, in1, op, r, w):
    return P.op(eng, lambda e: e.tensor_tensor(out=out, in0=in0, in1=in1, op=op), r, w)


def STT(P, out, in0, scalar, in1, op0, op1, r, w):
    return P.op("dve", lambda e: e.scalar_tensor_tensor(out=out, in0=in0, scalar=scalar, in1=in1, op0=op0, op1=op1), r, w)


def CP(P, eng, out, in_, r, w):
    if eng == "act":
        return P.op("act", lambda e: e.copy(out=out, in_=in_), r, w)
    return P.op(eng, lambda e: e.tensor_copy(out=out, in_=in_), r, w)


def DMA(P, eng, out, in_, r, w):
    return P.dma(eng, lambda e: e.dma_start(out=out, in_=in_), r, w)


def MEMSET(P, eng, ap, val, r, w):
    return P.op(eng, lambda e: e.memset(ap, val), r, w)


def ASEL(P, out, in_, pattern, cmp, fill, base, cm, r, w):
    P.fill_vals.add(float(fill))
    return P.op("pool", lambda e: e.affine_select(out=out, in_=in_, pattern=pattern, compare_op=cmp,
                                                  fill=P.fill_regs[float(fill)], base=base, channel_multiplier=cm), r, w)


def IOTA(P, out, pattern, base, cm, r, w):
    return P.op("pool", lambda e: e.iota(out, pattern=pattern, base=base, channel_multiplier=cm), r, w)


def RECIP(P, out, in_, r, w):
    return P.op("dve", lambda e: e.reciprocal(out=out, in_=in_), r, w)


def rstd_chain(P, ss, key):
    TS(P, "dve", ss[:, 1:2], ss[:, 0:1], 1.0 / D, EPS, ALU.mult, ALU.add, [key], [key])
    P.op("act", lambda e: e.sqrt(out=ss[:, 2:3], in_=ss[:, 1:2]), [key], [key])
    RECIP(P, ss[:, 3:4], ss[:, 2:3], [key], [key])


FM_QSB, FM_KSB, FM_QNSA, FM_KCMP, FM_VCMP, FM_KSLC, FM_KWIN, FM_QMEM = 0, 384, 768, 1152, 1280, 1408, 1536, 1664
FM_ROWS = 1920
FM_COLMAP = [(0, 0, 768), (768, 1152, 384), (1152, 1536, 128), (1280, 1664, 128), (1408, 1792, 128),
             (1536, 2048, 128), (1664, 2322, 256)]
TM_COLMAP = [(0, 768, 384), (384, 1920, 128), (512, 2176, 128), (640, 2304, 18)]
TM_COLS = 658
Q_CHUNKS = {0, 1, 2, 6, 7, 8, 13, 14}


def phase_A(c):
    nc, P = c.nc, c.P
    with ExitStack() as es:
        sb = lambda name, shape, dt: es.enter_context(nc.sbuf_tensor(name, shape, dt))
        Wfm = sb("Wfm", [128, 8, FM_ROWS], BF16)
        Wtm = sb("Wtm", [128, 8, TM_COLS], BF16)
        wst = Rot([sb(f"wst{i}", [128, 2578], F32) for i in range(2)], "wst")
        gcol = sb("gcolA", [128, 8], F32)
        xbuf = Rot([sb(f"xt{i}", [128, D], F32) for i in range(2)], "xt")
        xsbuf = Rot([sb(f"xs{i}", [128, D], F32) for i in range(2)], "xs")
        junk = sb("junkA", [128, D], F32)
        ssbuf = Rot([sb(f"ss{i}", [128, 4], F32) for i in range(4)], "ss")
        hTg = Rot([sb(f"hTg{i}", [128, 8, 512], BF16) for i in range(2)], "hTg")
        FMst = Rot([sb(f"FMst{i}", [128, 15, 512], BF16) for i in range(2)], "FMst")
        TMst = Rot([sb(f"TMst{i}", [128, 640], BF16) for i in range(3)], "TMst")
        gst = Rot([sb(f"gst{i}", [128, 18], F32) for i in range(3)], "gst")
        pstr = Rot(c.ps[0:2], "ps_tr")
        psfm = Rot(c.ps[2:5], "ps_fm")
        pstm = Rot(c.ps[5:8], "ps_tm")

        DMA(P, "sp", gcol[:], c.inp["mix_g"], [], ["gcol"])
        for kc in range(8):
            st, sk = wst.next()
            DMA(P, "sp", st[:], c.inp["w_in"][kc * 128:(kc + 1) * 128, 0:2578], [], [sk])
            n = 0
            for (dst, cm) in ((Wfm, FM_COLMAP), (Wtm, TM_COLMAP)):
                for (dc, sc, w) in cm:
                    if n % 2 == 0:
                        TS(P, "dve", dst[:, kc, dc:dc + w], st[:, sc:sc + w], gcol[:, kc:kc + 1], None, ALU.mult, None,
                           [sk, "gcol"], [("W", kc)])
                    else:
                        ACT(P, dst[:, kc, dc:dc + w], st[:, sc:sc + w], AF.Copy, [sk, "gcol"], [("W", kc)],
                            scale=gcol[:, kc:kc + 1])
                    n += 1
        Wkeys = [("W", kc) for kc in range(8)]

        for tg in range(8):
            hT, hk = hTg.next()
            hpieces = [(hk, s, half) for s in range(4) for half in range(2)]
            for s in range(4):
                i = tg * 4 + s
                xt, xk = xbuf.next()
                DMA(P, "sp", xt[:], c.inp["x"][i * 128:(i + 1) * 128, :], [], [xk])
                ss, ssk = ssbuf.next()
                ACT(P, junk[:], xt[:], AF.Square, [xk], ["junkA", ssk], accum=ss[:, 0:1])
                rstd_chain(P, ss, ssk)
                xs, xsk = xsbuf.next()
                TS(P, "dve", xs[:], xt[:], ss[:, 3:4], None, ALU.mult, None, [xk, ssk], [xsk])
                for half in range(2):
                    pt, ptk = pstr.next()
                    for j in range(4):
                        cc = half * 4 + j
                        TR(P, pt[:, j * 128:(j + 1) * 128], xs[:, cc * 128:(cc + 1) * 128], c.ident[:], [xsk, "ident"], [ptk])
                    dst = hT[:, half * 4:(half + 1) * 4, s * 128:(s + 1) * 128]
                    src = pt[:].rearrange("p (j t) -> p j t", j=4)
                    CP(P, "act" if half == 0 else "dve", dst, src, [ptk], [(hk, s, half)])
            fst, fsk = FMst.next()
            for ch in range(15):
                pf, pfk = psfm.next()
                for kc in range(8):
                    MM(P, pf[:, :], Wfm[:, kc, ch * 128:(ch + 1) * 128], hT[:, kc, :], kc == 0, kc == 7,
                       hpieces + [("W", kc)], [pfk])
                sc = 0.125 if ch in Q_CHUNKS else 1.0
                if ch % 2 == 0:
                    ACT(P, fst[:, ch, :], pf[:, :], AF.Copy, [pfk], [(fsk, ch)], scale=sc)
                else:
                    TS(P, "dve", fst[:, ch, :], pf[:, :], sc, None, ALU.mult, None, [pfk], [(fsk, ch)])
            DMA(P, "sp", c.FM.rearrange("(c p) t -> p c t", p=128)[:, :, tg * 512:(tg + 1) * 512], fst[:],
                [(fsk, ch) for ch in range(15)], ["FM"])
            DMA(P, "sp", c.HT.rearrange("(c p) t -> p c t", p=128)[:, :, tg * 512:(tg + 1) * 512], hT[:],
                hpieces, ["HT"])
            for s in range(4):
                i = tg * 4 + s
                pa, pak = pstm.next()
                for kc in range(8):
                    MM(P, pa[:, 0:512], hT[:, kc, s * 128:(s + 1) * 128], Wtm[:, kc, 0:512], kc == 0, kc == 7,
                       hpieces + [("W", kc)], [pak])
                tst, tsk = TMst.next()
                CP(P, "dve", tst[:, 0:512], pa[:, 0:512], [pak], [tsk])
                pb, pbk = pstm.next()
                for kc in range(8):
                    MM(P, pb[:, 0:146], hT[:, kc, s * 128:(s + 1) * 128], Wtm[:, kc, 512:658], kc == 0, kc == 7,
                       hpieces + [("W", kc)], [pbk])
                CP(P, "dve", tst[:, 512:640], pb[:, 0:128], [pbk], [tsk])
                gs, gsk = gst.next()
                ACT(P, gs[:], pb[:, 128:146], AF.Sigmoid, [pbk], [gsk])
                DMA(P, "sp", c.TMV[i * 128:(i + 1) * 128, :], tst[:], [tsk], ["TMV"])
                DMA(P, "sp", c.GATES[i * 128:(i + 1) * 128, :], gs[:], [gsk], ["GATES"])
        P.end_phase()
        P.alloc_sems(c.es0, c.sems)
        with nc.Block() as block:
            P.emit(block, c.sems)


def phase_B(c):
    nc, P = c.nc, c.P
    with ExitStack() as es:
        sb = lambda name, shape, dt: es.enter_context(nc.sbuf_tensor(name, shape, dt))
        qT = [sb(f"qTb{j}", [128, T], BF16) for j in range(3)]
        kT = [sb(f"kTb{j}", [128, T], BF16) for j in range(3)]
        V = sb("Vsb", [128, NT, 384], BF16)
        ntri = sb("ntri", [128, 128], BF16)
        nones = sb("nones", [128, 128], BF16)
        Ebuf = Rot([sb(f"E{i}", [128, 512], F32) for i in range(3)], "E")
        SPbuf = Rot([sb(f"SP{i}", [128, 512], BF16) for i in range(4)], "SP")
        Sbuf = Rot([sb(f"Ssum{i}", [128, 512], BF16) for i in range(3)], "Ssum")
        abuf = Rot([sb(f"aT{i}", [128, 512], BF16) for i in range(3)], "aT")
        obuf = Rot([sb(f"sbo{i}", [64, 512], BF16) for i in range(2)], "sbo")
        psA = Rot(c.ps[0:3], "psA")
        psB = Rot(c.ps[3:6], "psB")
        psO = Rot(c.ps[6:8], "psO")
        for j in range(3):
            DMA(P, "sp", qT[j][:], c.FM[FM_QSB + 128 * j:FM_QSB + 128 * (j + 1), :], ["FM"], [("qT", j)])
            DMA(P, "sp", kT[j][:], c.FM[FM_KSB + 128 * j:FM_KSB + 128 * (j + 1), :], ["FM"], [("kT", j)])
        DMA(P, "sp", V[:], c.TMV.rearrange("(c p) f -> p c f", p=128)[:, :, 0:384], ["TMV"], ["V"])
        MEMSET(P, "pool", nones[:], -1.0, [], ["nones"])
        MEMSET(P, "pool", ntri[:], -1.0, [], ["ntri"])
        ASEL(P, ntri[:], ntri[:], [[-1, 128]], ALU.is_ge, 0.0, 0, 1, ["ntri"], ["ntri"])
        uvst = Rot([sb(f"uvst{i}", [128, 2 * D], BF16) for i in range(4)], "uvst")

        def convert_chunk(ch):
            t_, tk = uvst.next()
            P.dma("pool", lambda e, o=t_[:], i_=c.inp["peer_uv"][ch * 128:(ch + 1) * 128, :]: e.dma_start(out=o, in_=i_), [], [tk])
            DMA(P, "sp", c.UVB[ch * 128:(ch + 1) * 128, :], t_[:], [tk], ["UVB"])
        steps = []
        for h in range(6):
            for g in range(8):
                nch = 4 * g + 4
                for idx_, cch in enumerate(range(nch - 1, -1, -1)):
                    steps.append(dict(h=h, g=g, cch=cch, first=idx_ == 0, last=cch == 0))
        N = len(steps)
        cur = dict(po=None, pok=None, ssum=None, ssumk=None)

        def S12(st):
            h, g, cch = st["h"], st["g"], st["cch"]
            j, half = h // 2, h % 2
            pr = slice(64 * half, 64 * half + 64)
            st["qs"] = qT[j][pr, g * 512:(g + 1) * 512]
            st["ks"] = kT[j][pr, cch * 128:(cch + 1) * 128]
            st["rk"] = [("kT", j), ("qT", j)]
            st["m"] = cch - 4 * g
            if st["first"]:
                cur["po"], cur["pok"] = psO.next()
                cur["ssum"] = cur["ssumk"] = None
            st["po"], st["pok"] = cur["po"], cur["pok"]
            pa, pak = psA.next()
            MM(P, pa[:, :], st["ks"], st["qs"], True, True, st["rk"], [pak])
            E, Ek = Ebuf.next()
            ACT(P, E[:], pa[:, :], AF.Exp, [pak], [Ek])
            SP, SPk = SPbuf.next()
            ACT(P, SP[:], E[:], AF.Ln, [Ek], [SPk], bias=1.0)
            if st["m"] >= 0:
                ASEL(P, SP[:], SP[:], [[1, 512]], ALU.is_gt, 0.0, -128 * st["m"], -1, [SPk], [SPk])
            st["SP"], st["SPk"] = SP, SPk
            st["ssum_prev"], st["ssum_prevk"] = cur["ssum"], cur["ssumk"]
            if not st["last"]:
                if st["first"]:
                    cur["ssum"], cur["ssumk"] = SP, SPk
                else:
                    sn, snk = Sbuf.next()
                    TT(P, "pool", sn[:], cur["ssum"][:], SP[:], ALU.add, [cur["ssumk"], SPk], [snk])
                    cur["ssum"], cur["ssumk"] = sn, snk

        def S34(st):
            pb, pbk = psB.next()
            MM(P, pb[:, :], ntri[:], st["SP"][:], True, False, ["ntri", st["SPk"]], [pbk])
            if not st["first"]:
                MM(P, pb[:, :], nones[:], st["ssum_prev"][:], False, False, ["nones", st["ssum_prevk"]], [pbk])
            MM(P, pb[:, :], st["ks"], st["qs"], False, True, st["rk"], [pbk])
            aT, aTk = abuf.next()
            ACT(P, aT[:], pb[:, :], AF.Exp, [pbk], [aTk])
            if st["m"] >= 0:
                ASEL(P, aT[:], aT[:], [[1, 512]], ALU.is_gt, 0.0, -128 * st["m"], -1, [aTk], [aTk])
            st["aT"], st["aTk"] = aT, aTk

        def S5(st):
            h, g, cch = st["h"], st["g"], st["cch"]
            MM(P, st["po"][0:64, :], V[:, cch, 64 * h:64 * h + 64], st["aT"][:], st["first"], st["last"], ["V", st["aTk"]], [st["pok"]])
            if st["last"]:
                ob, obk = obuf.next()
                CP(P, "dve", ob[:], st["po"][0:64, :], [st["pok"]], [obk])
                DMA(P, "sp", c.SBOT[64 * h:64 * h + 64, g * 512:(g + 1) * 512], ob[:], [obk], ["SBOT"])

        for n in range(N + 2):
            if n % 6 == 0 and n // 6 < 128:
                convert_chunk(n // 6)
            if n < N:
                S12(steps[n])
            if 0 <= n - 1 < N:
                S34(steps[n - 1])
            if 0 <= n - 2 < N:
                S5(steps[n - 2])
                steps[n - 2].clear()
        P.end_phase()
        P.alloc_sems(c.es0, c.sems)
        with nc.Block() as block:
            P.emit(block, c.sems)


def phase_C(c):
    nc, P = c.nc, c.P
    with ExitStack() as es:
        sb = lambda name, shape, dt: es.enter_context(nc.sbuf_tensor(name, shape, dt))
        qaug = [sb(f"qaug{h}", [128, T], BF16) for h in range(6)]
        kslc = [sb(f"kslc{g}", [128, T], BF16) for g in range(2)]
        kwin = [sb(f"kwin{g}", [64, T], BF16) for g in range(2)]
        kcmp = [sb(f"kcmp{g}", [64, T], BF16) for g in range(2)]
        vcmp = [sb(f"vcmp{g}", [64, T], BF16) for g in range(2)]
        kcT = [sb(f"kcT{g}", [64, 256], BF16) for g in range(2)]
        vcaug = [sb(f"vcaug{g}", [128, 2, 129], BF16) for g in range(2)]
        vwin = sb("vwin", [128, NT, 2, 65], BF16)
        vslc = sb("vslc", [128, NT, 2, 65], BF16)
        vstage = sb("vstage", [128, NT, 256], BF16)
        gates = sb("gatesC", [128, NT, 18], F32)
        pe = [sb("pek_sb", [64, 32], F32), sb("pev_sb", [64, 32], F32)]
        cwst = sb("cwst", [64, 2048], F32)
        cw = [sb("cwk", [64, 32, 64], BF16), sb("cwv", [64, 32, 64], BF16)]
        tmpb = Rot([sb(f"ctmp{i}", [64, 256], BF16) for i in range(3)], "ctmp")
        itmp = sb("itmp", [128, 64], I32)
        ftmp = sb("ftmp", [128, 64], F32)
        bias_sel = sb("bias_sel", [128, 6], F32)
        bias_win = sb("bias_win", [128, 6, 5], F32)
        bias_cmp = sb("bias_cmp", [128, 6, 64], F32)
        IOTi = sb("IOTi", [128, 128], I32)
        IOT = sb("IOT", [128, 128], F32)
        e32b = Rot([sb(f"e32_{i}", [128, 128], F32) for i in range(4)], "e32")
        ebb = Rot([sb(f"eb{i}", [128, 128], BF16) for i in range(7)], "eb")
        obuf = Rot([sb(f"oC{i}", [128, 384], F32) for i in range(2)], "oC")
        impb = Rot([sb(f"imp{i}", [128, 64], F32) for i in range(2)], "imp")
        wb = Rot([sb(f"wC{i}", [128, 4], F32) for i in range(4)], "wC")
        m8b = Rot([sb(f"m8_{i}", [128, 16], F32) for i in range(2)], "m8")
        repb = Rot([sb(f"rep{i}", [128, 64], F32) for i in range(2)], "rep")
        selb = Rot([sb(f"selp{i}", [128, 128], F32) for i in range(2)], "selp")
        Ctb = Rot([sb(f"Ct{i}", [128, 128], F32) for i in range(2)], "Ct")
        ostb = Rot([sb(f"ostC{i}", [128, 3, 128], BF16) for i in range(2)], "ostC")
        pss = Rot(c.ps[0:3], "pss")
        psacc = Rot(c.ps[3:6], "psacc")
        pstr = Rot(c.ps[6:8], "pstrC")
        pk = c.ps[6]

        for h in range(6):
            DMA(P, "sp", qaug[h][0:64, :], c.FM[FM_QNSA + 64 * h:FM_QNSA + 64 * (h + 1), :], ["FM"], [("q", h)])
        for g in range(2):
            DMA(P, "sp", kslc[g][0:64, :], c.FM[FM_KSLC + 64 * g:FM_KSLC + 64 * (g + 1), :], ["FM"], [("kslc", g)])
            DMA(P, "sp", kwin[g][:], c.FM[FM_KWIN + 64 * g:FM_KWIN + 64 * (g + 1), :], ["FM"], [("kwin", g)])
            DMA(P, "sp", kcmp[g][:], c.FM[FM_KCMP + 64 * g:FM_KCMP + 64 * (g + 1), :], ["FM"], [("kcmp", g)])
            DMA(P, "sp", vcmp[g][:], c.FM[FM_VCMP + 64 * g:FM_VCMP + 64 * (g + 1), :], ["FM"], [("vcmp", g)])
        DMA(P, "sp", vstage[:], c.TMV.rearrange("(c p) f -> p c f", p=128)[:, :, 384:640], ["TMV"], ["vstage"])
        DMA(P, "sp", gates[:], c.GATES.rearrange("(c p) f -> p c f", p=128), ["GATES"], ["gates"])
        DMA(P, "sp", pe[0][:], c.inp["pe_k"], [], ["pe0"])
        DMA(P, "sp", pe[1][:], c.inp["pe_v"], [], ["pe1"])
        for kv, nm in ((0, "cw_k"), (1, "cw_v")):
            DMA(P, "sp", cwst[:], c.inp[nm].rearrange("d l e -> d (l e)"), [], ["cwst"])
            CP(P, "dve", cw[kv][:].rearrange("d l e -> d (l e)"), cwst[:], ["cwst"], [("cw", kv)])
        CP(P, "dve", vslc[:, :, :, 0:64], vstage[:, :, 0:128].rearrange("p c (g d) -> p c g d", g=2), ["vstage"], ["vslc"])
        CP(P, "pool", vwin[:, :, :, 0:64], vstage[:, :, 128:256].rearrange("p c (g d) -> p c g d", g=2), ["vstage"], ["vwin"])
        MEMSET(P, "dve", vslc[:, :, :, 64:65], 1.0, ["vslc"], ["vslc"])
        MEMSET(P, "pool", vwin[:, :, :, 64:65], 1.0, ["vwin"], ["vwin"])
        for g in range(2):
            MEMSET(P, "pool", kslc[g][64:128, :], 1.0, [], [("kx", g)])
            ASEL(P, kslc[g][64:128, :], kslc[g][64:128, :], [[1, T]], ALU.is_ge, 0.0, 0, -64, [("kx", g)], [("kx", g)])
            ASEL(P, kslc[g][64:128, :], kslc[g][64:128, :], [[-1, T]], ALU.is_ge, 0.0, 63, 64, [("kx", g)], [("kx", g)])
        IOTA(P, itmp[0:64, 0:1], [[0, 1]], 0, 1, [], ["itmp"])
        IOTA(P, itmp[64:128, 0:1], [[0, 1]], 0, 1, ["itmp"], ["itmp"])
        CP(P, "dve", ftmp[:, 0:1], itmp[:, 0:1], ["itmp"], ["ftmp"])
        for h in range(6):
            TS(P, "dve", bias_sel[:, h:h + 1], ftmp[:, 0:1], SLOPES[h], None, ALU.mult, None, ["ftmp"], ["bias_sel"])
        IOTA(P, itmp[:, 0:5], [[128, 5]], -512, 1, ["itmp", "ftmp"], ["itmp"])
        CP(P, "dve", ftmp[:, 0:5], itmp[:, 0:5], ["itmp"], ["ftmp"])
        for h in range(6):
            TS(P, "dve", bias_win[:, h, :], ftmp[:, 0:5], SLOPES[h], None, ALU.mult, None, ["ftmp"], ["bias_win"])
        IOTA(P, itmp[:, 0:64], [[2048, 2], [-128, 32]], 31, 16, ["itmp", "ftmp"], ["itmp"])
        CP(P, "dve", ftmp[:, 0:64], itmp[:, 0:64], ["itmp"], ["ftmp"])
        for h in range(6):
            TS(P, "dve", bias_cmp[:, h, :], ftmp[:, 0:64], SLOPES[h], None, ALU.mult, None, ["ftmp"], ["bias_cmp"])
        IOTA(P, IOTi[64:128, :], [[-1, 128]], 0, 64, [], ["IOTi"])
        CP(P, "dve", IOT[64:128, :], IOTi[64:128, :], ["IOTi"], ["IOT"])
        for g in range(2):
            MEMSET(P, "pool", vcaug[g][:], 1.0, [], [("vcaug", g)])
            for ci in range(2):
                ASEL(P, vcaug[g][:, ci, 65:129], vcaug[g][:, ci, 65:129], [[-4, 64]], ALU.is_ge, 0.0, 128 * ci, 1,
                     [("vcaug", g)], [("vcaug", g)])
                ASEL(P, vcaug[g][:, ci, 65:129], vcaug[g][:, ci, 65:129], [[4, 64]], ALU.is_ge, 0.0, 3 - 128 * ci, -1,
                     [("vcaug", g)], [("vcaug", g)])
        for s_ in selb.tiles:
            pass
        for j in range(2):
            MEMSET(P, "dve", selb.tiles[j][:, 0:64], 0.0, [], [("selp", j)])
        for g in range(2):
            kview = kcmp[g][:].rearrange("p (n s) -> p n s", s=16)
            vview = vcmp[g][:].rearrange("p (n s) -> p n s", s=16)
            for l in range(32):
                tmp, tk = tmpb.next()
                TS(P, "dve" if l % 2 == 0 else "pool", tmp[:, 0:255], kview[:, l // 16:l // 16 + 255, l % 16],
                   pe[0][:, l:l + 1], None, ALU.add, None, [("kcmp", g), "pe0"], [tk])
                MM(P, pk[0:64, 0:255], cw[0][:, l, :], tmp[:, 0:255], l == 0, l == 31, [("cw", 0), tk], [("pstrC", 0)])
            CP(P, "dve", kcT[g][:, 0:255], pk[0:64, 0:255], [("pstrC", 0)], [("kcT", g)])
            for ci in range(2):
                rows = 128 if ci == 0 else 127
                for l in range(32):
                    tmp, tk = tmpb.next()
                    n0 = l // 16 + ci * 128
                    TS(P, "dve" if l % 2 == 0 else "pool", tmp[:, 0:rows], vview[:, n0:n0 + rows, l % 16],
                       pe[1][:, l:l + 1], None, ALU.add, None, [("vcmp", g), "pe1"], [tk])
                    MM(P, pk[0:rows, 256:320], tmp[:, 0:rows], cw[1][:, l, :], l == 0, l == 31, [("cw", 1), tk], [("pstrC", 0)])
                CP(P, "dve", vcaug[g][0:rows, ci, 0:64], pk[0:rows, 256:320], [("pstrC", 0)], [("vcaug", g)])

        def consume(pacc, pacck, o, ok, h, i, gcol, first):
            w, wk = wb.next()
            TS(P, "dve", w[:, 0:1], pacc[:, 64:65], 1e-30, None, ALU.max, None, [pacck], [wk])
            RECIP(P, w[:, 1:2], w[:, 0:1], [wk], [wk])
            TT(P, "dve", w[:, 2:3], w[:, 1:2], gates[:, i, gcol:gcol + 1], ALU.mult, [wk, "gates"], [wk])
            if first:
                TS(P, "dve", o[:, 64 * h:64 * h + 64], pacc[:, 0:64], w[:, 2:3], None, ALU.mult, None, [pacck, wk], [(ok, h)])
            else:
                STT(P, o[:, 64 * h:64 * h + 64], pacc[:, 0:64], w[:, 2:3], o[:, 64 * h:64 * h + 64], ALU.mult, ALU.add,
                    [pacck, wk, (ok, h)], [(ok, h)])
            return w, wk

        from collections import deque
        fifo = deque()
        LAGC = 3

        def defer(fn):
            fifo.append(fn)
            while len(fifo) > LAGC:
                fifo.popleft()()

        def flush():
            while fifo:
                fifo.popleft()()

        def consume_cmp(pc, pck, o, ok, h, i, hh, imp, impk):
            w, wk = consume(pc, pck, o, ok, h, i, 3 * h + 0, True)
            if hh == 0:
                TS(P, "dve", imp[:], pc[:, 65:129], w[:, 1:2], None, ALU.mult, None, [pck, wk], [impk])
            else:
                STT(P, imp[:], pc[:, 65:129], w[:, 1:2], imp[:], ALU.mult, ALU.add, [pck, wk, impk], [impk])

        for i in range(NT):
            t0 = 128 * i
            qc = slice(t0, t0 + 128)
            o, ok = obuf.next()
            for g in range(2):
                imp, impk = impb.next()
                for hh in range(3):
                    h = 3 * g + hh
                    nvalid = min(255, (t0 + 96) // 16 + 1)
                    chunks = [(0, min(128, nvalid))] + ([(1, nvalid - 128)] if nvalid > 128 else [])
                    pc, pck = psacc.next()
                    for ni, (ci, rows) in enumerate(chunks):
                        ps_, psk = pss.next()
                        MM(P, ps_[0:rows, 0:128], kcT[g][:, ci * 128:ci * 128 + rows], qaug[h][0:64, qc], True, True,
                           [("kcT", g), ("q", h)], [psk])
                        e32, e32k = e32b.next()
                        ACT(P, e32[0:rows, :], ps_[0:rows, 0:128], AF.Exp, [psk, "bias_cmp"], [e32k],
                            bias=bias_cmp[0:rows, h, ci * 32 + i:ci * 32 + i + 1])
                        eb, ebk = ebb.next()
                        ASEL(P, eb[0:rows, :], e32[0:rows, :], [[1, 128]], ALU.is_ge, 0.0, t0 - 2048 * ci - 31, -16,
                             [e32k], [ebk])
                        defer(lambda pc=pc, pck=pck, eb=eb, ebk=ebk, rows=rows, ci=ci, g=g, st_=(ni == 0), sp_=(ni == len(chunks) - 1):
                              MM(P, pc[:, 0:129], eb[0:rows, :], vcaug[g][0:rows, ci, :], st_, sp_, [ebk, ("vcaug", g)], [pck]))
                    defer(lambda pc=pc, pck=pck, o=o, ok=ok, h=h, i=i, hh=hh, imp=imp, impk=impk:
                          consume_cmp(pc, pck, o, ok, h, i, hh, imp, impk))
                    pw, pwk = psacc.next()
                    cl = list(range(max(0, i - 4), i + 1))
                    for ni, cch in enumerate(cl):
                        dc = cch - i
                        ps_, psk = pss.next()
                        MM(P, ps_[:, 0:128], kwin[g][:, cch * 128:(cch + 1) * 128], qaug[h][0:64, qc], True, True,
                           [("kwin", g), ("q", h)], [psk])
                        eb, ebk = ebb.next()
                        bw = bias_win[:, h, dc + 4:dc + 5]
                        if dc == 0 or dc == -4:
                            e32, e32k = e32b.next()
                            ACT(P, e32[:], ps_[:, 0:128], AF.Exp, [psk, "bias_win"], [e32k], bias=bw)
                            if dc == 0:
                                ASEL(P, eb[:], e32[:], [[1, 128]], ALU.is_ge, 0.0, 0, -1, [e32k], [ebk])
                            else:
                                ASEL(P, eb[:], e32[:], [[-1, 128]], ALU.is_gt, 0.0, 0, 1, [e32k], [ebk])
                        else:
                            ACT(P, eb[:], ps_[:, 0:128], AF.Exp, [psk, "bias_win"], [ebk], bias=bw)
                        defer(lambda pw=pw, pwk=pwk, eb=eb, ebk=ebk, cch=cch, g=g, st_=(ni == 0), sp_=(ni == len(cl) - 1):
                              MM(P, pw[:, 0:65], eb[:], vwin[:, cch, g, :], st_, sp_, [ebk, "vwin"], [pwk]))
                    defer(lambda pw=pw, pwk=pwk, o=o, ok=ok, h=h, i=i: consume(pw, pwk, o, ok, h, i, 3 * h + 2, False))
                flush()
                ASEL(P, imp[:], imp[:], [[-64, 64]], ALU.is_ge, 1e4, t0 - 128, 1, [impk], [impk])
                ASEL(P, imp[:], imp[:], [[-64, 64]], ALU.is_ge, -1.0, t0, 1, [impk], [impk])
                MEMSET(P, "pool", imp[:, 0:1], 1e4, [impk], [impk])
                m8, m8k = m8b.next()
                rep, repk = repb.next()
                P.op("dve", lambda e, m8=m8, imp=imp: e.max(out=m8[:, 0:8], in_=imp[:]), [impk], [m8k])
                P.op("dve", lambda e, m8=m8, imp=imp, rep=rep: e.match_replace(out=rep[:], in_to_replace=m8[:, 0:8],
                                                                                 in_values=imp[:], imm_value=-1e30),
                     [impk, m8k], [repk])
                P.op("dve", lambda e, m8=m8, rep=rep: e.max(out=m8[:, 8:16], in_=rep[:]), [repk, m8k], [m8k])
                selp, selk = selb.next()
                TS(P, "dve", selp[:, 64:128], imp[:], m8[:, 15:16], None, ALU.is_ge, None, [impk, m8k], [selk])
                pt_, ptk = pstr.next()
                TR(P, pt_[:, 0:128], selp[:], c.ident[:], [selk, "ident"], [ptk])
                for hh in range(3):
                    h = 3 * g + hh
                    Ct, Ctk = Ctb.next()
                    TS(P, "pool", Ct[64:128, :], IOT[64:128, :], SLOPES[h], -BIG - SLOPES[h] * t0, ALU.mult, ALU.add,
                       ["IOT"], [Ctk])
                    STT(P, qaug[h][64:128, qc], pt_[64:128, 0:128], BIG, Ct[64:128, :], ALU.mult, ALU.add,
                        [ptk, Ctk], [("qm", h, i)])
                for hh in range(3):
                    h = 3 * g + hh
                    psl, pslk = psacc.next()
                    for cch in range(i + 1):
                        ps_, psk = pss.next()
                        MM(P, ps_[:, 0:128], kslc[g][:, cch * 128:(cch + 1) * 128], qaug[h][:, qc], True, True,
                           [("kslc", g), ("kx", g), ("q", h), ("qm", h, i)], [psk])
                        eb, ebk = ebb.next()
                        if cch == i:
                            e32, e32k = e32b.next()
                            ACT(P, e32[:], ps_[:, 0:128], AF.Exp, [psk, "bias_sel"], [e32k], bias=bias_sel[:, h:h + 1])
                            ASEL(P, eb[:], e32[:], [[1, 128]], ALU.is_ge, 0.0, 0, -1, [e32k], [ebk])
                        else:
                            ACT(P, eb[:], ps_[:, 0:128], AF.Exp, [psk, "bias_sel"], [ebk], bias=bias_sel[:, h:h + 1])
                        defer(lambda psl=psl, pslk=pslk, eb=eb, ebk=ebk, cch=cch, g=g, st_=(cch == 0), sp_=(cch == i):
                              MM(P, psl[:, 0:65], eb[:], vslc[:, cch, g, :], st_, sp_, [ebk, "vslc"], [pslk]))
                    defer(lambda psl=psl, pslk=pslk, o=o, ok=ok, h=h, i=i: consume(psl, pslk, o, ok, h, i, 3 * h + 1, False))

            def finish_tile(o=o, ok=ok, qc=qc):
                pt2, pt2k = pstr.next()
                for j in range(3):
                    TR(P, pt2[:, j * 128:(j + 1) * 128], o[:, j * 128:(j + 1) * 128], c.ident[:],
                       [(ok, 2 * j), (ok, 2 * j + 1), "ident"], [pt2k])
                ost, ostk = ostb.next()
                CP(P, "act", ost[:], pt2[:, 0:384].rearrange("p (j t) -> p j t", j=3), [pt2k], [ostk])
                DMA(P, "sp", c.NSAOT.rearrange("(j p) t -> p j t", p=128)[:, :, qc], ost[:], [ostk], ["NSAOT"])
            defer(finish_tile)
        flush()
        P.end_phase()
        P.alloc_sems(c.es0, c.sems)
        with nc.Block() as block:
            P.emit(block, c.sems)


def phase_D(c):
    nc, P = c.nc, c.P
    with ExitStack() as es:
        sb = lambda name, shape, dt: es.enter_context(nc.sbuf_tensor(name, shape, dt))
        memt = sb("memt", [128, 2, D], F32)
        mems = sb("mems", [128, 2, D], F32)
        junk = sb("junkD", [128, D], F32)
        ssb = [sb(f"ssD{i}", [128, 4], F32) for i in range(2)]
        gcol = sb("gcolD", [128, 8], F32)
        mhT = sb("mhT", [128, 8, 256], BF16)
        wst = Rot([sb(f"wstD{i}", [128, 512], F32) for i in range(2)], "wstD")
        Wkv = sb("Wkv", [128, 8, 512], BF16)
        mkT = [sb(f"mkT{h}", [64, 256], BF16) for h in range(4)]
        mvaug = sb("mvaug", [128, 2, 4, 65], BF16)
        qm = [sb(f"qm{h}", [64, T], BF16) for h in range(4)]
        eb = Rot([sb(f"ebD{i}", [128, 512], BF16) for i in range(4)], "ebD")
        ob = Rot([sb(f"oD{i}", [128, 4, 256], F32) for i in range(2)], "oD")
        wb = Rot([sb(f"wD{i}", [128, 2], F32) for i in range(4)], "wD")
        ostb = Rot([sb(f"ostD{i}", [128, 2, 512], BF16) for i in range(2)], "ostD")
        pss = Rot(c.ps[0:2], "pssD")
        psacc = Rot(c.ps[2:5], "psaccD")
        pstr = Rot(c.ps[5:7], "pstrD")
        pmisc = c.ps[7]

        DMA(P, "sp", gcol[:], c.inp["mem_g"], [], ["gcol"])
        DMA(P, "sp", memt[:], c.inp["mem"].rearrange("(c p) d -> p c d", p=128), [], ["memt"])
        for h in range(4):
            DMA(P, "sp", qm[h][:], c.FM[FM_QMEM + 64 * h:FM_QMEM + 64 * (h + 1), :], ["FM"], [("qm", h)])
        for kc in range(8):
            st, sk = wst.next()
            DMA(P, "sp", st[:], c.inp["w_mem_kv"][kc * 128:(kc + 1) * 128, :], [], [sk])
            TS(P, "dve", Wkv[:, kc, :], st[:], gcol[:, kc:kc + 1], None, ALU.mult, None, [sk, "gcol"], [("Wkv", kc)])
        Wk = [("Wkv", kc) for kc in range(8)]
        for ci in range(2):
            ss = ssb[ci]
            ssk = ("ssD", ci)
            ACT(P, junk[:], memt[:, ci, :], AF.Square, ["memt"], ["junkD", ssk], accum=ss[:, 0:1])
            rstd_chain(P, ss, ssk)
            TS(P, "dve", mems[:, ci, :], memt[:, ci, :], ss[:, 3:4], None, ALU.mult, None, ["memt", ssk], [("mems", ci)])
            for half in range(2):
                pt, ptk = pstr.next()
                for j in range(4):
                    cc = half * 4 + j
                    TR(P, pt[:, j * 128:(j + 1) * 128], mems[:, ci, cc * 128:(cc + 1) * 128], c.ident[:],
                       [("mems", ci), "ident"], [ptk])
                CP(P, "dve", mhT[:, half * 4:(half + 1) * 4, ci * 128:(ci + 1) * 128],
                   pt[:].rearrange("p (j t) -> p j t", j=4), [ptk], [("mhT", ci, half)])
        mh = [("mhT", ci, half) for ci in range(2) for half in range(2)]
        for h in range(4):
            for kc in range(8):
                MM(P, pmisc[0:64, 0:256], Wkv[:, kc, 64 * h:64 * h + 64], mhT[:, kc, :], kc == 0, kc == 7, Wk + mh, ["pmisc"])
            CP(P, "dve", mkT[h][:], pmisc[0:64, 0:256], ["pmisc"], [("mkT", h)])
        for ci in range(2):
            for kc in range(8):
                MM(P, pmisc[:, 256:512], mhT[:, kc, ci * 128:(ci + 1) * 128], Wkv[:, kc, 256:512], kc == 0, kc == 7,
                   Wk + mh, ["pmisc"])
            CP(P, "dve", mvaug[:, ci, :, 0:64], pmisc[:, 256:512].rearrange("p (h d) -> p h d", h=4), ["pmisc"], ["mvaug"])
        MEMSET(P, "dve", mvaug[:, :, :, 64:65], 1.0, ["mvaug"], ["mvaug"])

        for g in range(8):
            o, ok = ob.next()
            for h in range(4):
                es_ = []
                for ci in range(2):
                    ps_, psk = pss.next()
                    MM(P, ps_[:, :], mkT[h][:, ci * 128:(ci + 1) * 128], qm[h][:, g * 512:(g + 1) * 512], True, True,
                       [("mkT", h), ("qm", h)], [psk])
                    e, ek = eb.next()
                    ACT(P, e[:], ps_[:, :], AF.Exp, [psk], [ek])
                    es_.append((e, ek))
                for sub in range(4):
                    pa, pak = psacc.next()
                    for ci in range(2):
                        MM(P, pa[:, 0:65], es_[ci][0][:, sub * 128:(sub + 1) * 128], mvaug[:, ci, h, :], ci == 0, ci == 1,
                           [es_[ci][1], "mvaug"], [pak])
                    w, wk = wb.next()
                    RECIP(P, w[:, 0:1], pa[:, 64:65], [pak], [wk])
                    TS(P, "dve", o[:, sub, 64 * h:64 * h + 64], pa[:, 0:64], w[:, 0:1], None, ALU.mult, None, [pak, wk],
                       [(ok, sub, h)])
            ost, ostk = ostb.next()
            for sub in range(4):
                pt, ptk = pstr.next()
                for j in range(2):
                    TR(P, pt[:, j * 128:(j + 1) * 128], o[:, sub, j * 128:(j + 1) * 128], c.ident[:],
                       [(ok, sub, 2 * j), (ok, sub, 2 * j + 1), "ident"], [ptk])
                CP(P, "act", ost[:, :, sub * 128:(sub + 1) * 128], pt[:, 0:256].rearrange("p (j t) -> p j t", j=2),
                   [ptk], [(ostk, sub)])
            DMA(P, "sp", c.MEMOT.rearrange("(j p) t -> p j t", p=128)[:, :, g * 512:(g + 1) * 512], ost[:],
                [(ostk, sub) for sub in range(4)], ["MEMOT"])
        P.end_phase()
        P.alloc_sems(c.es0, c.sems)
        with nc.Block() as block:
            P.emit(block, c.sems)


def phase_E(c):
    nc, P = c.nc, c.P
    with ExitStack() as es:
        sb = lambda name, shape, dt: es.enter_context(nc.sbuf_tensor(name, shape, dt))
        Wmg = sb("Wmg", [128, 8, 3072], BF16)
        Wbr = [sb("Wsb", [128, 3, D], BF16), sb("Wnsa", [128, 3, D], BF16), sb("Wmem", [128, 2, D], BF16)]
        Wout = sb("Wout", [128, 8, D], BF16)
        wst = Rot([sb(f"wstE{i}", [128, 1024], F32) for i in range(3)], "wstE")
        gcol = sb("gcolE", [128, 8], F32)
        bmg = sb("bmg", [128, 24], F32)
        hTb = Rot([sb(f"hTE{i}", [128, 8, 512], BF16) for i in range(2)], "hTE")
        srcb = [Rot([sb(f"srcE{b}_{i}", [128, 3 if b < 2 else 2, 512], BF16) for i in range(2)], f"srcE{b}") for b in range(3)]
        mgb = Rot([sb(f"mgT{i}", [128, 8, 512], BF16) for i in range(2)], "mgT")
        gateb = Rot([sb(f"gateE{i}", [128, 512], F32) for i in range(3)], "gateE")
        accb = Rot([sb(f"accE{i}", [128, 512], F32) for i in range(2)], "accE")
        tmpb = Rot([sb(f"tmpE{i}", [128, 512], F32) for i in range(2)], "tmpE")
        xb = Rot([sb(f"xE{i}", [128, D], F32) for i in range(2)], "xE")
        x1b = Rot([sb(f"x1E{i}", [128, D], F32) for i in range(2)], "x1E")
        psbr = Rot(c.ps[0:2], "psbr")
        psg = Rot(c.ps[2:5], "psg")
        psy = Rot(c.ps[5:8], "psy")

        DMA(P, "sp", gcol[:], c.inp["mix_g"], [], ["gcol"])
        DMA(P, "sp", bmg[:], c.inp["b_merge"], [], ["bmg"])
        n = 0
        for kc in range(8):
            for j in range(3):
                st, sk = wst.next()
                DMA(P, "sp", st[:], c.inp["w_in"][kc * 128:(kc + 1) * 128, 2578 + 1024 * j:2578 + 1024 * (j + 1)], [], [sk])
                if n % 2 == 0:
                    TS(P, "dve", Wmg[:, kc, 1024 * j:1024 * (j + 1)], st[:], gcol[:, kc:kc + 1], None, ALU.mult, None,
                       [sk, "gcol"], [("Wmg", kc)])
                else:
                    ACT(P, Wmg[:, kc, 1024 * j:1024 * (j + 1)], st[:], AF.Copy, [sk, "gcol"], [("Wmg", kc)],
                        scale=gcol[:, kc:kc + 1])
                n += 1
        for b, (nm, nf) in enumerate((("w_sb_br", 3), ("w_nsa_br", 3), ("w_mem_br", 2))):
            for f in range(nf):
                st, sk = wst.next()
                DMA(P, "sp", st[:], c.inp[nm][f * 128:(f + 1) * 128, :], [], [sk])
                CP(P, "dve" if n % 2 == 0 else "act", Wbr[b][:, f, :], st[:], [sk], [("Wbr", b)])
                n += 1
        for kc in range(8):
            st, sk = wst.next()
            DMA(P, "sp", st[:], c.inp["w_out"][kc * 128:(kc + 1) * 128, :], [], [sk])
            CP(P, "dve" if n % 2 == 0 else "act", Wout[:, kc, :], st[:], [sk], ["Wout"])
            n += 1
        Wmgk = [("Wmg", kc) for kc in range(8)]
        srcs = [(c.SBOT, 3, "SBOT"), (c.NSAOT, 3, "NSAOT"), (c.MEMOT, 2, "MEMOT")]
        for tg in range(8):
            tc_ = slice(tg * 512, (tg + 1) * 512)
            hT, hk = hTb.next()
            DMA(P, "sp", hT[:], c.HT.rearrange("(c p) t -> p c t", p=128)[:, :, tc_], ["HT"], [hk])
            src = []
            for b, (ap, nf, nm) in enumerate(srcs):
                t_, tk = srcb[b].next()
                DMA(P, "sp", t_[:], ap.rearrange("(f p) t -> p f t", p=128)[:, :, tc_], [nm], [tk])
                src.append((t_, tk, nf))
            mg, mgk = mgb.next()
            for dc in range(8):
                acc, acck = accb.next()
                for b in range(3):
                    t_, tk, nf = src[b]
                    pb, pbk = psbr.next()
                    for f in range(nf):
                        MM(P, pb[:, :], Wbr[b][:, f, dc * 128:(dc + 1) * 128], t_[:, f, :], f == 0, f == nf - 1,
                           [("Wbr", b), tk], [pbk])
                    pg, pgk = psg.next()
                    for kc in range(8):
                        MM(P, pg[:, :], Wmg[:, kc, b * 1024 + dc * 128:b * 1024 + (dc + 1) * 128], hT[:, kc, :], kc == 0, kc == 7,
                           Wmgk + [hk], [pgk])
                    gt, gtk = gateb.next()
                    ACT(P, gt[:], pg[:, :], AF.Sigmoid, [pgk, "bmg"], [gtk], bias=bmg[:, b * 8 + dc:b * 8 + dc + 1])
                    if b == 0:
                        TT(P, "dve", acc[:], gt[:], pb[:, :], ALU.mult, [gtk, pbk], [acck])
                    else:
                        tmp, tmpk = tmpb.next()
                        TT(P, "dve", tmp[:], gt[:], pb[:, :], ALU.mult, [gtk, pbk], [tmpk])
                        if b == 1:
                            TT(P, "pool", acc[:], acc[:], tmp[:], ALU.add, [acck, tmpk], [acck])
                        else:
                            TT(P, "pool", mg[:, dc, :], acc[:], tmp[:], ALU.add, [acck, tmpk], [(mgk, dc)])
            mgks = [(mgk, dc) for dc in range(8)]
            for s in range(4):
                i = tg * 4 + s
                xt, xk = xb.next()
                DMA(P, "sp", xt[:], c.inp["x"][i * 128:(i + 1) * 128, :], [], [xk])
                x1, x1k = x1b.next()
                for half in range(2):
                    py, pyk = psy.next()
                    for dc in range(8):
                        MM(P, py[:, :], mg[:, dc, s * 128:(s + 1) * 128], Wout[:, dc, half * 512:(half + 1) * 512], dc == 0, dc == 7,
                           mgks + ["Wout"], [pyk])
                    TT(P, "dve", x1[:, half * 512:(half + 1) * 512], xt[:, half * 512:(half + 1) * 512], py[:, :], ALU.add,
                       [xk, pyk], [(x1k, half)])
                DMA(P, "sp", c.X1[i * 128:(i + 1) * 128, :], x1[:], [(x1k, 0), (x1k, 1)], ["X1"])
        P.end_phase()
        P.alloc_sems(c.es0, c.sems)
        with nc.Block() as block:
            P.emit(block, c.sems)


def phase_F(c):
    nc, P = c.nc, c.P
    GS = 8
    with ExitStack() as es:
        sb = lambda name, shape, dt: es.enter_context(nc.sbuf_tensor(name, shape, dt))
        Wq = sb("Wq", [128, 8, 2048], BF16)
        subk = sb("subk", [128, 16, 128], BF16)
        g2b = sb("g2b", [128, D], F32)
        gFb = sb("gFb", [128, D], F32)
        keyidx = sb("keyidx", [128, 2048], I32)
        posidx = sb("posidx", [128, 2048], I32)
        iotaA = sb("iotaA", [128, 2048], F32)
        cI = sb("cI", [128, 8], I32)
        with ExitStack() as es1:
            sb1 = lambda name, shape, dt: es1.enter_context(nc.sbuf_tensor(name, shape, dt))
            wst = Rot([sb1(f"wstF{i}", [128, 2048], F32) for i in range(2)], "wstF")
            iotaAi = sb1("iotaAi", [128, 2048], I32)
            for kc in range(8):
                st, sk = wst.next()
                DMA(P, "sp", st[:], c.inp["peer_w_q"][kc * 128:(kc + 1) * 128, :], [], [sk])
                CP(P, "dve" if kc % 2 == 0 else "act", Wq[:, kc, :], st[:], [sk], [("Wq", kc)])
            st, sk = wst.next()
            DMA(P, "sp", st[:], c.inp["subkT"].rearrange("d b k -> d (b k)"), [], [sk])
            CP(P, "dve", subk[:].rearrange("d b k -> d (b k)"), st[:], [sk], ["subk"])
            DMA(P, "sp", g2b[:], c.inp["ffn_g"].partition_broadcast(128), [], ["g2b"])
            DMA(P, "sp", gFb[:], c.inp["final_g"].partition_broadcast(128), [], ["gFb"])
            IOTA(P, keyidx[:], [[0, 16], [1, 128]], 0, 0, [], ["keyidx"])
            IOTA(P, posidx[:], [[0, 8], [1, 256]], 0, 0, [], ["posidx"])
            IOTA(P, iotaAi[:], [[0, 128], [1, 16]], 0, 0, [], ["iotaAi"])
            CP(P, "dve", iotaA[:], iotaAi[:], ["iotaAi"], ["iotaA"])
            for j, v in enumerate((-128, -256, 127, 255, 15, 4)):
                IOTA(P, cI[:, j:j + 1], [[0, 1]], v, 0, ["cI"], ["cI"])
            P.end_phase()
            P.alloc_sems(c.es0, c.sems)
            with nc.Block() as block:
                P.emit(block, c.sems)
        x1b = Rot([sb(f"x1F{i}", [128, D], F32) for i in range(2)], "x1F")
        h2b = Rot([sb(f"h2F{i}", [128, D], F32) for i in range(1)], "h2F")
        ssb = Rot([sb(f"ssF{i}", [128, 4], F32) for i in range(4)], "ssF")
        junk = sb("junkF", [128, D], BF16)
        prodb = Rot([sb(f"prodF{i}", [128, D], BF16) for i in range(4)], "prodF")
        h2hb = Rot([sb(f"h2hF{i}", [128, D], BF16) for i in range(2)], "h2hF")
        h2Tb = Rot([sb(f"h2T{i}", [128, 8, 128], BF16) for i in range(2)], "h2T")
        qTb = sb("qTbF", [128, 16, 128], BF16)
        Sc = sb("Sc", [128, 2048], F32)
        rep = sb("repF", [128, 256], F32)
        stop = sb("stop", [128, 16, 16], F32)
        itop_i = sb("itop_i", [128, 256], I32)
        itop_f = sb("itop_f", [128, 16, 16], F32)
        tmpA = sb("tmpA", [128, 2048], F32)
        tmpB = Sc
        best = sb("best", [128, 8, 16], F32)
        pos_i = sb("pos_i", [128, 3, 128], I32)
        ab_f = sb("ab_f", [128, 2, 128], F32)
        sel_f = sb("sel_f", [128, 3, 128], F32)
        idxb = Rot([sb(f"idxF{i}", [128, 128], I32) for i in range(2)], "idxF")
        gwb = Rot([sb(f"gwF{i}", [128, 3, 128], F32) for i in range(2)], "gwF")
        gsum = sb("gsum", [128, 16], F32)
        ab = Rot([sb(f"aF{i}", [128, 2, 128], F32) for i in range(2)], "aF")
        uvb = Rot([sb(f"uvg{i}", [128, 2 * D], BF16) for i in range(16)], "uvg")
        dgb = Rot([sb(f"dg{i}", [128, 128], BF16) for i in range(6)], "dg")
        x2b = Rot([sb(f"x2F{i}", [128, D], F32) for i in range(1)], "x2F")
        ptq = Rot(c.ps[0:2], "ptq")
        psS = Rot(c.ps[2:4], "psS")
        pvb = Rot([(c.ps[4], c.ps[5]), (c.ps[6], c.ps[7])], "pv")
        Wqk = [("Wq", kc) for kc in range(8)]

        def route(i, st):
            x1, x1k = x1b.next()
            DMA(P, "sp", x1[:], c.X1[i * 128:(i + 1) * 128, :], ["X1"], [x1k])
            ss, ssk = ssb.next()
            ACT(P, junk[:], x1[:], AF.Square, [x1k], [ssk], accum=ss[:, 0:1])
            rstd_chain(P, ss, ssk)
            h2, h2k = h2b.next()
            STT(P, h2[:], x1[:], ss[:, 3:4], g2b[:], ALU.mult, ALU.mult, [x1k, ssk, "g2b"], [h2k])
            h2h, h2hk = h2hb.next()
            CP(P, "act", h2h[:], h2[:], [h2k], [h2hk])
            yield
            h2T, h2Tk = h2Tb.next()
            for half in range(2):
                pt, ptk = ptq.next()
                for j in range(4):
                    cc = half * 4 + j
                    TR(P, pt[:, j * 128:(j + 1) * 128], h2[:, cc * 128:(cc + 1) * 128], c.ident[:], [h2k, "ident"], [ptk])
                CP(P, "act", h2T[:, half * 4:(half + 1) * 4, :], pt[:].rearrange("p (j t) -> p j t", j=4), [ptk], [(h2Tk, half)])
            h2Tks = [(h2Tk, 0), (h2Tk, 1)]
            yield
            for b4 in range(4):
                pq, pqk = ptq.next()
                for j in range(4):
                    blk = b4 * 4 + j
                    for kc in range(8):
                        MM(P, pq[:, j * 128:(j + 1) * 128], Wq[:, kc, blk * 128:(blk + 1) * 128], h2T[:, kc, :], kc == 0, kc == 7,
                           Wqk + h2Tks, [pqk])
                CP(P, "act", qTb[:, b4 * 4:(b4 + 1) * 4, :], pq[:].rearrange("p (j t) -> p j t", j=4), [pqk], [("qTb", b4)])
                yield
            for b4 in range(4):
                pS, pSk = psS.next()
                for j in range(4):
                    blk = b4 * 4 + j
                    MM(P, pS[:, j * 128:(j + 1) * 128], qTb[:, blk, :], subk[:, blk, :], True, True, [("qTb", b4), "subk"], [pSk])
                STT(P, Sc[:, b4 * 512:(b4 + 1) * 512].bitcast(I32), pS[:, :].bitcast(I32), cI[:, 0:1],
                    keyidx[:, b4 * 512:(b4 + 1) * 512], ALU.bitwise_and, ALU.bitwise_or, [pSk, "cI", "keyidx"], [("Sc", b4), "tmpB"])
                yield
            for blk in range(16):
                sblk = Sc[:, blk * 128:(blk + 1) * 128]
                sck = ("Sc", blk // 4)
                P.op("dve", lambda e, o=stop[:, blk, 0:8], s=sblk: e.max(out=o, in_=s), [sck], ["stop"])
                P.op("dve", lambda e, o=rep[:, 0:128], m=stop[:, blk, 0:8], s=sblk: e.match_replace(
                    out=o, in_to_replace=m, in_values=s, imm_value=-1e30), [sck, "stop"], ["repF"])
                P.op("dve", lambda e, o=stop[:, blk, 8:16], s=rep[:, 0:128]: e.max(out=o, in_=s), ["repF"], ["stop"])
                yield
            stop2 = stop[:].rearrange("p b k -> p (b k)")
            TS(P, "dve", itop_i[:], stop2.bitcast(I32), cI[:, 2:3], None, ALU.bitwise_and, None, ["stop", "cI"], ["itop_i"])
            CP(P, "dve", itop_f[:].rearrange("p b k -> p (b k)"), itop_i[:], ["itop_i"], ["itop_f"])
            yield
            sv = stop[:].rearrange("p (h q) k -> p h q k", q=2)
            iv = itop_f[:].rearrange("p (h q) k -> p h q k", q=2)
            cand = tmpA[:].rearrange("p (h a b) -> p h a b", h=8, a=16)
            TT(P, "dve", cand, sv[:, :, 0, :].unsqueeze(3).to_broadcast([128, 8, 16, 16]),
               sv[:, :, 1, :].unsqueeze(2).to_broadcast([128, 8, 16, 16]), ALU.add, ["stop"], ["tmpA"])
            yield
            STT(P, tmpB[:].bitcast(I32), tmpA[:].bitcast(I32), cI[:, 1:2], posidx[:], ALU.bitwise_and, ALU.bitwise_or,
                ["tmpA", "cI", "posidx"], ["tmpB"] + [("Sc", b_) for b_ in range(4)])
            yield
            for h in range(8):
                sblk = tmpB[:, h * 256:(h + 1) * 256]
                P.op("dve", lambda e, o=best[:, h, 0:8], s=sblk: e.max(out=o, in_=s), ["tmpB"], ["best"])
                P.op("dve", lambda e, o=rep[:], m=best[:, h, 0:8], s=sblk: e.match_replace(
                    out=o, in_to_replace=m, in_values=s, imm_value=-1e30), ["tmpB", "best"], ["repF"])
                P.op("dve", lambda e, o=best[:, h, 8:16], s=rep[:]: e.max(out=o, in_=s), ["repF"], ["best"])
                yield
            best2 = best[:].rearrange("p h k -> p (h k)")
            TS(P, "dve", pos_i[:, 0, :], best2.bitcast(I32), cI[:, 3:4], None, ALU.bitwise_and, None, ["best", "cI"], ["pos_i"])
            TS(P, "dve", pos_i[:, 1, :], pos_i[:, 0, :], cI[:, 5:6], None, ALU.logical_shift_right, None, ["pos_i", "cI"], ["pos_i"])
            TS(P, "dve", pos_i[:, 2, :], pos_i[:, 0, :], cI[:, 4:5], None, ALU.bitwise_and, None, ["pos_i", "cI"], ["pos_i"])
            CP(P, "dve", ab_f[:], pos_i[:, 1:3, :], ["pos_i"], ["ab_f"])
            yield
            for q in range(2):
                akv = ab_f[:, q, :].rearrange("p (h k) -> p h k", h=8)
                eq = tmpA[:].rearrange("p (h k a) -> p h k a", h=8, k=16)
                TT(P, "dve", eq, akv.unsqueeze(3).to_broadcast([128, 8, 16, 16]),
                   iotaA[:].rearrange("p (h k a) -> p h k a", h=8, k=16), ALU.is_equal, ["ab_f", "iotaA"], ["tmpA"])
                yield
                pr = tmpB[:].rearrange("p (h k a) -> p h k a", h=8, k=16)
                TT(P, "dve", pr, eq, iv[:, :, q, :].unsqueeze(2).to_broadcast([128, 8, 16, 16]), ALU.mult,
                   ["tmpA", "itop_f"], ["tmpB"] + [("Sc", b_) for b_ in range(4)])
                yield
                P.op("dve", lambda e, o=sel_f[:, q, :], s=tmpB[:].rearrange("p (x a) -> p x a", a=16): e.tensor_reduce(
                    out=o, in_=s, axis=AX.X, op=ALU.add), ["tmpB"], ["sel_f"])
                yield
            STT(P, sel_f[:, 2, :], sel_f[:, 0, :], 128.0, sel_f[:, 1, :], ALU.mult, ALU.add, ["sel_f"], ["sel_f"])
            TS(P, "dve", sel_f[:, 2, :], sel_f[:, 2, :], 0.0, 16383.0, ALU.max, ALU.min, ["sel_f"], ["sel_f"])
            idx, idxk = idxb.next()
            CP(P, "dve", idx[:], sel_f[:, 2, :], ["sel_f"], [idxk])
            yield
            gw, gwk = gwb.next()
            v3 = lambda ap: ap.rearrange("p (h k) -> p h k", h=8)
            TT(P, "dve", v3(gw[:, 0, :]), best[:], best[:, :, 0:1].to_broadcast([128, 8, 16]), ALU.subtract, ["best"], [gwk])
            ACT(P, gw[:, 1, :], gw[:, 0, :], AF.Exp, [gwk], [gwk])
            yield
            P.op("dve", lambda e, o=gsum[:, 0:8], s=v3(gw[:, 1, :]): e.tensor_reduce(out=o, in_=s, axis=AX.X, op=ALU.add),
                 [gwk], ["gsum"])
            RECIP(P, gsum[:, 8:16], gsum[:, 0:8], ["gsum"], ["gsum"])
            TT(P, "dve", v3(gw[:, 2, :]), v3(gw[:, 1, :]), gsum[:, 8:16].unsqueeze(2).to_broadcast([128, 8, 16]), ALU.mult,
               [gwk, "gsum"], [gwk])
            st.update(x1=x1, x1k=x1k, h2=h2h, h2k=h2hk, idx=idx, idxk=idxk, gw=gw, gwk=gwk)
            yield

        def slots(i, st, bg):
            x1, x1k, h2, h2k, idx, idxk, gw, gwk = (st[k_] for k_ in ("x1", "x1k", "h2", "h2k", "idx", "idxk", "gw", "gwk"))
            a, ak = ab.next()
            (pv0, pv1), pvk = pvb.next()
            LAG = 6
            GSZ = 4
            held = {}
            for s in range(128 + LAG):
                if s < 128:
                    uv, uvk = uvb.next()
                    held[s] = (uv, uvk)
                    P.dma("pool", lambda e, o=uv[:], ix=idx[:, s:s + 1]: e.indirect_dma_start(
                        out=o, out_offset=None, in_=c.UVB,
                        in_offset=bass.IndirectOffsetOnAxis(ap=ix.bitcast(U32), axis=0)), [idxk, "UVB"], [uvk])
                    pd, pdk = prodb.next()
                    TT(P, "dve", pd[:], uv[:, 0:D], h2[:], ALU.mult, [uvk, h2k], [pdk])
                    ACT(P, junk[:], pd[:], AF.Copy, [pdk], [(ak, s)], accum=a[:, 0, s:s + 1])
                    if s % GSZ == GSZ - 1:
                        gs_ = slice(s - GSZ + 1, s + 1)
                        ACT(P, a[:, 1, gs_], a[:, 0, gs_], AF.Gelu, [(ak, s_) for s_ in range(s - GSZ + 1, s + 1)],
                            [(ak, "g", s // GSZ)])
                r_ = s - LAG
                if r_ >= 0:
                    uv, uvk = held.pop(r_)
                    dg, dgk = dgb.next()
                    TS(P, "dve", dg[:], c.identb[:], a[:, 1, r_:r_ + 1], gw[:, 2, r_:r_ + 1], ALU.mult, ALU.mult,
                       ["identb", (ak, "g", r_ // GSZ), gwk], [dgk])
                    MM(P, pv0[:, :], dg[:], uv[:, D:D + 512], r_ == 0, r_ == 127, [dgk, uvk], [(pvk, 0)])
                    MM(P, pv1[:, :], dg[:], uv[:, D + 512:2 * D], r_ == 0, r_ == 127, [dgk, uvk], [(pvk, 1)])
                if bg is not None and s % 2 == 1:
                    next(bg, None)
            if bg is not None:
                for _ in bg:
                    pass
            x2, x2k = x2b.next()
            TT(P, "dve", x2[:, 0:512], x1[:, 0:512], pv0[:, :], ALU.add, [x1k, (pvk, 0)], [(x2k, 0)])
            TT(P, "dve", x2[:, 512:1024], x1[:, 512:1024], pv1[:, :], ALU.add, [x1k, (pvk, 1)], [(x2k, 1)])
            ss2, ss2k = ssb.next()
            ACT(P, junk[:], x2[:], AF.Square, [(x2k, 0), (x2k, 1)], [ss2k], accum=ss2[:, 0:1])
            rstd_chain(P, ss2, ss2k)
            STT(P, x2[:], x2[:], ss2[:, 3:4], gFb[:], ALU.mult, ALU.mult, [(x2k, 0), (x2k, 1), ss2k, "gFb"], [(x2k, 0), (x2k, 1)])
            DMA(P, "sp", c.out[i * 128:(i + 1) * 128, :], x2[:], [(x2k, 0), (x2k, 1)], ["out"])

        states = [dict() for _ in range(NT)]
        for _ in route(0, states[0]):
            pass
        for i in range(NT):
            bg = route(i + 1, states[i + 1]) if i + 1 < NT else None
            slots(i, states[i], bg)
        P.end_phase()
        P.alloc_sems(c.es0, c.sems)
        with nc.Block() as block:
            P.emit(block, c.sems)


def build(upto="F", debug=False):
    nc = bass.Bass("TRN2", target_bir_lowering=False)
    c = Ctx()
    c.nc = nc
    c.P = Prog(nc, same_engine_sync=os.environ.get('MK_SES', '1') == '1')
    c.sems = {}
    inp = {}

    def din(name, shape, dt=F32):
        inp[name] = nc.dram_tensor(name, list(shape), dt, kind="ExternalInput").ap()

    din("x", [T, D])
    din("mem", [256, D])
    din("mix_g", [128, 8])
    din("mem_g", [128, 8])
    din("w_in", [D, IN_DIM])
    din("b_merge", [128, 24])
    din("pe_k", [64, 32])
    din("pe_v", [64, 32])
    din("cw_k", [64, 32, 64])
    din("cw_v", [64, 32, 64])
    din("w_mem_kv", [D, 512])
    din("w_sb_br", [384, D])
    din("w_nsa_br", [384, D])
    din("w_mem_br", [256, D])
    din("w_out", [D, D])
    din("ffn_g", [D])
    din("peer_w_q", [D, 2048])
    din("subkT", [128, 16, 128])
    din("peer_uv", [16384, 2 * D])
    din("final_g", [D])
    c.inp = inp
    kind = "ExternalOutput" if debug else "Internal"

    def scr(name, shape, dt):
        return nc.dram_tensor(name, list(shape), dt, kind=kind).ap()

    c.FM = scr("FM", [FM_ROWS, T], BF16)
    c.HT = scr("HT", [D, T], BF16)
    c.TMV = scr("TMV", [T, 640], BF16)
    c.GATES = scr("GATES", [T, 18], F32)
    c.SBOT = scr("SBOT", [384, T], BF16)
    c.NSAOT = scr("NSAOT", [384, T], BF16)
    c.MEMOT = scr("MEMOT", [256, T], BF16)
    c.X1 = scr("X1", [T, D], F32)
    c.UVB = nc.dram_tensor("UVB", [16384, 2 * D], BF16, kind="Internal").ap()
    c.out = nc.dram_tensor("out", [T, D], F32, kind="ExternalOutput").ap()

    with ExitStack() as es0:
        c.es0 = es0
        c.ps = [es0.enter_context(nc.psum_tensor(f"ps{i}", [128, 512], F32)) for i in range(8)]
        c.ident = es0.enter_context(nc.sbuf_tensor("ident", [128, 128], F32))
        c.identb = es0.enter_context(nc.sbuf_tensor("identb", [128, 128], BF16))
        P = c.P
        MEMSET(P, "pool", c.ident[:], 1.0, [], ["ident"])
        ASEL(P, c.ident[:], c.ident[:], [[1, 128]], ALU.is_equal, 0.0, 0, -1, ["ident"], ["ident"])
        CP(P, "pool", c.identb[:], c.ident[:], ["ident"], ["identb"])
        phases = [("A", phase_A), ("B", phase_B), ("C", phase_C), ("D", phase_D), ("E", phase_E), ("F", phase_F)]
        for name, fn in phases:
            fn(c)
            if name == upto:
                break
    return nc


def make_inputs(inputs, b):
    f = lambda a: np.ascontiguousarray(a, dtype=np.float32)
    gcol = lambda g: f(np.asarray(g).reshape(8, 128).T)
    m = {
        "x": f(inputs["x"][b]),
        "mem": f(inputs["mem"][b]),
        "mix_g": gcol(inputs["mix_norm_g"][0]),
        "mem_g": gcol(inputs["mem_norm_g"][0]),
        "w_in": f(inputs["w_in"][0]),
        "b_merge": f(np.asarray(inputs["b_merge"][0]).reshape(24, 128).T),
        "pe_k": f(np.asarray(inputs["cmp_pe_k"][0]).T),
        "pe_v": f(np.asarray(inputs["cmp_pe_v"][0]).T),
        "cw_k": f(np.asarray(inputs["cmp_w_k"][0]).transpose(1, 0, 2)),
        "cw_v": f(np.asarray(inputs["cmp_w_v"][0]).transpose(1, 0, 2)),
        "w_mem_kv": f(inputs["w_mem_kv"][0]),
        "w_sb_br": f(inputs["w_sb_br"][0]),
        "w_nsa_br": f(inputs["w_nsa_br"][0]),
        "w_mem_br": f(inputs["w_mem_br"][0]),
        "w_out": f(inputs["w_out"][0]),
        "ffn_g": f(inputs["ffn_norm_g"][0]),
        "peer_w_q": f(inputs["peer_w_q"][0]),
        "subkT": f(np.asarray(inputs["peer_subkeys"][0]).transpose(3, 0, 1, 2).reshape(128, 16, 128)),
        "peer_uv": np.ascontiguousarray(np.concatenate([np.asarray(inputs["peer_u"][0], dtype=np.float32),
                                                        np.asarray(inputs["peer_v"][0], dtype=np.float32)], axis=1)),
        "final_g": f(inputs["final_norm_g"]),
    }
    return m


def kernel(**inputs):
    nc = build()
    shared = None
    in_maps = []
    for b in range(8):
        m = make_inputs(inputs, b)
        if shared is None:
            shared = m
        else:
            for k in m:
                if k not in ("x", "mem"):
                    m[k] = shared[k]
        in_maps.append(m)
    res = run_bass_kernel_spmd(nc, in_maps, core_ids=list(range(8)))
    return np.stack([np.asarray(r["out"]) for r in res.results], axis=0).astype(np.float32)
```

```python
import sys
import numpy as np
from contextlib import ExitStack
import concourse.bass as bass
import concourse.mybir as mybir
from concourse.bass_utils import run_bass_kernel_spmd

F32 = mybir.dt.float32
BF16 = mybir.dt.bfloat16
I32 = mybir.dt.int32
U32 = mybir.dt.uint32
AF = mybir.ActivationFunctionType
ALU = mybir.AluOpType
AX = mybir.AxisListType

T = 4096
D = 1024
NT = T // 128
IN_DIM = 5650
EPS = 1e-6
SLOPES = [2.0 ** (-8.0 * (h + 1) / 6) for h in range(6)]
BIG = 30000.0

ENGS = ("pe", "act", "dve", "pool", "sp")
DMA_RING = 8


class Prog:
    def __init__(self, nc, same_engine_sync=True):
        self.nc = nc
        self.ops = {e: [] for e in ENGS}
        self.cnt = {e: 0 for e in ENGS}
        self.dma_n = {e: 0 for e in ENGS}
        self.last_w = {}
        self.readers = {}
        self.waited = {}
        self.same_engine_sync = same_engine_sync
        self.fill_vals = set()
        self.fill_regs = {}

    def _deps(self, eng, reads, writes):
        need = {}

        def add(tok):
            if tok is None:
                return
            sk, val, teng = tok
            if teng == eng and sk[0] == "c":
                if not self.same_engine_sync or eng == "pe":
                    return
            if need.get(sk, 0) < val:
                need[sk] = val

        for r in reads:
            add(self.last_w.get(r))
        for w in writes:
            add(self.last_w.get(w))
            for t in self.readers.get(w, ()):
                add(t)
        out = []
        for sk, val in need.items():
            if self.waited.get((eng, sk), 0) >= val:
                continue
            self.waited[(eng, sk)] = val
            out.append((sk, val))
        return out

    def _commit(self, tok, reads, writes):
        for r in reads:
            self.readers.setdefault(r, []).append(tok)
        for w in writes:
            self.last_w[w] = tok
            self.readers[w] = []

    def op(self, eng, fn, reads=(), writes=()):
        reads = tuple(reads)
        writes = tuple(writes)
        waits = self._deps(eng, reads, writes)
        self.cnt[eng] += 1
        tok = (("c", eng), self.cnt[eng], eng)
        fr = sys._getframe(1)
        self.ops[eng].append(dict(fn=fn, waits=waits, inc=(("c", eng), 1),
                                  where=(fr.f_lineno, fr.f_back.f_lineno if fr.f_back else 0)))
        self._commit(tok, reads, writes)
        return tok

    def dma(self, eng, fn, reads=(), writes=()):
        reads = tuple(reads)
        writes = tuple(writes)
        n = self.dma_n[eng]
        self.dma_n[eng] += 1
        sk = ("d", eng, n % DMA_RING)
        val = 16 * (n // DMA_RING + 1)
        waits = self._deps(eng, reads, writes)
        if val > 16 and self.waited.get((eng, sk), 0) < val - 16:
            self.waited[(eng, sk)] = val - 16
            waits.append((sk, val - 16))
        tok = (sk, val, eng)
        self.ops[eng].append(dict(fn=fn, waits=waits, inc=(sk, 16)))
        self._commit(tok, reads, writes)
        return tok

    def finish(self, eng, toks):
        self.ops[eng].append(dict(fn=None, waits=[(sk, val) for sk, val, _ in toks], inc=None))

    def end_phase(self):
        targets = []
        for e in ENGS:
            if self.cnt[e] > 0:
                targets.append((("c", e), self.cnt[e]))
            n = self.dma_n[e]
            for r in range(min(n, DMA_RING)):
                last = ((n - 1 - r) // DMA_RING) * DMA_RING + r
                targets.append((("d", e, r), 16 * (last // DMA_RING + 1)))
        for e in ENGS:
            waits = []
            for sk, val in targets:
                if self.waited.get((e, sk), 0) >= val:
                    continue
                self.waited[(e, sk)] = val
                waits.append((sk, val))
            self.ops[e].append(dict(fn=None, waits=waits, inc=None))
        self.last_w = {}
        self.readers = {}

    def sem_keys(self):
        keys = set()
        for e in ENGS:
            for o in self.ops[e]:
                if o["inc"]:
                    keys.add(o["inc"][0])
                for sk, _ in o["waits"]:
                    keys.add(sk)
        return sorted(keys)

    def alloc_sems(self, es, sems):
        for k in self.sem_keys():
            if k not in sems:
                sems[k] = es.enter_context(self.nc.semaphore("s_" + "_".join(map(str, k))))

    def emit(self, block, sems):
        engobj = {"pe": "tensor", "act": "scalar", "dve": "vector", "pool": "gpsimd", "sp": "sync"}

        def make(e):
            ops = self.ops[e]

            def body(eng):
                if e == "pool":
                    self.fill_regs = {v: eng.to_reg(v) for v in sorted(self.fill_vals)}
                for o in ops:
                    for sk, val in o["waits"]:
                        eng.wait_ge(sems[sk], val)
                    if o["fn"] is not None:
                        try:
                            ins = o["fn"](eng)
                        except Exception:
                            print("EMIT FAILED at lines", o.get("where"))
                            raise
                        if o["inc"]:
                            ins.then_inc(sems[o["inc"][0]], o["inc"][1])
            return body

        for e in ENGS:
            if self.ops[e]:
                getattr(block, engobj[e])(make(e))
        self.ops = {e: [] for e in ENGS}


class Ctx:
    pass


class Rot:
    def __init__(self, tiles, name):
        self.tiles = tiles
        self.name = name
        self.i = 0

    def next(self):
        j = self.i % len(self.tiles)
        self.i += 1
        return self.tiles[j], (self.name, j)


def MM(P, out, lhsT, rhs, start, stop, r, w):
    return P.op("pe", lambda e: e.matmul(out, lhsT=lhsT, rhs=rhs, start=start, stop=stop), r, w)


def TR(P, out, in_, ident, r, w):
    return P.op("pe", lambda e: e.transpose(out, in_, ident), r, w)


def ACT(P, out, in_, func, r, w, scale=None, bias=None, accum=None):
    kw = {}
    if scale is not None:
        kw["scale"] = scale
    if bias is not None:
        kw["bias"] = bias
    if accum is not None:
        kw["accum_out"] = accum
    return P.op("act", lambda e: e.activation(out=out, in_=in_, func=func, **kw), r, w)


def TS(P, eng, out, in0, s1, s2, op0, op1, r, w):
    if op1 is None:
        return P.op(eng, lambda e: e.tensor_scalar(out=out, in0=in0, scalar1=s1, scalar2=None, op0=op0), r, w)
    return P.op(eng, lambda e: e.tensor_scalar(out=out, in0=in0, scalar1=s1, scalar2=s2, op0=op0, op1=op1), r, w)


def TT(P, eng, out, in0, in1, op, r, w):
    return P.op(eng, lambda e: e.tensor_tensor(out=out, in0=in0, in1=in1, op=op), r, w)


def STT(P, out, in0, scalar, in1, op0, op1, r, w):
    return P.op("dve", lambda e: e.scalar_tensor_tensor(out=out, in0=in0, scalar=scalar, in1=in1, op0=op0, op1=op1), r, w)


def CP(P, eng, out, in_, r, w):
    if eng == "act":
        return P.op("act", lambda e: e.copy(out=out, in_=in_), r, w)
    return P.op(eng, lambda e: e.tensor_copy(out=out, in_=in_), r, w)


def DMA(P, eng, out, in_, r, w):
    return P.dma(eng, lambda e: e.dma_start(out=out, in_=in_), r, w)


def MEMSET(P, eng, ap, val, r, w):
    return P.op(eng, lambda e: e.memset(ap, val), r, w)


def ASEL(P, out, in_, pattern, cmp, fill, base, cm, r, w):
    P.fill_vals.add(float(fill))
    return P.op("pool", lambda e: e.affine_select(out=out, in_=in_, pattern=pattern, compare_op=cmp,
                                                  fill=P.fill_regs[float(fill)], base=base, channel_multiplier=cm), r, w)


def IOTA(P, out, pattern, base, cm, r, w):
    return P.op("pool", lambda e: e.iota(out, pattern=pattern, base=base, channel_multiplier=cm), r, w)


def RECIP(P, out, in_, r, w):
    return P.op("dve", lambda e: e.reciprocal(out=out, in_=in_), r, w)


def rstd_chain(P, ss, key):
    TS(P, "dve", ss[:, 1:2], ss[:, 0:1], 1.0 / D, EPS, ALU.mult, ALU.add, [key], [key])
    P.op("act", lambda e: e.sqrt(out=ss[:, 2:3], in_=ss[:, 1:2]), [key], [key])
    RECIP(P, ss[:, 3:4], ss[:, 2:3], [key], [key])


FM_QSB, FM_KSB, FM_QNSA, FM_KCMP, FM_VCMP, FM_KSLC, FM_KWIN, FM_QMEM = 0, 384, 768, 1152, 1280, 1408, 1536, 1664
FM_ROWS = 1920
FM_COLMAP = [(0, 0, 768), (768, 1152, 384), (1152, 1536, 128), (1280, 1664, 128), (1408, 1792, 128),
             (1536, 2048, 128), (1664, 2322, 256)]
TM_COLMAP = [(0, 768, 384), (384, 1920, 128), (512, 2176, 128), (640, 2304, 18)]
TM_COLS = 658
Q_CHUNKS = {0, 1, 2, 6, 7, 8, 13, 14}


def phase_A(c):
    nc, P = c.nc, c.P
    with ExitStack() as es:
        sb = lambda name, shape, dt: es.enter_context(nc.sbuf_tensor(name, shape, dt))
        Wfm = sb("Wfm", [128, 8, FM_ROWS], BF16)
        Wtm = sb("Wtm", [128, 8, TM_COLS], BF16)
        wst = Rot([sb(f"wst{i}", [128, 2578], F32) for i in range(2)], "wst")
        gcol = sb("gcolA", [128, 8], F32)
        xbuf = Rot([sb(f"xt{i}", [128, D], F32) for i in range(2)], "xt")
        xsbuf = Rot([sb(f"xs{i}", [128, D], F32) for i in range(2)], "xs")
        junk = sb("junkA", [128, D], F32)
        ssbuf = Rot([sb(f"ss{i}", [128, 4], F32) for i in range(4)], "ss")
        hTg = Rot([sb(f"hTg{i}", [128, 8, 512], BF16) for i in range(2)], "hTg")
        FMst = Rot([sb(f"FMst{i}", [128, 15, 512], BF16) for i in range(2)], "FMst")
        TMst = Rot([sb(f"TMst{i}", [128, 640], BF16) for i in range(3)], "TMst")
        gst = Rot([sb(f"gst{i}", [128, 18], F32) for i in range(3)], "gst")
        pstr = Rot(c.ps[0:2], "ps_tr")
        psfm = Rot(c.ps[2:5], "ps_fm")
        pstm = Rot(c.ps[5:8], "ps_tm")

        DMA(P, "sp", gcol[:], c.inp["mix_g"], [], ["gcol"])
        for kc in range(8):
            st, sk = wst.next()
            DMA(P, "sp", st[:], c.inp["w_in"][kc * 128:(kc + 1) * 128, 0:2578], [], [sk])
            n = 0
            for (dst, cm) in ((Wfm, FM_COLMAP), (Wtm, TM_COLMAP)):
                for (dc, sc, w) in cm:
                    if n % 2 == 0:
                        TS(P, "dve", dst[:, kc, dc:dc + w], st[:, sc:sc + w], gcol[:, kc:kc + 1], None, ALU.mult, None,
                           [sk, "gcol"], [("W", kc)])
                    else:
                        ACT(P, dst[:, kc, dc:dc + w], st[:, sc:sc + w], AF.Copy, [sk, "gcol"], [("W", kc)],
                            scale=gcol[:, kc:kc + 1])
                    n += 1
        Wkeys = [("W", kc) for kc in range(8)]

        for tg in range(8):
            hT, hk = hTg.next()
            hpieces = [(hk, s, half) for s in range(4) for half in range(2)]
            for s in range(4):
                i = tg * 4 + s
                xt, xk = xbuf.next()
                DMA(P, "sp", xt[:], c.inp["x"][i * 128:(i + 1) * 128, :], [], [xk])
                ss, ssk = ssbuf.next()
                ACT(P, junk[:], xt[:], AF.Square, [xk], ["junkA", ssk], accum=ss[:, 0:1])
                rstd_chain(P, ss, ssk)
                xs, xsk = xsbuf.next()
                TS(P, "dve", xs[:], xt[:], ss[:, 3:4], None, ALU.mult, None, [xk, ssk], [xsk])
                for half in range(2):
                    pt, ptk = pstr.next()
                    for j in range(4):
                        cc = half * 4 + j
                        TR(P, pt[:, j * 128:(j + 1) * 128], xs[:, cc * 128:(cc + 1) * 128], c.ident[:], [xsk, "ident"], [ptk])
                    dst = hT[:, half * 4:(half + 1) * 4, s * 128:(s + 1) * 128]
                    src = pt[:].rearrange("p (j t) -> p j t", j=4)
                    CP(P, "act" if half == 0 else "dve", dst, src, [ptk], [(hk, s, half)])
            fst, fsk = FMst.next()
            for ch in range(15):
                pf, pfk = psfm.next()
                for kc in range(8):
                    MM(P, pf[:, :], Wfm[:, kc, ch * 128:(ch + 1) * 128], hT[:, kc, :], kc == 0, kc == 7,
                       hpieces + [("W", kc)], [pfk])
                sc = 0.125 if ch in Q_CHUNKS else 1.0
                if ch % 2 == 0:
                    ACT(P, fst[:, ch, :], pf[:, :], AF.Copy, [pfk], [(fsk, ch)], scale=sc)
                else:
                    TS(P, "dve", fst[:, ch, :], pf[:, :], sc, None, ALU.mult, None, [pfk], [(fsk, ch)])
            DMA(P, "sp", c.FM.rearrange("(c p) t -> p c t", p=128)[:, :, tg * 512:(tg + 1) * 512], fst[:],
                [(fsk, ch) for ch in range(15)], ["FM"])
            DMA(P, "sp", c.HT.rearrange("(c p) t -> p c t", p=128)[:, :, tg * 512:(tg + 1) * 512], hT[:],
                hpieces, ["HT"])
            for s in range(4):
                i = tg * 4 + s
                pa, pak = pstm.next()
                for kc in range(8):
                    MM(P, pa[:, 0:512], hT[:, kc, s * 128:(s + 1) * 128], Wtm[:, kc, 0:512], kc == 0, kc == 7,
                       hpieces + [("W", kc)], [pak])
                tst, tsk = TMst.next()
                CP(P, "dve", tst[:, 0:512], pa[:, 0:512], [pak], [tsk])
                pb, pbk = pstm.next()
                for kc in range(8):
                    MM(P, pb[:, 0:146], hT[:, kc, s * 128:(s + 1) * 128], Wtm[:, kc, 512:658], kc == 0, kc == 7,
                       hpieces + [("W", kc)], [pbk])
                CP(P, "dve", tst[:, 512:640], pb[:, 0:128], [pbk], [tsk])
                gs, gsk = gst.next()
                ACT(P, gs[:], pb[:, 128:146], AF.Sigmoid, [pbk], [gsk])
                DMA(P, "sp", c.TMV[i * 128:(i + 1) * 128, :], tst[:], [tsk], ["TMV"])
                DMA(P, "sp", c.GATES[i * 128:(i + 1) * 128, :], gs[:], [gsk], ["GATES"])
        P.end_phase()
        P.alloc_sems(c.es0, c.sems)
        with nc.Block() as block:
            P.emit(block, c.sems)


def phase_B(c):
    nc, P = c.nc, c.P
    with ExitStack() as es:
        sb = lambda name, shape, dt: es.enter_context(nc.sbuf_tensor(name, shape, dt))
        qT = [sb(f"qTb{j}", [128, T], BF16) for j in range(3)]
        kT = [sb(f"kTb{j}", [128, T], BF16) for j in range(3)]
        V = sb("Vsb", [128, NT, 384], BF16)
        ntri = sb("ntri", [128, 128], BF16)
        nones = sb("nones", [128, 128], BF16)
        Ebuf = Rot([sb(f"E{i}", [128, 512], F32) for i in range(3)], "E")
        SPbuf = Rot([sb(f"SP{i}", [128, 512], BF16) for i in range(4)], "SP")
        Sbuf = Rot([sb(f"Ssum{i}", [128, 512], BF16) for i in range(3)], "Ssum")
        abuf = Rot([sb(f"aT{i}", [128, 512], BF16) for i in range(3)], "aT")
        obuf = Rot([sb(f"sbo{i}", [64, 512], BF16) for i in range(2)], "sbo")
        psA = Rot(c.ps[0:5], "psA")
        psO = Rot(c.ps[5:7], "psO")
        pswarm = c.ps[7]
        for j in range(3):
            DMA(P, "sp", qT[j][:], c.FM[FM_QSB + 128 * j:FM_QSB + 128 * (j + 1), :], ["FM"], [("qT", j)])
            DMA(P, "sp", kT[j][:], c.FM[FM_KSB + 128 * j:FM_KSB + 128 * (j + 1), :], ["FM"], [("kT", j)])
        DMA(P, "sp", V[:], c.TMV.rearrange("(c p) f -> p c f", p=128)[:, :, 0:384], ["TMV"], ["V"])
        MEMSET(P, "pool", nones[:], -1.0, [], ["nones"])
        MEMSET(P, "pool", ntri[:], -1.0, [], ["ntri"])
        ASEL(P, ntri[:], ntri[:], [[-1, 128]], ALU.is_ge, 0.0, 0, 1, ["ntri"], ["ntri"])
        uvst = Rot([sb(f"uvst{i}", [128, 2 * D], BF16) for i in range(4)], "uvst")

        def convert_chunk(ch):
            t_, tk = uvst.next()
            P.dma("pool", lambda e, o=t_[:], i_=c.inp["peer_uv"][ch * 128:(ch + 1) * 128, :]: e.dma_start(out=o, in_=i_), [], [tk])
            DMA(P, "sp", c.UVB[ch * 128:(ch + 1) * 128, :], t_[:], [tk], ["UVB"])
        steps = []
        for h in range(6):
            for g in range(8):
                nch = 4 * g + 4
                for idx_, cch in enumerate(range(nch - 1, -1, -1)):
                    steps.append(dict(h=h, g=g, cch=cch, first=idx_ == 0, last=cch == 0))
        N = len(steps)
        cur = dict(po=None, pok=None, ssum=None, ssumk=None)

        def S12(st):
            h, g, cch = st["h"], st["g"], st["cch"]
            j, half = h // 2, h % 2
            pr = slice(64 * half, 64 * half + 64)
            st["qs"] = qT[j][pr, g * 512:(g + 1) * 512]
            st["ks"] = kT[j][pr, cch * 128:(cch + 1) * 128]
            st["rk"] = [("kT", j), ("qT", j)]
            st["m"] = cch - 4 * g
            if st["first"]:
                cur["po"], cur["pok"] = psO.next()
                cur["ssum"] = cur["ssumk"] = None
            st["po"], st["pok"] = cur["po"], cur["pok"]
            pa, pak = psA.next()
            MM(P, pa[:, :], st["ks"], st["qs"], True, False, st["rk"], [pak])
            st["pa"], st["pak"] = pa, pak
            E, Ek = Ebuf.next()
            ACT(P, E[:], pa[:, :], AF.Exp, [pak], [Ek])
            SP, SPk = SPbuf.next()
            ACT(P, SP[:], E[:], AF.Ln, [Ek], [SPk], bias=1.0)
            if st["m"] >= 0:
                ASEL(P, SP[:], SP[:], [[1, 512]], ALU.is_gt, 0.0, -128 * st["m"], -1, [SPk], [SPk])
            st["SP"], st["SPk"] = SP, SPk
            st["ssum_prev"], st["ssum_prevk"] = cur["ssum"], cur["ssumk"]
            if not st["last"]:
                if st["first"]:
                    cur["ssum"], cur["ssumk"] = SP, SPk
                else:
                    sn, snk = Sbuf.next()
                    TT(P, "pool", sn[:], cur["ssum"][:], SP[:], ALU.add, [cur["ssumk"], SPk], [snk])
                    cur["ssum"], cur["ssumk"] = sn, snk

        def S34(st):
            pb, pbk = st["pa"], st["pak"]
            MM(P, pb[:, :], ntri[:], st["SP"][:], False, st["first"], ["ntri", st["SPk"]], [pbk])
            if not st["first"]:
                MM(P, pb[:, :], nones[:], st["ssum_prev"][:], False, True, ["nones", st["ssum_prevk"]], [pbk])
            aT, aTk = abuf.next()
            ACT(P, aT[:], pb[:, :], AF.Exp, [pbk], [aTk])
            if st["m"] >= 0:
                ASEL(P, aT[:], aT[:], [[1, 512]], ALU.is_gt, 0.0, -128 * st["m"], -1, [aTk], [aTk])
            st["aT"], st["aTk"] = aT, aTk

        NWARM = 2

        def S5(st):
            h, g, cch = st["h"], st["g"], st["cch"]
            for _ in range(NWARM):
                MM(P, pswarm[:, :], ntri[:], qT[0][:, 0:512], True, True, ["ntri", ("qT", 0)], ["pswarm"])
            MM(P, st["po"][0:64, :], V[:, cch, 64 * h:64 * h + 64], st["aT"][:], st["first"], st["last"], ["V", st["aTk"]], [st["pok"]])
            if st["last"]:
                ob, obk = obuf.next()
                CP(P, "dve", ob[:], st["po"][0:64, :], [st["pok"]], [obk])
                DMA(P, "sp", c.SBOT[64 * h:64 * h + 64, g * 512:(g + 1) * 512], ob[:], [obk], ["SBOT"])

        for n in range(N + 2):
            if n % 6 == 0 and n // 6 < 128:
                convert_chunk(n // 6)
            if n < N:
                S12(steps[n])
            if 0 <= n - 1 < N:
                S34(steps[n - 1])
            if 0 <= n - 2 < N:
                S5(steps[n - 2])
                steps[n - 2].clear()
        P.end_phase()
        P.alloc_sems(c.es0, c.sems)
        with nc.Block() as block:
            P.emit(block, c.sems)


def phase_C(c):
    nc, P = c.nc, c.P
    with ExitStack() as es:
        sb = lambda name, shape, dt: es.enter_context(nc.sbuf_tensor(name, shape, dt))
        qaug = [sb(f"qaug{h}", [128, T], BF16) for h in range(6)]
        kslc = [sb(f"kslc{g}", [128, T], BF16) for g in range(2)]
        kwin = [sb(f"kwin{g}", [64, T], BF16) for g in range(2)]
        kcmp = [sb(f"kcmp{g}", [64, T], BF16) for g in range(2)]
        vcmp = [sb(f"vcmp{g}", [64, T], BF16) for g in range(2)]
        kcT = [sb(f"kcT{g}", [64, 256], BF16) for g in range(2)]
        vcaug = [sb(f"vcaug{g}", [128, 2, 129], BF16) for g in range(2)]
        vwin = sb("vwin", [128, NT, 2, 65], BF16)
        vslc = sb("vslc", [128, NT, 2, 65], BF16)
        vstage = sb("vstage", [128, NT, 256], BF16)
        gates = sb("gatesC", [128, NT, 18], F32)
        pe = [sb("pek_sb", [64, 32], F32), sb("pev_sb", [64, 32], F32)]
        cwst = sb("cwst", [64, 2048], F32)
        cw = [sb("cwk", [64, 32, 64], BF16), sb("cwv", [64, 32, 64], BF16)]
        tmpb = Rot([sb(f"ctmp{i}", [64, 256], BF16) for i in range(3)], "ctmp")
        itmp = sb("itmp", [128, 64], I32)
        ftmp = sb("ftmp", [128, 64], F32)
        bias_sel = sb("bias_sel", [128, 6], F32)
        bias_win = sb("bias_win", [128, 6, 5], F32)
        bias_cmp = sb("bias_cmp", [128, 6, 64], F32)
        IOTi = sb("IOTi", [128, 128], I32)
        IOT = sb("IOT", [128, 128], F32)
        e32b = Rot([sb(f"e32_{i}", [128, 128], F32) for i in range(4)], "e32")
        ebb = Rot([sb(f"eb{i}", [128, 128], BF16) for i in range(7)], "eb")
        obuf = Rot([sb(f"oC{i}", [128, 384], F32) for i in range(2)], "oC")
        impb = Rot([sb(f"imp{i}", [128, 64], F32) for i in range(2)], "imp")
        wb = Rot([sb(f"wC{i}", [128, 4], F32) for i in range(4)], "wC")
        m8b = Rot([sb(f"m8_{i}", [128, 16], F32) for i in range(2)], "m8")
        repb = Rot([sb(f"rep{i}", [128, 64], F32) for i in range(2)], "rep")
        selb = Rot([sb(f"selp{i}", [128, 128], F32) for i in range(2)], "selp")
        Ctb = Rot([sb(f"Ct{i}", [128, 128], F32) for i in range(2)], "Ct")
        ostb = Rot([sb(f"ostC{i}", [128, 3, 128], BF16) for i in range(2)], "ostC")
        pss = Rot(c.ps[0:3], "pss")
        psacc = Rot(c.ps[3:6], "psacc")
        pstr = Rot(c.ps[6:8], "pstrC")
        pk = c.ps[6]

        for h in range(6):
            DMA(P, "sp", qaug[h][0:64, :], c.FM[FM_QNSA + 64 * h:FM_QNSA + 64 * (h + 1), :], ["FM"], [("q", h)])
        for g in range(2):
            DMA(P, "sp", kslc[g][0:64, :], c.FM[FM_KSLC + 64 * g:FM_KSLC + 64 * (g + 1), :], ["FM"], [("kslc", g)])
            DMA(P, "sp", kwin[g][:], c.FM[FM_KWIN + 64 * g:FM_KWIN + 64 * (g + 1), :], ["FM"], [("kwin", g)])
            DMA(P, "sp", kcmp[g][:], c.FM[FM_KCMP + 64 * g:FM_KCMP + 64 * (g + 1), :], ["FM"], [("kcmp", g)])
            DMA(P, "sp", vcmp[g][:], c.FM[FM_VCMP + 64 * g:FM_VCMP + 64 * (g + 1), :], ["FM"], [("vcmp", g)])
        DMA(P, "sp", vstage[:], c.TMV.rearrange("(c p) f -> p c f", p=128)[:, :, 384:640], ["TMV"], ["vstage"])
        DMA(P, "sp", gates[:], c.GATES.rearrange("(c p) f -> p c f", p=128), ["GATES"], ["gates"])
        DMA(P, "sp", pe[0][:], c.inp["pe_k"], [], ["pe0"])
        DMA(P, "sp", pe[1][:], c.inp["pe_v"], [], ["pe1"])
        for kv, nm in ((0, "cw_k"), (1, "cw_v")):
            DMA(P, "sp", cwst[:], c.inp[nm].rearrange("d l e -> d (l e)"), [], ["cwst"])
            CP(P, "dve", cw[kv][:].rearrange("d l e -> d (l e)"), cwst[:], ["cwst"], [("cw", kv)])
        CP(P, "dve", vslc[:, :, :, 0:64], vstage[:, :, 0:128].rearrange("p c (g d) -> p c g d", g=2), ["vstage"], ["vslc"])
        CP(P, "pool", vwin[:, :, :, 0:64], vstage[:, :, 128:256].rearrange("p c (g d) -> p c g d", g=2), ["vstage"], ["vwin"])
        MEMSET(P, "dve", vslc[:, :, :, 64:65], 1.0, ["vslc"], ["vslc"])
        MEMSET(P, "pool", vwin[:, :, :, 64:65], 1.0, ["vwin"], ["vwin"])
        for g in range(2):
            MEMSET(P, "pool", kslc[g][64:128, :], 1.0, [], [("kx", g)])
            ASEL(P, kslc[g][64:128, :], kslc[g][64:128, :], [[1, T]], ALU.is_ge, 0.0, 0, -64, [("kx", g)], [("kx", g)])
            ASEL(P, kslc[g][64:128, :], kslc[g][64:128, :], [[-1, T]], ALU.is_ge, 0.0, 63, 64, [("kx", g)], [("kx", g)])
        IOTA(P, itmp[0:64, 0:1], [[0, 1]], 0, 1, [], ["itmp"])
        IOTA(P, itmp[64:128, 0:1], [[0, 1]], 0, 1, ["itmp"], ["itmp"])
        CP(P, "dve", ftmp[:, 0:1], itmp[:, 0:1], ["itmp"], ["ftmp"])
        for h in range(6):
            TS(P, "dve", bias_sel[:, h:h + 1], ftmp[:, 0:1], SLOPES[h], None, ALU.mult, None, ["ftmp"], ["bias_sel"])
        IOTA(P, itmp[:, 0:5], [[128, 5]], -512, 1, ["itmp", "ftmp"], ["itmp"])
        CP(P, "dve", ftmp[:, 0:5], itmp[:, 0:5], ["itmp"], ["ftmp"])
        for h in range(6):
            TS(P, "dve", bias_win[:, h, :], ftmp[:, 0:5], SLOPES[h], None, ALU.mult, None, ["ftmp"], ["bias_win"])
        IOTA(P, itmp[:, 0:64], [[2048, 2], [-128, 32]], 31, 16, ["itmp", "ftmp"], ["itmp"])
        CP(P, "dve", ftmp[:, 0:64], itmp[:, 0:64], ["itmp"], ["ftmp"])
        for h in range(6):
            TS(P, "dve", bias_cmp[:, h, :], ftmp[:, 0:64], SLOPES[h], None, ALU.mult, None, ["ftmp"], ["bias_cmp"])
        IOTA(P, IOTi[64:128, :], [[-1, 128]], 0, 64, [], ["IOTi"])
        CP(P, "dve", IOT[64:128, :], IOTi[64:128, :], ["IOTi"], ["IOT"])
        for g in range(2):
            MEMSET(P, "pool", vcaug[g][:], 1.0, [], [("vcaug", g)])
            for ci in range(2):
                ASEL(P, vcaug[g][:, ci, 65:129], vcaug[g][:, ci, 65:129], [[-4, 64]], ALU.is_ge, 0.0, 128 * ci, 1,
                     [("vcaug", g)], [("vcaug", g)])
                ASEL(P, vcaug[g][:, ci, 65:129], vcaug[g][:, ci, 65:129], [[4, 64]], ALU.is_ge, 0.0, 3 - 128 * ci, -1,
                     [("vcaug", g)], [("vcaug", g)])
        for s_ in selb.tiles:
            pass
        for j in range(2):
            MEMSET(P, "dve", selb.tiles[j][:, 0:64], 0.0, [], [("selp", j)])
        for g in range(2):
            kview = kcmp[g][:].rearrange("p (n s) -> p n s", s=16)
            vview = vcmp[g][:].rearrange("p (n s) -> p n s", s=16)
            for l in range(32):
                tmp, tk = tmpb.next()
                TS(P, "dve" if l % 2 == 0 else "pool", tmp[:, 0:255], kview[:, l // 16:l // 16 + 255, l % 16],
                   pe[0][:, l:l + 1], None, ALU.add, None, [("kcmp", g), "pe0"], [tk])
                MM(P, pk[0:64, 0:255], cw[0][:, l, :], tmp[:, 0:255], l == 0, l == 31, [("cw", 0), tk], [("pstrC", 0)])
            CP(P, "dve", kcT[g][:, 0:255], pk[0:64, 0:255], [("pstrC", 0)], [("kcT", g)])
            for ci in range(2):
                rows = 128 if ci == 0 else 127
                for l in range(32):
                    tmp, tk = tmpb.next()
                    n0 = l // 16 + ci * 128
                    TS(P, "dve" if l % 2 == 0 else "pool", tmp[:, 0:rows], vview[:, n0:n0 + rows, l % 16],
                       pe[1][:, l:l + 1], None, ALU.add, None, [("vcmp", g), "pe1"], [tk])
                    MM(P, pk[0:rows, 256:320], tmp[:, 0:rows], cw[1][:, l, :], l == 0, l == 31, [("cw", 1), tk], [("pstrC", 0)])
                CP(P, "dve", vcaug[g][0:rows, ci, 0:64], pk[0:rows, 256:320], [("pstrC", 0)], [("vcaug", g)])

        def consume(pacc, pacck, o, ok, h, i, gcol, first):
            w, wk = wb.next()
            TS(P, "dve", w[:, 0:1], pacc[:, 64:65], 1e-30, None, ALU.max, None, [pacck], [wk])
            RECIP(P, w[:, 1:2], w[:, 0:1], [wk], [wk])
            TT(P, "dve", w[:, 2:3], w[:, 1:2], gates[:, i, gcol:gcol + 1], ALU.mult, [wk, "gates"], [wk])
            if first:
                TS(P, "dve", o[:, 64 * h:64 * h + 64], pacc[:, 0:64], w[:, 2:3], None, ALU.mult, None, [pacck, wk], [(ok, h)])
            else:
                STT(P, o[:, 64 * h:64 * h + 64], pacc[:, 0:64], w[:, 2:3], o[:, 64 * h:64 * h + 64], ALU.mult, ALU.add,
                    [pacck, wk, (ok, h)], [(ok, h)])
            return w, wk

        from collections import deque
        fifo = deque()
        LAGC = 3

        def defer(fn):
            fifo.append(fn)
            while len(fifo) > LAGC:
                fifo.popleft()()

        def flush():
            while fifo:
                fifo.popleft()()

        def consume_cmp(pc, pck, o, ok, h, i, hh, imp, impk):
            w, wk = consume(pc, pck, o, ok, h, i, 3 * h + 0, True)
            if hh == 0:
                TS(P, "dve", imp[:], pc[:, 65:129], w[:, 1:2], None, ALU.mult, None, [pck, wk], [impk])
            else:
                STT(P, imp[:], pc[:, 65:129], w[:, 1:2], imp[:], ALU.mult, ALU.add, [pck, wk, impk], [impk])

        for i in range(NT):
            t0 = 128 * i
            qc = slice(t0, t0 + 128)
            o, ok = obuf.next()
            for g in range(2):
                imp, impk = impb.next()
                for hh in range(3):
                    h = 3 * g + hh
                    nvalid = min(255, (t0 + 96) // 16 + 1)
                    chunks = [(0, min(128, nvalid))] + ([(1, nvalid - 128)] if nvalid > 128 else [])
                    pc, pck = psacc.next()
                    for ni, (ci, rows) in enumerate(chunks):
                        ps_, psk = pss.next()
                        MM(P, ps_[0:rows, 0:128], kcT[g][:, ci * 128:ci * 128 + rows], qaug[h][0:64, qc], True, True,
                           [("kcT", g), ("q", h)], [psk])
                        e32, e32k = e32b.next()
                        ACT(P, e32[0:rows, :], ps_[0:rows, 0:128], AF.Exp, [psk, "bias_cmp"], [e32k],
                            bias=bias_cmp[0:rows, h, ci * 32 + i:ci * 32 + i + 1])
                        eb, ebk = ebb.next()
                        ASEL(P, eb[0:rows, :], e32[0:rows, :], [[1, 128]], ALU.is_ge, 0.0, t0 - 2048 * ci - 31, -16,
                             [e32k], [ebk])
                        defer(lambda pc=pc, pck=pck, eb=eb, ebk=ebk, rows=rows, ci=ci, g=g, st_=(ni == 0), sp_=(ni == len(chunks) - 1):
                              MM(P, pc[:, 0:129], eb[0:rows, :], vcaug[g][0:rows, ci, :], st_, sp_, [ebk, ("vcaug", g)], [pck]))
                    defer(lambda pc=pc, pck=pck, o=o, ok=ok, h=h, i=i, hh=hh, imp=imp, impk=impk:
                          consume_cmp(pc, pck, o, ok, h, i, hh, imp, impk))
                    pw, pwk = psacc.next()
                    cl = list(range(max(0, i - 4), i + 1))
                    for ni, cch in enumerate(cl):
                        dc = cch - i
                        ps_, psk = pss.next()
                        MM(P, ps_[:, 0:128], kwin[g][:, cch * 128:(cch + 1) * 128], qaug[h][0:64, qc], True, True,
                           [("kwin", g), ("q", h)], [psk])
                        eb, ebk = ebb.next()
                        bw = bias_win[:, h, dc + 4:dc + 5]
                        if dc == 0 or dc == -4:
                            e32, e32k = e32b.next()
                            ACT(P, e32[:], ps_[:, 0:128], AF.Exp, [psk, "bias_win"], [e32k], bias=bw)
                            if dc == 0:
                                ASEL(P, eb[:], e32[:], [[1, 128]], ALU.is_ge, 0.0, 0, -1, [e32k], [ebk])
                            else:
                                ASEL(P, eb[:], e32[:], [[-1, 128]], ALU.is_gt, 0.0, 0, 1, [e32k], [ebk])
                        else:
                            ACT(P, eb[:], ps_[:, 0:128], AF.Exp, [psk, "bias_win"], [ebk], bias=bw)
                        defer(lambda pw=pw, pwk=pwk, eb=eb, ebk=ebk, cch=cch, g=g, st_=(ni == 0), sp_=(ni == len(cl) - 1):
                              MM(P, pw[:, 0:65], eb[:], vwin[:, cch, g, :], st_, sp_, [ebk, "vwin"], [pwk]))
                    defer(lambda pw=pw, pwk=pwk, o=o, ok=ok, h=h, i=i: consume(pw, pwk, o, ok, h, i, 3 * h + 2, False))
                flush()
                ASEL(P, imp[:], imp[:], [[-64, 64]], ALU.is_ge, 1e4, t0 - 128, 1, [impk], [impk])
                ASEL(P, imp[:], imp[:], [[-64, 64]], ALU.is_ge, -1.0, t0, 1, [impk], [impk])
                MEMSET(P, "pool", imp[:, 0:1], 1e4, [impk], [impk])
                m8, m8k = m8b.next()
                rep, repk = repb.next()
                P.op("dve", lambda e, m8=m8, imp=imp: e.max(out=m8[:, 0:8], in_=imp[:]), [impk], [m8k])
                P.op("dve", lambda e, m8=m8, imp=imp, rep=rep: e.match_replace(out=rep[:], in_to_replace=m8[:, 0:8],
                                                                                 in_values=imp[:], imm_value=-1e30),
                     [impk, m8k], [repk])
                P.op("dve", lambda e, m8=m8, rep=rep: e.max(out=m8[:, 8:16], in_=rep[:]), [repk, m8k], [m8k])
                selp, selk = selb.next()
                TS(P, "dve", selp[:, 64:128], imp[:], m8[:, 15:16], None, ALU.is_ge, None, [impk, m8k], [selk])
                pt_, ptk = pstr.next()
                TR(P, pt_[:, 0:128], selp[:], c.ident[:], [selk, "ident"], [ptk])
                for hh in range(3):
                    h = 3 * g + hh
                    Ct, Ctk = Ctb.next()
                    TS(P, "pool", Ct[64:128, :], IOT[64:128, :], SLOPES[h], -BIG - SLOPES[h] * t0, ALU.mult, ALU.add,
                       ["IOT"], [Ctk])
                    STT(P, qaug[h][64:128, qc], pt_[64:128, 0:128], BIG, Ct[64:128, :], ALU.mult, ALU.add,
                        [ptk, Ctk], [("qm", h, i)])
                for hh in range(3):
                    h = 3 * g + hh
                    psl, pslk = psacc.next()
                    for cch in range(i + 1):
                        ps_, psk = pss.next()
                        MM(P, ps_[:, 0:128], kslc[g][:, cch * 128:(cch + 1) * 128], qaug[h][:, qc], True, True,
                           [("kslc", g), ("kx", g), ("q", h), ("qm", h, i)], [psk])
                        eb, ebk = ebb.next()
                        if cch == i:
                            e32, e32k = e32b.next()
                            ACT(P, e32[:], ps_[:, 0:128], AF.Exp, [psk, "bias_sel"], [e32k], bias=bias_sel[:, h:h + 1])
                            ASEL(P, eb[:], e32[:], [[1, 128]], ALU.is_ge, 0.0, 0, -1, [e32k], [ebk])
                        else:
                            ACT(P, eb[:], ps_[:, 0:128], AF.Exp, [psk, "bias_sel"], [ebk], bias=bias_sel[:, h:h + 1])
                        defer(lambda psl=psl, pslk=pslk, eb=eb, ebk=ebk, cch=cch, g=g, st_=(cch == 0), sp_=(cch == i):
                              MM(P, psl[:, 0:65], eb[:], vslc[:, cch, g, :], st_, sp_, [ebk, "vslc"], [pslk]))
                    defer(lambda psl=psl, pslk=pslk, o=o, ok=ok, h=h, i=i: consume(psl, pslk, o, ok, h, i, 3 * h + 1, False))

            def finish_tile(o=o, ok=ok, qc=qc):
                pt2, pt2k = pstr.next()
                for j in range(3):
                    TR(P, pt2[:, j * 128:(j + 1) * 128], o[:, j * 128:(j + 1) * 128], c.ident[:],
                       [(ok, 2 * j), (ok, 2 * j + 1), "ident"], [pt2k])
                ost, ostk = ostb.next()
                CP(P, "act", ost[:], pt2[:, 0:384].rearrange("p (j t) -> p j t", j=3), [pt2k], [ostk])
                DMA(P, "sp", c.NSAOT.rearrange("(j p) t -> p j t", p=128)[:, :, qc], ost[:], [ostk], ["NSAOT"])
            defer(finish_tile)
        flush()
        P.end_phase()
        P.alloc_sems(c.es0, c.sems)
        with nc.Block() as block:
            P.emit(block, c.sems)


def phase_D(c):
    nc, P = c.nc, c.P
    with ExitStack() as es:
        sb = lambda name, shape, dt: es.enter_context(nc.sbuf_tensor(name, shape, dt))
        memt = sb("memt", [128, 2, D], F32)
        mems = sb("mems", [128, 2, D], F32)
        junk = sb("junkD", [128, D], F32)
        ssb = [sb(f"ssD{i}", [128, 4], F32) for i in range(2)]
        gcol = sb("gcolD", [128, 8], F32)
        mhT = sb("mhT", [128, 8, 256], BF16)
        wst = Rot([sb(f"wstD{i}", [128, 512], F32) for i in range(2)], "wstD")
        Wkv = sb("Wkv", [128, 8, 512], BF16)
        mkT = [sb(f"mkT{h}", [64, 256], BF16) for h in range(4)]
        mvaug = sb("mvaug", [128, 2, 4, 65], BF16)
        qm = [sb(f"qm{h}", [64, T], BF16) for h in range(4)]
        eb = Rot([sb(f"ebD{i}", [128, 512], BF16) for i in range(4)], "ebD")
        ob = Rot([sb(f"oD{i}", [128, 4, 256], F32) for i in range(2)], "oD")
        wb = Rot([sb(f"wD{i}", [128, 2], F32) for i in range(4)], "wD")
        ostb = Rot([sb(f"ostD{i}", [128, 2, 512], BF16) for i in range(2)], "ostD")
        pss = Rot(c.ps[0:2], "pssD")
        psacc = Rot(c.ps[2:5], "psaccD")
        pstr = Rot(c.ps[5:7], "pstrD")
        pmisc = c.ps[7]

        DMA(P, "sp", gcol[:], c.inp["mem_g"], [], ["gcol"])
        DMA(P, "sp", memt[:], c.inp["mem"].rearrange("(c p) d -> p c d", p=128), [], ["memt"])
        for h in range(4):
            DMA(P, "sp", qm[h][:], c.FM[FM_QMEM + 64 * h:FM_QMEM + 64 * (h + 1), :], ["FM"], [("qm", h)])
        for kc in range(8):
            st, sk = wst.next()
            DMA(P, "sp", st[:], c.inp["w_mem_kv"][kc * 128:(kc + 1) * 128, :], [], [sk])
            TS(P, "dve", Wkv[:, kc, :], st[:], gcol[:, kc:kc + 1], None, ALU.mult, None, [sk, "gcol"], [("Wkv", kc)])
        Wk = [("Wkv", kc) for kc in range(8)]
        for ci in range(2):
            ss = ssb[ci]
            ssk = ("ssD", ci)
            ACT(P, junk[:], memt[:, ci, :], AF.Square, ["memt"], ["junkD", ssk], accum=ss[:, 0:1])
            rstd_chain(P, ss, ssk)
            TS(P, "dve", mems[:, ci, :], memt[:, ci, :], ss[:, 3:4], None, ALU.mult, None, ["memt", ssk], [("mems", ci)])
            for half in range(2):
                pt, ptk = pstr.next()
                for j in range(4):
                    cc = half * 4 + j
                    TR(P, pt[:, j * 128:(j + 1) * 128], mems[:, ci, cc * 128:(cc + 1) * 128], c.ident[:],
                       [("mems", ci), "ident"], [ptk])
                CP(P, "dve", mhT[:, half * 4:(half + 1) * 4, ci * 128:(ci + 1) * 128],
                   pt[:].rearrange("p (j t) -> p j t", j=4), [ptk], [("mhT", ci, half)])
        mh = [("mhT", ci, half) for ci in range(2) for half in range(2)]
        for h in range(4):
            for kc in range(8):
                MM(P, pmisc[0:64, 0:256], Wkv[:, kc, 64 * h:64 * h + 64], mhT[:, kc, :], kc == 0, kc == 7, Wk + mh, ["pmisc"])
            CP(P, "dve", mkT[h][:], pmisc[0:64, 0:256], ["pmisc"], [("mkT", h)])
        for ci in range(2):
            for kc in range(8):
                MM(P, pmisc[:, 256:512], mhT[:, kc, ci * 128:(ci + 1) * 128], Wkv[:, kc, 256:512], kc == 0, kc == 7,
                   Wk + mh, ["pmisc"])
            CP(P, "dve", mvaug[:, ci, :, 0:64], pmisc[:, 256:512].rearrange("p (h d) -> p h d", h=4), ["pmisc"], ["mvaug"])
        MEMSET(P, "dve", mvaug[:, :, :, 64:65], 1.0, ["mvaug"], ["mvaug"])

        for g in range(8):
            o, ok = ob.next()
            for h in range(4):
                es_ = []
                for ci in range(2):
                    ps_, psk = pss.next()
                    MM(P, ps_[:, :], mkT[h][:, ci * 128:(ci + 1) * 128], qm[h][:, g * 512:(g + 1) * 512], True, True,
                       [("mkT", h), ("qm", h)], [psk])
                    e, ek = eb.next()
                    ACT(P, e[:], ps_[:, :], AF.Exp, [psk], [ek])
                    es_.append((e, ek))
                for sub in range(4):
                    pa, pak = psacc.next()
                    for ci in range(2):
                        MM(P, pa[:, 0:65], es_[ci][0][:, sub * 128:(sub + 1) * 128], mvaug[:, ci, h, :], ci == 0, ci == 1,
                           [es_[ci][1], "mvaug"], [pak])
                    w, wk = wb.next()
                    RECIP(P, w[:, 0:1], pa[:, 64:65], [pak], [wk])
                    TS(P, "dve", o[:, sub, 64 * h:64 * h + 64], pa[:, 0:64], w[:, 0:1], None, ALU.mult, None, [pak, wk],
                       [(ok, sub, h)])
            ost, ostk = ostb.next()
            for sub in range(4):
                pt, ptk = pstr.next()
                for j in range(2):
                    TR(P, pt[:, j * 128:(j + 1) * 128], o[:, sub, j * 128:(j + 1) * 128], c.ident[:],
                       [(ok, sub, 2 * j), (ok, sub, 2 * j + 1), "ident"], [ptk])
                CP(P, "act", ost[:, :, sub * 128:(sub + 1) * 128], pt[:, 0:256].rearrange("p (j t) -> p j t", j=2),
                   [ptk], [(ostk, sub)])
            DMA(P, "sp", c.MEMOT.rearrange("(j p) t -> p j t", p=128)[:, :, g * 512:(g + 1) * 512], ost[:],
                [(ostk, sub) for sub in range(4)], ["MEMOT"])
        P.end_phase()
        P.alloc_sems(c.es0, c.sems)
        with nc.Block() as block:
            P.emit(block, c.sems)


def phase_E(c):
    nc, P = c.nc, c.P
    with ExitStack() as es:
        sb = lambda name, shape, dt: es.enter_context(nc.sbuf_tensor(name, shape, dt))
        Wmg = sb("Wmg", [128, 8, 3072], BF16)
        Wbr = [sb("Wsb", [128, 3, D], BF16), sb("Wnsa", [128, 3, D], BF16), sb("Wmem", [128, 2, D], BF16)]
        Wout = sb("Wout", [128, 8, D], BF16)
        wst = Rot([sb(f"wstE{i}", [128, 1024], F32) for i in range(3)], "wstE")
        gcol = sb("gcolE", [128, 8], F32)
        bmg = sb("bmg", [128, 24], F32)
        hTb = Rot([sb(f"hTE{i}", [128, 8, 512], BF16) for i in range(2)], "hTE")
        srcb = [Rot([sb(f"srcE{b}_{i}", [128, 3 if b < 2 else 2, 512], BF16) for i in range(2)], f"srcE{b}") for b in range(3)]
        mgb = Rot([sb(f"mgT{i}", [128, 8, 512], BF16) for i in range(2)], "mgT")
        gateb = Rot([sb(f"gateE{i}", [128, 512], F32) for i in range(3)], "gateE")
        accb = Rot([sb(f"accE{i}", [128, 512], F32) for i in range(2)], "accE")
        tmpb = Rot([sb(f"tmpE{i}", [128, 512], F32) for i in range(2)], "tmpE")
        xb = Rot([sb(f"xE{i}", [128, D], F32) for i in range(2)], "xE")
        x1b = Rot([sb(f"x1E{i}", [128, D], F32) for i in range(2)], "x1E")
        psbr = Rot(c.ps[0:2], "psbr")
        psg = Rot(c.ps[2:5], "psg")
        psy = Rot(c.ps[5:8], "psy")

        DMA(P, "sp", gcol[:], c.inp["mix_g"], [], ["gcol"])
        DMA(P, "sp", bmg[:], c.inp["b_merge"], [], ["bmg"])
        n = 0
        for kc in range(8):
            for j in range(3):
                st, sk = wst.next()
                DMA(P, "sp", st[:], c.inp["w_in"][kc * 128:(kc + 1) * 128, 2578 + 1024 * j:2578 + 1024 * (j + 1)], [], [sk])
                if n % 2 == 0:
                    TS(P, "dve", Wmg[:, kc, 1024 * j:1024 * (j + 1)], st[:], gcol[:, kc:kc + 1], None, ALU.mult, None,
                       [sk, "gcol"], [("Wmg", kc)])
                else:
                    ACT(P, Wmg[:, kc, 1024 * j:1024 * (j + 1)], st[:], AF.Copy, [sk, "gcol"], [("Wmg", kc)],
                        scale=gcol[:, kc:kc + 1])
                n += 1
        for b, (nm, nf) in enumerate((("w_sb_br", 3), ("w_nsa_br", 3), ("w_mem_br", 2))):
            for f in range(nf):
                st, sk = wst.next()
                DMA(P, "sp", st[:], c.inp[nm][f * 128:(f + 1) * 128, :], [], [sk])
                CP(P, "dve" if n % 2 == 0 else "act", Wbr[b][:, f, :], st[:], [sk], [("Wbr", b)])
                n += 1
        for kc in range(8):
            st, sk = wst.next()
            DMA(P, "sp", st[:], c.inp["w_out"][kc * 128:(kc + 1) * 128, :], [], [sk])
            CP(P, "dve" if n % 2 == 0 else "act", Wout[:, kc, :], st[:], [sk], ["Wout"])
            n += 1
        Wmgk = [("Wmg", kc) for kc in range(8)]
        srcs = [(c.SBOT, 3, "SBOT"), (c.NSAOT, 3, "NSAOT"), (c.MEMOT, 2, "MEMOT")]
        for tg in range(8):
            tc_ = slice(tg * 512, (tg + 1) * 512)
            hT, hk = hTb.next()
            DMA(P, "sp", hT[:], c.HT.rearrange("(c p) t -> p c t", p=128)[:, :, tc_], ["HT"], [hk])
            src = []
            for b, (ap, nf, nm) in enumerate(srcs):
                t_, tk = srcb[b].next()
                DMA(P, "sp", t_[:], ap.rearrange("(f p) t -> p f t", p=128)[:, :, tc_], [nm], [tk])
                src.append((t_, tk, nf))
            mg, mgk = mgb.next()
            for dc in range(8):
                acc, acck = accb.next()
                for b in range(3):
                    t_, tk, nf = src[b]
                    pb, pbk = psbr.next()
                    for f in range(nf):
                        MM(P, pb[:, :], Wbr[b][:, f, dc * 128:(dc + 1) * 128], t_[:, f, :], f == 0, f == nf - 1,
                           [("Wbr", b), tk], [pbk])
                    pg, pgk = psg.next()
                    for kc in range(8):
                        MM(P, pg[:, :], Wmg[:, kc, b * 1024 + dc * 128:b * 1024 + (dc + 1) * 128], hT[:, kc, :], kc == 0, kc == 7,
                           Wmgk + [hk], [pgk])
                    gt, gtk = gateb.next()
                    ACT(P, gt[:], pg[:, :], AF.Sigmoid, [pgk, "bmg"], [gtk], bias=bmg[:, b * 8 + dc:b * 8 + dc + 1])
                    if b == 0:
                        TT(P, "dve", acc[:], gt[:], pb[:, :], ALU.mult, [gtk, pbk], [acck])
                    else:
                        tmp, tmpk = tmpb.next()
                        TT(P, "dve", tmp[:], gt[:], pb[:, :], ALU.mult, [gtk, pbk], [tmpk])
                        if b == 1:
                            TT(P, "pool", acc[:], acc[:], tmp[:], ALU.add, [acck, tmpk], [acck])
                        else:
                            TT(P, "pool", mg[:, dc, :], acc[:], tmp[:], ALU.add, [acck, tmpk], [(mgk, dc)])
            mgks = [(mgk, dc) for dc in range(8)]
            for s in range(4):
                i = tg * 4 + s
                xt, xk = xb.next()
                DMA(P, "sp", xt[:], c.inp["x"][i * 128:(i + 1) * 128, :], [], [xk])
                x1, x1k = x1b.next()
                for half in range(2):
                    py, pyk = psy.next()
                    for dc in range(8):
                        MM(P, py[:, :], mg[:, dc, s * 128:(s + 1) * 128], Wout[:, dc, half * 512:(half + 1) * 512], dc == 0, dc == 7,
                           mgks + ["Wout"], [pyk])
                    TT(P, "dve", x1[:, half * 512:(half + 1) * 512], xt[:, half * 512:(half + 1) * 512], py[:, :], ALU.add,
                       [xk, pyk], [(x1k, half)])
                DMA(P, "sp", c.X1[i * 128:(i + 1) * 128, :], x1[:], [(x1k, 0), (x1k, 1)], ["X1"])
        P.end_phase()
        P.alloc_sems(c.es0, c.sems)
        with nc.Block() as block:
            P.emit(block, c.sems)


def phase_F(c):
    nc, P = c.nc, c.P
    GS = 8
    with ExitStack() as es:
        sb = lambda name, shape, dt: es.enter_context(nc.sbuf_tensor(name, shape, dt))
        Wq = sb("Wq", [128, 8, 2048], BF16)
        subk = sb("subk", [128, 16, 128], BF16)
        g2b = sb("g2b", [128, D], F32)
        gFb = sb("gFb", [128, D], F32)
        keyidx = sb("keyidx", [128, 2048], I32)
        posidx = sb("posidx", [128, 2048], I32)
        iotaA = sb("iotaA", [128, 2048], F32)
        cI = sb("cI", [128, 8], I32)
        with ExitStack() as es1:
            sb1 = lambda name, shape, dt: es1.enter_context(nc.sbuf_tensor(name, shape, dt))
            wst = Rot([sb1(f"wstF{i}", [128, 2048], F32) for i in range(2)], "wstF")
            iotaAi = sb1("iotaAi", [128, 2048], I32)
            for kc in range(8):
                st, sk = wst.next()
                DMA(P, "sp", st[:], c.inp["peer_w_q"][kc * 128:(kc + 1) * 128, :], [], [sk])
                CP(P, "dve" if kc % 2 == 0 else "act", Wq[:, kc, :], st[:], [sk], [("Wq", kc)])
            st, sk = wst.next()
            DMA(P, "sp", st[:], c.inp["subkT"].rearrange("d b k -> d (b k)"), [], [sk])
            CP(P, "dve", subk[:].rearrange("d b k -> d (b k)"), st[:], [sk], ["subk"])
            DMA(P, "sp", g2b[:], c.inp["ffn_g"].partition_broadcast(128), [], ["g2b"])
            DMA(P, "sp", gFb[:], c.inp["final_g"].partition_broadcast(128), [], ["gFb"])
            IOTA(P, keyidx[:], [[0, 16], [1, 128]], 0, 0, [], ["keyidx"])
            IOTA(P, posidx[:], [[0, 8], [1, 256]], 0, 0, [], ["posidx"])
            IOTA(P, iotaAi[:], [[0, 128], [1, 16]], 0, 0, [], ["iotaAi"])
            CP(P, "dve", iotaA[:], iotaAi[:], ["iotaAi"], ["iotaA"])
            for j, v in enumerate((-128, -256, 127, 255, 15, 4)):
                IOTA(P, cI[:, j:j + 1], [[0, 1]], v, 0, ["cI"], ["cI"])
            P.end_phase()
            P.alloc_sems(c.es0, c.sems)
            with nc.Block() as block:
                P.emit(block, c.sems)
        x1b = Rot([sb(f"x1F{i}", [128, D], F32) for i in range(2)], "x1F")
        h2b = Rot([sb(f"h2F{i}", [128, D], F32) for i in range(1)], "h2F")
        ssb = Rot([sb(f"ssF{i}", [128, 4], F32) for i in range(4)], "ssF")
        junk = sb("junkF", [128, D], BF16)
        prodb = Rot([sb(f"prodF{i}", [128, D], BF16) for i in range(4)], "prodF")
        h2hb = Rot([sb(f"h2hF{i}", [128, D], BF16) for i in range(2)], "h2hF")
        h2Tb = Rot([sb(f"h2T{i}", [128, 8, 128], BF16) for i in range(2)], "h2T")
        qTb = sb("qTbF", [128, 16, 128], BF16)
        Sc = sb("Sc", [128, 2048], F32)
        rep = sb("repF", [128, 256], F32)
        stop = sb("stop", [128, 16, 16], F32)
        itop_i = sb("itop_i", [128, 256], I32)
        itop_f = sb("itop_f", [128, 16, 16], F32)
        tmpA = sb("tmpA", [128, 2048], F32)
        tmpB = Sc
        best = sb("best", [128, 8, 16], F32)
        pos_i = sb("pos_i", [128, 3, 128], I32)
        ab_f = sb("ab_f", [128, 2, 128], F32)
        sel_f = sb("sel_f", [128, 3, 128], F32)
        idxb = Rot([sb(f"idxF{i}", [128, 128], I32) for i in range(2)], "idxF")
        gwb = Rot([sb(f"gwF{i}", [128, 3, 128], F32) for i in range(2)], "gwF")
        gsum = sb("gsum", [128, 16], F32)
        ab = Rot([sb(f"aF{i}", [128, 2, 128], F32) for i in range(2)], "aF")
        uvb = Rot([sb(f"uvg{i}", [128, 2 * D], BF16) for i in range(16)], "uvg")
        dgb = Rot([sb(f"dg{i}", [128, 128], BF16) for i in range(6)], "dg")
        x2b = Rot([sb(f"x2F{i}", [128, D], F32) for i in range(1)], "x2F")
        ptq = Rot(c.ps[0:2], "ptq")
        psS = Rot(c.ps[2:4], "psS")
        pvb = Rot([(c.ps[4], c.ps[5]), (c.ps[6], c.ps[7])], "pv")
        Wqk = [("Wq", kc) for kc in range(8)]

        def route(i, st):
            x1, x1k = x1b.next()
            DMA(P, "sp", x1[:], c.X1[i * 128:(i + 1) * 128, :], ["X1"], [x1k])
            ss, ssk = ssb.next()
            ACT(P, junk[:], x1[:], AF.Square, [x1k], [ssk], accum=ss[:, 0:1])
            rstd_chain(P, ss, ssk)
            h2, h2k = h2b.next()
            STT(P, h2[:], x1[:], ss[:, 3:4], g2b[:], ALU.mult, ALU.mult, [x1k, ssk, "g2b"], [h2k])
            h2h, h2hk = h2hb.next()
            CP(P, "act", h2h[:], h2[:], [h2k], [h2hk])
            yield
            h2T, h2Tk = h2Tb.next()
            for half in range(2):
                pt, ptk = ptq.next()
                for j in range(4):
                    cc = half * 4 + j
                    TR(P, pt[:, j * 128:(j + 1) * 128], h2[:, cc * 128:(cc + 1) * 128], c.ident[:], [h2k, "ident"], [ptk])
                CP(P, "act", h2T[:, half * 4:(half + 1) * 4, :], pt[:].rearrange("p (j t) -> p j t", j=4), [ptk], [(h2Tk, half)])
            h2Tks = [(h2Tk, 0), (h2Tk, 1)]
            yield
            for b4 in range(4):
                pq, pqk = ptq.next()
                for j in range(4):
                    blk = b4 * 4 + j
                    for kc in range(8):
                        MM(P, pq[:, j * 128:(j + 1) * 128], Wq[:, kc, blk * 128:(blk + 1) * 128], h2T[:, kc, :], kc == 0, kc == 7,
                           Wqk + h2Tks, [pqk])
                CP(P, "act", qTb[:, b4 * 4:(b4 + 1) * 4, :], pq[:].rearrange("p (j t) -> p j t", j=4), [pqk], [("qTb", b4)])
                yield
            for b4 in range(4):
                pS, pSk = psS.next()
                for j in range(4):
                    blk = b4 * 4 + j
                    MM(P, pS[:, j * 128:(j + 1) * 128], qTb[:, blk, :], subk[:, blk, :], True, True, [("qTb", b4), "subk"], [pSk])
                STT(P, Sc[:, b4 * 512:(b4 + 1) * 512].bitcast(I32), pS[:, :].bitcast(I32), cI[:, 0:1],
                    keyidx[:, b4 * 512:(b4 + 1) * 512], ALU.bitwise_and, ALU.bitwise_or, [pSk, "cI", "keyidx"], [("Sc", b4), "tmpB"])
                yield
            for blk in range(16):
                sblk = Sc[:, blk * 128:(blk + 1) * 128]
                sck = ("Sc", blk // 4)
                P.op("dve", lambda e, o=stop[:, blk, 0:8], s=sblk: e.max(out=o, in_=s), [sck], ["stop"])
                P.op("dve", lambda e, o=rep[:, 0:128], m=stop[:, blk, 0:8], s=sblk: e.match_replace(
                    out=o, in_to_replace=m, in_values=s, imm_value=-1e30), [sck, "stop"], ["repF"])
                P.op("dve", lambda e, o=stop[:, blk, 8:16], s=rep[:, 0:128]: e.max(out=o, in_=s), ["repF"], ["stop"])
                yield
            stop2 = stop[:].rearrange("p b k -> p (b k)")
            TS(P, "dve", itop_i[:], stop2.bitcast(I32), cI[:, 2:3], None, ALU.bitwise_and, None, ["stop", "cI"], ["itop_i"])
            CP(P, "dve", itop_f[:].rearrange("p b k -> p (b k)"), itop_i[:], ["itop_i"], ["itop_f"])
            yield
            sv = stop[:].rearrange("p (h q) k -> p h q k", q=2)
            iv = itop_f[:].rearrange("p (h q) k -> p h q k", q=2)
            cand = tmpA[:].rearrange("p (h a b) -> p h a b", h=8, a=16)
            TT(P, "dve", cand, sv[:, :, 0, :].unsqueeze(3).to_broadcast([128, 8, 16, 16]),
               sv[:, :, 1, :].unsqueeze(2).to_broadcast([128, 8, 16, 16]), ALU.add, ["stop"], ["tmpA"])
            yield
            STT(P, tmpB[:].bitcast(I32), tmpA[:].bitcast(I32), cI[:, 1:2], posidx[:], ALU.bitwise_and, ALU.bitwise_or,
                ["tmpA", "cI", "posidx"], ["tmpB"] + [("Sc", b_) for b_ in range(4)])
            yield
            for h in range(8):
                sblk = tmpB[:, h * 256:(h + 1) * 256]
                P.op("dve", lambda e, o=best[:, h, 0:8], s=sblk: e.max(out=o, in_=s), ["tmpB"], ["best"])
                P.op("dve", lambda e, o=rep[:], m=best[:, h, 0:8], s=sblk: e.match_replace(
                    out=o, in_to_replace=m, in_values=s, imm_value=-1e30), ["tmpB", "best"], ["repF"])
                P.op("dve", lambda e, o=best[:, h, 8:16], s=rep[:]: e.max(out=o, in_=s), ["repF"], ["best"])
                yield
            best2 = best[:].rearrange("p h k -> p (h k)")
            TS(P, "dve", pos_i[:, 0, :], best2.bitcast(I32), cI[:, 3:4], None, ALU.bitwise_and, None, ["best", "cI"], ["pos_i"])
            TS(P, "dve", pos_i[:, 1, :], pos_i[:, 0, :], cI[:, 5:6], None, ALU.logical_shift_right, None, ["pos_i", "cI"], ["pos_i"])
            TS(P, "dve", pos_i[:, 2, :], pos_i[:, 0, :], cI[:, 4:5], None, ALU.bitwise_and, None, ["pos_i", "cI"], ["pos_i"])
            CP(P, "dve", ab_f[:], pos_i[:, 1:3, :], ["pos_i"], ["ab_f"])
            yield
            for q in range(2):
                akv = ab_f[:, q, :].rearrange("p (h k) -> p h k", h=8)
                eq = tmpA[:].rearrange("p (h k a) -> p h k a", h=8, k=16)
                TT(P, "dve", eq, akv.unsqueeze(3).to_broadcast([128, 8, 16, 16]),
                   iotaA[:].rearrange("p (h k a) -> p h k a", h=8, k=16), ALU.is_equal, ["ab_f", "iotaA"], ["tmpA"])
                yield
                pr = tmpB[:].rearrange("p (h k a) -> p h k a", h=8, k=16)
                TT(P, "dve", pr, eq, iv[:, :, q, :].unsqueeze(2).to_broadcast([128, 8, 16, 16]), ALU.mult,
                   ["tmpA", "itop_f"], ["tmpB"] + [("Sc", b_) for b_ in range(4)])
                yield
                P.op("dve", lambda e, o=sel_f[:, q, :], s=tmpB[:].rearrange("p (x a) -> p x a", a=16): e.tensor_reduce(
                    out=o, in_=s, axis=AX.X, op=ALU.add), ["tmpB"], ["sel_f"])
                yield
            STT(P, sel_f[:, 2, :], sel_f[:, 0, :], 128.0, sel_f[:, 1, :], ALU.mult, ALU.add, ["sel_f"], ["sel_f"])
            TS(P, "dve", sel_f[:, 2, :], sel_f[:, 2, :], 0.0, 16383.0, ALU.max, ALU.min, ["sel_f"], ["sel_f"])
            idx, idxk = idxb.next()
            CP(P, "dve", idx[:], sel_f[:, 2, :], ["sel_f"], [idxk])
            yield
            gw, gwk = gwb.next()
            v3 = lambda ap: ap.rearrange("p (h k) -> p h k", h=8)
            TT(P, "dve", v3(gw[:, 0, :]), best[:], best[:, :, 0:1].to_broadcast([128, 8, 16]), ALU.subtract, ["best"], [gwk])
            ACT(P, gw[:, 1, :], gw[:, 0, :], AF.Exp, [gwk], [gwk])
            yield
            P.op("dve", lambda e, o=gsum[:, 0:8], s=v3(gw[:, 1, :]): e.tensor_reduce(out=o, in_=s, axis=AX.X, op=ALU.add),
                 [gwk], ["gsum"])
            RECIP(P, gsum[:, 8:16], gsum[:, 0:8], ["gsum"], ["gsum"])
            TT(P, "dve", v3(gw[:, 2, :]), v3(gw[:, 1, :]), gsum[:, 8:16].unsqueeze(2).to_broadcast([128, 8, 16]), ALU.mult,
               [gwk, "gsum"], [gwk])
            st.update(x1=x1, x1k=x1k, h2=h2h, h2k=h2hk, idx=idx, idxk=idxk, gw=gw, gwk=gwk)
            yield

        def slots(i, st, bg):
            x1, x1k, h2, h2k, idx, idxk, gw, gwk = (st[k_] for k_ in ("x1", "x1k", "h2", "h2k", "idx", "idxk", "gw", "gwk"))
            a, ak = ab.next()
            (pv0, pv1), pvk = pvb.next()
            LAG = 6
            GSZ = 4
            held = {}
            for s in range(128 + LAG):
                if s < 128:
                    uv, uvk = uvb.next()
                    held[s] = (uv, uvk)
                    P.dma("pool", lambda e, o=uv[:], ix=idx[:, s:s + 1]: e.indirect_dma_start(
                        out=o, out_offset=None, in_=c.UVB,
                        in_offset=bass.IndirectOffsetOnAxis(ap=ix.bitcast(U32), axis=0)), [idxk, "UVB"], [uvk])
                    pd, pdk = prodb.next()
                    TT(P, "dve", pd[:], uv[:, 0:D], h2[:], ALU.mult, [uvk, h2k], [pdk])
                    ACT(P, junk[:], pd[:], AF.Copy, [pdk], [(ak, s)], accum=a[:, 0, s:s + 1])
                    if s % GSZ == GSZ - 1:
                        gs_ = slice(s - GSZ + 1, s + 1)
                        ACT(P, a[:, 1, gs_], a[:, 0, gs_], AF.Gelu, [(ak, s_) for s_ in range(s - GSZ + 1, s + 1)],
                            [(ak, "g", s // GSZ)])
                r_ = s - LAG
                if r_ >= 0:
                    uv, uvk = held.pop(r_)
                    dg, dgk = dgb.next()
                    TS(P, "dve", dg[:], c.identb[:], a[:, 1, r_:r_ + 1], gw[:, 2, r_:r_ + 1], ALU.mult, ALU.mult,
                       ["identb", (ak, "g", r_ // GSZ), gwk], [dgk])
                    MM(P, pv0[:, :], dg[:], uv[:, D:D + 512], r_ == 0, r_ == 127, [dgk, uvk], [(pvk, 0)])
                    MM(P, pv1[:, :], dg[:], uv[:, D + 512:2 * D], r_ == 0, r_ == 127, [dgk, uvk], [(pvk, 1)])
                if bg is not None and s % 2 == 1:
                    next(bg, None)
            if bg is not None:
                for _ in bg:
                    pass
            x2, x2k = x2b.next()
            TT(P, "dve", x2[:, 0:512], x1[:, 0:512], pv0[:, :], ALU.add, [x1k, (pvk, 0)], [(x2k, 0)])
            TT(P, "dve", x2[:, 512:1024], x1[:, 512:1024], pv1[:, :], ALU.add, [x1k, (pvk, 1)], [(x2k, 1)])
            ss2, ss2k = ssb.next()
            ACT(P, junk[:], x2[:], AF.Square, [(x2k, 0), (x2k, 1)], [ss2k], accum=ss2[:, 0:1])
            rstd_chain(P, ss2, ss2k)
            STT(P, x2[:], x2[:], ss2[:, 3:4], gFb[:], ALU.mult, ALU.mult, [(x2k, 0), (x2k, 1), ss2k, "gFb"], [(x2k, 0), (x2k, 1)])
            DMA(P, "sp", c.out[i * 128:(i + 1) * 128, :], x2[:], [(x2k, 0), (x2k, 1)], ["out"])

        states = [dict() for _ in range(NT)]
        for _ in route(0, states[0]):
            pass
        for i in range(NT):
            bg = route(i + 1, states[i + 1]) if i + 1 < NT else None
            slots(i, states[i], bg)
        P.end_phase()
        P.alloc_sems(c.es0, c.sems)
        with nc.Block() as block:
            P.emit(block, c.sems)


def build(upto="F", debug=False):
    nc = bass.Bass("TRN2", target_bir_lowering=False)
    c = Ctx()
    c.nc = nc
    c.P = Prog(nc)
    c.sems = {}
    inp = {}

    def din(name, shape, dt=F32):
        inp[name] = nc.dram_tensor(name, list(shape), dt, kind="ExternalInput").ap()

    din("x", [T, D])
    din("mem", [256, D])
    din("mix_g", [128, 8])
    din("mem_g", [128, 8])
    din("w_in", [D, IN_DIM])
    din("b_merge", [128, 24])
    din("pe_k", [64, 32])
    din("pe_v", [64, 32])
    din("cw_k", [64, 32, 64])
    din("cw_v", [64, 32, 64])
    din("w_mem_kv", [D, 512])
    din("w_sb_br", [384, D])
    din("w_nsa_br", [384, D])
    din("w_mem_br", [256, D])
    din("w_out", [D, D])
    din("ffn_g", [D])
    din("peer_w_q", [D, 2048])
    din("subkT", [128, 16, 128])
    din("peer_uv", [16384, 2 * D])
    din("final_g", [D])
    c.inp = inp
    kind = "ExternalOutput" if debug else "Internal"

    def scr(name, shape, dt):
        return nc.dram_tensor(name, list(shape), dt, kind=kind).ap()

    c.FM = scr("FM", [FM_ROWS, T], BF16)
    c.HT = scr("HT", [D, T], BF16)
    c.TMV = scr("TMV", [T, 640], BF16)
    c.GATES = scr("GATES", [T, 18], F32)
    c.SBOT = scr("SBOT", [384, T], BF16)
    c.NSAOT = scr("NSAOT", [384, T], BF16)
    c.MEMOT = scr("MEMOT", [256, T], BF16)
    c.X1 = scr("X1", [T, D], F32)
    c.UVB = nc.dram_tensor("UVB", [16384, 2 * D], BF16, kind="Internal").ap()
    c.out = nc.dram_tensor("out", [T, D], F32, kind="ExternalOutput").ap()

    with ExitStack() as es0:
        c.es0 = es0
        c.ps = [es0.enter_context(nc.psum_tensor(f"ps{i}", [128, 512], F32)) for i in range(8)]
        c.ident = es0.enter_context(nc.sbuf_tensor("ident", [128, 128], F32))
        c.identb = es0.enter_context(nc.sbuf_tensor("identb", [128, 128], BF16))
        P = c.P
        MEMSET(P, "pool", c.ident[:], 1.0, [], ["ident"])
        ASEL(P, c.ident[:], c.ident[:], [[1, 128]], ALU.is_equal, 0.0, 0, -1, ["ident"], ["ident"])
        CP(P, "pool", c.identb[:], c.ident[:], ["ident"], ["identb"])
        phases = [("A", phase_A), ("B", phase_B), ("C", phase_C), ("D", phase_D), ("E", phase_E), ("F", phase_F)]
        for name, fn in phases:
            fn(c)
            if name == upto:
                break
    return nc


def make_inputs(inputs, b):
    f = lambda a: np.ascontiguousarray(a, dtype=np.float32)
    gcol = lambda g: f(np.asarray(g).reshape(8, 128).T)
    m = {
        "x": f(inputs["x"][b]),
        "mem": f(inputs["mem"][b]),
        "mix_g": gcol(inputs["mix_norm_g"][0]),
        "mem_g": gcol(inputs["mem_norm_g"][0]),
        "w_in": f(inputs["w_in"][0]),
        "b_merge": f(np.asarray(inputs["b_merge"][0]).reshape(24, 128).T),
        "pe_k": f(np.asarray(inputs["cmp_pe_k"][0]).T),
        "pe_v": f(np.asarray(inputs["cmp_pe_v"][0]).T),
        "cw_k": f(np.asarray(inputs["cmp_w_k"][0]).transpose(1, 0, 2)),
        "cw_v": f(np.asarray(inputs["cmp_w_v"][0]).transpose(1, 0, 2)),
        "w_mem_kv": f(inputs["w_mem_kv"][0]),
        "w_sb_br": f(inputs["w_sb_br"][0]),
        "w_nsa_br": f(inputs["w_nsa_br"][0]),
        "w_mem_br": f(inputs["w_mem_br"][0]),
        "w_out": f(inputs["w_out"][0]),
        "ffn_g": f(inputs["ffn_norm_g"][0]),
        "peer_w_q": f(inputs["peer_w_q"][0]),
        "subkT": f(np.asarray(inputs["peer_subkeys"][0]).transpose(3, 0, 1, 2).reshape(128, 16, 128)),
        "peer_uv": np.ascontiguousarray(np.concatenate([np.asarray(inputs["peer_u"][0], dtype=np.float32),
                                                        np.asarray(inputs["peer_v"][0], dtype=np.float32)], axis=1)),
        "final_g": f(inputs["final_norm_g"]),
    }
    return m


def kernel(**inputs):
    nc = build()
    shared = None
    in_maps = []
    for b in range(8):
        m = make_inputs(inputs, b)
        if shared is None:
            shared = m
        else:
            for k in m:
                if k not in ("x", "mem"):
                    m[k] = shared[k]
        in_maps.append(m)
    res = run_bass_kernel_spmd(nc, in_maps, core_ids=list(range(8)))
    return np.stack([np.asarray(r["out"]) for r in res.results], axis=0).astype(np.float32)
```

```python
import sys
import numpy as np
from contextlib import ExitStack
import concourse.bass as bass
import concourse.mybir as mybir
from concourse.bass_utils import run_bass_kernel_spmd

F32 = mybir.dt.float32
BF16 = mybir.dt.bfloat16
I32 = mybir.dt.int32
U32 = mybir.dt.uint32
AF = mybir.ActivationFunctionType
ALU = mybir.AluOpType
AX = mybir.AxisListType

T = 4096
D = 1024
NT = T // 128
IN_DIM = 5650
EPS = 1e-6
SLOPES = [2.0 ** (-8.0 * (h + 1) / 6) for h in range(6)]
BIG = 30000.0

ENGS = ("pe", "act", "dve", "pool", "sp")
DMA_RING = 8


class Prog:
    def __init__(self, nc, same_engine_sync=True):
        self.nc = nc
        self.ops = {e: [] for e in ENGS}
        self.cnt = {e: 0 for e in ENGS}
        self.dma_n = {e: 0 for e in ENGS}
        self.last_w = {}
        self.readers = {}
        self.waited = {}
        self.same_engine_sync = same_engine_sync
        self.fill_vals = set()
        self.fill_regs = {}

    def _deps(self, eng, reads, writes):
        need = {}

        def add(tok):
            if tok is None:
                return
            sk, val, teng = tok
            if teng == eng and sk[0] == "c":
                if not self.same_engine_sync or eng == "pe":
                    return
            if need.get(sk, 0) < val:
                need[sk] = val

        for r in reads:
            add(self.last_w.get(r))
        for w in writes:
            add(self.last_w.get(w))
            for t in self.readers.get(w, ()):
                add(t)
        out = []
        for sk, val in need.items():
            if self.waited.get((eng, sk), 0) >= val:
                continue
            self.waited[(eng, sk)] = val
            out.append((sk, val))
        return out

    def _commit(self, tok, reads, writes):
        for r in reads:
            self.readers.setdefault(r, []).append(tok)
        for w in writes:
            self.last_w[w] = tok
            self.readers[w] = []

    def op(self, eng, fn, reads=(), writes=()):
        reads = tuple(reads)
        writes = tuple(writes)
        waits = self._deps(eng, reads, writes)
        self.cnt[eng] += 1
        tok = (("c", eng), self.cnt[eng], eng)
        fr = sys._getframe(1)
        self.ops[eng].append(dict(fn=fn, waits=waits, inc=(("c", eng), 1),
                                  where=(fr.f_lineno, fr.f_back.f_lineno if fr.f_back else 0)))
        self._commit(tok, reads, writes)
        return tok

    def dma(self, eng, fn, reads=(), writes=()):
        reads = tuple(reads)
        writes = tuple(writes)
        n = self.dma_n[eng]
        self.dma_n[eng] += 1
        sk = ("d", eng, n % DMA_RING)
        val = 16 * (n // DMA_RING + 1)
        waits = self._deps(eng, reads, writes)
        if val > 16 and self.waited.get((eng, sk), 0) < val - 16:
            self.waited[(eng, sk)] = val - 16
            waits.append((sk, val - 16))
        tok = (sk, val, eng)
        self.ops[eng].append(dict(fn=fn, waits=waits, inc=(sk, 16)))
        self._commit(tok, reads, writes)
        return tok

    def finish(self, eng, toks):
        self.ops[eng].append(dict(fn=None, waits=[(sk, val) for sk, val, _ in toks], inc=None))

    def end_phase(self):
        targets = []
        for e in ENGS:
            if self.cnt[e] > 0:
                targets.append((("c", e), self.cnt[e]))
            n = self.dma_n[e]
            for r in range(min(n, DMA_RING)):
                last = ((n - 1 - r) // DMA_RING) * DMA_RING + r
                targets.append((("d", e, r), 16 * (last // DMA_RING + 1)))
        for e in ENGS:
            waits = []
            for sk, val in targets:
                if self.waited.get((e, sk), 0) >= val:
                    continue
                self.waited[(e, sk)] = val
                waits.append((sk, val))
            self.ops[e].append(dict(fn=None, waits=waits, inc=None))
        self.last_w = {}
        self.readers = {}

    def sem_keys(self):
        keys = set()
        for e in ENGS:
            for o in self.ops[e]:
                if o["inc"]:
                    keys.add(o["inc"][0])
                for sk, _ in o["waits"]:
                    keys.add(sk)
        return sorted(keys)

    def alloc_sems(self, es, sems):
        for k in self.sem_keys():
            if k not in sems:
                sems[k] = es.enter_context(self.nc.semaphore("s_" + "_".join(map(str, k))))

    def emit(self, block, sems):
        engobj = {"pe": "tensor", "act": "scalar", "dve": "vector", "pool": "gpsimd", "sp": "sync"}

        def make(e):
            ops = self.ops[e]

            def body(eng):
                if e == "pool":
                    self.fill_regs = {v: eng.to_reg(v) for v in sorted(self.fill_vals)}
                for o in ops:
                    for sk, val in o["waits"]:
                        eng.wait_ge(sems[sk], val)
                    if o["fn"] is not None:
                        try:
                            ins = o["fn"](eng)
                        except Exception:
                            print("EMIT FAILED at lines", o.get("where"))
                            raise
                        if o["inc"]:
                            ins.then_inc(sems[o["inc"][0]], o["inc"][1])
            return body

        for e in ENGS:
            if self.ops[e]:
                getattr(block, engobj[e])(make(e))
        self.ops = {e: [] for e in ENGS}


class Ctx:
    pass


class Rot:
    def __init__(self, tiles, name):
        self.tiles = tiles
        self.name = name
        self.i = 0

    def next(self):
        j = self.i % len(self.tiles)
        self.i += 1
        return self.tiles[j], (self.name, j)


def MM(P, out, lhsT, rhs, start, stop, r, w, sgc=False):
    return P.op("pe", lambda e: e.matmul(out, lhsT=lhsT, rhs=rhs, start=start, stop=stop, skip_group_check=sgc), r, w)


def TR(P, out, in_, ident, r, w):
    return P.op("pe", lambda e: e.transpose(out, in_, ident), r, w)


def ACT(P, out, in_, func, r, w, scale=None, bias=None, accum=None):
    kw = {}
    if scale is not None:
        kw["scale"] = scale
    if bias is not None:
        kw["bias"] = bias
    if accum is not None:
        kw["accum_out"] = accum
    return P.op("act", lambda e: e.activation(out=out, in_=in_, func=func, **kw), r, w)


def TS(P, eng, out, in0, s1, s2, op0, op1, r, w):
    if op1 is None:
        return P.op(eng, lambda e: e.tensor_scalar(out=out, in0=in0, scalar1=s1, scalar2=None, op0=op0), r, w)
    return P.op(eng, lambda e: e.tensor_scalar(out=out, in0=in0, scalar1=s1, scalar2=s2, op0=op0, op1=op1), r, w)


def TT(P, eng, out, in0, in1, op, r, w):
    return P.op(eng, lambda e: e.tensor_tensor(out=out, in0=in0, in1=in1, op=op), r, w)


def STT(P, out, in0, scalar, in1, op0, op1, r, w):
    return P.op("dve", lambda e: e.scalar_tensor_tensor(out=out, in0=in0, scalar=scalar, in1=in1, op0=op0, op1=op1), r, w)


def CP(P, eng, out, in_, r, w):
    if eng == "act":
        return P.op("act", lambda e: e.copy(out=out, in_=in_), r, w)
    return P.op(eng, lambda e: e.tensor_copy(out=out, in_=in_), r, w)


def DMA(P, eng, out, in_, r, w):
    return P.dma(eng, lambda e: e.dma_start(out=out, in_=in_), r, w)


def MEMSET(P, eng, ap, val, r, w):
    return P.op(eng, lambda e: e.memset(ap, val), r, w)


def ASEL(P, out, in_, pattern, cmp, fill, base, cm, r, w):
    P.fill_vals.add(float(fill))
    return P.op("pool", lambda e: e.affine_select(out=out, in_=in_, pattern=pattern, compare_op=cmp,
                                                  fill=P.fill_regs[float(fill)], base=base, channel_multiplier=cm), r, w)


def IOTA(P, out, pattern, base, cm, r, w):
    return P.op("pool", lambda e: e.iota(out, pattern=pattern, base=base, channel_multiplier=cm), r, w)


def RECIP(P, out, in_, r, w):
    return P.op("dve", lambda e: e.reciprocal(out=out, in_=in_), r, w)


def rstd_chain(P, ss, key):
    TS(P, "dve", ss[:, 1:2], ss[:, 0:1], 1.0 / D, EPS, ALU.mult, ALU.add, [key], [key])
    P.op("act", lambda e: e.sqrt(out=ss[:, 2:3], in_=ss[:, 1:2]), [key], [key])
    RECIP(P, ss[:, 3:4], ss[:, 2:3], [key], [key])


FM_QSB, FM_KSB, FM_QNSA, FM_KCMP, FM_VCMP, FM_KSLC, FM_KWIN, FM_QMEM = 0, 384, 768, 1152, 1280, 1408, 1536, 1664
FM_ROWS = 1920
FM_COLMAP = [(0, 0, 768), (768, 1152, 384), (1152, 1536, 128), (1280, 1664, 128), (1408, 1792, 128),
             (1536, 2048, 128), (1664, 2322, 256)]
TM_COLMAP = [(0, 768, 384), (384, 1920, 128), (512, 2176, 128), (640, 2304, 18)]
TM_COLS = 658
Q_CHUNKS = {0, 1, 2, 6, 7, 8, 13, 14}


def phase_A(c):
    nc, P = c.nc, c.P
    with ExitStack() as es:
        sb = lambda name, shape, dt: es.enter_context(nc.sbuf_tensor(name, shape, dt))
        Wfm = sb("Wfm", [128, 8, FM_ROWS], BF16)
        Wtm = sb("Wtm", [128, 8, TM_COLS], BF16)
        wst = Rot([sb(f"wst{i}", [128, 2578], F32) for i in range(2)], "wst")
        gcol = sb("gcolA", [128, 8], F32)
        xbuf = Rot([sb(f"xt{i}", [128, D], F32) for i in range(2)], "xt")
        xsbuf = Rot([sb(f"xs{i}", [128, D], F32) for i in range(2)], "xs")
        junk = sb("junkA", [128, D], F32)
        ssbuf = Rot([sb(f"ss{i}", [128, 4], F32) for i in range(4)], "ss")
        hTg = Rot([sb(f"hTg{i}", [128, 8, 512], BF16) for i in range(2)], "hTg")
        FMst = Rot([sb(f"FMst{i}", [128, 15, 512], BF16) for i in range(2)], "FMst")
        TMst = Rot([sb(f"TMst{i}", [128, 640], BF16) for i in range(3)], "TMst")
        gst = Rot([sb(f"gst{i}", [128, 18], F32) for i in range(3)], "gst")
        pstr = Rot(c.ps[0:2], "ps_tr")
        psfm = Rot(c.ps[2:5], "ps_fm")
        pstm = Rot(c.ps[5:8], "ps_tm")

        DMA(P, "sp", gcol[:], c.inp["mix_g"], [], ["gcol"])
        for kc in range(8):
            st, sk = wst.next()
            DMA(P, "sp", st[:], c.inp["w_in"][kc * 128:(kc + 1) * 128, 0:2578], [], [sk])
            n = 0
            for (dst, cm) in ((Wfm, FM_COLMAP), (Wtm, TM_COLMAP)):
                for (dc, sc, w) in cm:
                    if n % 2 == 0:
                        TS(P, "dve", dst[:, kc, dc:dc + w], st[:, sc:sc + w], gcol[:, kc:kc + 1], None, ALU.mult, None,
                           [sk, "gcol"], [("W", kc)])
                    else:
                        ACT(P, dst[:, kc, dc:dc + w], st[:, sc:sc + w], AF.Copy, [sk, "gcol"], [("W", kc)],
                            scale=gcol[:, kc:kc + 1])
                    n += 1
        Wkeys = [("W", kc) for kc in range(8)]

        for tg in range(8):
            hT, hk = hTg.next()
            hpieces = [(hk, s, half) for s in range(4) for half in range(2)]
            for s in range(4):
                i = tg * 4 + s
                xt, xk = xbuf.next()
                DMA(P, "sp", xt[:], c.inp["x"][i * 128:(i + 1) * 128, :], [], [xk])
                ss, ssk = ssbuf.next()
                ACT(P, junk[:], xt[:], AF.Square, [xk], ["junkA", ssk], accum=ss[:, 0:1])
                rstd_chain(P, ss, ssk)
                xs, xsk = xsbuf.next()
                TS(P, "dve", xs[:], xt[:], ss[:, 3:4], None, ALU.mult, None, [xk, ssk], [xsk])
                for half in range(2):
                    pt, ptk = pstr.next()
                    for j in range(4):
                        cc = half * 4 + j
                        TR(P, pt[:, j * 128:(j + 1) * 128], xs[:, cc * 128:(cc + 1) * 128], c.ident[:], [xsk, "ident"], [ptk])
                    dst = hT[:, half * 4:(half + 1) * 4, s * 128:(s + 1) * 128]
                    src = pt[:].rearrange("p (j t) -> p j t", j=4)
                    CP(P, "act" if half == 0 else "dve", dst, src, [ptk], [(hk, s, half)])
            fst, fsk = FMst.next()
            for ch in range(15):
                pf, pfk = psfm.next()
                for kc in range(8):
                    MM(P, pf[:, :], Wfm[:, kc, ch * 128:(ch + 1) * 128], hT[:, kc, :], kc == 0, kc == 7,
                       hpieces + [("W", kc)], [pfk])
                sc = 0.125 if ch in Q_CHUNKS else 1.0
                if ch % 2 == 0:
                    ACT(P, fst[:, ch, :], pf[:, :], AF.Copy, [pfk], [(fsk, ch)], scale=sc)
                else:
                    TS(P, "dve", fst[:, ch, :], pf[:, :], sc, None, ALU.mult, None, [pfk], [(fsk, ch)])
            DMA(P, "sp", c.FM.rearrange("(c p) t -> p c t", p=128)[:, :, tg * 512:(tg + 1) * 512], fst[:],
                [(fsk, ch) for ch in range(15)], ["FM"])
            DMA(P, "sp", c.HT.rearrange("(c p) t -> p c t", p=128)[:, :, tg * 512:(tg + 1) * 512], hT[:],
                hpieces, ["HT"])
            for s in range(4):
                i = tg * 4 + s
                pa, pak = pstm.next()
                for kc in range(8):
                    MM(P, pa[:, 0:512], hT[:, kc, s * 128:(s + 1) * 128], Wtm[:, kc, 0:512], kc == 0, kc == 7,
                       hpieces + [("W", kc)], [pak])
                tst, tsk = TMst.next()
                CP(P, "dve", tst[:, 0:512], pa[:, 0:512], [pak], [tsk])
                pb, pbk = pstm.next()
                for kc in range(8):
                    MM(P, pb[:, 0:146], hT[:, kc, s * 128:(s + 1) * 128], Wtm[:, kc, 512:658], kc == 0, kc == 7,
                       hpieces + [("W", kc)], [pbk])
                CP(P, "dve", tst[:, 512:640], pb[:, 0:128], [pbk], [tsk])
                gs, gsk = gst.next()
                ACT(P, gs[:], pb[:, 128:146], AF.Sigmoid, [pbk], [gsk])
                DMA(P, "sp", c.TMV[i * 128:(i + 1) * 128, :], tst[:], [tsk], ["TMV"])
                DMA(P, "sp", c.GATES[i * 128:(i + 1) * 128, :], gs[:], [gsk], ["GATES"])
        P.end_phase()
        P.alloc_sems(c.es0, c.sems)
        with nc.Block() as block:
            P.emit(block, c.sems)


def phase_B(c):
    nc, P = c.nc, c.P
    with ExitStack() as es:
        sb = lambda name, shape, dt: es.enter_context(nc.sbuf_tensor(name, shape, dt))
        qT = [sb(f"qTb{j}", [128, T], BF16) for j in range(3)]
        kT = [sb(f"kTb{j}", [128, T], BF16) for j in range(3)]
        V = sb("Vsb", [128, NT, 384], BF16)
        ntri = sb("ntri", [128, 128], BF16)
        nones = sb("nones", [128, 128], BF16)
        Ebuf = Rot([sb(f"E{i}", [128, 512], F32) for i in range(3)], "E")
        SPbuf = Rot([sb(f"SP{i}", [128, 512], BF16) for i in range(4)], "SP")
        Sbuf = Rot([sb(f"Ssum{i}", [128, 512], BF16) for i in range(3)], "Ssum")
        abuf = Rot([sb(f"aT{i}", [128, 512], BF16) for i in range(3)], "aT")
        obuf = Rot([sb(f"sbo{i}", [64, 512], BF16) for i in range(2)], "sbo")
        psA = Rot(c.ps[0:5], "psA")
        psO = Rot(c.ps[5:7], "psO")
        pswarm = c.ps[7]
        for j in range(3):
            DMA(P, "sp", qT[j][:], c.FM[FM_QSB + 128 * j:FM_QSB + 128 * (j + 1), :], ["FM"], [("qT", j)])
            DMA(P, "sp", kT[j][:], c.FM[FM_KSB + 128 * j:FM_KSB + 128 * (j + 1), :], ["FM"], [("kT", j)])
        DMA(P, "sp", V[:], c.TMV.rearrange("(c p) f -> p c f", p=128)[:, :, 0:384], ["TMV"], ["V"])
        MEMSET(P, "pool", nones[:], -1.0, [], ["nones"])
        MEMSET(P, "pool", ntri[:], -1.0, [], ["ntri"])
        ASEL(P, ntri[:], ntri[:], [[-1, 128]], ALU.is_ge, 0.0, 0, 1, ["ntri"], ["ntri"])
        uvst = Rot([sb(f"uvst{i}", [128, 2 * D], BF16) for i in range(4)], "uvst")

        def convert_chunk(ch):
            t_, tk = uvst.next()
            P.dma("pool", lambda e, o=t_[:], i_=c.inp["peer_uv"][ch * 128:(ch + 1) * 128, :]: e.dma_start(out=o, in_=i_), [], [tk])
            DMA(P, "sp", c.UVB[ch * 128:(ch + 1) * 128, :], t_[:], [tk], ["UVB"])
        steps = []
        for h in range(6):
            for g in range(8):
                nch = 4 * g + 4
                for idx_, cch in enumerate(range(nch - 1, -1, -1)):
                    steps.append(dict(h=h, g=g, cch=cch, first=idx_ == 0, last=cch == 0))
        N = len(steps)
        cur = dict(po=None, pok=None, ssum=None, ssumk=None)

        def S12(st):
            h, g, cch = st["h"], st["g"], st["cch"]
            j, half = h // 2, h % 2
            pr = slice(64 * half, 64 * half + 64)
            st["qs"] = qT[j][pr, g * 512:(g + 1) * 512]
            st["ks"] = kT[j][pr, cch * 128:(cch + 1) * 128]
            st["rk"] = [("kT", j), ("qT", j)]
            st["m"] = cch - 4 * g
            if st["first"]:
                cur["po"], cur["pok"] = psO.next()
                cur["ssum"] = cur["ssumk"] = None
            st["po"], st["pok"] = cur["po"], cur["pok"]
            pa, pak = psA.next()
            MM(P, pa[:, :], st["ks"], st["qs"], True, False, st["rk"], [pak])
            st["pa"], st["pak"] = pa, pak
            E, Ek = Ebuf.next()
            ACT(P, E[:], pa[:, :], AF.Exp, [pak], [Ek])
            SP, SPk = SPbuf.next()
            ACT(P, SP[:], E[:], AF.Ln, [Ek], [SPk], bias=1.0)
            if st["m"] >= 0:
                ASEL(P, SP[:], SP[:], [[1, 512]], ALU.is_gt, 0.0, -128 * st["m"], -1, [SPk], [SPk])
            st["SP"], st["SPk"] = SP, SPk
            st["ssum_prev"], st["ssum_prevk"] = cur["ssum"], cur["ssumk"]
            if not st["last"]:
                if st["first"]:
                    cur["ssum"], cur["ssumk"] = SP, SPk
                else:
                    sn, snk = Sbuf.next()
                    TT(P, "pool", sn[:], cur["ssum"][:], SP[:], ALU.add, [cur["ssumk"], SPk], [snk])
                    cur["ssum"], cur["ssumk"] = sn, snk

        def S34(st):
            pb, pbk = st["pa"], st["pak"]
            MM(P, pb[:, :], ntri[:], st["SP"][:], False, st["first"], ["ntri", st["SPk"]], [pbk])
            if not st["first"]:
                MM(P, pb[:, :], nones[:], st["ssum_prev"][:], False, True, ["nones", st["ssum_prevk"]], [pbk])
            aT, aTk = abuf.next()
            ACT(P, aT[:], pb[:, :], AF.Exp, [pbk], [aTk])
            if st["m"] >= 0:
                ASEL(P, aT[:], aT[:], [[1, 512]], ALU.is_gt, 0.0, -128 * st["m"], -1, [aTk], [aTk])
            st["aT"], st["aTk"] = aT, aTk

        NWARM = 2

        def S5(st):
            h, g, cch = st["h"], st["g"], st["cch"]
            for _ in range(NWARM):
                MM(P, pswarm[:, :], ntri[:], qT[0][:, 0:512], True, True, ["ntri", ("qT", 0)], ["pswarm"])
            MM(P, st["po"][0:64, :], V[:, cch, 64 * h:64 * h + 64], st["aT"][:], st["first"], st["last"], ["V", st["aTk"]], [st["pok"]])
            if st["last"]:
                ob, obk = obuf.next()
                CP(P, "dve", ob[:], st["po"][0:64, :], [st["pok"]], [obk])
                DMA(P, "sp", c.SBOT[64 * h:64 * h + 64, g * 512:(g + 1) * 512], ob[:], [obk], ["SBOT"])

        for n in range(N + 2):
            if n % 6 == 0 and n // 6 < 128:
                convert_chunk(n // 6)
            if n < N:
                S12(steps[n])
            if 0 <= n - 1 < N:
                S34(steps[n - 1])
            if 0 <= n - 2 < N:
                S5(steps[n - 2])
                steps[n - 2].clear()
        P.end_phase()
        P.alloc_sems(c.es0, c.sems)
        with nc.Block() as block:
            P.emit(block, c.sems)


def phase_C(c):
    nc, P = c.nc, c.P
    with ExitStack() as es:
        sb = lambda name, shape, dt: es.enter_context(nc.sbuf_tensor(name, shape, dt))
        qaug = [sb(f"qaug{h}", [128, T], BF16) for h in range(6)]
        kslc = [sb(f"kslc{g}", [128, T], BF16) for g in range(2)]
        kwin = [sb(f"kwin{g}", [64, T], BF16) for g in range(2)]
        kcmp = [sb(f"kcmp{g}", [64, T], BF16) for g in range(2)]
        vcmp = [sb(f"vcmp{g}", [64, T], BF16) for g in range(2)]
        kcT = [sb(f"kcT{g}", [64, 256], BF16) for g in range(2)]
        vcaug = [sb(f"vcaug{g}", [128, 2, 129], BF16) for g in range(2)]
        vwin = sb("vwin", [128, NT, 2, 65], BF16)
        vslc = sb("vslc", [128, NT, 2, 65], BF16)
        vstage = sb("vstage", [128, NT, 256], BF16)
        gates = sb("gatesC", [128, NT, 18], F32)
        pe = [sb("pek_sb", [64, 32], F32), sb("pev_sb", [64, 32], F32)]
        cwst = sb("cwst", [64, 2048], F32)
        cw = [sb("cwk", [64, 32, 64], BF16), sb("cwv", [64, 32, 64], BF16)]
        tmpb = Rot([sb(f"ctmp{i}", [64, 256], BF16) for i in range(3)], "ctmp")
        itmp = sb("itmp", [128, 64], I32)
        ftmp = sb("ftmp", [128, 64], F32)
        bias_sel = sb("bias_sel", [128, 6], F32)
        bias_win = sb("bias_win", [128, 6, 5], F32)
        bias_cmp = sb("bias_cmp", [128, 6, 64], F32)
        IOTi = sb("IOTi", [128, 128], I32)
        IOT = sb("IOT", [128, 128], F32)
        e32b = Rot([sb(f"e32_{i}", [128, 128], F32) for i in range(4)], "e32")
        ebb = Rot([sb(f"eb{i}", [128, 128], BF16) for i in range(7)], "eb")
        obuf = Rot([sb(f"oC{i}", [128, 384], F32) for i in range(8)], "oC")
        ewb = Rot([sb(f"ew{i}", [128, 512], BF16) for i in range(5)], "ew")
        otiles = {}
        impb = Rot([sb(f"imp{i}", [128, 64], F32) for i in range(2)], "imp")
        wb = Rot([sb(f"wC{i}", [128, 4], F32) for i in range(4)], "wC")
        m8b = Rot([sb(f"m8_{i}", [128, 16], F32) for i in range(2)], "m8")
        repb = Rot([sb(f"rep{i}", [128, 64], F32) for i in range(2)], "rep")
        selb = Rot([sb(f"selp{i}", [128, 128], F32) for i in range(2)], "selp")
        Ctb = Rot([sb(f"Ct{i}", [128, 128], F32) for i in range(2)], "Ct")
        ostb = Rot([sb(f"ostC{i}", [128, 3, 128], BF16) for i in range(2)], "ostC")
        pss = Rot(c.ps[0:3], "pss")
        psacc = Rot(c.ps[3:6], "psacc")
        pstr = Rot(c.ps[6:8], "pstrC")
        pk = c.ps[6]

        for h in range(6):
            DMA(P, "sp", qaug[h][0:64, :], c.FM[FM_QNSA + 64 * h:FM_QNSA + 64 * (h + 1), :], ["FM"], [("q", h)])
        for g in range(2):
            DMA(P, "sp", kslc[g][0:64, :], c.FM[FM_KSLC + 64 * g:FM_KSLC + 64 * (g + 1), :], ["FM"], [("kslc", g)])
            DMA(P, "sp", kwin[g][:], c.FM[FM_KWIN + 64 * g:FM_KWIN + 64 * (g + 1), :], ["FM"], [("kwin", g)])
            DMA(P, "sp", kcmp[g][:], c.FM[FM_KCMP + 64 * g:FM_KCMP + 64 * (g + 1), :], ["FM"], [("kcmp", g)])
            DMA(P, "sp", vcmp[g][:], c.FM[FM_VCMP + 64 * g:FM_VCMP + 64 * (g + 1), :], ["FM"], [("vcmp", g)])
        DMA(P, "sp", vstage[:], c.TMV.rearrange("(c p) f -> p c f", p=128)[:, :, 384:640], ["TMV"], ["vstage"])
        DMA(P, "sp", gates[:], c.GATES.rearrange("(c p) f -> p c f", p=128), ["GATES"], ["gates"])
        DMA(P, "sp", pe[0][:], c.inp["pe_k"], [], ["pe0"])
        DMA(P, "sp", pe[1][:], c.inp["pe_v"], [], ["pe1"])
        for kv, nm in ((0, "cw_k"), (1, "cw_v")):
            DMA(P, "sp", cwst[:], c.inp[nm].rearrange("d l e -> d (l e)"), [], ["cwst"])
            CP(P, "dve", cw[kv][:].rearrange("d l e -> d (l e)"), cwst[:], ["cwst"], [("cw", kv)])
        CP(P, "dve", vslc[:, :, :, 0:64], vstage[:, :, 0:128].rearrange("p c (g d) -> p c g d", g=2), ["vstage"], ["vslc"])
        CP(P, "pool", vwin[:, :, :, 0:64], vstage[:, :, 128:256].rearrange("p c (g d) -> p c g d", g=2), ["vstage"], ["vwin"])
        MEMSET(P, "dve", vslc[:, :, :, 64:65], 1.0, ["vslc"], ["vslc"])
        MEMSET(P, "pool", vwin[:, :, :, 64:65], 1.0, ["vwin"], ["vwin"])
        for g in range(2):
            MEMSET(P, "pool", kslc[g][64:128, :], 1.0, [], [("kx", g)])
            ASEL(P, kslc[g][64:128, :], kslc[g][64:128, :], [[1, T]], ALU.is_ge, 0.0, 0, -64, [("kx", g)], [("kx", g)])
            ASEL(P, kslc[g][64:128, :], kslc[g][64:128, :], [[-1, T]], ALU.is_ge, 0.0, 63, 64, [("kx", g)], [("kx", g)])
        IOTA(P, itmp[0:64, 0:1], [[0, 1]], 0, 1, [], ["itmp"])
        IOTA(P, itmp[64:128, 0:1], [[0, 1]], 0, 1, ["itmp"], ["itmp"])
        CP(P, "dve", ftmp[:, 0:1], itmp[:, 0:1], ["itmp"], ["ftmp"])
        for h in range(6):
            TS(P, "dve", bias_sel[:, h:h + 1], ftmp[:, 0:1], SLOPES[h], None, ALU.mult, None, ["ftmp"], ["bias_sel"])
        IOTA(P, itmp[:, 0:5], [[128, 5]], -512, 1, ["itmp", "ftmp"], ["itmp"])
        CP(P, "dve", ftmp[:, 0:5], itmp[:, 0:5], ["itmp"], ["ftmp"])
        for h in range(6):
            TS(P, "dve", bias_win[:, h, :], ftmp[:, 0:5], SLOPES[h], None, ALU.mult, None, ["ftmp"], ["bias_win"])
        IOTA(P, itmp[:, 0:64], [[2048, 2], [-128, 32]], 31, 16, ["itmp", "ftmp"], ["itmp"])
        CP(P, "dve", ftmp[:, 0:64], itmp[:, 0:64], ["itmp"], ["ftmp"])
        for h in range(6):
            TS(P, "dve", bias_cmp[:, h, :], ftmp[:, 0:64], SLOPES[h], None, ALU.mult, None, ["ftmp"], ["bias_cmp"])
        IOTA(P, IOTi[64:128, :], [[-1, 128]], 0, 64, [], ["IOTi"])
        CP(P, "dve", IOT[64:128, :], IOTi[64:128, :], ["IOTi"], ["IOT"])
        for g in range(2):
            MEMSET(P, "pool", vcaug[g][:], 1.0, [], [("vcaug", g)])
            for ci in range(2):
                ASEL(P, vcaug[g][:, ci, 65:129], vcaug[g][:, ci, 65:129], [[-4, 64]], ALU.is_ge, 0.0, 128 * ci, 1,
                     [("vcaug", g)], [("vcaug", g)])
                ASEL(P, vcaug[g][:, ci, 65:129], vcaug[g][:, ci, 65:129], [[4, 64]], ALU.is_ge, 0.0, 3 - 128 * ci, -1,
                     [("vcaug", g)], [("vcaug", g)])
        for s_ in selb.tiles:
            pass
        for j in range(2):
            MEMSET(P, "dve", selb.tiles[j][:, 0:64], 0.0, [], [("selp", j)])
        for g in range(2):
            kview = kcmp[g][:].rearrange("p (n s) -> p n s", s=16)
            vview = vcmp[g][:].rearrange("p (n s) -> p n s", s=16)
            for l in range(32):
                tmp, tk = tmpb.next()
                TS(P, "dve" if l % 2 == 0 else "pool", tmp[:, 0:255], kview[:, l // 16:l // 16 + 255, l % 16],
                   pe[0][:, l:l + 1], None, ALU.add, None, [("kcmp", g), "pe0"], [tk])
                MM(P, pk[0:64, 0:255], cw[0][:, l, :], tmp[:, 0:255], l == 0, l == 31, [("cw", 0), tk], [("pstrC", 0)])
            CP(P, "dve", kcT[g][:, 0:255], pk[0:64, 0:255], [("pstrC", 0)], [("kcT", g)])
            for ci in range(2):
                rows = 128 if ci == 0 else 127
                for l in range(32):
                    tmp, tk = tmpb.next()
                    n0 = l // 16 + ci * 128
                    TS(P, "dve" if l % 2 == 0 else "pool", tmp[:, 0:rows], vview[:, n0:n0 + rows, l % 16],
                       pe[1][:, l:l + 1], None, ALU.add, None, [("vcmp", g), "pe1"], [tk])
                    MM(P, pk[0:rows, 256:320], tmp[:, 0:rows], cw[1][:, l, :], l == 0, l == 31, [("cw", 1), tk], [("pstrC", 0)])
                CP(P, "dve", vcaug[g][0:rows, ci, 0:64], pk[0:rows, 256:320], [("pstrC", 0)], [("vcaug", g)])

        def consume(pacc, pacck, o, ok, h, i, gcol, first):
            w, wk = wb.next()
            TS(P, "dve", w[:, 0:1], pacc[:, 64:65], 1e-30, None, ALU.max, None, [pacck], [wk])
            RECIP(P, w[:, 1:2], w[:, 0:1], [wk], [wk])
            TT(P, "dve", w[:, 2:3], w[:, 1:2], gates[:, i, gcol:gcol + 1], ALU.mult, [wk, "gates"], [wk])
            if first:
                TS(P, "dve", o[:, 64 * h:64 * h + 64], pacc[:, 0:64], w[:, 2:3], None, ALU.mult, None, [pacck, wk], [(ok, h)])
            else:
                STT(P, o[:, 64 * h:64 * h + 64], pacc[:, 0:64], w[:, 2:3], o[:, 64 * h:64 * h + 64], ALU.mult, ALU.add,
                    [pacck, wk, (ok, h)], [(ok, h)])
            return w, wk

        from collections import deque
        fifo = deque()
        LAGC = 3

        def defer(fn):
            fifo.append(fn)
            while len(fifo) > LAGC:
                fifo.popleft()()

        def flush():
            while fifo:
                fifo.popleft()()

        def consume_cmp(pc, pck, o, ok, h, i, hh, imp, impk):
            w, wk = consume(pc, pck, o, ok, h, i, 3 * h + 0, True)
            if hh == 0:
                TS(P, "dve", imp[:], pc[:, 65:129], w[:, 1:2], None, ALU.mult, None, [pck, wk], [impk])
            else:
                STT(P, imp[:], pc[:, 65:129], w[:, 1:2], imp[:], ALU.mult, ALU.add, [pck, wk, impk], [impk])

        for i in range(NT):
            t0 = 128 * i
            qc = slice(t0, t0 + 128)
            o, ok = obuf.next()
            for g in range(2):
                imp, impk = impb.next()
                for hh in range(3):
                    h = 3 * g + hh
                    nvalid = min(255, (t0 + 96) // 16 + 1)
                    chunks = [(0, min(128, nvalid))] + ([(1, nvalid - 128)] if nvalid > 128 else [])
                    pc, pck = psacc.next()
                    for ni, (ci, rows) in enumerate(chunks):
                        ps_, psk = pss.next()
                        MM(P, ps_[0:rows, 0:128], kcT[g][:, ci * 128:ci * 128 + rows], qaug[h][0:64, qc], True, True,
                           [("kcT", g), ("q", h)], [psk])
                        e32, e32k = e32b.next()
                        ACT(P, e32[0:rows, :], ps_[0:rows, 0:128], AF.Exp, [psk, "bias_cmp"], [e32k],
                            bias=bias_cmp[0:rows, h, ci * 32 + i:ci * 32 + i + 1])
                        eb, ebk = ebb.next()
                        ASEL(P, eb[0:rows, :], e32[0:rows, :], [[1, 128]], ALU.is_ge, 0.0, t0 - 2048 * ci - 31, -16,
                             [e32k], [ebk])
                        defer(lambda pc=pc, pck=pck, eb=eb, ebk=ebk, rows=rows, ci=ci, g=g, st_=(ni == 0), sp_=(ni == len(chunks) - 1):
                              MM(P, pc[:, 0:129], eb[0:rows, :], vcaug[g][0:rows, ci, :], st_, sp_, [ebk, ("vcaug", g)], [pck]))
                    defer(lambda pc=pc, pck=pck, o=o, ok=ok, h=h, i=i, hh=hh, imp=imp, impk=impk:
                          consume_cmp(pc, pck, o, ok, h, i, hh, imp, impk))
                    pw, pwk = psacc.next()
                    cl = list(range(max(0, i - 4), i + 1))
                    for ni, cch in enumerate(cl):
                        dc = cch - i
                        ps_, psk = pss.next()
                        MM(P, ps_[:, 0:128], kwin[g][:, cch * 128:(cch + 1) * 128], qaug[h][0:64, qc], True, True,
                           [("kwin", g), ("q", h)], [psk])
                        eb, ebk = ebb.next()
                        bw = bias_win[:, h, dc + 4:dc + 5]
                        if dc == 0 or dc == -4:
                            e32, e32k = e32b.next()
                            ACT(P, e32[:], ps_[:, 0:128], AF.Exp, [psk, "bias_win"], [e32k], bias=bw)
                            if dc == 0:
                                ASEL(P, eb[:], e32[:], [[1, 128]], ALU.is_ge, 0.0, 0, -1, [e32k], [ebk])
                            else:
                                ASEL(P, eb[:], e32[:], [[-1, 128]], ALU.is_gt, 0.0, 0, 1, [e32k], [ebk])
                        else:
                            ACT(P, eb[:], ps_[:, 0:128], AF.Exp, [psk, "bias_win"], [ebk], bias=bw)
                        defer(lambda pw=pw, pwk=pwk, eb=eb, ebk=ebk, cch=cch, g=g, st_=(ni == 0), sp_=(ni == len(cl) - 1):
                              MM(P, pw[:, 0:65], eb[:], vwin[:, cch, g, :], st_, sp_, [ebk, "vwin"], [pwk]))
                    defer(lambda pw=pw, pwk=pwk, o=o, ok=ok, h=h, i=i: consume(pw, pwk, o, ok, h, i, 3 * h + 2, False))
                flush()
                ASEL(P, imp[:], imp[:], [[-64, 64]], ALU.is_ge, 1e4, t0 - 128, 1, [impk], [impk])
                ASEL(P, imp[:], imp[:], [[-64, 64]], ALU.is_ge, -1.0, t0, 1, [impk], [impk])
                MEMSET(P, "pool", imp[:, 0:1], 1e4, [impk], [impk])
                m8, m8k = m8b.next()
                rep, repk = repb.next()
                P.op("dve", lambda e, m8=m8, imp=imp: e.max(out=m8[:, 0:8], in_=imp[:]), [impk], [m8k])
                P.op("dve", lambda e, m8=m8, imp=imp, rep=rep: e.match_replace(out=rep[:], in_to_replace=m8[:, 0:8],
                                                                                 in_values=imp[:], imm_value=-1e30),
                     [impk, m8k], [repk])
                P.op("dve", lambda e, m8=m8, rep=rep: e.max(out=m8[:, 8:16], in_=rep[:]), [repk, m8k], [m8k])
                selp, selk = selb.next()
                TS(P, "dve", selp[:, 64:128], imp[:], m8[:, 15:16], None, ALU.is_ge, None, [impk, m8k], [selk])
                pt_, ptk = pstr.next()
                TR(P, pt_[:, 0:128], selp[:], c.ident[:], [selk, "ident"], [ptk])
                for hh in range(3):
                    h = 3 * g + hh
                    Ct, Ctk = Ctb.next()
                    TS(P, "pool", Ct[64:128, :], IOT[64:128, :], SLOPES[h], -BIG - SLOPES[h] * t0, ALU.mult, ALU.add,
                       ["IOT"], [Ctk])
                    STT(P, qaug[h][64:128, qc], pt_[64:128, 0:128], BIG, Ct[64:128, :], ALU.mult, ALU.add,
                        [ptk, Ctk], [("qm", h, i)])

            otiles[i] = (o, ok)
            if i % 4 != 3:
                continue
            G4 = i // 4
            t0g = 512 * G4
            for g in range(2):
                for hh in range(3):
                    h = 3 * g + hh
                    psl, pslk = psacc.next()
                    nchk = 4 * G4 + 4
                    for cch in range(nchk):
                        jmin = max(0, cch - 4 * G4)
                        ncol = (4 - jmin) * 128
                        qcs = slice(t0g + jmin * 128, t0g + 512)
                        ps_, psk = pss.next()
                        MM(P, ps_[:, 0:ncol], kslc[g][:, cch * 128:(cch + 1) * 128], qaug[h][:, qcs], True, True,
                           [("kslc", g), ("kx", g), ("q", h)] + [("qm", h, 4 * G4 + j) for j in range(jmin, 4)], [psk])
                        ew, ewk = ewb.next()
                        ACT(P, ew[:, 0:ncol], ps_[:, 0:ncol], AF.Exp, [psk, "bias_sel"], [ewk], bias=bias_sel[:, h:h + 1])
                        if cch >= 4 * G4:
                            ASEL(P, ew[:, 0:128], ew[:, 0:128], [[1, 128]], ALU.is_ge, 0.0, 0, -1, [ewk], [ewk])

                        def av(psl=psl, pslk=pslk, ew=ew, ewk=ewk, cch=cch, jmin=jmin, g=g, G4=G4):
                            for j in range(jmin, 4):
                                MM(P, psl[:, j * 128:j * 128 + 65], ew[:, (j - jmin) * 128:(j - jmin + 1) * 128],
                                   vslc[:, cch, g, :], cch == 0 and j == 0, cch == 4 * G4 + j, [ewk, "vslc"], [pslk], sgc=True)
                        defer(av)
                    for j in range(4):
                        ti = 4 * G4 + j
                        defer(lambda psl=psl, pslk=pslk, j=j, ti=ti, h=h:
                              consume(psl[:, j * 128:j * 128 + 65], pslk, otiles[ti][0], otiles[ti][1], h, ti, 3 * h + 1, False))
            for ti in range(4 * G4, 4 * G4 + 4):
                def finish_tile(o=otiles[ti][0], ok=otiles[ti][1], qc=slice(128 * ti, 128 * ti + 128)):
                    pt2, pt2k = pstr.next()
                    for j in range(3):
                        TR(P, pt2[:, j * 128:(j + 1) * 128], o[:, j * 128:(j + 1) * 128], c.ident[:],
                           [(ok, 2 * j), (ok, 2 * j + 1), "ident"], [pt2k])
                    ost, ostk = ostb.next()
                    CP(P, "act", ost[:], pt2[:, 0:384].rearrange("p (j t) -> p j t", j=3), [pt2k], [ostk])
                    DMA(P, "sp", c.NSAOT.rearrange("(j p) t -> p j t", p=128)[:, :, qc], ost[:], [ostk], ["NSAOT"])
                defer(finish_tile)
        flush()
        P.end_phase()
        P.alloc_sems(c.es0, c.sems)
        with nc.Block() as block:
            P.emit(block, c.sems)


def phase_D(c):
    nc, P = c.nc, c.P
    with ExitStack() as es:
        sb = lambda name, shape, dt: es.enter_context(nc.sbuf_tensor(name, shape, dt))
        memt = sb("memt", [128, 2, D], F32)
        mems = sb("mems", [128, 2, D], F32)
        junk = sb("junkD", [128, D], F32)
        ssb = [sb(f"ssD{i}", [128, 4], F32) for i in range(2)]
        gcol = sb("gcolD", [128, 8], F32)
        mhT = sb("mhT", [128, 8, 256], BF16)
        wst = Rot([sb(f"wstD{i}", [128, 512], F32) for i in range(2)], "wstD")
        Wkv = sb("Wkv", [128, 8, 512], BF16)
        mkT = [sb(f"mkT{h}", [64, 256], BF16) for h in range(4)]
        mvaug = sb("mvaug", [128, 2, 4, 65], BF16)
        qm = [sb(f"qm{h}", [64, T], BF16) for h in range(4)]
        eb = Rot([sb(f"ebD{i}", [128, 512], BF16) for i in range(4)], "ebD")
        ob = Rot([sb(f"oD{i}", [128, 4, 256], F32) for i in range(2)], "oD")
        wb = Rot([sb(f"wD{i}", [128, 2], F32) for i in range(4)], "wD")
        ostb = Rot([sb(f"ostD{i}", [128, 2, 512], BF16) for i in range(2)], "ostD")
        pss = Rot(c.ps[0:2], "pssD")
        psacc = Rot(c.ps[2:5], "psaccD")
        pstr = Rot(c.ps[5:7], "pstrD")
        pmisc = c.ps[7]

        DMA(P, "sp", gcol[:], c.inp["mem_g"], [], ["gcol"])
        DMA(P, "sp", memt[:], c.inp["mem"].rearrange("(c p) d -> p c d", p=128), [], ["memt"])
        for h in range(4):
            DMA(P, "sp", qm[h][:], c.FM[FM_QMEM + 64 * h:FM_QMEM + 64 * (h + 1), :], ["FM"], [("qm", h)])
        for kc in range(8):
            st, sk = wst.next()
            DMA(P, "sp", st[:], c.inp["w_mem_kv"][kc * 128:(kc + 1) * 128, :], [], [sk])
            TS(P, "dve", Wkv[:, kc, :], st[:], gcol[:, kc:kc + 1], None, ALU.mult, None, [sk, "gcol"], [("Wkv", kc)])
        Wk = [("Wkv", kc) for kc in range(8)]
        for ci in range(2):
            ss = ssb[ci]
            ssk = ("ssD", ci)
            ACT(P, junk[:], memt[:, ci, :], AF.Square, ["memt"], ["junkD", ssk], accum=ss[:, 0:1])
            rstd_chain(P, ss, ssk)
            TS(P, "dve", mems[:, ci, :], memt[:, ci, :], ss[:, 3:4], None, ALU.mult, None, ["memt", ssk], [("mems", ci)])
            for half in range(2):
                pt, ptk = pstr.next()
                for j in range(4):
                    cc = half * 4 + j
                    TR(P, pt[:, j * 128:(j + 1) * 128], mems[:, ci, cc * 128:(cc + 1) * 128], c.ident[:],
                       [("mems", ci), "ident"], [ptk])
                CP(P, "dve", mhT[:, half * 4:(half + 1) * 4, ci * 128:(ci + 1) * 128],
                   pt[:].rearrange("p (j t) -> p j t", j=4), [ptk], [("mhT", ci, half)])
        mh = [("mhT", ci, half) for ci in range(2) for half in range(2)]
        for h in range(4):
            for kc in range(8):
                MM(P, pmisc[0:64, 0:256], Wkv[:, kc, 64 * h:64 * h + 64], mhT[:, kc, :], kc == 0, kc == 7, Wk + mh, ["pmisc"])
            CP(P, "dve", mkT[h][:], pmisc[0:64, 0:256], ["pmisc"], [("mkT", h)])
        for ci in range(2):
            for kc in range(8):
                MM(P, pmisc[:, 256:512], mhT[:, kc, ci * 128:(ci + 1) * 128], Wkv[:, kc, 256:512], kc == 0, kc == 7,
                   Wk + mh, ["pmisc"])
            CP(P, "dve", mvaug[:, ci, :, 0:64], pmisc[:, 256:512].rearrange("p (h d) -> p h d", h=4), ["pmisc"], ["mvaug"])
        MEMSET(P, "dve", mvaug[:, :, :, 64:65], 1.0, ["mvaug"], ["mvaug"])

        for g in range(8):
            o, ok = ob.next()
            for h in range(4):
                es_ = []
                for ci in range(2):
                    ps_, psk = pss.next()
                    MM(P, ps_[:, :], mkT[h][:, ci * 128:(ci + 1) * 128], qm[h][:, g * 512:(g + 1) * 512], True, True,
                       [("mkT", h), ("qm", h)], [psk])
                    e, ek = eb.next()
                    ACT(P, e[:], ps_[:, :], AF.Exp, [psk], [ek])
                    es_.append((e, ek))
                for sub in range(4):
                    pa, pak = psacc.next()
                    for ci in range(2):
                        MM(P, pa[:, 0:65], es_[ci][0][:, sub * 128:(sub + 1) * 128], mvaug[:, ci, h, :], ci == 0, ci == 1,
                           [es_[ci][1], "mvaug"], [pak])
                    w, wk = wb.next()
                    RECIP(P, w[:, 0:1], pa[:, 64:65], [pak], [wk])
                    TS(P, "dve", o[:, sub, 64 * h:64 * h + 64], pa[:, 0:64], w[:, 0:1], None, ALU.mult, None, [pak, wk],
                       [(ok, sub, h)])
            ost, ostk = ostb.next()
            for sub in range(4):
                pt, ptk = pstr.next()
                for j in range(2):
                    TR(P, pt[:, j * 128:(j + 1) * 128], o[:, sub, j * 128:(j + 1) * 128], c.ident[:],
                       [(ok, sub, 2 * j), (ok, sub, 2 * j + 1), "ident"], [ptk])
                CP(P, "act", ost[:, :, sub * 128:(sub + 1) * 128], pt[:, 0:256].rearrange("p (j t) -> p j t", j=2),
                   [ptk], [(ostk, sub)])
            DMA(P, "sp", c.MEMOT.rearrange("(j p) t -> p j t", p=128)[:, :, g * 512:(g + 1) * 512], ost[:],
                [(ostk, sub) for sub in range(4)], ["MEMOT"])
        P.end_phase()
        P.alloc_sems(c.es0, c.sems)
        with nc.Block() as block:
            P.emit(block, c.sems)


def phase_E(c):
    nc, P = c.nc, c.P
    with ExitStack() as es:
        sb = lambda name, shape, dt: es.enter_context(nc.sbuf_tensor(name, shape, dt))
        Wmg = sb("Wmg", [128, 8, 3072], BF16)
        Wbr = [sb("Wsb", [128, 3, D], BF16), sb("Wnsa", [128, 3, D], BF16), sb("Wmem", [128, 2, D], BF16)]
        Wout = sb("Wout", [128, 8, D], BF16)
        wst = Rot([sb(f"wstE{i}", [128, 1024], F32) for i in range(3)], "wstE")
        gcol = sb("gcolE", [128, 8], F32)
        bmg = sb("bmg", [128, 24], F32)
        hTb = Rot([sb(f"hTE{i}", [128, 8, 512], BF16) for i in range(2)], "hTE")
        srcb = [Rot([sb(f"srcE{b}_{i}", [128, 3 if b < 2 else 2, 512], BF16) for i in range(2)], f"srcE{b}") for b in range(3)]
        mgb = Rot([sb(f"mgT{i}", [128, 8, 512], BF16) for i in range(2)], "mgT")
        gateb = Rot([sb(f"gateE{i}", [128, 512], F32) for i in range(3)], "gateE")
        accb = Rot([sb(f"accE{i}", [128, 512], F32) for i in range(2)], "accE")
        tmpb = Rot([sb(f"tmpE{i}", [128, 512], F32) for i in range(2)], "tmpE")
        xb = Rot([sb(f"xE{i}", [128, D], F32) for i in range(2)], "xE")
        x1b = Rot([sb(f"x1E{i}", [128, D], F32) for i in range(2)], "x1E")
        psbr = Rot(c.ps[0:2], "psbr")
        psg = Rot(c.ps[2:5], "psg")
        psy = Rot(c.ps[5:8], "psy")

        DMA(P, "sp", gcol[:], c.inp["mix_g"], [], ["gcol"])
        DMA(P, "sp", bmg[:], c.inp["b_merge"], [], ["bmg"])
        n = 0
        for kc in range(8):
            for j in range(3):
                st, sk = wst.next()
                DMA(P, "sp", st[:], c.inp["w_in"][kc * 128:(kc + 1) * 128, 2578 + 1024 * j:2578 + 1024 * (j + 1)], [], [sk])
                if n % 2 == 0:
                    TS(P, "dve", Wmg[:, kc, 1024 * j:1024 * (j + 1)], st[:], gcol[:, kc:kc + 1], None, ALU.mult, None,
                       [sk, "gcol"], [("Wmg", kc)])
                else:
                    ACT(P, Wmg[:, kc, 1024 * j:1024 * (j + 1)], st[:], AF.Copy, [sk, "gcol"], [("Wmg", kc)],
                        scale=gcol[:, kc:kc + 1])
                n += 1
        for b, (nm, nf) in enumerate((("w_sb_br", 3), ("w_nsa_br", 3), ("w_mem_br", 2))):
            for f in range(nf):
                st, sk = wst.next()
                DMA(P, "sp", st[:], c.inp[nm][f * 128:(f + 1) * 128, :], [], [sk])
                CP(P, "dve" if n % 2 == 0 else "act", Wbr[b][:, f, :], st[:], [sk], [("Wbr", b)])
                n += 1
        for kc in range(8):
            st, sk = wst.next()
            DMA(P, "sp", st[:], c.inp["w_out"][kc * 128:(kc + 1) * 128, :], [], [sk])
            CP(P, "dve" if n % 2 == 0 else "act", Wout[:, kc, :], st[:], [sk], ["Wout"])
            n += 1
        Wmgk = [("Wmg", kc) for kc in range(8)]
        srcs = [(c.SBOT, 3, "SBOT"), (c.NSAOT, 3, "NSAOT"), (c.MEMOT, 2, "MEMOT")]
        for tg in range(8):
            tc_ = slice(tg * 512, (tg + 1) * 512)
            hT, hk = hTb.next()
            DMA(P, "sp", hT[:], c.HT.rearrange("(c p) t -> p c t", p=128)[:, :, tc_], ["HT"], [hk])
            src = []
            for b, (ap, nf, nm) in enumerate(srcs):
                t_, tk = srcb[b].next()
                DMA(P, "sp", t_[:], ap.rearrange("(f p) t -> p f t", p=128)[:, :, tc_], [nm], [tk])
                src.append((t_, tk, nf))
            mg, mgk = mgb.next()
            for dc in range(8):
                acc, acck = accb.next()
                for b in range(3):
                    t_, tk, nf = src[b]
                    pb, pbk = psbr.next()
                    for f in range(nf):
                        MM(P, pb[:, :], Wbr[b][:, f, dc * 128:(dc + 1) * 128], t_[:, f, :], f == 0, f == nf - 1,
                           [("Wbr", b), tk], [pbk])
                    pg, pgk = psg.next()
                    for kc in range(8):
                        MM(P, pg[:, :], Wmg[:, kc, b * 1024 + dc * 128:b * 1024 + (dc + 1) * 128], hT[:, kc, :], kc == 0, kc == 7,
                           Wmgk + [hk], [pgk])
                    gt, gtk = gateb.next()
                    ACT(P, gt[:], pg[:, :], AF.Sigmoid, [pgk, "bmg"], [gtk], bias=bmg[:, b * 8 + dc:b * 8 + dc + 1])
                    if b == 0:
                        TT(P, "dve", acc[:], gt[:], pb[:, :], ALU.mult, [gtk, pbk], [acck])
                    else:
                        tmp, tmpk = tmpb.next()
                        TT(P, "dve", tmp[:], gt[:], pb[:, :], ALU.mult, [gtk, pbk], [tmpk])
                        if b == 1:
                            TT(P, "pool", acc[:], acc[:], tmp[:], ALU.add, [acck, tmpk], [acck])
                        else:
                            TT(P, "pool", mg[:, dc, :], acc[:], tmp[:], ALU.add, [acck, tmpk], [(mgk, dc)])
            mgks = [(mgk, dc) for dc in range(8)]
            for s in range(4):
                i = tg * 4 + s
                xt, xk = xb.next()
                DMA(P, "sp", xt[:], c.inp["x"][i * 128:(i + 1) * 128, :], [], [xk])
                x1, x1k = x1b.next()
                for half in range(2):
                    py, pyk = psy.next()
                    for dc in range(8):
                        MM(P, py[:, :], mg[:, dc, s * 128:(s + 1) * 128], Wout[:, dc, half * 512:(half + 1) * 512], dc == 0, dc == 7,
                           mgks + ["Wout"], [pyk])
                    TT(P, "dve", x1[:, half * 512:(half + 1) * 512], xt[:, half * 512:(half + 1) * 512], py[:, :], ALU.add,
                       [xk, pyk], [(x1k, half)])
                DMA(P, "sp", c.X1[i * 128:(i + 1) * 128, :], x1[:], [(x1k, 0), (x1k, 1)], ["X1"])
        P.end_phase()
        P.alloc_sems(c.es0, c.sems)
        with nc.Block() as block:
            P.emit(block, c.sems)


def phase_F(c):
    nc, P = c.nc, c.P
    GS = 8
    with ExitStack() as es:
        sb = lambda name, shape, dt: es.enter_context(nc.sbuf_tensor(name, shape, dt))
        Wq = sb("Wq", [128, 8, 2048], BF16)
        subk = sb("subk", [128, 16, 128], BF16)
        g2b = sb("g2b", [128, D], F32)
        gFb = sb("gFb", [128, D], F32)
        keyidx = sb("keyidx", [128, 2048], I32)
        posidx = sb("posidx", [128, 2048], I32)
        iotaA = sb("iotaA", [128, 2048], F32)
        cI = sb("cI", [128, 8], I32)
        with ExitStack() as es1:
            sb1 = lambda name, shape, dt: es1.enter_context(nc.sbuf_tensor(name, shape, dt))
            wst = Rot([sb1(f"wstF{i}", [128, 2048], F32) for i in range(2)], "wstF")
            iotaAi = sb1("iotaAi", [128, 2048], I32)
            for kc in range(8):
                st, sk = wst.next()
                DMA(P, "sp", st[:], c.inp["peer_w_q"][kc * 128:(kc + 1) * 128, :], [], [sk])
                CP(P, "dve" if kc % 2 == 0 else "act", Wq[:, kc, :], st[:], [sk], [("Wq", kc)])
            st, sk = wst.next()
            DMA(P, "sp", st[:], c.inp["subkT"].rearrange("d b k -> d (b k)"), [], [sk])
            CP(P, "dve", subk[:].rearrange("d b k -> d (b k)"), st[:], [sk], ["subk"])
            DMA(P, "sp", g2b[:], c.inp["ffn_g"].partition_broadcast(128), [], ["g2b"])
            DMA(P, "sp", gFb[:], c.inp["final_g"].partition_broadcast(128), [], ["gFb"])
            IOTA(P, keyidx[:], [[0, 16], [1, 128]], 0, 0, [], ["keyidx"])
            IOTA(P, posidx[:], [[0, 8], [1, 256]], 0, 0, [], ["posidx"])
            IOTA(P, iotaAi[:], [[0, 128], [1, 16]], 0, 0, [], ["iotaAi"])
            CP(P, "dve", iotaA[:], iotaAi[:], ["iotaAi"], ["iotaA"])
            for j, v in enumerate((-128, -256, 127, 255, 15, 4)):
                IOTA(P, cI[:, j:j + 1], [[0, 1]], v, 0, ["cI"], ["cI"])
            P.end_phase()
            P.alloc_sems(c.es0, c.sems)
            with nc.Block() as block:
                P.emit(block, c.sems)
        x1b = Rot([sb(f"x1F{i}", [128, D], F32) for i in range(2)], "x1F")
        h2b = Rot([sb(f"h2F{i}", [128, D], F32) for i in range(1)], "h2F")
        ssb = Rot([sb(f"ssF{i}", [128, 4], F32) for i in range(4)], "ssF")
        junk = sb("junkF", [128, D], BF16)
        prodb = Rot([sb(f"prodF{i}", [128, D], BF16) for i in range(4)], "prodF")
        h2hb = Rot([sb(f"h2hF{i}", [128, D], BF16) for i in range(2)], "h2hF")
        h2Tb = Rot([sb(f"h2T{i}", [128, 8, 128], BF16) for i in range(2)], "h2T")
        qTb = sb("qTbF", [128, 16, 128], BF16)
        Sc = sb("Sc", [128, 2048], F32)
        rep = sb("repF", [128, 256], F32)
        stop = sb("stop", [128, 16, 16], F32)
        itop_i = sb("itop_i", [128, 256], I32)
        itop_f = sb("itop_f", [128, 16, 16], F32)
        tmpA = sb("tmpA", [128, 2048], F32)
        tmpB = Sc
        best = sb("best", [128, 8, 16], F32)
        pos_i = sb("pos_i", [128, 3, 128], I32)
        ab_f = sb("ab_f", [128, 2, 128], F32)
        sel_f = sb("sel_f", [128, 3, 128], F32)
        idxb = Rot([sb(f"idxF{i}", [128, 128], I32) for i in range(2)], "idxF")
        gwb = Rot([sb(f"gwF{i}", [128, 3, 128], F32) for i in range(2)], "gwF")
        gsum = sb("gsum", [128, 16], F32)
        ab = Rot([sb(f"aF{i}", [128, 2, 128], F32) for i in range(2)], "aF")
        uvb = Rot([sb(f"uvg{i}", [128, 2 * D], BF16) for i in range(16)], "uvg")
        dgb = Rot([sb(f"dg{i}", [128, 128], BF16) for i in range(6)], "dg")
        x2b = Rot([sb(f"x2F{i}", [128, D], F32) for i in range(1)], "x2F")
        ptq = Rot(c.ps[0:2], "ptq")
        psS = Rot(c.ps[2:4], "psS")
        pvb = Rot([(c.ps[4], c.ps[5]), (c.ps[6], c.ps[7])], "pv")
        Wqk = [("Wq", kc) for kc in range(8)]

        def route(i, st):
            x1, x1k = x1b.next()
            DMA(P, "sp", x1[:], c.X1[i * 128:(i + 1) * 128, :], ["X1"], [x1k])
            ss, ssk = ssb.next()
            ACT(P, junk[:], x1[:], AF.Square, [x1k], [ssk], accum=ss[:, 0:1])
            rstd_chain(P, ss, ssk)
            h2, h2k = h2b.next()
            STT(P, h2[:], x1[:], ss[:, 3:4], g2b[:], ALU.mult, ALU.mult, [x1k, ssk, "g2b"], [h2k])
            h2h, h2hk = h2hb.next()
            CP(P, "act", h2h[:], h2[:], [h2k], [h2hk])
            yield
            h2T, h2Tk = h2Tb.next()
            for half in range(2):
                pt, ptk = ptq.next()
                for j in range(4):
                    cc = half * 4 + j
                    TR(P, pt[:, j * 128:(j + 1) * 128], h2[:, cc * 128:(cc + 1) * 128], c.ident[:], [h2k, "ident"], [ptk])
                CP(P, "act", h2T[:, half * 4:(half + 1) * 4, :], pt[:].rearrange("p (j t) -> p j t", j=4), [ptk], [(h2Tk, half)])
            h2Tks = [(h2Tk, 0), (h2Tk, 1)]
            yield
            for b4 in range(4):
                pq, pqk = ptq.next()
                for j in range(4):
                    blk = b4 * 4 + j
                    for kc in range(8):
                        MM(P, pq[:, j * 128:(j + 1) * 128], Wq[:, kc, blk * 128:(blk + 1) * 128], h2T[:, kc, :], kc == 0, kc == 7,
                           Wqk + h2Tks, [pqk])
                CP(P, "act", qTb[:, b4 * 4:(b4 + 1) * 4, :], pq[:].rearrange("p (j t) -> p j t", j=4), [pqk], [("qTb", b4)])
                yield
            for b4 in range(4):
                pS, pSk = psS.next()
                for j in range(4):
                    blk = b4 * 4 + j
                    MM(P, pS[:, j * 128:(j + 1) * 128], qTb[:, blk, :], subk[:, blk, :], True, True, [("qTb", b4), "subk"], [pSk])
                STT(P, Sc[:, b4 * 512:(b4 + 1) * 512].bitcast(I32), pS[:, :].bitcast(I32), cI[:, 0:1],
                    keyidx[:, b4 * 512:(b4 + 1) * 512], ALU.bitwise_and, ALU.bitwise_or, [pSk, "cI", "keyidx"], [("Sc", b4), "tmpB"])
                yield
            for blk in range(16):
                sblk = Sc[:, blk * 128:(blk + 1) * 128]
                sck = ("Sc", blk // 4)
                P.op("dve", lambda e, o=stop[:, blk, 0:8], s=sblk: e.max(out=o, in_=s), [sck], ["stop"])
                P.op("dve", lambda e, o=rep[:, 0:128], m=stop[:, blk, 0:8], s=sblk: e.match_replace(
                    out=o, in_to_replace=m, in_values=s, imm_value=-1e30), [sck, "stop"], ["repF"])
                P.op("dve", lambda e, o=stop[:, blk, 8:16], s=rep[:, 0:128]: e.max(out=o, in_=s), ["repF"], ["stop"])
                yield
            stop2 = stop[:].rearrange("p b k -> p (b k)")
            TS(P, "dve", itop_i[:], stop2.bitcast(I32), cI[:, 2:3], None, ALU.bitwise_and, None, ["stop", "cI"], ["itop_i"])
            CP(P, "dve", itop_f[:].rearrange("p b k -> p (b k)"), itop_i[:], ["itop_i"], ["itop_f"])
            yield
            sv = stop[:].rearrange("p (h q) k -> p h q k", q=2)
            iv = itop_f[:].rearrange("p (h q) k -> p h q k", q=2)
            cand = tmpA[:].rearrange("p (h a b) -> p h a b", h=8, a=16)
            TT(P, "dve", cand, sv[:, :, 0, :].unsqueeze(3).to_broadcast([128, 8, 16, 16]),
               sv[:, :, 1, :].unsqueeze(2).to_broadcast([128, 8, 16, 16]), ALU.add, ["stop"], ["tmpA"])
            yield
            STT(P, tmpB[:].bitcast(I32), tmpA[:].bitcast(I32), cI[:, 1:2], posidx[:], ALU.bitwise_and, ALU.bitwise_or,
                ["tmpA", "cI", "posidx"], ["tmpB"] + [("Sc", b_) for b_ in range(4)])
            yield
            for h in range(8):
                sblk = tmpB[:, h * 256:(h + 1) * 256]
                P.op("dve", lambda e, o=best[:, h, 0:8], s=sblk: e.max(out=o, in_=s), ["tmpB"], ["best"])
                P.op("dve", lambda e, o=rep[:], m=best[:, h, 0:8], s=sblk: e.match_replace(
                    out=o, in_to_replace=m, in_values=s, imm_value=-1e30), ["tmpB", "best"], ["repF"])
                P.op("dve", lambda e, o=best[:, h, 8:16], s=rep[:]: e.max(out=o, in_=s), ["repF"], ["best"])
                yield
            best2 = best[:].rearrange("p h k -> p (h k)")
            TS(P, "dve", pos_i[:, 0, :], best2.bitcast(I32), cI[:, 3:4], None, ALU.bitwise_and, None, ["best", "cI"], ["pos_i"])
            TS(P, "dve", pos_i[:, 1, :], pos_i[:, 0, :], cI[:, 5:6], None, ALU.logical_shift_right, None, ["pos_i", "cI"], ["pos_i"])
            TS(P, "dve", pos_i[:, 2, :], pos_i[:, 0, :], cI[:, 4:5], None, ALU.bitwise_and, None, ["pos_i", "cI"], ["pos_i"])
            CP(P, "dve", ab_f[:], pos_i[:, 1:3, :], ["pos_i"], ["ab_f"])
            yield
            for q in range(2):
                akv = ab_f[:, q, :].rearrange("p (h k) -> p h k", h=8)
                eq = tmpA[:].rearrange("p (h k a) -> p h k a", h=8, k=16)
                TT(P, "dve", eq, akv.unsqueeze(3).to_broadcast([128, 8, 16, 16]),
                   iotaA[:].rearrange("p (h k a) -> p h k a", h=8, k=16), ALU.is_equal, ["ab_f", "iotaA"], ["tmpA"])
                yield
                pr = tmpB[:].rearrange("p (h k a) -> p h k a", h=8, k=16)
                TT(P, "dve", pr, eq, iv[:, :, q, :].unsqueeze(2).to_broadcast([128, 8, 16, 16]), ALU.mult,
                   ["tmpA", "itop_f"], ["tmpB"] + [("Sc", b_) for b_ in range(4)])
                yield
                P.op("dve", lambda e, o=sel_f[:, q, :], s=tmpB[:].rearrange("p (x a) -> p x a", a=16): e.tensor_reduce(
                    out=o, in_=s, axis=AX.X, op=ALU.add), ["tmpB"], ["sel_f"])
                yield
            STT(P, sel_f[:, 2, :], sel_f[:, 0, :], 128.0, sel_f[:, 1, :], ALU.mult, ALU.add, ["sel_f"], ["sel_f"])
            TS(P, "dve", sel_f[:, 2, :], sel_f[:, 2, :], 0.0, 16383.0, ALU.max, ALU.min, ["sel_f"], ["sel_f"])
            idx, idxk = idxb.next()
            CP(P, "dve", idx[:], sel_f[:, 2, :], ["sel_f"], [idxk])
            yield
            gw, gwk = gwb.next()
            v3 = lambda ap: ap.rearrange("p (h k) -> p h k", h=8)
            TT(P, "dve", v3(gw[:, 0, :]), best[:], best[:, :, 0:1].to_broadcast([128, 8, 16]), ALU.subtract, ["best"], [gwk])
            ACT(P, gw[:, 1, :], gw[:, 0, :], AF.Exp, [gwk], [gwk])
            yield
            P.op("dve", lambda e, o=gsum[:, 0:8], s=v3(gw[:, 1, :]): e.tensor_reduce(out=o, in_=s, axis=AX.X, op=ALU.add),
                 [gwk], ["gsum"])
            RECIP(P, gsum[:, 8:16], gsum[:, 0:8], ["gsum"], ["gsum"])
            TT(P, "dve", v3(gw[:, 2, :]), v3(gw[:, 1, :]), gsum[:, 8:16].unsqueeze(2).to_broadcast([128, 8, 16]), ALU.mult,
               [gwk, "gsum"], [gwk])
            st.update(x1=x1, x1k=x1k, h2=h2h, h2k=h2hk, idx=idx, idxk=idxk, gw=gw, gwk=gwk)
            yield

        def slots(i, st, bg):
            x1, x1k, h2, h2k, idx, idxk, gw, gwk = (st[k_] for k_ in ("x1", "x1k", "h2", "h2k", "idx", "idxk", "gw", "gwk"))
            a, ak = ab.next()
            (pv0, pv1), pvk = pvb.next()
            LAG = 6
            GSZ = 4
            held = {}
            for s in range(128 + LAG):
                if s < 128:
                    uv, uvk = uvb.next()
                    held[s] = (uv, uvk)
                    P.dma("pool", lambda e, o=uv[:], ix=idx[:, s:s + 1]: e.indirect_dma_start(
                        out=o, out_offset=None, in_=c.UVB,
                        in_offset=bass.IndirectOffsetOnAxis(ap=ix.bitcast(U32), axis=0)), [idxk, "UVB"], [uvk])
                    pd, pdk = prodb.next()
                    TT(P, "dve", pd[:], uv[:, 0:D], h2[:], ALU.mult, [uvk, h2k], [pdk])
                    ACT(P, junk[:], pd[:], AF.Copy, [pdk], [(ak, s)], accum=a[:, 0, s:s + 1])
                    if s % GSZ == GSZ - 1:
                        gs_ = slice(s - GSZ + 1, s + 1)
                        ACT(P, a[:, 1, gs_], a[:, 0, gs_], AF.Gelu, [(ak, s_) for s_ in range(s - GSZ + 1, s + 1)],
                            [(ak, "g", s // GSZ)])
                r_ = s - LAG
                if r_ >= 0:
                    uv, uvk = held.pop(r_)
                    dg, dgk = dgb.next()
                    TS(P, "dve", dg[:], c.identb[:], a[:, 1, r_:r_ + 1], gw[:, 2, r_:r_ + 1], ALU.mult, ALU.mult,
                       ["identb", (ak, "g", r_ // GSZ), gwk], [dgk])
                    MM(P, pv0[:, :], dg[:], uv[:, D:D + 512], r_ == 0, r_ == 127, [dgk, uvk], [(pvk, 0)])
                    MM(P, pv1[:, :], dg[:], uv[:, D + 512:2 * D], r_ == 0, r_ == 127, [dgk, uvk], [(pvk, 1)])
                if bg is not None and s % 2 == 1:
                    next(bg, None)
            if bg is not None:
                for _ in bg:
                    pass
            x2, x2k = x2b.next()
            TT(P, "dve", x2[:, 0:512], x1[:, 0:512], pv0[:, :], ALU.add, [x1k, (pvk, 0)], [(x2k, 0)])
            TT(P, "dve", x2[:, 512:1024], x1[:, 512:1024], pv1[:, :], ALU.add, [x1k, (pvk, 1)], [(x2k, 1)])
            ss2, ss2k = ssb.next()
            ACT(P, junk[:], x2[:], AF.Square, [(x2k, 0), (x2k, 1)], [ss2k], accum=ss2[:, 0:1])
            rstd_chain(P, ss2, ss2k)
            STT(P, x2[:], x2[:], ss2[:, 3:4], gFb[:], ALU.mult, ALU.mult, [(x2k, 0), (x2k, 1), ss2k, "gFb"], [(x2k, 0), (x2k, 1)])
            DMA(P, "sp", c.out[i * 128:(i + 1) * 128, :], x2[:], [(x2k, 0), (x2k, 1)], ["out"])

        states = [dict() for _ in range(NT)]
        for _ in route(0, states[0]):
            pass
        for i in range(NT):
            bg = route(i + 1, states[i + 1]) if i + 1 < NT else None
            slots(i, states[i], bg)
        P.end_phase()
        P.alloc_sems(c.es0, c.sems)
        with nc.Block() as block:
            P.emit(block, c.sems)


def build(upto="F", debug=False):
    nc = bass.Bass("TRN2", target_bir_lowering=False)
    c = Ctx()
    c.nc = nc
    c.P = Prog(nc)
    c.sems = {}
    inp = {}

    def din(name, shape, dt=F32):
        inp[name] = nc.dram_tensor(name, list(shape), dt, kind="ExternalInput").ap()

    din("x", [T, D])
    din("mem", [256, D])
    din("mix_g", [128, 8])
    din("mem_g", [128, 8])
    din("w_in", [D, IN_DIM])
    din("b_merge", [128, 24])
    din("pe_k", [64, 32])
    din("pe_v", [64, 32])
    din("cw_k", [64, 32, 64])
    din("cw_v", [64, 32, 64])
    din("w_mem_kv", [D, 512])
    din("w_sb_br", [384, D])
    din("w_nsa_br", [384, D])
    din("w_mem_br", [256, D])
    din("w_out", [D, D])
    din("ffn_g", [D])
    din("peer_w_q", [D, 2048])
    din("subkT", [128, 16, 128])
    din("peer_uv", [16384, 2 * D])
    din("final_g", [D])
    c.inp = inp
    kind = "ExternalOutput" if debug else "Internal"

    def scr(name, shape, dt):
        return nc.dram_tensor(name, list(shape), dt, kind=kind).ap()

    c.FM = scr("FM", [FM_ROWS, T], BF16)
    c.HT = scr("HT", [D, T], BF16)
    c.TMV = scr("TMV", [T, 640], BF16)
    c.GATES = scr("GATES", [T, 18], F32)
    c.SBOT = scr("SBOT", [384, T], BF16)
    c.NSAOT = scr("NSAOT", [384, T], BF16)
    c.MEMOT = scr("MEMOT", [256, T], BF16)
    c.X1 = scr("X1", [T, D], F32)
    c.UVB = nc.dram_tensor("UVB", [16384, 2 * D], BF16, kind="Internal").ap()
    c.out = nc.dram_tensor("out", [T, D], F32, kind="ExternalOutput").ap()

    with ExitStack() as es0:
        c.es0 = es0
        c.ps = [es0.enter_context(nc.psum_tensor(f"ps{i}", [128, 512], F32)) for i in range(8)]
        c.ident = es0.enter_context(nc.sbuf_tensor("ident", [128, 128], F32))
        c.identb = es0.enter_context(nc.sbuf_tensor("identb", [128, 128], BF16))
        P = c.P
        MEMSET(P, "pool", c.ident[:], 1.0, [], ["ident"])
        ASEL(P, c.ident[:], c.ident[:], [[1, 128]], ALU.is_equal, 0.0, 0, -1, ["ident"], ["ident"])
        CP(P, "pool", c.identb[:], c.ident[:], ["ident"], ["identb"])
        phases = [("A", phase_A), ("B", phase_B), ("C", phase_C), ("D", phase_D), ("E", phase_E), ("F", phase_F)]
        for name, fn in phases:
            fn(c)
            if name == upto:
                break
    return nc


def make_inputs(inputs, b):
    f = lambda a: np.ascontiguousarray(a, dtype=np.float32)
    gcol = lambda g: f(np.asarray(g).reshape(8, 128).T)
    m = {
        "x": f(inputs["x"][b]),
        "mem": f(inputs["mem"][b]),
        "mix_g": gcol(inputs["mix_norm_g"][0]),
        "mem_g": gcol(inputs["mem_norm_g"][0]),
        "w_in": f(inputs["w_in"][0]),
        "b_merge": f(np.asarray(inputs["b_merge"][0]).reshape(24, 128).T),
        "pe_k": f(np.asarray(inputs["cmp_pe_k"][0]).T),
        "pe_v": f(np.asarray(inputs["cmp_pe_v"][0]).T),
        "cw_k": f(np.asarray(inputs["cmp_w_k"][0]).transpose(1, 0, 2)),
        "cw_v": f(np.asarray(inputs["cmp_w_v"][0]).transpose(1, 0, 2)),
        "w_mem_kv": f(inputs["w_mem_kv"][0]),
        "w_sb_br": f(inputs["w_sb_br"][0]),
        "w_nsa_br": f(inputs["w_nsa_br"][0]),
        "w_mem_br": f(inputs["w_mem_br"][0]),
        "w_out": f(inputs["w_out"][0]),
        "ffn_g": f(inputs["ffn_norm_g"][0]),
        "peer_w_q": f(inputs["peer_w_q"][0]),
        "subkT": f(np.asarray(inputs["peer_subkeys"][0]).transpose(3, 0, 1, 2).reshape(128, 16, 128)),
        "peer_uv": np.ascontiguousarray(np.concatenate([np.asarray(inputs["peer_u"][0], dtype=np.float32),
                                                        np.asarray(inputs["peer_v"][0], dtype=np.float32)], axis=1)),
        "final_g": f(inputs["final_norm_g"]),
    }
    return m


def kernel(**inputs):
    nc = build()
    shared = None
    in_maps = []
    for b in range(8):
        m = make_inputs(inputs, b)
        if shared is None:
            shared = m
        else:
            for k in m:
                if k not in ("x", "mem"):
                    m[k] = shared[k]
        in_maps.append(m)
    res = run_bass_kernel_spmd(nc, in_maps, core_ids=list(range(8)))
    return np.stack([np.asarray(r["out"]) for r in res.results], axis=0).astype(np.float32)
```

```python
import sys
import numpy as np
from contextlib import ExitStack
import concourse.bass as bass
import concourse.mybir as mybir
from concourse.bass_utils import run_bass_kernel_spmd

F32 = mybir.dt.float32
BF16 = mybir.dt.bfloat16
I32 = mybir.dt.int32
U32 = mybir.dt.uint32
AF = mybir.ActivationFunctionType
ALU = mybir.AluOpType
AX = mybir.AxisListType

T = 4096
D = 1024
NT = T // 128
IN_DIM = 5650
EPS = 1e-6
SLOPES = [2.0 ** (-8.0 * (h + 1) / 6) for h in range(6)]
BIG = 30000.0

ENGS = ("pe", "act", "dve", "pool", "sp")
DMA_RING = 8


class Prog:
    def __init__(self, nc, same_engine_sync=True):
        self.nc = nc
        self.ops = {e: [] for e in ENGS}
        self.cnt = {e: 0 for e in ENGS}
        self.dma_n = {e: 0 for e in ENGS}
        self.last_w = {}
        self.readers = {}
        self.waited = {}
        self.same_engine_sync = same_engine_sync
        self.fill_vals = set()
        self.fill_regs = {}

    def _deps(self, eng, reads, writes):
        need = {}

        def add(tok):
            if tok is None:
                return
            sk, val, teng = tok
            if teng == eng and sk[0] == "c":
                if not self.same_engine_sync or eng == "pe":
                    return
            if need.get(sk, 0) < val:
                need[sk] = val

        for r in reads:
            add(self.last_w.get(r))
        for w in writes:
            add(self.last_w.get(w))
            for t in self.readers.get(w, ()):
                add(t)
        out = []
        for sk, val in need.items():
            if self.waited.get((eng, sk), 0) >= val:
                continue
            self.waited[(eng, sk)] = val
            out.append((sk, val))
        return out

    def _commit(self, tok, reads, writes):
        for r in reads:
            self.readers.setdefault(r, []).append(tok)
        for w in writes:
            self.last_w[w] = tok
            self.readers[w] = []

    def op(self, eng, fn, reads=(), writes=()):
        reads = tuple(reads)
        writes = tuple(writes)
        waits = self._deps(eng, reads, writes)
        self.cnt[eng] += 1
        tok = (("c", eng), self.cnt[eng], eng)
        fr = sys._getframe(1)
        self.ops[eng].append(dict(fn=fn, waits=waits, inc=(("c", eng), 1),
                                  where=(fr.f_lineno, fr.f_back.f_lineno if fr.f_back else 0)))
        self._commit(tok, reads, writes)
        return tok

    def dma(self, eng, fn, reads=(), writes=()):
        reads = tuple(reads)
        writes = tuple(writes)
        n = self.dma_n[eng]
        self.dma_n[eng] += 1
        sk = ("d", eng, n % DMA_RING)
        val = 16 * (n // DMA_RING + 1)
        waits = self._deps(eng, reads, writes)
        if val > 16 and self.waited.get((eng, sk), 0) < val - 16:
            self.waited[(eng, sk)] = val - 16
            waits.append((sk, val - 16))
        tok = (sk, val, eng)
        self.ops[eng].append(dict(fn=fn, waits=waits, inc=(sk, 16)))
        self._commit(tok, reads, writes)
        return tok

    def finish(self, eng, toks):
        self.ops[eng].append(dict(fn=None, waits=[(sk, val) for sk, val, _ in toks], inc=None))

    def end_phase(self):
        targets = []
        for e in ENGS:
            if self.cnt[e] > 0:
                targets.append((("c", e), self.cnt[e]))
            n = self.dma_n[e]
            for r in range(min(n, DMA_RING)):
                last = ((n - 1 - r) // DMA_RING) * DMA_RING + r
                targets.append((("d", e, r), 16 * (last // DMA_RING + 1)))
        for e in ENGS:
            waits = []
            for sk, val in targets:
                if self.waited.get((e, sk), 0) >= val:
                    continue
                self.waited[(e, sk)] = val
                waits.append((sk, val))
            self.ops[e].append(dict(fn=None, waits=waits, inc=None))
        self.last_w = {}
        self.readers = {}

    def sem_keys(self):
        keys = set()
        for e in ENGS:
            for o in self.ops[e]:
                if o["inc"]:
                    keys.add(o["inc"][0])
                for sk, _ in o["waits"]:
                    keys.add(sk)
        return sorted(keys)

    def alloc_sems(self, es, sems):
        for k in self.sem_keys():
            if k not in sems:
                sems[k] = es.enter_context(self.nc.semaphore("s_" + "_".join(map(str, k))))

    def emit(self, block, sems):
        engobj = {"pe": "tensor", "act": "scalar", "dve": "vector", "pool": "gpsimd", "sp": "sync"}

        def make(e):
            ops = self.ops[e]

            def body(eng):
                if e == "pool":
                    self.fill_regs = {v: eng.to_reg(v) for v in sorted(self.fill_vals)}
                for o in ops:
                    for sk, val in o["waits"]:
                        eng.wait_ge(sems[sk], val)
                    if o["fn"] is not None:
                        try:
                            ins = o["fn"](eng)
                        except Exception:
                            print("EMIT FAILED at lines", o.get("where"))
                            raise
                        if o["inc"]:
                            ins.then_inc(sems[o["inc"][0]], o["inc"][1])
            return body

        for e in ENGS:
            if self.ops[e]:
                getattr(block, engobj[e])(make(e))
        self.ops = {e: [] for e in ENGS}


class Ctx:
    pass


class Rot:
    def __init__(self, tiles, name):
        self.tiles = tiles
        self.name = name
        self.i = 0

    def next(self):
        j = self.i % len(self.tiles)
        self.i += 1
        return self.tiles[j], (self.name, j)


def MM(P, out, lhsT, rhs, start, stop, r, w, sgc=False):
    return P.op("pe", lambda e: e.matmul(out, lhsT=lhsT, rhs=rhs, start=start, stop=stop, skip_group_check=sgc), r, w)


def TR(P, out, in_, ident, r, w):
    return P.op("pe", lambda e: e.transpose(out, in_, ident), r, w)


def ACT(P, out, in_, func, r, w, scale=None, bias=None, accum=None):
    kw = {}
    if scale is not None:
        kw["scale"] = scale
    if bias is not None:
        kw["bias"] = bias
    if accum is not None:
        kw["accum_out"] = accum
    return P.op("act", lambda e: e.activation(out=out, in_=in_, func=func, **kw), r, w)


def TS(P, eng, out, in0, s1, s2, op0, op1, r, w):
    if op1 is None:
        return P.op(eng, lambda e: e.tensor_scalar(out=out, in0=in0, scalar1=s1, scalar2=None, op0=op0), r, w)
    return P.op(eng, lambda e: e.tensor_scalar(out=out, in0=in0, scalar1=s1, scalar2=s2, op0=op0, op1=op1), r, w)


def TT(P, eng, out, in0, in1, op, r, w):
    return P.op(eng, lambda e: e.tensor_tensor(out=out, in0=in0, in1=in1, op=op), r, w)


def STT(P, out, in0, scalar, in1, op0, op1, r, w):
    return P.op("dve", lambda e: e.scalar_tensor_tensor(out=out, in0=in0, scalar=scalar, in1=in1, op0=op0, op1=op1), r, w)


def CP(P, eng, out, in_, r, w):
    if eng == "act":
        return P.op("act", lambda e: e.copy(out=out, in_=in_), r, w)
    return P.op(eng, lambda e: e.tensor_copy(out=out, in_=in_), r, w)


def DMA(P, eng, out, in_, r, w):
    return P.dma(eng, lambda e: e.dma_start(out=out, in_=in_), r, w)


def MEMSET(P, eng, ap, val, r, w):
    return P.op(eng, lambda e: e.memset(ap, val), r, w)


def ASEL(P, out, in_, pattern, cmp, fill, base, cm, r, w):
    P.fill_vals.add(float(fill))
    return P.op("pool", lambda e: e.affine_select(out=out, in_=in_, pattern=pattern, compare_op=cmp,
                                                  fill=P.fill_regs[float(fill)], base=base, channel_multiplier=cm), r, w)


def IOTA(P, out, pattern, base, cm, r, w):
    return P.op("pool", lambda e: e.iota(out, pattern=pattern, base=base, channel_multiplier=cm), r, w)


def RECIP(P, out, in_, r, w):
    return P.op("dve", lambda e: e.reciprocal(out=out, in_=in_), r, w)


def rstd_chain(P, ss, key):
    TS(P, "dve", ss[:, 1:2], ss[:, 0:1], 1.0 / D, EPS, ALU.mult, ALU.add, [key], [key])
    P.op("act", lambda e: e.sqrt(out=ss[:, 2:3], in_=ss[:, 1:2]), [key], [key])
    RECIP(P, ss[:, 3:4], ss[:, 2:3], [key], [key])


FM_QSB, FM_KSB, FM_QNSA, FM_KCMP, FM_VCMP, FM_KSLC, FM_KWIN, FM_QMEM = 0, 384, 768, 1152, 1280, 1408, 1536, 1664
FM_ROWS = 1920
FM_COLMAP = [(0, 0, 768), (768, 1152, 384), (1152, 1536, 128), (1280, 1664, 128), (1408, 1792, 128),
             (1536, 2048, 128), (1664, 2322, 256)]
TM_COLMAP = [(0, 768, 384), (384, 1920, 128), (512, 2176, 128), (640, 2304, 18)]
TM_COLS = 658
Q_CHUNKS = {0, 1, 2, 6, 7, 8, 13, 14}


def phase_A(c):
    nc, P = c.nc, c.P
    with ExitStack() as es:
        sb = lambda name, shape, dt: es.enter_context(nc.sbuf_tensor(name, shape, dt))
        Wfm = sb("Wfm", [128, 8, FM_ROWS], BF16)
        Wtm = sb("Wtm", [128, 8, TM_COLS], BF16)
        wst = Rot([sb(f"wst{i}", [128, 2578], F32) for i in range(2)], "wst")
        gcol = sb("gcolA", [128, 8], F32)
        xbuf = Rot([sb(f"xt{i}", [128, D], F32) for i in range(2)], "xt")
        xsbuf = Rot([sb(f"xs{i}", [128, D], F32) for i in range(2)], "xs")
        junk = sb("junkA", [128, D], F32)
        ssbuf = Rot([sb(f"ss{i}", [128, 4], F32) for i in range(4)], "ss")
        hTg = Rot([sb(f"hTg{i}", [128, 8, 512], BF16) for i in range(2)], "hTg")
        FMst = Rot([sb(f"FMst{i}", [128, 15, 512], BF16) for i in range(2)], "FMst")
        TMst = Rot([sb(f"TMst{i}", [128, 640], BF16) for i in range(3)], "TMst")
        gst = Rot([sb(f"gst{i}", [128, 18], F32) for i in range(3)], "gst")
        pstr = Rot(c.ps[0:2], "ps_tr")
        psfm = Rot(c.ps[2:5], "ps_fm")
        pstm = Rot(c.ps[5:8], "ps_tm")

        DMA(P, "sp", gcol[:], c.inp["mix_g"], [], ["gcol"])
        for kc in range(8):
            st, sk = wst.next()
            DMA(P, "sp", st[:], c.inp["w_in"][kc * 128:(kc + 1) * 128, 0:2578], [], [sk])
            n = 0
            for (dst, cm) in ((Wfm, FM_COLMAP), (Wtm, TM_COLMAP)):
                for (dc, sc, w) in cm:
                    if n % 2 == 0:
                        TS(P, "dve", dst[:, kc, dc:dc + w], st[:, sc:sc + w], gcol[:, kc:kc + 1], None, ALU.mult, None,
                           [sk, "gcol"], [("W", kc)])
                    else:
                        ACT(P, dst[:, kc, dc:dc + w], st[:, sc:sc + w], AF.Copy, [sk, "gcol"], [("W", kc)],
                            scale=gcol[:, kc:kc + 1])
                    n += 1
        Wkeys = [("W", kc) for kc in range(8)]

        for tg in range(8):
            hT, hk = hTg.next()
            hpieces = [(hk, s, half) for s in range(4) for half in range(2)]
            for s in range(4):
                i = tg * 4 + s
                xt, xk = xbuf.next()
                DMA(P, "sp", xt[:], c.inp["x"][i * 128:(i + 1) * 128, :], [], [xk])
                ss, ssk = ssbuf.next()
                ACT(P, junk[:], xt[:], AF.Square, [xk], ["junkA", ssk], accum=ss[:, 0:1])
                rstd_chain(P, ss, ssk)
                xs, xsk = xsbuf.next()
                TS(P, "dve", xs[:], xt[:], ss[:, 3:4], None, ALU.mult, None, [xk, ssk], [xsk])
                for half in range(2):
                    pt, ptk = pstr.next()
                    for j in range(4):
                        cc = half * 4 + j
                        TR(P, pt[:, j * 128:(j + 1) * 128], xs[:, cc * 128:(cc + 1) * 128], c.ident[:], [xsk, "ident"], [ptk])
                    dst = hT[:, half * 4:(half + 1) * 4, s * 128:(s + 1) * 128]
                    src = pt[:].rearrange("p (j t) -> p j t", j=4)
                    CP(P, "act" if half == 0 else "dve", dst, src, [ptk], [(hk, s, half)])
            fst, fsk = FMst.next()
            for ch in range(15):
                pf, pfk = psfm.next()
                for kc in range(8):
                    MM(P, pf[:, :], Wfm[:, kc, ch * 128:(ch + 1) * 128], hT[:, kc, :], kc == 0, kc == 7,
                       hpieces + [("W", kc)], [pfk])
                sc = 0.125 if ch in Q_CHUNKS else 1.0
                if ch % 2 == 0:
                    ACT(P, fst[:, ch, :], pf[:, :], AF.Copy, [pfk], [(fsk, ch)], scale=sc)
                else:
                    TS(P, "dve", fst[:, ch, :], pf[:, :], sc, None, ALU.mult, None, [pfk], [(fsk, ch)])
            DMA(P, "sp", c.FM.rearrange("(c p) t -> p c t", p=128)[:, :, tg * 512:(tg + 1) * 512], fst[:],
                [(fsk, ch) for ch in range(15)], ["FM"])
            DMA(P, "sp", c.HT.rearrange("(c p) t -> p c t", p=128)[:, :, tg * 512:(tg + 1) * 512], hT[:],
                hpieces, ["HT"])
            for s in range(4):
                i = tg * 4 + s
                pa, pak = pstm.next()
                for kc in range(8):
                    MM(P, pa[:, 0:512], hT[:, kc, s * 128:(s + 1) * 128], Wtm[:, kc, 0:512], kc == 0, kc == 7,
                       hpieces + [("W", kc)], [pak])
                tst, tsk = TMst.next()
                CP(P, "dve", tst[:, 0:512], pa[:, 0:512], [pak], [tsk])
                pb, pbk = pstm.next()
                for kc in range(8):
                    MM(P, pb[:, 0:146], hT[:, kc, s * 128:(s + 1) * 128], Wtm[:, kc, 512:658], kc == 0, kc == 7,
                       hpieces + [("W", kc)], [pbk])
                CP(P, "dve", tst[:, 512:640], pb[:, 0:128], [pbk], [tsk])
                gs, gsk = gst.next()
                ACT(P, gs[:], pb[:, 128:146], AF.Sigmoid, [pbk], [gsk])
                DMA(P, "sp", c.TMV[i * 128:(i + 1) * 128, :], tst[:], [tsk], ["TMV"])
                DMA(P, "sp", c.GATES[i * 128:(i + 1) * 128, :], gs[:], [gsk], ["GATES"])
        P.end_phase()
        P.alloc_sems(c.es0, c.sems)
        with nc.Block() as block:
            P.emit(block, c.sems)


def phase_B(c):
    nc, P = c.nc, c.P
    with ExitStack() as es:
        sb = lambda name, shape, dt: es.enter_context(nc.sbuf_tensor(name, shape, dt))
        qT = [sb(f"qTb{j}", [128, T], BF16) for j in range(3)]
        kT = [sb(f"kTb{j}", [128, T], BF16) for j in range(3)]
        V = sb("Vsb", [128, NT, 384], BF16)
        ntri = sb("ntri", [128, 128], BF16)
        nones = sb("nones", [128, 128], BF16)
        Ebuf = Rot([sb(f"E{i}", [128, 512], F32) for i in range(3)], "E")
        SPbuf = Rot([sb(f"SP{i}", [128, 512], BF16) for i in range(4)], "SP")
        Sbuf = Rot([sb(f"Ssum{i}", [128, 512], BF16) for i in range(3)], "Ssum")
        abuf = Rot([sb(f"aT{i}", [128, 512], BF16) for i in range(3)], "aT")
        obuf = Rot([sb(f"sbo{i}", [64, 512], BF16) for i in range(2)], "sbo")
        psA = Rot(c.ps[0:5], "psA")
        psO = Rot(c.ps[5:7], "psO")
        pswarm = c.ps[7]
        for j in range(3):
            DMA(P, "sp", qT[j][:], c.FM[FM_QSB + 128 * j:FM_QSB + 128 * (j + 1), :], ["FM"], [("qT", j)])
            DMA(P, "sp", kT[j][:], c.FM[FM_KSB + 128 * j:FM_KSB + 128 * (j + 1), :], ["FM"], [("kT", j)])
        DMA(P, "sp", V[:], c.TMV.rearrange("(c p) f -> p c f", p=128)[:, :, 0:384], ["TMV"], ["V"])
        MEMSET(P, "pool", nones[:], -1.0, [], ["nones"])
        MEMSET(P, "pool", ntri[:], -1.0, [], ["ntri"])
        ASEL(P, ntri[:], ntri[:], [[-1, 128]], ALU.is_ge, 0.0, 0, 1, ["ntri"], ["ntri"])
        uvst = Rot([sb(f"uvst{i}", [128, 2 * D], BF16) for i in range(4)], "uvst")

        def convert_chunk(ch):
            t_, tk = uvst.next()
            P.dma("pool", lambda e, o=t_[:], i_=c.inp["peer_uv"][ch * 128:(ch + 1) * 128, :]: e.dma_start(out=o, in_=i_), [], [tk])
            DMA(P, "sp", c.UVB[ch * 128:(ch + 1) * 128, :], t_[:], [tk], ["UVB"])
        steps = []
        for h in range(6):
            for g in range(8):
                nch = 4 * g + 4
                for idx_, cch in enumerate(range(nch - 1, -1, -1)):
                    steps.append(dict(h=h, g=g, cch=cch, first=idx_ == 0, last=cch == 0))
        N = len(steps)
        cur = dict(po=None, pok=None, ssum=None, ssumk=None)

        def S12(st):
            h, g, cch = st["h"], st["g"], st["cch"]
            j, half = h // 2, h % 2
            pr = slice(64 * half, 64 * half + 64)
            st["qs"] = qT[j][pr, g * 512:(g + 1) * 512]
            st["ks"] = kT[j][pr, cch * 128:(cch + 1) * 128]
            st["rk"] = [("kT", j), ("qT", j)]
            st["m"] = cch - 4 * g
            if st["first"]:
                cur["po"], cur["pok"] = psO.next()
                cur["ssum"] = cur["ssumk"] = None
            st["po"], st["pok"] = cur["po"], cur["pok"]
            pa, pak = psA.next()
            MM(P, pa[:, :], st["ks"], st["qs"], True, False, st["rk"], [pak])
            st["pa"], st["pak"] = pa, pak
            E, Ek = Ebuf.next()
            ACT(P, E[:], pa[:, :], AF.Exp, [pak], [Ek])
            SP, SPk = SPbuf.next()
            ACT(P, SP[:], E[:], AF.Ln, [Ek], [SPk], bias=1.0)
            if st["m"] >= 0:
                ASEL(P, SP[:], SP[:], [[1, 512]], ALU.is_gt, 0.0, -128 * st["m"], -1, [SPk], [SPk])
            st["SP"], st["SPk"] = SP, SPk
            st["ssum_prev"], st["ssum_prevk"] = cur["ssum"], cur["ssumk"]
            if not st["last"]:
                if st["first"]:
                    cur["ssum"], cur["ssumk"] = SP, SPk
                else:
                    sn, snk = Sbuf.next()
                    TT(P, "pool", sn[:], cur["ssum"][:], SP[:], ALU.add, [cur["ssumk"], SPk], [snk])
                    cur["ssum"], cur["ssumk"] = sn, snk

        def S34(st):
            pb, pbk = st["pa"], st["pak"]
            MM(P, pb[:, :], ntri[:], st["SP"][:], False, st["first"], ["ntri", st["SPk"]], [pbk])
            if not st["first"]:
                MM(P, pb[:, :], nones[:], st["ssum_prev"][:], False, True, ["nones", st["ssum_prevk"]], [pbk])
            aT, aTk = abuf.next()
            ACT(P, aT[:], pb[:, :], AF.Exp, [pbk], [aTk])
            if st["m"] >= 0:
                ASEL(P, aT[:], aT[:], [[1, 512]], ALU.is_gt, 0.0, -128 * st["m"], -1, [aTk], [aTk])
            st["aT"], st["aTk"] = aT, aTk

        NWARM = 2

        def S5(st):
            h, g, cch = st["h"], st["g"], st["cch"]
            for _ in range(NWARM):
                MM(P, pswarm[:, :], ntri[:], qT[0][:, 0:512], True, True, ["ntri", ("qT", 0)], ["pswarm"])
            MM(P, st["po"][0:64, :], V[:, cch, 64 * h:64 * h + 64], st["aT"][:], st["first"], st["last"], ["V", st["aTk"]], [st["pok"]])
            if st["last"]:
                ob, obk = obuf.next()
                CP(P, "dve", ob[:], st["po"][0:64, :], [st["pok"]], [obk])
                DMA(P, "sp", c.SBOT[64 * h:64 * h + 64, g * 512:(g + 1) * 512], ob[:], [obk], ["SBOT"])

        for n in range(N + 2):
            if n % 6 == 0 and n // 6 < 128:
                convert_chunk(n // 6)
            if n < N:
                S12(steps[n])
            if 0 <= n - 1 < N:
                S34(steps[n - 1])
            if 0 <= n - 2 < N:
                S5(steps[n - 2])
                steps[n - 2].clear()
        P.end_phase()
        P.alloc_sems(c.es0, c.sems)
        with nc.Block() as block:
            P.emit(block, c.sems)


def phase_C(c):
    nc, P = c.nc, c.P
    with ExitStack() as es:
        sb = lambda name, shape, dt: es.enter_context(nc.sbuf_tensor(name, shape, dt))
        qaug = [sb(f"qaug{h}", [128, T], BF16) for h in range(6)]
        kslc = [sb(f"kslc{g}", [128, T], BF16) for g in range(2)]
        kwin = [sb(f"kwin{g}", [64, T], BF16) for g in range(2)]
        kcmp = [sb(f"kcmp{g}", [64, T], BF16) for g in range(2)]
        vcmp = [sb(f"vcmp{g}", [64, T], BF16) for g in range(2)]
        kcT = [sb(f"kcT{g}", [64, 256], BF16) for g in range(2)]
        vcaug = [sb(f"vcaug{g}", [128, 2, 129], BF16) for g in range(2)]
        vwin = sb("vwin", [128, NT, 2, 65], BF16)
        vslc = sb("vslc", [128, NT, 2, 65], BF16)
        vstage = sb("vstage", [128, NT, 256], BF16)
        gates = sb("gatesC", [128, NT, 18], F32)
        pe = [sb("pek_sb", [64, 32], F32), sb("pev_sb", [64, 32], F32)]
        cwst = sb("cwst", [64, 2048], F32)
        cw = [sb("cwk", [64, 32, 64], BF16), sb("cwv", [64, 32, 64], BF16)]
        tmpb = Rot([sb(f"ctmp{i}", [64, 256], BF16) for i in range(3)], "ctmp")
        itmp = sb("itmp", [128, 64], I32)
        ftmp = sb("ftmp", [128, 64], F32)
        bias_sel = sb("bias_sel", [128, 6], F32)
        bias_win = sb("bias_win", [128, 6, 5], F32)
        bias_cmp = sb("bias_cmp", [128, 6, 64], F32)
        IOTi = sb("IOTi", [128, 128], I32)
        IOT = sb("IOT", [128, 128], F32)
        e32b = Rot([sb(f"e32_{i}", [128, 128], F32) for i in range(4)], "e32")
        ebb = Rot([sb(f"eb{i}", [128, 128], BF16) for i in range(7)], "eb")
        obuf = Rot([sb(f"oC{i}", [128, 384], F32) for i in range(8)], "oC")
        ewb = Rot([sb(f"ew{i}", [128, 512], BF16) for i in range(5)], "ew")
        otiles = {}
        impb = Rot([sb(f"imp{i}", [128, 64], F32) for i in range(2)], "imp")
        wb = Rot([sb(f"wC{i}", [128, 4], F32) for i in range(4)], "wC")
        m8b = Rot([sb(f"m8_{i}", [128, 16], F32) for i in range(2)], "m8")
        repb = Rot([sb(f"rep{i}", [128, 64], F32) for i in range(2)], "rep")
        selb = Rot([sb(f"selp{i}", [128, 128], F32) for i in range(4)], "selp")
        Ctb = Rot([sb(f"Ct{i}", [128, 128], F32) for i in range(2)], "Ct")
        ostb = Rot([sb(f"ostC{i}", [128, 3, 128], BF16) for i in range(2)], "ostC")
        pss = Rot(c.ps[0:3], "pss")
        psacc = Rot(c.ps[3:6], "psacc")
        pstr = Rot(c.ps[6:8], "pstrC")
        pk = c.ps[6]

        for h in range(6):
            DMA(P, "sp", qaug[h][0:64, :], c.FM[FM_QNSA + 64 * h:FM_QNSA + 64 * (h + 1), :], ["FM"], [("q", h)])
        for g in range(2):
            DMA(P, "sp", kslc[g][0:64, :], c.FM[FM_KSLC + 64 * g:FM_KSLC + 64 * (g + 1), :], ["FM"], [("kslc", g)])
            DMA(P, "sp", kwin[g][:], c.FM[FM_KWIN + 64 * g:FM_KWIN + 64 * (g + 1), :], ["FM"], [("kwin", g)])
            DMA(P, "sp", kcmp[g][:], c.FM[FM_KCMP + 64 * g:FM_KCMP + 64 * (g + 1), :], ["FM"], [("kcmp", g)])
            DMA(P, "sp", vcmp[g][:], c.FM[FM_VCMP + 64 * g:FM_VCMP + 64 * (g + 1), :], ["FM"], [("vcmp", g)])
        DMA(P, "sp", vstage[:], c.TMV.rearrange("(c p) f -> p c f", p=128)[:, :, 384:640], ["TMV"], ["vstage"])
        DMA(P, "sp", gates[:], c.GATES.rearrange("(c p) f -> p c f", p=128), ["GATES"], ["gates"])
        DMA(P, "sp", pe[0][:], c.inp["pe_k"], [], ["pe0"])
        DMA(P, "sp", pe[1][:], c.inp["pe_v"], [], ["pe1"])
        for kv, nm in ((0, "cw_k"), (1, "cw_v")):
            DMA(P, "sp", cwst[:], c.inp[nm].rearrange("d l e -> d (l e)"), [], ["cwst"])
            CP(P, "dve", cw[kv][:].rearrange("d l e -> d (l e)"), cwst[:], ["cwst"], [("cw", kv)])
        CP(P, "dve", vslc[:, :, :, 0:64], vstage[:, :, 0:128].rearrange("p c (g d) -> p c g d", g=2), ["vstage"], ["vslc"])
        CP(P, "pool", vwin[:, :, :, 0:64], vstage[:, :, 128:256].rearrange("p c (g d) -> p c g d", g=2), ["vstage"], ["vwin"])
        MEMSET(P, "dve", vslc[:, :, :, 64:65], 1.0, ["vslc"], ["vslc"])
        MEMSET(P, "pool", vwin[:, :, :, 64:65], 1.0, ["vwin"], ["vwin"])
        for g in range(2):
            MEMSET(P, "pool", kslc[g][64:128, :], 1.0, [], [("kx", g)])
            ASEL(P, kslc[g][64:128, :], kslc[g][64:128, :], [[1, T]], ALU.is_ge, 0.0, 0, -64, [("kx", g)], [("kx", g)])
            ASEL(P, kslc[g][64:128, :], kslc[g][64:128, :], [[-1, T]], ALU.is_ge, 0.0, 63, 64, [("kx", g)], [("kx", g)])
        IOTA(P, itmp[0:64, 0:1], [[0, 1]], 0, 1, [], ["itmp"])
        IOTA(P, itmp[64:128, 0:1], [[0, 1]], 0, 1, ["itmp"], ["itmp"])
        CP(P, "dve", ftmp[:, 0:1], itmp[:, 0:1], ["itmp"], ["ftmp"])
        for h in range(6):
            TS(P, "dve", bias_sel[:, h:h + 1], ftmp[:, 0:1], SLOPES[h], None, ALU.mult, None, ["ftmp"], ["bias_sel"])
        IOTA(P, itmp[:, 0:5], [[128, 5]], -512, 1, ["itmp", "ftmp"], ["itmp"])
        CP(P, "dve", ftmp[:, 0:5], itmp[:, 0:5], ["itmp"], ["ftmp"])
        for h in range(6):
            TS(P, "dve", bias_win[:, h, :], ftmp[:, 0:5], SLOPES[h], None, ALU.mult, None, ["ftmp"], ["bias_win"])
        IOTA(P, itmp[:, 0:64], [[2048, 2], [-128, 32]], 31, 16, ["itmp", "ftmp"], ["itmp"])
        CP(P, "dve", ftmp[:, 0:64], itmp[:, 0:64], ["itmp"], ["ftmp"])
        for h in range(6):
            TS(P, "dve", bias_cmp[:, h, :], ftmp[:, 0:64], SLOPES[h], None, ALU.mult, None, ["ftmp"], ["bias_cmp"])
        IOTA(P, IOTi[64:128, :], [[-1, 128]], 0, 64, [], ["IOTi"])
        CP(P, "dve", IOT[64:128, :], IOTi[64:128, :], ["IOTi"], ["IOT"])
        for g in range(2):
            MEMSET(P, "pool", vcaug[g][:], 1.0, [], [("vcaug", g)])
            for ci in range(2):
                ASEL(P, vcaug[g][:, ci, 65:129], vcaug[g][:, ci, 65:129], [[-4, 64]], ALU.is_ge, 0.0, 128 * ci, 1,
                     [("vcaug", g)], [("vcaug", g)])
                ASEL(P, vcaug[g][:, ci, 65:129], vcaug[g][:, ci, 65:129], [[4, 64]], ALU.is_ge, 0.0, 3 - 128 * ci, -1,
                     [("vcaug", g)], [("vcaug", g)])
        for s_ in selb.tiles:
            pass
        for j in range(4):
            MEMSET(P, "dve", selb.tiles[j][:, 0:64], 0.0, [], [("selp", j)])
        for g in range(2):
            kview = kcmp[g][:].rearrange("p (n s) -> p n s", s=16)
            vview = vcmp[g][:].rearrange("p (n s) -> p n s", s=16)
            for l in range(32):
                tmp, tk = tmpb.next()
                TS(P, "dve" if l % 2 == 0 else "pool", tmp[:, 0:255], kview[:, l // 16:l // 16 + 255, l % 16],
                   pe[0][:, l:l + 1], None, ALU.add, None, [("kcmp", g), "pe0"], [tk])
                MM(P, pk[0:64, 0:255], cw[0][:, l, :], tmp[:, 0:255], l == 0, l == 31, [("cw", 0), tk], [("pstrC", 0)])
            CP(P, "dve", kcT[g][:, 0:255], pk[0:64, 0:255], [("pstrC", 0)], [("kcT", g)])
            for ci in range(2):
                rows = 128 if ci == 0 else 127
                for l in range(32):
                    tmp, tk = tmpb.next()
                    n0 = l // 16 + ci * 128
                    TS(P, "dve" if l % 2 == 0 else "pool", tmp[:, 0:rows], vview[:, n0:n0 + rows, l % 16],
                       pe[1][:, l:l + 1], None, ALU.add, None, [("vcmp", g), "pe1"], [tk])
                    MM(P, pk[0:rows, 256:320], tmp[:, 0:rows], cw[1][:, l, :], l == 0, l == 31, [("cw", 1), tk], [("pstrC", 0)])
                CP(P, "dve", vcaug[g][0:rows, ci, 0:64], pk[0:rows, 256:320], [("pstrC", 0)], [("vcaug", g)])

        def consume(pacc, pacck, o, ok, h, i, gcol, first):
            w, wk = wb.next()
            TS(P, "dve", w[:, 0:1], pacc[:, 64:65], 1e-30, None, ALU.max, None, [pacck], [wk])
            RECIP(P, w[:, 1:2], w[:, 0:1], [wk], [wk])
            TT(P, "dve", w[:, 2:3], w[:, 1:2], gates[:, i, gcol:gcol + 1], ALU.mult, [wk, "gates"], [wk])
            if first:
                TS(P, "dve", o[:, 64 * h:64 * h + 64], pacc[:, 0:64], w[:, 2:3], None, ALU.mult, None, [pacck, wk], [(ok, h)])
            else:
                STT(P, o[:, 64 * h:64 * h + 64], pacc[:, 0:64], w[:, 2:3], o[:, 64 * h:64 * h + 64], ALU.mult, ALU.add,
                    [pacck, wk, (ok, h)], [(ok, h)])
            return w, wk

        from collections import deque
        fifo = deque()
        LAGC = 3

        def defer(fn):
            fifo.append(fn)
            while len(fifo) > LAGC:
                fifo.popleft()()

        def flush():
            while fifo:
                fifo.popleft()()

        def consume_cmp(pc, pck, o, ok, h, i, hh, imp, impk):
            w, wk = consume(pc, pck, o, ok, h, i, 3 * h + 0, True)
            if hh == 0:
                TS(P, "dve", imp[:], pc[:, 65:129], w[:, 1:2], None, ALU.mult, None, [pck, wk], [impk])
            else:
                STT(P, imp[:], pc[:, 65:129], w[:, 1:2], imp[:], ALU.mult, ALU.add, [pck, wk, impk], [impk])

        for i in range(NT):
            t0 = 128 * i
            qc = slice(t0, t0 + 128)
            o, ok = obuf.next()
            for g in range(2):
                imp, impk = impb.next()
                for hh in range(3):
                    h = 3 * g + hh
                    nvalid = min(255, (t0 + 96) // 16 + 1)
                    chunks = [(0, min(128, nvalid))] + ([(1, nvalid - 128)] if nvalid > 128 else [])
                    pc, pck = psacc.next()
                    for ni, (ci, rows) in enumerate(chunks):
                        ps_, psk = pss.next()
                        MM(P, ps_[0:rows, 0:128], kcT[g][:, ci * 128:ci * 128 + rows], qaug[h][0:64, qc], True, True,
                           [("kcT", g), ("q", h)], [psk])
                        e32, e32k = e32b.next()
                        ACT(P, e32[0:rows, :], ps_[0:rows, 0:128], AF.Exp, [psk, "bias_cmp"], [e32k],
                            bias=bias_cmp[0:rows, h, ci * 32 + i:ci * 32 + i + 1])
                        eb, ebk = ebb.next()
                        ASEL(P, eb[0:rows, :], e32[0:rows, :], [[1, 128]], ALU.is_ge, 0.0, t0 - 2048 * ci - 31, -16,
                             [e32k], [ebk])
                        defer(lambda pc=pc, pck=pck, eb=eb, ebk=ebk, rows=rows, ci=ci, g=g, st_=(ni == 0), sp_=(ni == len(chunks) - 1):
                              MM(P, pc[:, 0:129], eb[0:rows, :], vcaug[g][0:rows, ci, :], st_, sp_, [ebk, ("vcaug", g)], [pck]))
                    defer(lambda pc=pc, pck=pck, o=o, ok=ok, h=h, i=i, hh=hh, imp=imp, impk=impk:
                          consume_cmp(pc, pck, o, ok, h, i, hh, imp, impk))
                    pw, pwk = psacc.next()
                    cl = list(range(max(0, i - 4), i + 1))
                    for ni, cch in enumerate(cl):
                        dc = cch - i
                        ps_, psk = pss.next()
                        MM(P, ps_[:, 0:128], kwin[g][:, cch * 128:(cch + 1) * 128], qaug[h][0:64, qc], True, True,
                           [("kwin", g), ("q", h)], [psk])
                        eb, ebk = ebb.next()
                        bw = bias_win[:, h, dc + 4:dc + 5]
                        if dc == 0 or dc == -4:
                            e32, e32k = e32b.next()
                            ACT(P, e32[:], ps_[:, 0:128], AF.Exp, [psk, "bias_win"], [e32k], bias=bw)
                            if dc == 0:
                                ASEL(P, eb[:], e32[:], [[1, 128]], ALU.is_ge, 0.0, 0, -1, [e32k], [ebk])
                            else:
                                ASEL(P, eb[:], e32[:], [[-1, 128]], ALU.is_gt, 0.0, 0, 1, [e32k], [ebk])
                        else:
                            ACT(P, eb[:], ps_[:, 0:128], AF.Exp, [psk, "bias_win"], [ebk], bias=bw)
                        defer(lambda pw=pw, pwk=pwk, eb=eb, ebk=ebk, cch=cch, g=g, st_=(ni == 0), sp_=(ni == len(cl) - 1):
                              MM(P, pw[:, 0:65], eb[:], vwin[:, cch, g, :], st_, sp_, [ebk, "vwin"], [pwk]))
                    defer(lambda pw=pw, pwk=pwk, o=o, ok=ok, h=h, i=i: consume(pw, pwk, o, ok, h, i, 3 * h + 2, False))
                flush()
                ASEL(P, imp[:], imp[:], [[-64, 64]], ALU.is_ge, 1e4, t0 - 128, 1, [impk], [impk])
                ASEL(P, imp[:], imp[:], [[-64, 64]], ALU.is_ge, -1.0, t0, 1, [impk], [impk])
                MEMSET(P, "pool", imp[:, 0:1], 1e4, [impk], [impk])
                m8, m8k = m8b.next()
                rep, repk = repb.next()
                P.op("dve", lambda e, m8=m8, imp=imp: e.max(out=m8[:, 0:8], in_=imp[:]), [impk], [m8k])
                P.op("dve", lambda e, m8=m8, imp=imp, rep=rep: e.match_replace(out=rep[:], in_to_replace=m8[:, 0:8],
                                                                                 in_values=imp[:], imm_value=-1e30),
                     [impk, m8k], [repk])
                P.op("dve", lambda e, m8=m8, rep=rep: e.max(out=m8[:, 8:16], in_=rep[:]), [repk, m8k], [m8k])
                selp, selk = selb.next()
                TS(P, "dve", selp[:, 64:128], imp[:], m8[:, 15:16], None, ALU.is_ge, None, [impk, m8k], [selk])

                def sel_tail(selp=selp, selk=selk, g=g, i=i, t0=t0, qc=qc):
                    pt_, ptk = pstr.next()
                    TR(P, pt_[:, 0:128], selp[:], c.ident[:], [selk, "ident"], [ptk])
                    for hh in range(3):
                        h = 3 * g + hh
                        Ct, Ctk = Ctb.next()
                        TS(P, "pool", Ct[64:128, :], IOT[64:128, :], SLOPES[h], -BIG - SLOPES[h] * t0, ALU.mult, ALU.add,
                           ["IOT"], [Ctk])
                        STT(P, qaug[h][64:128, qc], pt_[64:128, 0:128], BIG, Ct[64:128, :], ALU.mult, ALU.add,
                            [ptk, Ctk], [("qm", h, i)])
                defer(sel_tail)

            otiles[i] = (o, ok)
            if i % 4 != 3:
                continue
            G4 = i // 4
            t0g = 512 * G4
            flush()
            for g in range(2):
                for hh in range(3):
                    h = 3 * g + hh
                    psl, pslk = psacc.next()
                    nchk = 4 * G4 + 4
                    for cch in range(nchk):
                        jmin = max(0, cch - 4 * G4)
                        ncol = (4 - jmin) * 128
                        qcs = slice(t0g + jmin * 128, t0g + 512)
                        ps_, psk = pss.next()
                        MM(P, ps_[:, 0:ncol], kslc[g][:, cch * 128:(cch + 1) * 128], qaug[h][:, qcs], True, True,
                           [("kslc", g), ("kx", g), ("q", h)] + [("qm", h, 4 * G4 + j) for j in range(jmin, 4)], [psk])
                        ew, ewk = ewb.next()
                        ACT(P, ew[:, 0:ncol], ps_[:, 0:ncol], AF.Exp, [psk, "bias_sel"], [ewk], bias=bias_sel[:, h:h + 1])
                        if cch >= 4 * G4:
                            ASEL(P, ew[:, 0:128], ew[:, 0:128], [[1, 128]], ALU.is_ge, 0.0, 0, -1, [ewk], [ewk])

                        def av(psl=psl, pslk=pslk, ew=ew, ewk=ewk, cch=cch, jmin=jmin, g=g, G4=G4):
                            for j in range(jmin, 4):
                                MM(P, psl[:, j * 128:j * 128 + 65], ew[:, (j - jmin) * 128:(j - jmin + 1) * 128],
                                   vslc[:, cch, g, :], cch == 0 and j == 0, cch == 4 * G4 + j, [ewk, "vslc"], [pslk], sgc=True)
                        defer(av)
                    for j in range(4):
                        ti = 4 * G4 + j
                        defer(lambda psl=psl, pslk=pslk, j=j, ti=ti, h=h:
                              consume(psl[:, j * 128:j * 128 + 65], pslk, otiles[ti][0], otiles[ti][1], h, ti, 3 * h + 1, False))
            for ti in range(4 * G4, 4 * G4 + 4):
                def finish_tile(o=otiles[ti][0], ok=otiles[ti][1], qc=slice(128 * ti, 128 * ti + 128)):
                    pt2, pt2k = pstr.next()
                    for j in range(3):
                        TR(P, pt2[:, j * 128:(j + 1) * 128], o[:, j * 128:(j + 1) * 128], c.ident[:],
                           [(ok, 2 * j), (ok, 2 * j + 1), "ident"], [pt2k])
                    ost, ostk = ostb.next()
                    CP(P, "act", ost[:], pt2[:, 0:384].rearrange("p (j t) -> p j t", j=3), [pt2k], [ostk])
                    DMA(P, "sp", c.NSAOT.rearrange("(j p) t -> p j t", p=128)[:, :, qc], ost[:], [ostk], ["NSAOT"])
                defer(finish_tile)
        flush()
        P.end_phase()
        P.alloc_sems(c.es0, c.sems)
        with nc.Block() as block:
            P.emit(block, c.sems)


def phase_D(c):
    nc, P = c.nc, c.P
    with ExitStack() as es:
        sb = lambda name, shape, dt: es.enter_context(nc.sbuf_tensor(name, shape, dt))
        memt = sb("memt", [128, 2, D], F32)
        mems = sb("mems", [128, 2, D], F32)
        junk = sb("junkD", [128, D], F32)
        ssb = [sb(f"ssD{i}", [128, 4], F32) for i in range(2)]
        gcol = sb("gcolD", [128, 8], F32)
        mhT = sb("mhT", [128, 8, 256], BF16)
        wst = Rot([sb(f"wstD{i}", [128, 512], F32) for i in range(2)], "wstD")
        Wkv = sb("Wkv", [128, 8, 512], BF16)
        mkT = [sb(f"mkT{h}", [64, 256], BF16) for h in range(4)]
        mvaug = sb("mvaug", [128, 2, 4, 65], BF16)
        qm = [sb(f"qm{h}", [64, T], BF16) for h in range(4)]
        eb = Rot([sb(f"ebD{i}", [128, 512], BF16) for i in range(4)], "ebD")
        ob = Rot([sb(f"oD{i}", [128, 4, 256], F32) for i in range(2)], "oD")
        wb = Rot([sb(f"wD{i}", [128, 2], F32) for i in range(4)], "wD")
        ostb = Rot([sb(f"ostD{i}", [128, 2, 512], BF16) for i in range(2)], "ostD")
        pss = Rot(c.ps[0:2], "pssD")
        psacc = Rot(c.ps[2:5], "psaccD")
        pstr = Rot(c.ps[5:7], "pstrD")
        pmisc = c.ps[7]

        DMA(P, "sp", gcol[:], c.inp["mem_g"], [], ["gcol"])
        DMA(P, "sp", memt[:], c.inp["mem"].rearrange("(c p) d -> p c d", p=128), [], ["memt"])
        for h in range(4):
            DMA(P, "sp", qm[h][:], c.FM[FM_QMEM + 64 * h:FM_QMEM + 64 * (h + 1), :], ["FM"], [("qm", h)])
        for kc in range(8):
            st, sk = wst.next()
            DMA(P, "sp", st[:], c.inp["w_mem_kv"][kc * 128:(kc + 1) * 128, :], [], [sk])
            TS(P, "dve", Wkv[:, kc, :], st[:], gcol[:, kc:kc + 1], None, ALU.mult, None, [sk, "gcol"], [("Wkv", kc)])
        Wk = [("Wkv", kc) for kc in range(8)]
        for ci in range(2):
            ss = ssb[ci]
            ssk = ("ssD", ci)
            ACT(P, junk[:], memt[:, ci, :], AF.Square, ["memt"], ["junkD", ssk], accum=ss[:, 0:1])
            rstd_chain(P, ss, ssk)
            TS(P, "dve", mems[:, ci, :], memt[:, ci, :], ss[:, 3:4], None, ALU.mult, None, ["memt", ssk], [("mems", ci)])
            for half in range(2):
                pt, ptk = pstr.next()
                for j in range(4):
                    cc = half * 4 + j
                    TR(P, pt[:, j * 128:(j + 1) * 128], mems[:, ci, cc * 128:(cc + 1) * 128], c.ident[:],
                       [("mems", ci), "ident"], [ptk])
                CP(P, "dve", mhT[:, half * 4:(half + 1) * 4, ci * 128:(ci + 1) * 128],
                   pt[:].rearrange("p (j t) -> p j t", j=4), [ptk], [("mhT", ci, half)])
        mh = [("mhT", ci, half) for ci in range(2) for half in range(2)]
        for h in range(4):
            for kc in range(8):
                MM(P, pmisc[0:64, 0:256], Wkv[:, kc, 64 * h:64 * h + 64], mhT[:, kc, :], kc == 0, kc == 7, Wk + mh, ["pmisc"])
            CP(P, "dve", mkT[h][:], pmisc[0:64, 0:256], ["pmisc"], [("mkT", h)])
        for ci in range(2):
            for kc in range(8):
                MM(P, pmisc[:, 256:512], mhT[:, kc, ci * 128:(ci + 1) * 128], Wkv[:, kc, 256:512], kc == 0, kc == 7,
                   Wk + mh, ["pmisc"])
            CP(P, "dve", mvaug[:, ci, :, 0:64], pmisc[:, 256:512].rearrange("p (h d) -> p h d", h=4), ["pmisc"], ["mvaug"])
        MEMSET(P, "dve", mvaug[:, :, :, 64:65], 1.0, ["mvaug"], ["mvaug"])

        for g in range(8):
            o, ok = ob.next()
            for h in range(4):
                es_ = []
                for ci in range(2):
                    ps_, psk = pss.next()
                    MM(P, ps_[:, :], mkT[h][:, ci * 128:(ci + 1) * 128], qm[h][:, g * 512:(g + 1) * 512], True, True,
                       [("mkT", h), ("qm", h)], [psk])
                    e, ek = eb.next()
                    ACT(P, e[:], ps_[:, :], AF.Exp, [psk], [ek])
                    es_.append((e, ek))
                for sub in range(4):
                    pa, pak = psacc.next()
                    for ci in range(2):
                        MM(P, pa[:, 0:65], es_[ci][0][:, sub * 128:(sub + 1) * 128], mvaug[:, ci, h, :], ci == 0, ci == 1,
                           [es_[ci][1], "mvaug"], [pak])
                    w, wk = wb.next()
                    RECIP(P, w[:, 0:1], pa[:, 64:65], [pak], [wk])
                    TS(P, "dve", o[:, sub, 64 * h:64 * h + 64], pa[:, 0:64], w[:, 0:1], None, ALU.mult, None, [pak, wk],
                       [(ok, sub, h)])
            ost, ostk = ostb.next()
            for sub in range(4):
                pt, ptk = pstr.next()
                for j in range(2):
                    TR(P, pt[:, j * 128:(j + 1) * 128], o[:, sub, j * 128:(j + 1) * 128], c.ident[:],
                       [(ok, sub, 2 * j), (ok, sub, 2 * j + 1), "ident"], [ptk])
                CP(P, "act", ost[:, :, sub * 128:(sub + 1) * 128], pt[:, 0:256].rearrange("p (j t) -> p j t", j=2),
                   [ptk], [(ostk, sub)])
            DMA(P, "sp", c.MEMOT.rearrange("(j p) t -> p j t", p=128)[:, :, g * 512:(g + 1) * 512], ost[:],
                [(ostk, sub) for sub in range(4)], ["MEMOT"])
        P.end_phase()
        P.alloc_sems(c.es0, c.sems)
        with nc.Block() as block:
            P.emit(block, c.sems)


def phase_E(c):
    nc, P = c.nc, c.P
    with ExitStack() as es:
        sb = lambda name, shape, dt: es.enter_context(nc.sbuf_tensor(name, shape, dt))
        Wmg = sb("Wmg", [128, 8, 3072], BF16)
        Wbr = [sb("Wsb", [128, 3, D], BF16), sb("Wnsa", [128, 3, D], BF16), sb("Wmem", [128, 2, D], BF16)]
        Wout = sb("Wout", [128, 8, D], BF16)
        wst = Rot([sb(f"wstE{i}", [128, 1024], F32) for i in range(3)], "wstE")
        gcol = sb("gcolE", [128, 8], F32)
        bmg = sb("bmg", [128, 24], F32)
        hTb = Rot([sb(f"hTE{i}", [128, 8, 512], BF16) for i in range(2)], "hTE")
        srcb = [Rot([sb(f"srcE{b}_{i}", [128, 3 if b < 2 else 2, 512], BF16) for i in range(2)], f"srcE{b}") for b in range(3)]
        mgb = Rot([sb(f"mgT{i}", [128, 8, 512], BF16) for i in range(2)], "mgT")
        gateb = Rot([sb(f"gateE{i}", [128, 512], F32) for i in range(3)], "gateE")
        accb = Rot([sb(f"accE{i}", [128, 512], F32) for i in range(2)], "accE")
        tmpb = Rot([sb(f"tmpE{i}", [128, 512], F32) for i in range(2)], "tmpE")
        xb = Rot([sb(f"xE{i}", [128, D], F32) for i in range(2)], "xE")
        x1b = Rot([sb(f"x1E{i}", [128, D], F32) for i in range(2)], "x1E")
        psbr = Rot(c.ps[0:2], "psbr")
        psg = Rot(c.ps[2:5], "psg")
        psy = Rot(c.ps[5:8], "psy")

        DMA(P, "sp", gcol[:], c.inp["mix_g"], [], ["gcol"])
        DMA(P, "sp", bmg[:], c.inp["b_merge"], [], ["bmg"])
        n = 0
        for kc in range(8):
            for j in range(3):
                st, sk = wst.next()
                DMA(P, "sp", st[:], c.inp["w_in"][kc * 128:(kc + 1) * 128, 2578 + 1024 * j:2578 + 1024 * (j + 1)], [], [sk])
                if n % 2 == 0:
                    TS(P, "dve", Wmg[:, kc, 1024 * j:1024 * (j + 1)], st[:], gcol[:, kc:kc + 1], None, ALU.mult, None,
                       [sk, "gcol"], [("Wmg", kc)])
                else:
                    ACT(P, Wmg[:, kc, 1024 * j:1024 * (j + 1)], st[:], AF.Copy, [sk, "gcol"], [("Wmg", kc)],
                        scale=gcol[:, kc:kc + 1])
                n += 1
        for b, (nm, nf) in enumerate((("w_sb_br", 3), ("w_nsa_br", 3), ("w_mem_br", 2))):
            for f in range(nf):
                st, sk = wst.next()
                DMA(P, "sp", st[:], c.inp[nm][f * 128:(f + 1) * 128, :], [], [sk])
                CP(P, "dve" if n % 2 == 0 else "act", Wbr[b][:, f, :], st[:], [sk], [("Wbr", b)])
                n += 1
        for kc in range(8):
            st, sk = wst.next()
            DMA(P, "sp", st[:], c.inp["w_out"][kc * 128:(kc + 1) * 128, :], [], [sk])
            CP(P, "dve" if n % 2 == 0 else "act", Wout[:, kc, :], st[:], [sk], ["Wout"])
            n += 1
        Wmgk = [("Wmg", kc) for kc in range(8)]
        srcs = [(c.SBOT, 3, "SBOT"), (c.NSAOT, 3, "NSAOT"), (c.MEMOT, 2, "MEMOT")]
        for tg in range(8):
            tc_ = slice(tg * 512, (tg + 1) * 512)
            hT, hk = hTb.next()
            DMA(P, "sp", hT[:], c.HT.rearrange("(c p) t -> p c t", p=128)[:, :, tc_], ["HT"], [hk])
            src = []
            for b, (ap, nf, nm) in enumerate(srcs):
                t_, tk = srcb[b].next()
                DMA(P, "sp", t_[:], ap.rearrange("(f p) t -> p f t", p=128)[:, :, tc_], [nm], [tk])
                src.append((t_, tk, nf))
            mg, mgk = mgb.next()
            for dc in range(8):
                acc, acck = accb.next()
                for b in range(3):
                    t_, tk, nf = src[b]
                    pb, pbk = psbr.next()
                    for f in range(nf):
                        MM(P, pb[:, :], Wbr[b][:, f, dc * 128:(dc + 1) * 128], t_[:, f, :], f == 0, f == nf - 1,
                           [("Wbr", b), tk], [pbk])
                    pg, pgk = psg.next()
                    for kc in range(8):
                        MM(P, pg[:, :], Wmg[:, kc, b * 1024 + dc * 128:b * 1024 + (dc + 1) * 128], hT[:, kc, :], kc == 0, kc == 7,
                           Wmgk + [hk], [pgk])
                    gt, gtk = gateb.next()
                    ACT(P, gt[:], pg[:, :], AF.Sigmoid, [pgk, "bmg"], [gtk], bias=bmg[:, b * 8 + dc:b * 8 + dc + 1])
                    if b == 0:
                        TT(P, "dve", acc[:], gt[:], pb[:, :], ALU.mult, [gtk, pbk], [acck])
                    else:
                        tmp, tmpk = tmpb.next()
                        TT(P, "dve", tmp[:], gt[:], pb[:, :], ALU.mult, [gtk, pbk], [tmpk])
                        if b == 1:
                            TT(P, "pool", acc[:], acc[:], tmp[:], ALU.add, [acck, tmpk], [acck])
                        else:
                            TT(P, "pool", mg[:, dc, :], acc[:], tmp[:], ALU.add, [acck, tmpk], [(mgk, dc)])
            mgks = [(mgk, dc) for dc in range(8)]
            for s in range(4):
                i = tg * 4 + s
                xt, xk = xb.next()
                DMA(P, "sp", xt[:], c.inp["x"][i * 128:(i + 1) * 128, :], [], [xk])
                x1, x1k = x1b.next()
                for half in range(2):
                    py, pyk = psy.next()
                    for dc in range(8):
                        MM(P, py[:, :], mg[:, dc, s * 128:(s + 1) * 128], Wout[:, dc, half * 512:(half + 1) * 512], dc == 0, dc == 7,
                           mgks + ["Wout"], [pyk])
                    TT(P, "dve", x1[:, half * 512:(half + 1) * 512], xt[:, half * 512:(half + 1) * 512], py[:, :], ALU.add,
                       [xk, pyk], [(x1k, half)])
                DMA(P, "sp", c.X1[i * 128:(i + 1) * 128, :], x1[:], [(x1k, 0), (x1k, 1)], ["X1"])
        P.end_phase()
        P.alloc_sems(c.es0, c.sems)
        with nc.Block() as block:
            P.emit(block, c.sems)


def phase_F(c):
    nc, P = c.nc, c.P
    GS = 8
    with ExitStack() as es:
        sb = lambda name, shape, dt: es.enter_context(nc.sbuf_tensor(name, shape, dt))
        Wq = sb("Wq", [128, 8, 2048], BF16)
        subk = sb("subk", [128, 16, 128], BF16)
        g2b = sb("g2b", [128, D], F32)
        gFb = sb("gFb", [128, D], F32)
        keyidx = sb("keyidx", [128, 2048], I32)
        posidx = sb("posidx", [128, 2048], I32)
        iotaA = sb("iotaA", [128, 2048], F32)
        cI = sb("cI", [128, 8], I32)
        with ExitStack() as es1:
            sb1 = lambda name, shape, dt: es1.enter_context(nc.sbuf_tensor(name, shape, dt))
            wst = Rot([sb1(f"wstF{i}", [128, 2048], F32) for i in range(2)], "wstF")
            iotaAi = sb1("iotaAi", [128, 2048], I32)
            for kc in range(8):
                st, sk = wst.next()
                DMA(P, "sp", st[:], c.inp["peer_w_q"][kc * 128:(kc + 1) * 128, :], [], [sk])
                CP(P, "dve" if kc % 2 == 0 else "act", Wq[:, kc, :], st[:], [sk], [("Wq", kc)])
            st, sk = wst.next()
            DMA(P, "sp", st[:], c.inp["subkT"].rearrange("d b k -> d (b k)"), [], [sk])
            CP(P, "dve", subk[:].rearrange("d b k -> d (b k)"), st[:], [sk], ["subk"])
            DMA(P, "sp", g2b[:], c.inp["ffn_g"].partition_broadcast(128), [], ["g2b"])
            DMA(P, "sp", gFb[:], c.inp["final_g"].partition_broadcast(128), [], ["gFb"])
            IOTA(P, keyidx[:], [[0, 16], [1, 128]], 0, 0, [], ["keyidx"])
            IOTA(P, posidx[:], [[0, 8], [1, 256]], 0, 0, [], ["posidx"])
            IOTA(P, iotaAi[:], [[0, 128], [1, 16]], 0, 0, [], ["iotaAi"])
            CP(P, "dve", iotaA[:], iotaAi[:], ["iotaAi"], ["iotaA"])
            for j, v in enumerate((-128, -256, 127, 255, 15, 4)):
                IOTA(P, cI[:, j:j + 1], [[0, 1]], v, 0, ["cI"], ["cI"])
            P.end_phase()
            P.alloc_sems(c.es0, c.sems)
            with nc.Block() as block:
                P.emit(block, c.sems)
        x1b = Rot([sb(f"x1F{i}", [128, D], F32) for i in range(2)], "x1F")
        h2b = Rot([sb(f"h2F{i}", [128, D], F32) for i in range(1)], "h2F")
        ssb = Rot([sb(f"ssF{i}", [128, 4], F32) for i in range(4)], "ssF")
        junk = sb("junkF", [128, D], BF16)
        prodb = Rot([sb(f"prodF{i}", [128, D], BF16) for i in range(4)], "prodF")
        h2hb = Rot([sb(f"h2hF{i}", [128, D], BF16) for i in range(2)], "h2hF")
        h2Tb = Rot([sb(f"h2T{i}", [128, 8, 128], BF16) for i in range(2)], "h2T")
        qTb = sb("qTbF", [128, 16, 128], BF16)
        Sc = sb("Sc", [128, 2048], F32)
        rep = sb("repF", [128, 256], F32)
        stop = sb("stop", [128, 16, 16], F32)
        itop_i = sb("itop_i", [128, 256], I32)
        itop_f = sb("itop_f", [128, 16, 16], F32)
        tmpA = sb("tmpA", [128, 2048], F32)
        tmpB = Sc
        best = sb("best", [128, 8, 16], F32)
        pos_i = sb("pos_i", [128, 3, 128], I32)
        ab_f = sb("ab_f", [128, 2, 128], F32)
        sel_f = sb("sel_f", [128, 3, 128], F32)
        idxb = Rot([sb(f"idxF{i}", [128, 128], I32) for i in range(2)], "idxF")
        gwb = Rot([sb(f"gwF{i}", [128, 3, 128], F32) for i in range(2)], "gwF")
        gsum = sb("gsum", [128, 16], F32)
        ab = Rot([sb(f"aF{i}", [128, 2, 128], F32) for i in range(2)], "aF")
        uvb = Rot([sb(f"uvg{i}", [128, 2 * D], BF16) for i in range(16)], "uvg")
        dgb = Rot([sb(f"dg{i}", [128, 128], BF16) for i in range(6)], "dg")
        x2b = Rot([sb(f"x2F{i}", [128, D], F32) for i in range(1)], "x2F")
        ptq = Rot(c.ps[0:2], "ptq")
        psS = Rot(c.ps[2:4], "psS")
        pvb = Rot([(c.ps[4], c.ps[5]), (c.ps[6], c.ps[7])], "pv")
        Wqk = [("Wq", kc) for kc in range(8)]

        def route(i, st):
            x1, x1k = x1b.next()
            DMA(P, "sp", x1[:], c.X1[i * 128:(i + 1) * 128, :], ["X1"], [x1k])
            ss, ssk = ssb.next()
            ACT(P, junk[:], x1[:], AF.Square, [x1k], [ssk], accum=ss[:, 0:1])
            rstd_chain(P, ss, ssk)
            h2, h2k = h2b.next()
            STT(P, h2[:], x1[:], ss[:, 3:4], g2b[:], ALU.mult, ALU.mult, [x1k, ssk, "g2b"], [h2k])
            h2h, h2hk = h2hb.next()
            CP(P, "act", h2h[:], h2[:], [h2k], [h2hk])
            yield
            h2T, h2Tk = h2Tb.next()
            for half in range(2):
                pt, ptk = ptq.next()
                for j in range(4):
                    cc = half * 4 + j
                    TR(P, pt[:, j * 128:(j + 1) * 128], h2[:, cc * 128:(cc + 1) * 128], c.ident[:], [h2k, "ident"], [ptk])
                CP(P, "act", h2T[:, half * 4:(half + 1) * 4, :], pt[:].rearrange("p (j t) -> p j t", j=4), [ptk], [(h2Tk, half)])
            h2Tks = [(h2Tk, 0), (h2Tk, 1)]
            yield
            for b4 in range(4):
                pq, pqk = ptq.next()
                for j in range(4):
                    blk = b4 * 4 + j
                    for kc in range(8):
                        MM(P, pq[:, j * 128:(j + 1) * 128], Wq[:, kc, blk * 128:(blk + 1) * 128], h2T[:, kc, :], kc == 0, kc == 7,
                           Wqk + h2Tks, [pqk])
                CP(P, "act", qTb[:, b4 * 4:(b4 + 1) * 4, :], pq[:].rearrange("p (j t) -> p j t", j=4), [pqk], [("qTb", b4)])
                yield
            for b4 in range(4):
                pS, pSk = psS.next()
                for j in range(4):
                    blk = b4 * 4 + j
                    MM(P, pS[:, j * 128:(j + 1) * 128], qTb[:, blk, :], subk[:, blk, :], True, True, [("qTb", b4), "subk"], [pSk])
                STT(P, Sc[:, b4 * 512:(b4 + 1) * 512].bitcast(I32), pS[:, :].bitcast(I32), cI[:, 0:1],
                    keyidx[:, b4 * 512:(b4 + 1) * 512], ALU.bitwise_and, ALU.bitwise_or, [pSk, "cI", "keyidx"], [("Sc", b4), "tmpB"])
                yield
            for blk in range(16):
                sblk = Sc[:, blk * 128:(blk + 1) * 128]
                sck = ("Sc", blk // 4)
                P.op("dve", lambda e, o=stop[:, blk, 0:8], s=sblk: e.max(out=o, in_=s), [sck], ["stop"])
                P.op("dve", lambda e, o=rep[:, 0:128], m=stop[:, blk, 0:8], s=sblk: e.match_replace(
                    out=o, in_to_replace=m, in_values=s, imm_value=-1e30), [sck, "stop"], ["repF"])
                P.op("dve", lambda e, o=stop[:, blk, 8:16], s=rep[:, 0:128]: e.max(out=o, in_=s), ["repF"], ["stop"])
                yield
            stop2 = stop[:].rearrange("p b k -> p (b k)")
            TS(P, "dve", itop_i[:], stop2.bitcast(I32), cI[:, 2:3], None, ALU.bitwise_and, None, ["stop", "cI"], ["itop_i"])
            CP(P, "dve", itop_f[:].rearrange("p b k -> p (b k)"), itop_i[:], ["itop_i"], ["itop_f"])
            yield
            sv = stop[:].rearrange("p (h q) k -> p h q k", q=2)
            iv = itop_f[:].rearrange("p (h q) k -> p h q k", q=2)
            cand = tmpA[:].rearrange("p (h a b) -> p h a b", h=8, a=16)
            TT(P, "dve", cand, sv[:, :, 0, :].unsqueeze(3).to_broadcast([128, 8, 16, 16]),
               sv[:, :, 1, :].unsqueeze(2).to_broadcast([128, 8, 16, 16]), ALU.add, ["stop"], ["tmpA"])
            yield
            STT(P, tmpB[:].bitcast(I32), tmpA[:].bitcast(I32), cI[:, 1:2], posidx[:], ALU.bitwise_and, ALU.bitwise_or,
                ["tmpA", "cI", "posidx"], ["tmpB"] + [("Sc", b_) for b_ in range(4)])
            yield
            for h in range(8):
                sblk = tmpB[:, h * 256:(h + 1) * 256]
                P.op("dve", lambda e, o=best[:, h, 0:8], s=sblk: e.max(out=o, in_=s), ["tmpB"], ["best"])
                P.op("dve", lambda e, o=rep[:], m=best[:, h, 0:8], s=sblk: e.match_replace(
                    out=o, in_to_replace=m, in_values=s, imm_value=-1e30), ["tmpB", "best"], ["repF"])
                P.op("dve", lambda e, o=best[:, h, 8:16], s=rep[:]: e.max(out=o, in_=s), ["repF"], ["best"])
                yield
            best2 = best[:].rearrange("p h k -> p (h k)")
            TS(P, "dve", pos_i[:, 0, :], best2.bitcast(I32), cI[:, 3:4], None, ALU.bitwise_and, None, ["best", "cI"], ["pos_i"])
            TS(P, "dve", pos_i[:, 1, :], pos_i[:, 0, :], cI[:, 5:6], None, ALU.logical_shift_right, None, ["pos_i", "cI"], ["pos_i"])
            TS(P, "dve", pos_i[:, 2, :], pos_i[:, 0, :], cI[:, 4:5], None, ALU.bitwise_and, None, ["pos_i", "cI"], ["pos_i"])
            CP(P, "dve", ab_f[:], pos_i[:, 1:3, :], ["pos_i"], ["ab_f"])
            yield
            for q in range(2):
                akv = ab_f[:, q, :].rearrange("p (h k) -> p h k", h=8)
                eq = tmpA[:].rearrange("p (h k a) -> p h k a", h=8, k=16)
                TT(P, "dve", eq, akv.unsqueeze(3).to_broadcast([128, 8, 16, 16]),
                   iotaA[:].rearrange("p (h k a) -> p h k a", h=8, k=16), ALU.is_equal, ["ab_f", "iotaA"], ["tmpA"])
                yield
                pr = tmpB[:].rearrange("p (h k a) -> p h k a", h=8, k=16)
                TT(P, "dve", pr, eq, iv[:, :, q, :].unsqueeze(2).to_broadcast([128, 8, 16, 16]), ALU.mult,
                   ["tmpA", "itop_f"], ["tmpB"] + [("Sc", b_) for b_ in range(4)])
                yield
                P.op("dve", lambda e, o=sel_f[:, q, :], s=tmpB[:].rearrange("p (x a) -> p x a", a=16): e.tensor_reduce(
                    out=o, in_=s, axis=AX.X, op=ALU.add), ["tmpB"], ["sel_f"])
                yield
            STT(P, sel_f[:, 2, :], sel_f[:, 0, :], 128.0, sel_f[:, 1, :], ALU.mult, ALU.add, ["sel_f"], ["sel_f"])
            TS(P, "dve", sel_f[:, 2, :], sel_f[:, 2, :], 0.0, 16383.0, ALU.max, ALU.min, ["sel_f"], ["sel_f"])
            idx, idxk = idxb.next()
            CP(P, "dve", idx[:], sel_f[:, 2, :], ["sel_f"], [idxk])
            yield
            gw, gwk = gwb.next()
            v3 = lambda ap: ap.rearrange("p (h k) -> p h k", h=8)
            TT(P, "dve", v3(gw[:, 0, :]), best[:], best[:, :, 0:1].to_broadcast([128, 8, 16]), ALU.subtract, ["best"], [gwk])
            ACT(P, gw[:, 1, :], gw[:, 0, :], AF.Exp, [gwk], [gwk])
            yield
            P.op("dve", lambda e, o=gsum[:, 0:8], s=v3(gw[:, 1, :]): e.tensor_reduce(out=o, in_=s, axis=AX.X, op=ALU.add),
                 [gwk], ["gsum"])
            RECIP(P, gsum[:, 8:16], gsum[:, 0:8], ["gsum"], ["gsum"])
            TT(P, "dve", v3(gw[:, 2, :]), v3(gw[:, 1, :]), gsum[:, 8:16].unsqueeze(2).to_broadcast([128, 8, 16]), ALU.mult,
               [gwk, "gsum"], [gwk])
            st.update(x1=x1, x1k=x1k, h2=h2h, h2k=h2hk, idx=idx, idxk=idxk, gw=gw, gwk=gwk)
            yield

        def slots(i, st, bg):
            x1, x1k, h2, h2k, idx, idxk, gw, gwk = (st[k_] for k_ in ("x1", "x1k", "h2", "h2k", "idx", "idxk", "gw", "gwk"))
            a, ak = ab.next()
            (pv0, pv1), pvk = pvb.next()
            LAG = 6
            GSZ = 4
            held = {}
            for s in range(128 + LAG):
                if s < 128:
                    uv, uvk = uvb.next()
                    held[s] = (uv, uvk)
                    P.dma("pool", lambda e, o=uv[:], ix=idx[:, s:s + 1]: e.indirect_dma_start(
                        out=o, out_offset=None, in_=c.UVB,
                        in_offset=bass.IndirectOffsetOnAxis(ap=ix.bitcast(U32), axis=0)), [idxk, "UVB"], [uvk])
                    pd, pdk = prodb.next()
                    TT(P, "dve", pd[:], uv[:, 0:D], h2[:], ALU.mult, [uvk, h2k], [pdk])
                    ACT(P, junk[:], pd[:], AF.Copy, [pdk], [(ak, s)], accum=a[:, 0, s:s + 1])
                    if s % GSZ == GSZ - 1:
                        gs_ = slice(s - GSZ + 1, s + 1)
                        ACT(P, a[:, 1, gs_], a[:, 0, gs_], AF.Gelu, [(ak, s_) for s_ in range(s - GSZ + 1, s + 1)],
                            [(ak, "g", s // GSZ)])
                r_ = s - LAG
                if r_ >= 0:
                    uv, uvk = held.pop(r_)
                    dg, dgk = dgb.next()
                    TS(P, "dve", dg[:], c.identb[:], a[:, 1, r_:r_ + 1], gw[:, 2, r_:r_ + 1], ALU.mult, ALU.mult,
                       ["identb", (ak, "g", r_ // GSZ), gwk], [dgk])
                    MM(P, pv0[:, :], dg[:], uv[:, D:D + 512], r_ == 0, r_ == 127, [dgk, uvk], [(pvk, 0)])
                    MM(P, pv1[:, :], dg[:], uv[:, D + 512:2 * D], r_ == 0, r_ == 127, [dgk, uvk], [(pvk, 1)])
                if bg is not None and s % 2 == 1:
                    next(bg, None)
            if bg is not None:
                for _ in bg:
                    pass
            x2, x2k = x2b.next()
            TT(P, "dve", x2[:, 0:512], x1[:, 0:512], pv0[:, :], ALU.add, [x1k, (pvk, 0)], [(x2k, 0)])
            TT(P, "dve", x2[:, 512:1024], x1[:, 512:1024], pv1[:, :], ALU.add, [x1k, (pvk, 1)], [(x2k, 1)])
            ss2, ss2k = ssb.next()
            ACT(P, junk[:], x2[:], AF.Square, [(x2k, 0), (x2k, 1)], [ss2k], accum=ss2[:, 0:1])
            rstd_chain(P, ss2, ss2k)
            STT(P, x2[:], x2[:], ss2[:, 3:4], gFb[:], ALU.mult, ALU.mult, [(x2k, 0), (x2k, 1), ss2k, "gFb"], [(x2k, 0), (x2k, 1)])
            DMA(P, "sp", c.out[i * 128:(i + 1) * 128, :], x2[:], [(x2k, 0), (x2k, 1)], ["out"])

        states = [dict() for _ in range(NT)]
        for _ in route(0, states[0]):
            pass
        for i in range(NT):
            bg = route(i + 1, states[i + 1]) if i + 1 < NT else None
            slots(i, states[i], bg)
        P.end_phase()
        P.alloc_sems(c.es0, c.sems)
        with nc.Block() as block:
            P.emit(block, c.sems)


def build(upto="F", debug=False):
    nc = bass.Bass("TRN2", target_bir_lowering=False)
    c = Ctx()
    c.nc = nc
    c.P = Prog(nc)
    c.sems = {}
    inp = {}

    def din(name, shape, dt=F32):
        inp[name] = nc.dram_tensor(name, list(shape), dt, kind="ExternalInput").ap()

    din("x", [T, D])
    din("mem", [256, D])
    din("mix_g", [128, 8])
    din("mem_g", [128, 8])
    din("w_in", [D, IN_DIM])
    din("b_merge", [128, 24])
    din("pe_k", [64, 32])
    din("pe_v", [64, 32])
    din("cw_k", [64, 32, 64])
    din("cw_v", [64, 32, 64])
    din("w_mem_kv", [D, 512])
    din("w_sb_br", [384, D])
    din("w_nsa_br", [384, D])
    din("w_mem_br", [256, D])
    din("w_out", [D, D])
    din("ffn_g", [D])
    din("peer_w_q", [D, 2048])
    din("subkT", [128, 16, 128])
    din("peer_uv", [16384, 2 * D])
    din("final_g", [D])
    c.inp = inp
    kind = "ExternalOutput" if debug else "Internal"

    def scr(name, shape, dt):
        return nc.dram_tensor(name, list(shape), dt, kind=kind).ap()

    c.FM = scr("FM", [FM_ROWS, T], BF16)
    c.HT = scr("HT", [D, T], BF16)
    c.TMV = scr("TMV", [T, 640], BF16)
    c.GATES = scr("GATES", [T, 18], F32)
    c.SBOT = scr("SBOT", [384, T], BF16)
    c.NSAOT = scr("NSAOT", [384, T], BF16)
    c.MEMOT = scr("MEMOT", [256, T], BF16)
    c.X1 = scr("X1", [T, D], F32)
    c.UVB = nc.dram_tensor("UVB", [16384, 2 * D], BF16, kind="Internal").ap()
    c.out = nc.dram_tensor("out", [T, D], F32, kind="ExternalOutput").ap()

    with ExitStack() as es0:
        c.es0 = es0
        c.ps = [es0.enter_context(nc.psum_tensor(f"ps{i}", [128, 512], F32)) for i in range(8)]
        c.ident = es0.enter_context(nc.sbuf_tensor("ident", [128, 128], F32))
        c.identb = es0.enter_context(nc.sbuf_tensor("identb", [128, 128], BF16))
        P = c.P
        MEMSET(P, "pool", c.ident[:], 1.0, [], ["ident"])
        ASEL(P, c.ident[:], c.ident[:], [[1, 128]], ALU.is_equal, 0.0, 0, -1, ["ident"], ["ident"])
        CP(P, "pool", c.identb[:], c.ident[:], ["ident"], ["identb"])
        phases = [("A", phase_A), ("B", phase_B), ("C", phase_C), ("D", phase_D), ("E", phase_E), ("F", phase_F)]
        for name, fn in phases:
            fn(c)
            if name == upto:
                break
    return nc


def make_inputs(inputs, b):
    f = lambda a: np.ascontiguousarray(a, dtype=np.float32)
    gcol = lambda g: f(np.asarray(g).reshape(8, 128).T)
    m = {
        "x": f(inputs["x"][b]),
        "mem": f(inputs["mem"][b]),
        "mix_g": gcol(inputs["mix_norm_g"][0]),
        "mem_g": gcol(inputs["mem_norm_g"][0]),
        "w_in": f(inputs["w_in"][0]),
        "b_merge": f(np.asarray(inputs["b_merge"][0]).reshape(24, 128).T),
        "pe_k": f(np.asarray(inputs["cmp_pe_k"][0]).T),
        "pe_v": f(np.asarray(inputs["cmp_pe_v"][0]).T),
        "cw_k": f(np.asarray(inputs["cmp_w_k"][0]).transpose(1, 0, 2)),
        "cw_v": f(np.asarray(inputs["cmp_w_v"][0]).transpose(1, 0, 2)),
        "w_mem_kv": f(inputs["w_mem_kv"][0]),
        "w_sb_br": f(inputs["w_sb_br"][0]),
        "w_nsa_br": f(inputs["w_nsa_br"][0]),
        "w_mem_br": f(inputs["w_mem_br"][0]),
        "w_out": f(inputs["w_out"][0]),
        "ffn_g": f(inputs["ffn_norm_g"][0]),
        "peer_w_q": f(inputs["peer_w_q"][0]),
        "subkT": f(np.asarray(inputs["peer_subkeys"][0]).transpose(3, 0, 1, 2).reshape(128, 16, 128)),
        "peer_uv": np.ascontiguousarray(np.concatenate([np.asarray(inputs["peer_u"][0], dtype=np.float32),
                                                        np.asarray(inputs["peer_v"][0], dtype=np.float32)], axis=1)),
        "final_g": f(inputs["final_norm_g"]),
    }
    return m


def kernel(**inputs):
    nc = build()
    shared = None
    in_maps = []
    for b in range(8):
        m = make_inputs(inputs, b)
        if shared is None:
            shared = m
        else:
            for k in m:
                if k not in ("x", "mem"):
                    m[k] = shared[k]
        in_maps.append(m)
    res = run_bass_kernel_spmd(nc, in_maps, core_ids=list(range(8)))
    return np.stack([np.asarray(r["out"]) for r in res.results], axis=0).astype(np.float32)
```

```python
import sys
import numpy as np
from contextlib import ExitStack
import concourse.bass as bass
import concourse.mybir as mybir
from concourse.bass_utils import run_bass_kernel_spmd

F32 = mybir.dt.float32
BF16 = mybir.dt.bfloat16
I32 = mybir.dt.int32
U32 = mybir.dt.uint32
AF = mybir.ActivationFunctionType
ALU = mybir.AluOpType
AX = mybir.AxisListType

T = 4096
D = 1024
NT = T // 128
IN_DIM = 5650
EPS = 1e-6
SLOPES = [2.0 ** (-8.0 * (h + 1) / 6) for h in range(6)]
BIG = 30000.0

ENGS = ("pe", "act", "dve", "pool", "sp")
DMA_RING = 8


class Prog:
    def __init__(self, nc, same_engine_sync=True):
        self.nc = nc
        self.ops = {e: [] for e in ENGS}
        self.cnt = {e: 0 for e in ENGS}
        self.dma_n = {e: 0 for e in ENGS}
        self.last_w = {}
        self.readers = {}
        self.waited = {}
        self.same_engine_sync = same_engine_sync
        self.fill_vals = set()
        self.fill_regs = {}

    def _deps(self, eng, reads, writes):
        need = {}

        def add(tok):
            if tok is None:
                return
            sk, val, teng = tok
            if teng == eng and sk[0] == "c":
                if not self.same_engine_sync or eng == "pe":
                    return
            if need.get(sk, 0) < val:
                need[sk] = val

        for r in reads:
            add(self.last_w.get(r))
        for w in writes:
            add(self.last_w.get(w))
            for t in self.readers.get(w, ()):
                add(t)
        out = []
        for sk, val in need.items():
            if self.waited.get((eng, sk), 0) >= val:
                continue
            self.waited[(eng, sk)] = val
            out.append((sk, val))
        return out

    def _commit(self, tok, reads, writes):
        for r in reads:
            self.readers.setdefault(r, []).append(tok)
        for w in writes:
            self.last_w[w] = tok
            self.readers[w] = []

    def op(self, eng, fn, reads=(), writes=()):
        reads = tuple(reads)
        writes = tuple(writes)
        waits = self._deps(eng, reads, writes)
        self.cnt[eng] += 1
        tok = (("c", eng), self.cnt[eng], eng)
        fr = sys._getframe(1)
        self.ops[eng].append(dict(fn=fn, waits=waits, inc=(("c", eng), 1),
                                  where=(fr.f_lineno, fr.f_back.f_lineno if fr.f_back else 0)))
        self._commit(tok, reads, writes)
        return tok

    def dma(self, eng, fn, reads=(), writes=()):
        reads = tuple(reads)
        writes = tuple(writes)
        n = self.dma_n[eng]
        self.dma_n[eng] += 1
        sk = ("d", eng, n % DMA_RING)
        val = 16 * (n // DMA_RING + 1)
        waits = self._deps(eng, reads, writes)
        if val > 16 and self.waited.get((eng, sk), 0) < val - 16:
            self.waited[(eng, sk)] = val - 16
            waits.append((sk, val - 16))
        tok = (sk, val, eng)
        self.ops[eng].append(dict(fn=fn, waits=waits, inc=(sk, 16)))
        self._commit(tok, reads, writes)
        return tok

    def finish(self, eng, toks):
        self.ops[eng].append(dict(fn=None, waits=[(sk, val) for sk, val, _ in toks], inc=None))

    def end_phase(self):
        targets = []
        for e in ENGS:
            if self.cnt[e] > 0:
                targets.append((("c", e), self.cnt[e]))
            n = self.dma_n[e]
            for r in range(min(n, DMA_RING)):
                last = ((n - 1 - r) // DMA_RING) * DMA_RING + r
                targets.append((("d", e, r), 16 * (last // DMA_RING + 1)))
        for e in ENGS:
            waits = []
            for sk, val in targets:
                if self.waited.get((e, sk), 0) >= val:
                    continue
                self.waited[(e, sk)] = val
                waits.append((sk, val))
            self.ops[e].append(dict(fn=None, waits=waits, inc=None))
        self.last_w = {}
        self.readers = {}

    def sem_keys(self):
        keys = set()
        for e in ENGS:
            for o in self.ops[e]:
                if o["inc"]:
                    keys.add(o["inc"][0])
                for sk, _ in o["waits"]:
                    keys.add(sk)
        return sorted(keys)

    def alloc_sems(self, es, sems):
        for k in self.sem_keys():
            if k not in sems:
                sems[k] = es.enter_context(self.nc.semaphore("s_" + "_".join(map(str, k))))

    def emit(self, block, sems):
        engobj = {"pe": "tensor", "act": "scalar", "dve": "vector", "pool": "gpsimd", "sp": "sync"}

        def make(e):
            ops = self.ops[e]

            def body(eng):
                if e == "pool":
                    self.fill_regs = {v: eng.to_reg(v) for v in sorted(self.fill_vals)}
                for o in ops:
                    for sk, val in o["waits"]:
                        eng.wait_ge(sems[sk], val)
                    if o["fn"] is not None:
                        try:
                            ins = o["fn"](eng)
                        except Exception:
                            print("EMIT FAILED at lines", o.get("where"))
                            raise
                        if o["inc"]:
                            ins.then_inc(sems[o["inc"][0]], o["inc"][1])
            return body

        for e in ENGS:
            if self.ops[e]:
                getattr(block, engobj[e])(make(e))
        self.ops = {e: [] for e in ENGS}


class Ctx:
    pass


class Rot:
    def __init__(self, tiles, name):
        self.tiles = tiles
        self.name = name
        self.i = 0

    def next(self):
        j = self.i % len(self.tiles)
        self.i += 1
        return self.tiles[j], (self.name, j)


def MM(P, out, lhsT, rhs, start, stop, r, w, sgc=False):
    return P.op("pe", lambda e: e.matmul(out, lhsT=lhsT, rhs=rhs, start=start, stop=stop, skip_group_check=sgc), r, w)


def TR(P, out, in_, ident, r, w):
    return P.op("pe", lambda e: e.transpose(out, in_, ident), r, w)


def ACT(P, out, in_, func, r, w, scale=None, bias=None, accum=None):
    kw = {}
    if scale is not None:
        kw["scale"] = scale
    if bias is not None:
        kw["bias"] = bias
    if accum is not None:
        kw["accum_out"] = accum
    return P.op("act", lambda e: e.activation(out=out, in_=in_, func=func, **kw), r, w)


def TS(P, eng, out, in0, s1, s2, op0, op1, r, w):
    if op1 is None:
        return P.op(eng, lambda e: e.tensor_scalar(out=out, in0=in0, scalar1=s1, scalar2=None, op0=op0), r, w)
    return P.op(eng, lambda e: e.tensor_scalar(out=out, in0=in0, scalar1=s1, scalar2=s2, op0=op0, op1=op1), r, w)


def TT(P, eng, out, in0, in1, op, r, w):
    return P.op(eng, lambda e: e.tensor_tensor(out=out, in0=in0, in1=in1, op=op), r, w)


def STT(P, out, in0, scalar, in1, op0, op1, r, w):
    return P.op("dve", lambda e: e.scalar_tensor_tensor(out=out, in0=in0, scalar=scalar, in1=in1, op0=op0, op1=op1), r, w)


def CP(P, eng, out, in_, r, w):
    if eng == "act":
        return P.op("act", lambda e: e.copy(out=out, in_=in_), r, w)
    return P.op(eng, lambda e: e.tensor_copy(out=out, in_=in_), r, w)


def DMA(P, eng, out, in_, r, w):
    return P.dma(eng, lambda e: e.dma_start(out=out, in_=in_), r, w)


def MEMSET(P, eng, ap, val, r, w):
    return P.op(eng, lambda e: e.memset(ap, val), r, w)


def ASEL(P, out, in_, pattern, cmp, fill, base, cm, r, w):
    P.fill_vals.add(float(fill))
    return P.op("pool", lambda e: e.affine_select(out=out, in_=in_, pattern=pattern, compare_op=cmp,
                                                  fill=P.fill_regs[float(fill)], base=base, channel_multiplier=cm), r, w)


def IOTA(P, out, pattern, base, cm, r, w):
    return P.op("pool", lambda e: e.iota(out, pattern=pattern, base=base, channel_multiplier=cm), r, w)


def RECIP(P, out, in_, r, w):
    return P.op("dve", lambda e: e.reciprocal(out=out, in_=in_), r, w)


def rstd_chain(P, ss, key):
    TS(P, "dve", ss[:, 1:2], ss[:, 0:1], 1.0 / D, EPS, ALU.mult, ALU.add, [key], [key])
    P.op("act", lambda e: e.sqrt(out=ss[:, 2:3], in_=ss[:, 1:2]), [key], [key])
    RECIP(P, ss[:, 3:4], ss[:, 2:3], [key], [key])


FM_QSB, FM_KSB, FM_QNSA, FM_KCMP, FM_VCMP, FM_KSLC, FM_KWIN, FM_QMEM = 0, 384, 768, 1152, 1280, 1408, 1536, 1664
FM_ROWS = 1920
FM_COLMAP = [(0, 0, 768), (768, 1152, 384), (1152, 1536, 128), (1280, 1664, 128), (1408, 1792, 128),
             (1536, 2048, 128), (1664, 2322, 256)]
TM_COLMAP = [(0, 768, 384), (384, 1920, 128), (512, 2176, 128), (640, 2304, 18)]
TM_COLS = 658
Q_CHUNKS = {0, 1, 2, 6, 7, 8, 13, 14}


def phase_A(c):
    nc, P = c.nc, c.P
    with ExitStack() as es:
        sb = lambda name, shape, dt: es.enter_context(nc.sbuf_tensor(name, shape, dt))
        Wfm = sb("Wfm", [128, 8, FM_ROWS], BF16)
        Wtm = sb("Wtm", [128, 8, TM_COLS], BF16)
        wst = Rot([sb(f"wst{i}", [128, 2578], F32) for i in range(2)], "wst")
        gcol = sb("gcolA", [128, 8], F32)
        xbuf = Rot([sb(f"xt{i}", [128, D], F32) for i in range(2)], "xt")
        xsbuf = Rot([sb(f"xs{i}", [128, D], F32) for i in range(2)], "xs")
        junk = sb("junkA", [128, D], F32)
        ssbuf = Rot([sb(f"ss{i}", [128, 4], F32) for i in range(4)], "ss")
        hTg = Rot([sb(f"hTg{i}", [128, 8, 512], BF16) for i in range(2)], "hTg")
        FMst = Rot([sb(f"FMst{i}", [128, 15, 512], BF16) for i in range(2)], "FMst")
        TMst = Rot([sb(f"TMst{i}", [128, 640], BF16) for i in range(3)], "TMst")
        gst = Rot([sb(f"gst{i}", [128, 18], F32) for i in range(3)], "gst")
        pstr = Rot(c.ps[0:2], "ps_tr")
        psfm = Rot(c.ps[2:5], "ps_fm")
        pstm = Rot(c.ps[5:8], "ps_tm")

        DMA(P, "sp", gcol[:], c.inp["mix_g"], [], ["gcol"])
        for kc in range(8):
            st, sk = wst.next()
            DMA(P, "sp", st[:], c.inp["w_in"][kc * 128:(kc + 1) * 128, 0:2578], [], [sk])
            n = 0
            for (dst, cm) in ((Wfm, FM_COLMAP), (Wtm, TM_COLMAP)):
                for (dc, sc, w) in cm:
                    if n % 2 == 0:
                        TS(P, "dve", dst[:, kc, dc:dc + w], st[:, sc:sc + w], gcol[:, kc:kc + 1], None, ALU.mult, None,
                           [sk, "gcol"], [("W", kc)])
                    else:
                        ACT(P, dst[:, kc, dc:dc + w], st[:, sc:sc + w], AF.Copy, [sk, "gcol"], [("W", kc)],
                            scale=gcol[:, kc:kc + 1])
                    n += 1
        Wkeys = [("W", kc) for kc in range(8)]

        for tg in range(8):
            hT, hk = hTg.next()
            hpieces = [(hk, s, half) for s in range(4) for half in range(2)]
            for s in range(4):
                i = tg * 4 + s
                xt, xk = xbuf.next()
                DMA(P, "sp", xt[:], c.inp["x"][i * 128:(i + 1) * 128, :], [], [xk])
                ss, ssk = ssbuf.next()
                ACT(P, junk[:], xt[:], AF.Square, [xk], ["junkA", ssk], accum=ss[:, 0:1])
                rstd_chain(P, ss, ssk)
                xs, xsk = xsbuf.next()
                TS(P, "dve", xs[:], xt[:], ss[:, 3:4], None, ALU.mult, None, [xk, ssk], [xsk])
                for half in range(2):
                    pt, ptk = pstr.next()
                    for j in range(4):
                        cc = half * 4 + j
                        TR(P, pt[:, j * 128:(j + 1) * 128], xs[:, cc * 128:(cc + 1) * 128], c.ident[:], [xsk, "ident"], [ptk])
                    dst = hT[:, half * 4:(half + 1) * 4, s * 128:(s + 1) * 128]
                    src = pt[:].rearrange("p (j t) -> p j t", j=4)
                    CP(P, "act" if half == 0 else "dve", dst, src, [ptk], [(hk, s, half)])
            fst, fsk = FMst.next()
            for ch in range(15):
                pf, pfk = psfm.next()
                for kc in range(8):
                    MM(P, pf[:, :], Wfm[:, kc, ch * 128:(ch + 1) * 128], hT[:, kc, :], kc == 0, kc == 7,
                       hpieces + [("W", kc)], [pfk])
                sc = 0.125 if ch in Q_CHUNKS else 1.0
                if ch % 2 == 0:
                    ACT(P, fst[:, ch, :], pf[:, :], AF.Copy, [pfk], [(fsk, ch)], scale=sc)
                else:
                    TS(P, "dve", fst[:, ch, :], pf[:, :], sc, None, ALU.mult, None, [pfk], [(fsk, ch)])
            DMA(P, "sp", c.FM.rearrange("(c p) t -> p c t", p=128)[:, :, tg * 512:(tg + 1) * 512], fst[:],
                [(fsk, ch) for ch in range(15)], ["FM"])
            DMA(P, "sp", c.HT.rearrange("(c p) t -> p c t", p=128)[:, :, tg * 512:(tg + 1) * 512], hT[:],
                hpieces, ["HT"])
            for s in range(4):
                i = tg * 4 + s
                pa, pak = pstm.next()
                for kc in range(8):
                    MM(P, pa[:, 0:512], hT[:, kc, s * 128:(s + 1) * 128], Wtm[:, kc, 0:512], kc == 0, kc == 7,
                       hpieces + [("W", kc)], [pak])
                tst, tsk = TMst.next()
                CP(P, "dve", tst[:, 0:512], pa[:, 0:512], [pak], [tsk])
                pb, pbk = pstm.next()
                for kc in range(8):
                    MM(P, pb[:, 0:146], hT[:, kc, s * 128:(s + 1) * 128], Wtm[:, kc, 512:658], kc == 0, kc == 7,
                       hpieces + [("W", kc)], [pbk])
                CP(P, "dve", tst[:, 512:640], pb[:, 0:128], [pbk], [tsk])
                gs, gsk = gst.next()
                ACT(P, gs[:], pb[:, 128:146], AF.Sigmoid, [pbk], [gsk])
                DMA(P, "sp", c.TMV[i * 128:(i + 1) * 128, :], tst[:], [tsk], ["TMV"])
                DMA(P, "sp", c.GATES[i * 128:(i + 1) * 128, :], gs[:], [gsk], ["GATES"])
        P.end_phase()
        P.alloc_sems(c.es0, c.sems)
        with nc.Block() as block:
            P.emit(block, c.sems)


def phase_B(c):
    nc, P = c.nc, c.P
    with ExitStack() as es:
        sb = lambda name, shape, dt: es.enter_context(nc.sbuf_tensor(name, shape, dt))
        qT = [sb(f"qTb{j}", [128, T], BF16) for j in range(3)]
        kT = [sb(f"kTb{j}", [128, T], BF16) for j in range(3)]
        V = sb("Vsb", [128, NT, 384], BF16)
        ntri = sb("ntri", [128, 128], BF16)
        nones = sb("nones", [128, 128], BF16)
        Ebuf = Rot([sb(f"E{i}", [128, 512], F32) for i in range(3)], "E")
        SPbuf = Rot([sb(f"SP{i}", [128, 512], BF16) for i in range(4)], "SP")
        Sbuf = Rot([sb(f"Ssum{i}", [128, 512], BF16) for i in range(3)], "Ssum")
        abuf = Rot([sb(f"aT{i}", [128, 512], BF16) for i in range(3)], "aT")
        obuf = Rot([sb(f"sbo{i}", [64, 512], BF16) for i in range(2)], "sbo")
        psA = Rot(c.ps[0:5], "psA")
        psO = Rot(c.ps[5:7], "psO")
        pswarm = c.ps[7]
        for j in range(3):
            DMA(P, "sp", qT[j][:], c.FM[FM_QSB + 128 * j:FM_QSB + 128 * (j + 1), :], ["FM"], [("qT", j)])
            DMA(P, "sp", kT[j][:], c.FM[FM_KSB + 128 * j:FM_KSB + 128 * (j + 1), :], ["FM"], [("kT", j)])
        DMA(P, "sp", V[:], c.TMV.rearrange("(c p) f -> p c f", p=128)[:, :, 0:384], ["TMV"], ["V"])
        MEMSET(P, "pool", nones[:], -1.0, [], ["nones"])
        MEMSET(P, "pool", ntri[:], -1.0, [], ["ntri"])
        ASEL(P, ntri[:], ntri[:], [[-1, 128]], ALU.is_ge, 0.0, 0, 1, ["ntri"], ["ntri"])
        uvst = Rot([sb(f"uvst{i}", [128, 2 * D], BF16) for i in range(4)], "uvst")

        def convert_chunk(ch):
            t_, tk = uvst.next()
            P.dma("pool", lambda e, o=t_[:], i_=c.inp["peer_uv"][ch * 128:(ch + 1) * 128, :]: e.dma_start(out=o, in_=i_), [], [tk])
            DMA(P, "sp", c.UVB[ch * 128:(ch + 1) * 128, :], t_[:], [tk], ["UVB"])
        steps = []
        for h in range(6):
            for g in range(8):
                nch = 4 * g + 4
                for idx_, cch in enumerate(range(nch - 1, -1, -1)):
                    steps.append(dict(h=h, g=g, cch=cch, first=idx_ == 0, last=cch == 0))
        N = len(steps)
        cur = dict(po=None, pok=None, ssum=None, ssumk=None)

        def S12(st):
            h, g, cch = st["h"], st["g"], st["cch"]
            j, half = h // 2, h % 2
            pr = slice(64 * half, 64 * half + 64)
            st["qs"] = qT[j][pr, g * 512:(g + 1) * 512]
            st["ks"] = kT[j][pr, cch * 128:(cch + 1) * 128]
            st["rk"] = [("kT", j), ("qT", j)]
            st["m"] = cch - 4 * g
            if st["first"]:
                cur["po"], cur["pok"] = psO.next()
                cur["ssum"] = cur["ssumk"] = None
            st["po"], st["pok"] = cur["po"], cur["pok"]
            pa, pak = psA.next()
            MM(P, pa[:, :], st["ks"], st["qs"], True, False, st["rk"], [pak])
            st["pa"], st["pak"] = pa, pak
            E, Ek = Ebuf.next()
            ACT(P, E[:], pa[:, :], AF.Exp, [pak], [Ek])
            SP, SPk = SPbuf.next()
            ACT(P, SP[:], E[:], AF.Ln, [Ek], [SPk], bias=1.0)
            if st["m"] >= 0:
                ASEL(P, SP[:], SP[:], [[1, 512]], ALU.is_gt, 0.0, -128 * st["m"], -1, [SPk], [SPk])
            st["SP"], st["SPk"] = SP, SPk
            st["ssum_prev"], st["ssum_prevk"] = cur["ssum"], cur["ssumk"]
            if not st["last"]:
                if st["first"]:
                    cur["ssum"], cur["ssumk"] = SP, SPk
                else:
                    sn, snk = Sbuf.next()
                    TT(P, "pool", sn[:], cur["ssum"][:], SP[:], ALU.add, [cur["ssumk"], SPk], [snk])
                    cur["ssum"], cur["ssumk"] = sn, snk

        def S34(st):
            pb, pbk = st["pa"], st["pak"]
            MM(P, pb[:, :], ntri[:], st["SP"][:], False, st["first"], ["ntri", st["SPk"]], [pbk])
            if not st["first"]:
                MM(P, pb[:, :], nones[:], st["ssum_prev"][:], False, True, ["nones", st["ssum_prevk"]], [pbk])
            aT, aTk = abuf.next()
            ACT(P, aT[:], pb[:, :], AF.Exp, [pbk], [aTk])
            if st["m"] >= 0:
                ASEL(P, aT[:], aT[:], [[1, 512]], ALU.is_gt, 0.0, -128 * st["m"], -1, [aTk], [aTk])
            st["aT"], st["aTk"] = aT, aTk

        NWARM = 2

        def S5(st):
            h, g, cch = st["h"], st["g"], st["cch"]
            for _ in range(NWARM):
                MM(P, pswarm[:, :], ntri[:], qT[0][:, 0:512], True, True, ["ntri", ("qT", 0)], ["pswarm"])
            MM(P, st["po"][0:64, :], V[:, cch, 64 * h:64 * h + 64], st["aT"][:], st["first"], st["last"], ["V", st["aTk"]], [st["pok"]])
            if st["last"]:
                ob, obk = obuf.next()
                CP(P, "dve", ob[:], st["po"][0:64, :], [st["pok"]], [obk])
                DMA(P, "sp", c.SBOT[64 * h:64 * h + 64, g * 512:(g + 1) * 512], ob[:], [obk], ["SBOT"])

        for n in range(N + 2):
            if n % 6 == 0 and n // 6 < 128:
                convert_chunk(n // 6)
            if n < N:
                S12(steps[n])
            if 0 <= n - 1 < N:
                S34(steps[n - 1])
            if 0 <= n - 2 < N:
                S5(steps[n - 2])
                steps[n - 2].clear()
        P.end_phase()
        P.alloc_sems(c.es0, c.sems)
        with nc.Block() as block:
            P.emit(block, c.sems)


def phase_C(c):
    nc, P = c.nc, c.P
    with ExitStack() as es:
        sb = lambda name, shape, dt: es.enter_context(nc.sbuf_tensor(name, shape, dt))
        qaug = [sb(f"qaug{h}", [128, T], BF16) for h in range(6)]
        kslc = [sb(f"kslc{g}", [128, T], BF16) for g in range(2)]
        kwin = [sb(f"kwin{g}", [64, T], BF16) for g in range(2)]
        kcmp = [sb(f"kcmp{g}", [64, T], BF16) for g in range(2)]
        vcmp = [sb(f"vcmp{g}", [64, T], BF16) for g in range(2)]
        kcT = [sb(f"kcT{g}", [64, 256], BF16) for g in range(2)]
        vcaug = [sb(f"vcaug{g}", [128, 2, 129], BF16) for g in range(2)]
        vwin = sb("vwin", [128, NT, 2, 65], BF16)
        vslc = sb("vslc", [128, NT, 2, 65], BF16)
        vstage = sb("vstage", [128, NT, 256], BF16)
        gates = sb("gatesC", [128, NT, 18], F32)
        pe = [sb("pek_sb", [64, 32], F32), sb("pev_sb", [64, 32], F32)]
        cwst = sb("cwst", [64, 2048], F32)
        cw = [sb("cwk", [64, 32, 64], BF16), sb("cwv", [64, 32, 64], BF16)]
        tmpb = Rot([sb(f"ctmp{i}", [64, 256], BF16) for i in range(3)], "ctmp")
        itmp = sb("itmp", [128, 64], I32)
        ftmp = sb("ftmp", [128, 64], F32)
        bias_sel = sb("bias_sel", [128, 6], F32)
        bias_win = sb("bias_win", [128, 6, 5], F32)
        bias_cmp = sb("bias_cmp", [128, 6, 64], F32)
        IOTi = sb("IOTi", [128, 128], I32)
        IOT = sb("IOT", [128, 128], F32)
        e32b = Rot([sb(f"e32_{i}", [128, 128], F32) for i in range(4)], "e32")
        ebb = Rot([sb(f"eb{i}", [128, 128], BF16) for i in range(7)], "eb")
        obuf = Rot([sb(f"oC{i}", [128, 384], F32) for i in range(8)], "oC")
        ewb = Rot([sb(f"ew{i}", [128, 512], BF16) for i in range(5)], "ew")
        otiles = {}
        impb = Rot([sb(f"imp{i}", [128, 64], F32) for i in range(2)], "imp")
        wb = Rot([sb(f"wC{i}", [128, 4], F32) for i in range(4)], "wC")
        m8b = Rot([sb(f"m8_{i}", [128, 16], F32) for i in range(2)], "m8")
        repb = Rot([sb(f"rep{i}", [128, 64], F32) for i in range(2)], "rep")
        selb = Rot([sb(f"selp{i}", [128, 128], F32) for i in range(4)], "selp")
        Ctb = Rot([sb(f"Ct{i}", [128, 128], F32) for i in range(2)], "Ct")
        ostb = Rot([sb(f"ostC{i}", [128, 3, 128], BF16) for i in range(2)], "ostC")
        pss = Rot(c.ps[0:3], "pss")
        psacc = Rot(c.ps[3:6], "psacc")
        pstr = Rot(c.ps[6:8], "pstrC")
        pk = c.ps[6]

        for h in range(6):
            DMA(P, "sp", qaug[h][0:64, :], c.FM[FM_QNSA + 64 * h:FM_QNSA + 64 * (h + 1), :], ["FM"], [("q", h)])
        for g in range(2):
            DMA(P, "sp", kslc[g][0:64, :], c.FM[FM_KSLC + 64 * g:FM_KSLC + 64 * (g + 1), :], ["FM"], [("kslc", g)])
            DMA(P, "sp", kwin[g][:], c.FM[FM_KWIN + 64 * g:FM_KWIN + 64 * (g + 1), :], ["FM"], [("kwin", g)])
            DMA(P, "sp", kcmp[g][:], c.FM[FM_KCMP + 64 * g:FM_KCMP + 64 * (g + 1), :], ["FM"], [("kcmp", g)])
            DMA(P, "sp", vcmp[g][:], c.FM[FM_VCMP + 64 * g:FM_VCMP + 64 * (g + 1), :], ["FM"], [("vcmp", g)])
        DMA(P, "sp", vstage[:], c.TMV.rearrange("(c p) f -> p c f", p=128)[:, :, 384:640], ["TMV"], ["vstage"])
        DMA(P, "sp", gates[:], c.GATES.rearrange("(c p) f -> p c f", p=128), ["GATES"], ["gates"])
        DMA(P, "sp", pe[0][:], c.inp["pe_k"], [], ["pe0"])
        DMA(P, "sp", pe[1][:], c.inp["pe_v"], [], ["pe1"])
        for kv, nm in ((0, "cw_k"), (1, "cw_v")):
            DMA(P, "sp", cwst[:], c.inp[nm].rearrange("d l e -> d (l e)"), [], ["cwst"])
            CP(P, "dve", cw[kv][:].rearrange("d l e -> d (l e)"), cwst[:], ["cwst"], [("cw", kv)])
        CP(P, "dve", vslc[:, :, :, 0:64], vstage[:, :, 0:128].rearrange("p c (g d) -> p c g d", g=2), ["vstage"], ["vslc"])
        CP(P, "pool", vwin[:, :, :, 0:64], vstage[:, :, 128:256].rearrange("p c (g d) -> p c g d", g=2), ["vstage"], ["vwin"])
        MEMSET(P, "dve", vslc[:, :, :, 64:65], 1.0, ["vslc"], ["vslc"])
        MEMSET(P, "pool", vwin[:, :, :, 64:65], 1.0, ["vwin"], ["vwin"])
        for g in range(2):
            MEMSET(P, "pool", kslc[g][64:128, :], 1.0, [], [("kx", g)])
            ASEL(P, kslc[g][64:128, :], kslc[g][64:128, :], [[1, T]], ALU.is_ge, 0.0, 0, -64, [("kx", g)], [("kx", g)])
            ASEL(P, kslc[g][64:128, :], kslc[g][64:128, :], [[-1, T]], ALU.is_ge, 0.0, 63, 64, [("kx", g)], [("kx", g)])
        IOTA(P, itmp[0:64, 0:1], [[0, 1]], 0, 1, [], ["itmp"])
        IOTA(P, itmp[64:128, 0:1], [[0, 1]], 0, 1, ["itmp"], ["itmp"])
        CP(P, "dve", ftmp[:, 0:1], itmp[:, 0:1], ["itmp"], ["ftmp"])
        for h in range(6):
            TS(P, "dve", bias_sel[:, h:h + 1], ftmp[:, 0:1], SLOPES[h], None, ALU.mult, None, ["ftmp"], ["bias_sel"])
        IOTA(P, itmp[:, 0:5], [[128, 5]], -512, 1, ["itmp", "ftmp"], ["itmp"])
        CP(P, "dve", ftmp[:, 0:5], itmp[:, 0:5], ["itmp"], ["ftmp"])
        for h in range(6):
            TS(P, "dve", bias_win[:, h, :], ftmp[:, 0:5], SLOPES[h], None, ALU.mult, None, ["ftmp"], ["bias_win"])
        IOTA(P, itmp[:, 0:64], [[2048, 2], [-128, 32]], 31, 16, ["itmp", "ftmp"], ["itmp"])
        CP(P, "dve", ftmp[:, 0:64], itmp[:, 0:64], ["itmp"], ["ftmp"])
        for h in range(6):
            TS(P, "dve", bias_cmp[:, h, :], ftmp[:, 0:64], SLOPES[h], None, ALU.mult, None, ["ftmp"], ["bias_cmp"])
        IOTA(P, IOTi[64:128, :], [[-1, 128]], 0, 64, [], ["IOTi"])
        CP(P, "dve", IOT[64:128, :], IOTi[64:128, :], ["IOTi"], ["IOT"])
        for g in range(2):
            MEMSET(P, "pool", vcaug[g][:], 1.0, [], [("vcaug", g)])
            for ci in range(2):
                ASEL(P, vcaug[g][:, ci, 65:129], vcaug[g][:, ci, 65:129], [[-4, 64]], ALU.is_ge, 0.0, 128 * ci, 1,
                     [("vcaug", g)], [("vcaug", g)])
                ASEL(P, vcaug[g][:, ci, 65:129], vcaug[g][:, ci, 65:129], [[4, 64]], ALU.is_ge, 0.0, 3 - 128 * ci, -1,
                     [("vcaug", g)], [("vcaug", g)])
        for s_ in selb.tiles:
            pass
        for j in range(4):
            MEMSET(P, "dve", selb.tiles[j][:, 0:64], 0.0, [], [("selp", j)])
        for g in range(2):
            kview = kcmp[g][:].rearrange("p (n s) -> p n s", s=16)
            vview = vcmp[g][:].rearrange("p (n s) -> p n s", s=16)
            for l in range(32):
                tmp, tk = tmpb.next()
                TS(P, "dve" if l % 2 == 0 else "pool", tmp[:, 0:255], kview[:, l // 16:l // 16 + 255, l % 16],
                   pe[0][:, l:l + 1], None, ALU.add, None, [("kcmp", g), "pe0"], [tk])
                MM(P, pk[0:64, 0:255], cw[0][:, l, :], tmp[:, 0:255], l == 0, l == 31, [("cw", 0), tk], [("pstrC", 0)])
            CP(P, "dve", kcT[g][:, 0:255], pk[0:64, 0:255], [("pstrC", 0)], [("kcT", g)])
            for ci in range(2):
                rows = 128 if ci == 0 else 127
                for l in range(32):
                    tmp, tk = tmpb.next()
                    n0 = l // 16 + ci * 128
                    TS(P, "dve" if l % 2 == 0 else "pool", tmp[:, 0:rows], vview[:, n0:n0 + rows, l % 16],
                       pe[1][:, l:l + 1], None, ALU.add, None, [("vcmp", g), "pe1"], [tk])
                    MM(P, pk[0:rows, 256:320], tmp[:, 0:rows], cw[1][:, l, :], l == 0, l == 31, [("cw", 1), tk], [("pstrC", 0)])
                CP(P, "dve", vcaug[g][0:rows, ci, 0:64], pk[0:rows, 256:320], [("pstrC", 0)], [("vcaug", g)])

        def consume(pacc, pacck, o, ok, h, i, gcol, first):
            w, wk = wb.next()
            TS(P, "dve", w[:, 0:1], pacc[:, 64:65], 1e-30, None, ALU.max, None, [pacck], [wk])
            RECIP(P, w[:, 1:2], w[:, 0:1], [wk], [wk])
            TT(P, "dve", w[:, 2:3], w[:, 1:2], gates[:, i, gcol:gcol + 1], ALU.mult, [wk, "gates"], [wk])
            if first:
                TS(P, "dve", o[:, 64 * h:64 * h + 64], pacc[:, 0:64], w[:, 2:3], None, ALU.mult, None, [pacck, wk], [(ok, h)])
            else:
                STT(P, o[:, 64 * h:64 * h + 64], pacc[:, 0:64], w[:, 2:3], o[:, 64 * h:64 * h + 64], ALU.mult, ALU.add,
                    [pacck, wk, (ok, h)], [(ok, h)])
            return w, wk

        from collections import deque
        fifo = deque()
        LAGC = 3

        def defer(fn):
            fifo.append(fn)
            while len(fifo) > LAGC:
                fifo.popleft()()

        def flush():
            while fifo:
                fifo.popleft()()

        def consume_cmp(pc, pck, o, ok, h, i, hh, imp, impk):
            w, wk = consume(pc, pck, o, ok, h, i, 3 * h + 0, True)
            if hh == 0:
                TS(P, "dve", imp[:], pc[:, 65:129], w[:, 1:2], None, ALU.mult, None, [pck, wk], [impk])
            else:
                STT(P, imp[:], pc[:, 65:129], w[:, 1:2], imp[:], ALU.mult, ALU.add, [pck, wk, impk], [impk])

        for i in range(NT):
            t0 = 128 * i
            qc = slice(t0, t0 + 128)
            o, ok = obuf.next()
            for g in range(2):
                imp, impk = impb.next()
                for hh in range(3):
                    h = 3 * g + hh
                    nvalid = min(255, (t0 + 96) // 16 + 1)
                    chunks = [(0, min(128, nvalid))] + ([(1, nvalid - 128)] if nvalid > 128 else [])
                    pc, pck = psacc.next()
                    for ni, (ci, rows) in enumerate(chunks):
                        ps_, psk = pss.next()
                        MM(P, ps_[0:rows, 0:128], kcT[g][:, ci * 128:ci * 128 + rows], qaug[h][0:64, qc], True, True,
                           [("kcT", g), ("q", h)], [psk])
                        e32, e32k = e32b.next()
                        ACT(P, e32[0:rows, :], ps_[0:rows, 0:128], AF.Exp, [psk, "bias_cmp"], [e32k],
                            bias=bias_cmp[0:rows, h, ci * 32 + i:ci * 32 + i + 1])
                        eb, ebk = ebb.next()
                        ASEL(P, eb[0:rows, :], e32[0:rows, :], [[1, 128]], ALU.is_ge, 0.0, t0 - 2048 * ci - 31, -16,
                             [e32k], [ebk])
                        defer(lambda pc=pc, pck=pck, eb=eb, ebk=ebk, rows=rows, ci=ci, g=g, st_=(ni == 0), sp_=(ni == len(chunks) - 1):
                              MM(P, pc[:, 0:129], eb[0:rows, :], vcaug[g][0:rows, ci, :], st_, sp_, [ebk, ("vcaug", g)], [pck]))
                    defer(lambda pc=pc, pck=pck, o=o, ok=ok, h=h, i=i, hh=hh, imp=imp, impk=impk:
                          consume_cmp(pc, pck, o, ok, h, i, hh, imp, impk))
                    pw, pwk = psacc.next()
                    cl = list(range(max(0, i - 4), i + 1))
                    for ni, cch in enumerate(cl):
                        dc = cch - i
                        ps_, psk = pss.next()
                        MM(P, ps_[:, 0:128], kwin[g][:, cch * 128:(cch + 1) * 128], qaug[h][0:64, qc], True, True,
                           [("kwin", g), ("q", h)], [psk])
                        eb, ebk = ebb.next()
                        bw = bias_win[:, h, dc + 4:dc + 5]
                        if dc == 0 or dc == -4:
                            e32, e32k = e32b.next()
                            ACT(P, e32[:], ps_[:, 0:128], AF.Exp, [psk, "bias_win"], [e32k], bias=bw)
                            if dc == 0:
                                ASEL(P, eb[:], e32[:], [[1, 128]], ALU.is_ge, 0.0, 0, -1, [e32k], [ebk])
                            else:
                                ASEL(P, eb[:], e32[:], [[-1, 128]], ALU.is_gt, 0.0, 0, 1, [e32k], [ebk])
                        else:
                            ACT(P, eb[:], ps_[:, 0:128], AF.Exp, [psk, "bias_win"], [ebk], bias=bw)
                        defer(lambda pw=pw, pwk=pwk, eb=eb, ebk=ebk, cch=cch, g=g, st_=(ni == 0), sp_=(ni == len(cl) - 1):
                              MM(P, pw[:, 0:65], eb[:], vwin[:, cch, g, :], st_, sp_, [ebk, "vwin"], [pwk]))
                    defer(lambda pw=pw, pwk=pwk, o=o, ok=ok, h=h, i=i: consume(pw, pwk, o, ok, h, i, 3 * h + 2, False))
                flush()
                ASEL(P, imp[:], imp[:], [[-64, 64]], ALU.is_ge, 1e4, t0 - 128, 1, [impk], [impk])
                ASEL(P, imp[:], imp[:], [[-64, 64]], ALU.is_ge, -1.0, t0, 1, [impk], [impk])
                MEMSET(P, "pool", imp[:, 0:1], 1e4, [impk], [impk])
                m8, m8k = m8b.next()
                rep, repk = repb.next()
                P.op("dve", lambda e, m8=m8, imp=imp: e.max(out=m8[:, 0:8], in_=imp[:]), [impk], [m8k])
                P.op("dve", lambda e, m8=m8, imp=imp, rep=rep: e.match_replace(out=rep[:], in_to_replace=m8[:, 0:8],
                                                                                 in_values=imp[:], imm_value=-1e30),
                     [impk, m8k], [repk])
                P.op("dve", lambda e, m8=m8, rep=rep: e.max(out=m8[:, 8:16], in_=rep[:]), [repk, m8k], [m8k])
                selp, selk = selb.next()
                TS(P, "dve", selp[:, 64:128], imp[:], m8[:, 15:16], None, ALU.is_ge, None, [impk, m8k], [selk])

                def sel_tail(selp=selp, selk=selk, g=g, i=i, t0=t0, qc=qc):
                    pt_, ptk = pstr.next()
                    TR(P, pt_[:, 0:128], selp[:], c.ident[:], [selk, "ident"], [ptk])
                    for hh in range(3):
                        h = 3 * g + hh
                        Ct, Ctk = Ctb.next()
                        TS(P, "pool", Ct[64:128, :], IOT[64:128, :], SLOPES[h], -BIG - SLOPES[h] * t0, ALU.mult, ALU.add,
                           ["IOT"], [Ctk])
                        STT(P, qaug[h][64:128, qc], pt_[64:128, 0:128], BIG, Ct[64:128, :], ALU.mult, ALU.add,
                            [ptk, Ctk], [("qm", h, i)])
                defer(sel_tail)

            otiles[i] = (o, ok)
            if i % 4 != 3:
                continue
            G4 = i // 4
            t0g = 512 * G4
            flush()
            for g in range(2):
                for hh in range(3):
                    h = 3 * g + hh
                    psl, pslk = psacc.next()
                    nchk = 4 * G4 + 4
                    for cch in range(nchk):
                        jmin = max(0, cch - 4 * G4)
                        ncol = (4 - jmin) * 128
                        qcs = slice(t0g + jmin * 128, t0g + 512)
                        ps_, psk = pss.next()
                        MM(P, ps_[:, 0:ncol], kslc[g][:, cch * 128:(cch + 1) * 128], qaug[h][:, qcs], True, True,
                           [("kslc", g), ("kx", g), ("q", h)] + [("qm", h, 4 * G4 + j) for j in range(jmin, 4)], [psk])
                        ew, ewk = ewb.next()
                        ACT(P, ew[:, 0:ncol], ps_[:, 0:ncol], AF.Exp, [psk, "bias_sel"], [ewk], bias=bias_sel[:, h:h + 1])
                        if cch >= 4 * G4:
                            ASEL(P, ew[:, 0:128], ew[:, 0:128], [[1, 128]], ALU.is_ge, 0.0, 0, -1, [ewk], [ewk])

                        def av(psl=psl, pslk=pslk, ew=ew, ewk=ewk, cch=cch, jmin=jmin, g=g, G4=G4):
                            for j in range(jmin, 4):
                                MM(P, psl[:, j * 128:j * 128 + 65], ew[:, (j - jmin) * 128:(j - jmin + 1) * 128],
                                   vslc[:, cch, g, :], cch == 0 and j == 0, cch == 4 * G4 + j, [ewk, "vslc"], [pslk], sgc=True)
                        defer(av)
                    for j in range(4):
                        ti = 4 * G4 + j
                        defer(lambda psl=psl, pslk=pslk, j=j, ti=ti, h=h:
                              consume(psl[:, j * 128:j * 128 + 65], pslk, otiles[ti][0], otiles[ti][1], h, ti, 3 * h + 1, False))
            for ti in range(4 * G4, 4 * G4 + 4):
                def finish_tile(o=otiles[ti][0], ok=otiles[ti][1], qc=slice(128 * ti, 128 * ti + 128)):
                    pt2, pt2k = pstr.next()
                    for j in range(3):
                        TR(P, pt2[:, j * 128:(j + 1) * 128], o[:, j * 128:(j + 1) * 128], c.ident[:],
                           [(ok, 2 * j), (ok, 2 * j + 1), "ident"], [pt2k])
                    ost, ostk = ostb.next()
                    CP(P, "act", ost[:], pt2[:, 0:384].rearrange("p (j t) -> p j t", j=3), [pt2k], [ostk])
                    DMA(P, "sp", c.NSAOT.rearrange("(j p) t -> p j t", p=128)[:, :, qc], ost[:], [ostk], ["NSAOT"])
                defer(finish_tile)
        flush()
        P.end_phase()
        P.alloc_sems(c.es0, c.sems)
        with nc.Block() as block:
            P.emit(block, c.sems)


def phase_D(c):
    nc, P = c.nc, c.P
    with ExitStack() as es:
        sb = lambda name, shape, dt: es.enter_context(nc.sbuf_tensor(name, shape, dt))
        memt = sb("memt", [128, 2, D], F32)
        mems = sb("mems", [128, 2, D], F32)
        junk = sb("junkD", [128, D], F32)
        ssb = [sb(f"ssD{i}", [128, 4], F32) for i in range(2)]
        gcol = sb("gcolD", [128, 8], F32)
        mhT = sb("mhT", [128, 8, 256], BF16)
        wst = Rot([sb(f"wstD{i}", [128, 512], F32) for i in range(2)], "wstD")
        Wkv = sb("Wkv", [128, 8, 512], BF16)
        mkT = [sb(f"mkT{h}", [64, 256], BF16) for h in range(4)]
        mvaug = sb("mvaug", [128, 2, 4, 65], BF16)
        qm = [sb(f"qm{h}", [64, T], BF16) for h in range(4)]
        eb = Rot([sb(f"ebD{i}", [128, 512], BF16) for i in range(4)], "ebD")
        ob = Rot([sb(f"oD{i}", [128, 4, 256], F32) for i in range(2)], "oD")
        wb = Rot([sb(f"wD{i}", [128, 2], F32) for i in range(4)], "wD")
        ostb = Rot([sb(f"ostD{i}", [128, 2, 512], BF16) for i in range(2)], "ostD")
        pss = Rot(c.ps[0:2], "pssD")
        psacc = Rot(c.ps[2:5], "psaccD")
        pstr = Rot(c.ps[5:7], "pstrD")
        pmisc = c.ps[7]

        DMA(P, "sp", gcol[:], c.inp["mem_g"], [], ["gcol"])
        DMA(P, "sp", memt[:], c.inp["mem"].rearrange("(c p) d -> p c d", p=128), [], ["memt"])
        for h in range(4):
            DMA(P, "sp", qm[h][:], c.FM[FM_QMEM + 64 * h:FM_QMEM + 64 * (h + 1), :], ["FM"], [("qm", h)])
        for kc in range(8):
            st, sk = wst.next()
            DMA(P, "sp", st[:], c.inp["w_mem_kv"][kc * 128:(kc + 1) * 128, :], [], [sk])
            TS(P, "dve", Wkv[:, kc, :], st[:], gcol[:, kc:kc + 1], None, ALU.mult, None, [sk, "gcol"], [("Wkv", kc)])
        Wk = [("Wkv", kc) for kc in range(8)]
        for ci in range(2):
            ss = ssb[ci]
            ssk = ("ssD", ci)
            ACT(P, junk[:], memt[:, ci, :], AF.Square, ["memt"], ["junkD", ssk], accum=ss[:, 0:1])
            rstd_chain(P, ss, ssk)
            TS(P, "dve", mems[:, ci, :], memt[:, ci, :], ss[:, 3:4], None, ALU.mult, None, ["memt", ssk], [("mems", ci)])
            for half in range(2):
                pt, ptk = pstr.next()
                for j in range(4):
                    cc = half * 4 + j
                    TR(P, pt[:, j * 128:(j + 1) * 128], mems[:, ci, cc * 128:(cc + 1) * 128], c.ident[:],
                       [("mems", ci), "ident"], [ptk])
                CP(P, "dve", mhT[:, half * 4:(half + 1) * 4, ci * 128:(ci + 1) * 128],
                   pt[:].rearrange("p (j t) -> p j t", j=4), [ptk], [("mhT", ci, half)])
        mh = [("mhT", ci, half) for ci in range(2) for half in range(2)]
        for h in range(4):
            for kc in range(8):
                MM(P, pmisc[0:64, 0:256], Wkv[:, kc, 64 * h:64 * h + 64], mhT[:, kc, :], kc == 0, kc == 7, Wk + mh, ["pmisc"])
            CP(P, "dve", mkT[h][:], pmisc[0:64, 0:256], ["pmisc"], [("mkT", h)])
        for ci in range(2):
            for kc in range(8):
                MM(P, pmisc[:, 256:512], mhT[:, kc, ci * 128:(ci + 1) * 128], Wkv[:, kc, 256:512], kc == 0, kc == 7,
                   Wk + mh, ["pmisc"])
            CP(P, "dve", mvaug[:, ci, :, 0:64], pmisc[:, 256:512].rearrange("p (h d) -> p h d", h=4), ["pmisc"], ["mvaug"])
        MEMSET(P, "dve", mvaug[:, :, :, 64:65], 1.0, ["mvaug"], ["mvaug"])

        for g in range(8):
            o, ok = ob.next()
            for h in range(4):
                es_ = []
                for ci in range(2):
                    ps_, psk = pss.next()
                    MM(P, ps_[:, :], mkT[h][:, ci * 128:(ci + 1) * 128], qm[h][:, g * 512:(g + 1) * 512], True, True,
                       [("mkT", h), ("qm", h)], [psk])
                    e, ek = eb.next()
                    ACT(P, e[:], ps_[:, :], AF.Exp, [psk], [ek])
                    es_.append((e, ek))
                for sub in range(4):
                    pa, pak = psacc.next()
                    for ci in range(2):
                        MM(P, pa[:, 0:65], es_[ci][0][:, sub * 128:(sub + 1) * 128], mvaug[:, ci, h, :], ci == 0, ci == 1,
                           [es_[ci][1], "mvaug"], [pak])
                    w, wk = wb.next()
                    RECIP(P, w[:, 0:1], pa[:, 64:65], [pak], [wk])
                    TS(P, "dve", o[:, sub, 64 * h:64 * h + 64], pa[:, 0:64], w[:, 0:1], None, ALU.mult, None, [pak, wk],
                       [(ok, sub, h)])
            ost, ostk = ostb.next()
            for sub in range(4):
                pt, ptk = pstr.next()
                for j in range(2):
                    TR(P, pt[:, j * 128:(j + 1) * 128], o[:, sub, j * 128:(j + 1) * 128], c.ident[:],
                       [(ok, sub, 2 * j), (ok, sub, 2 * j + 1), "ident"], [ptk])
                CP(P, "act", ost[:, :, sub * 128:(sub + 1) * 128], pt[:, 0:256].rearrange("p (j t) -> p j t", j=2),
                   [ptk], [(ostk, sub)])
            DMA(P, "sp", c.MEMOT.rearrange("(j p) t -> p j t", p=128)[:, :, g * 512:(g + 1) * 512], ost[:],
                [(ostk, sub) for sub in range(4)], ["MEMOT"])
        P.end_phase()
        P.alloc_sems(c.es0, c.sems)
        with nc.Block() as block:
            P.emit(block, c.sems)


def phase_E(c):
    nc, P = c.nc, c.P
    with ExitStack() as es:
        sb = lambda name, shape, dt: es.enter_context(nc.sbuf_tensor(name, shape, dt))
        Wmg = sb("Wmg", [128, 8, 3072], BF16)
        Wbr = [sb("Wsb", [128, 3, D], BF16), sb("Wnsa", [128, 3, D], BF16), sb("Wmem", [128, 2, D], BF16)]
        Wout = sb("Wout", [128, 8, D], BF16)
        wst = Rot([sb(f"wstE{i}", [128, 1024], F32) for i in range(3)], "wstE")
        gcol = sb("gcolE", [128, 8], F32)
        bmg = sb("bmg", [128, 24], F32)
        hTb = Rot([sb(f"hTE{i}", [128, 8, 512], BF16) for i in range(2)], "hTE")
        srcb = [Rot([sb(f"srcE{b}_{i}", [128, 3 if b < 2 else 2, 512], BF16) for i in range(2)], f"srcE{b}") for b in range(3)]
        mgb = Rot([sb(f"mgT{i}", [128, 8, 512], BF16) for i in range(2)], "mgT")
        gateb = Rot([sb(f"gateE{i}", [128, 512], F32) for i in range(3)], "gateE")
        accb = Rot([sb(f"accE{i}", [128, 512], F32) for i in range(2)], "accE")
        tmpb = Rot([sb(f"tmpE{i}", [128, 512], F32) for i in range(2)], "tmpE")
        xb = Rot([sb(f"xE{i}", [128, D], F32) for i in range(2)], "xE")
        x1b = Rot([sb(f"x1E{i}", [128, D], F32) for i in range(2)], "x1E")
        psbr = Rot(c.ps[0:2], "psbr")
        psg = Rot(c.ps[2:5], "psg")
        psy = Rot(c.ps[5:8], "psy")

        DMA(P, "sp", gcol[:], c.inp["mix_g"], [], ["gcol"])
        DMA(P, "sp", bmg[:], c.inp["b_merge"], [], ["bmg"])
        n = 0
        for kc in range(8):
            for j in range(3):
                st, sk = wst.next()
                DMA(P, "sp", st[:], c.inp["w_in"][kc * 128:(kc + 1) * 128, 2578 + 1024 * j:2578 + 1024 * (j + 1)], [], [sk])
                if n % 2 == 0:
                    TS(P, "dve", Wmg[:, kc, 1024 * j:1024 * (j + 1)], st[:], gcol[:, kc:kc + 1], None, ALU.mult, None,
                       [sk, "gcol"], [("Wmg", kc)])
                else:
                    ACT(P, Wmg[:, kc, 1024 * j:1024 * (j + 1)], st[:], AF.Copy, [sk, "gcol"], [("Wmg", kc)],
                        scale=gcol[:, kc:kc + 1])
                n += 1
        for b, (nm, nf) in enumerate((("w_sb_br", 3), ("w_nsa_br", 3), ("w_mem_br", 2))):
            for f in range(nf):
                st, sk = wst.next()
                DMA(P, "sp", st[:], c.inp[nm][f * 128:(f + 1) * 128, :], [], [sk])
                CP(P, "dve" if n % 2 == 0 else "act", Wbr[b][:, f, :], st[:], [sk], [("Wbr", b)])
                n += 1
        for kc in range(8):
            st, sk = wst.next()
            DMA(P, "sp", st[:], c.inp["w_out"][kc * 128:(kc + 1) * 128, :], [], [sk])
            CP(P, "dve" if n % 2 == 0 else "act", Wout[:, kc, :], st[:], [sk], ["Wout"])
            n += 1
        Wmgk = [("Wmg", kc) for kc in range(8)]
        srcs = [(c.SBOT, 3, "SBOT"), (c.NSAOT, 3, "NSAOT"), (c.MEMOT, 2, "MEMOT")]
        for tg in range(8):
            tc_ = slice(tg * 512, (tg + 1) * 512)
            hT, hk = hTb.next()
            DMA(P, "sp", hT[:], c.HT.rearrange("(c p) t -> p c t", p=128)[:, :, tc_], ["HT"], [hk])
            src = []
            for b, (ap, nf, nm) in enumerate(srcs):
                t_, tk = srcb[b].next()
                DMA(P, "sp", t_[:], ap.rearrange("(f p) t -> p f t", p=128)[:, :, tc_], [nm], [tk])
                src.append((t_, tk, nf))
            mg, mgk = mgb.next()
            for dc in range(8):
                acc, acck = accb.next()
                for b in range(3):
                    t_, tk, nf = src[b]
                    pb, pbk = psbr.next()
                    for f in range(nf):
                        MM(P, pb[:, :], Wbr[b][:, f, dc * 128:(dc + 1) * 128], t_[:, f, :], f == 0, f == nf - 1,
                           [("Wbr", b), tk], [pbk])
                    pg, pgk = psg.next()
                    for kc in range(8):
                        MM(P, pg[:, :], Wmg[:, kc, b * 1024 + dc * 128:b * 1024 + (dc + 1) * 128], hT[:, kc, :], kc == 0, kc == 7,
                           Wmgk + [hk], [pgk])
                    gt, gtk = gateb.next()
                    ACT(P, gt[:], pg[:, :], AF.Sigmoid, [pgk, "bmg"], [gtk], bias=bmg[:, b * 8 + dc:b * 8 + dc + 1])
                    if b == 0:
                        TT(P, "dve", acc[:], gt[:], pb[:, :], ALU.mult, [gtk, pbk], [acck])
                    else:
                        tmp, tmpk = tmpb.next()
                        TT(P, "dve", tmp[:], gt[:], pb[:, :], ALU.mult, [gtk, pbk], [tmpk])
                        if b == 1:
                            TT(P, "pool", acc[:], acc[:], tmp[:], ALU.add, [acck, tmpk], [acck])
                        else:
                            TT(P, "pool", mg[:, dc, :], acc[:], tmp[:], ALU.add, [acck, tmpk], [(mgk, dc)])
            mgks = [(mgk, dc) for dc in range(8)]
            for s in range(4):
                i = tg * 4 + s
                xt, xk = xb.next()
                DMA(P, "sp", xt[:], c.inp["x"][i * 128:(i + 1) * 128, :], [], [xk])
                x1, x1k = x1b.next()
                for half in range(2):
                    py, pyk = psy.next()
                    for dc in range(8):
                        MM(P, py[:, :], mg[:, dc, s * 128:(s + 1) * 128], Wout[:, dc, half * 512:(half + 1) * 512], dc == 0, dc == 7,
                           mgks + ["Wout"], [pyk])
                    TT(P, "dve", x1[:, half * 512:(half + 1) * 512], xt[:, half * 512:(half + 1) * 512], py[:, :], ALU.add,
                       [xk, pyk], [(x1k, half)])
                DMA(P, "sp", c.X1[i * 128:(i + 1) * 128, :], x1[:], [(x1k, 0), (x1k, 1)], ["X1"])
        P.end_phase()
        P.alloc_sems(c.es0, c.sems)
        with nc.Block() as block:
            P.emit(block, c.sems)


def phase_F(c):
    nc, P = c.nc, c.P
    GS = 8
    with ExitStack() as es:
        sb = lambda name, shape, dt: es.enter_context(nc.sbuf_tensor(name, shape, dt))
        Wq = sb("Wq", [128, 8, 2048], BF16)
        subk = sb("subk", [128, 16, 128], BF16)
        g2b = sb("g2b", [128, D], F32)
        gFb = sb("gFb", [128, D], F32)
        keyidx = sb("keyidx", [128, 2048], I32)
        posidx = sb("posidx", [128, 2048], I32)
        iotaA = sb("iotaA", [128, 2048], F32)
        cI = sb("cI", [128, 8], I32)
        with ExitStack() as es1:
            sb1 = lambda name, shape, dt: es1.enter_context(nc.sbuf_tensor(name, shape, dt))
            wst = Rot([sb1(f"wstF{i}", [128, 2048], F32) for i in range(2)], "wstF")
            iotaAi = sb1("iotaAi", [128, 2048], I32)
            for kc in range(8):
                st, sk = wst.next()
                DMA(P, "sp", st[:], c.inp["peer_w_q"][kc * 128:(kc + 1) * 128, :], [], [sk])
                CP(P, "dve" if kc % 2 == 0 else "act", Wq[:, kc, :], st[:], [sk], [("Wq", kc)])
            st, sk = wst.next()
            DMA(P, "sp", st[:], c.inp["subkT"].rearrange("d b k -> d (b k)"), [], [sk])
            CP(P, "dve", subk[:].rearrange("d b k -> d (b k)"), st[:], [sk], ["subk"])
            DMA(P, "sp", g2b[:], c.inp["ffn_g"].partition_broadcast(128), [], ["g2b"])
            DMA(P, "sp", gFb[:], c.inp["final_g"].partition_broadcast(128), [], ["gFb"])
            IOTA(P, keyidx[:], [[0, 16], [1, 128]], 0, 0, [], ["keyidx"])
            IOTA(P, posidx[:], [[0, 8], [1, 256]], 0, 0, [], ["posidx"])
            IOTA(P, iotaAi[:], [[0, 128], [1, 16]], 0, 0, [], ["iotaAi"])
            CP(P, "dve", iotaA[:], iotaAi[:], ["iotaAi"], ["iotaA"])
            for j, v in enumerate((-128, -256, 127, 255, 15, 4)):
                IOTA(P, cI[:, j:j + 1], [[0, 1]], v, 0, ["cI"], ["cI"])
            P.end_phase()
            P.alloc_sems(c.es0, c.sems)
            with nc.Block() as block:
                P.emit(block, c.sems)
        x1b = Rot([sb(f"x1F{i}", [128, D], F32) for i in range(2)], "x1F")
        h2b = Rot([sb(f"h2F{i}", [128, D], F32) for i in range(1)], "h2F")
        ssb = Rot([sb(f"ssF{i}", [128, 4], F32) for i in range(4)], "ssF")
        junk = sb("junkF", [128, D], BF16)
        prodb = Rot([sb(f"prodF{i}", [128, D], BF16) for i in range(4)], "prodF")
        h2hb = Rot([sb(f"h2hF{i}", [128, D], BF16) for i in range(2)], "h2hF")
        h2Tb = Rot([sb(f"h2T{i}", [128, 8, 128], BF16) for i in range(2)], "h2T")
        qTb = sb("qTbF", [128, 16, 128], BF16)
        Sc = sb("Sc", [128, 2048], F32)
        rep = sb("repF", [128, 256], F32)
        stop = sb("stop", [128, 16, 16], F32)
        itop_i = sb("itop_i", [128, 256], I32)
        itop_f = sb("itop_f", [128, 16, 16], F32)
        tmpA = sb("tmpA", [128, 2048], F32)
        tmpB = Sc
        best = sb("best", [128, 8, 16], F32)
        pos_i = sb("pos_i", [128, 3, 128], I32)
        ab_f = sb("ab_f", [128, 2, 128], F32)
        sel_f = sb("sel_f", [128, 3, 128], F32)
        idxb = Rot([sb(f"idxF{i}", [128, 128], I32) for i in range(2)], "idxF")
        gwb = Rot([sb(f"gwF{i}", [128, 3, 128], F32) for i in range(2)], "gwF")
        gsum = sb("gsum", [128, 16], F32)
        ab = Rot([sb(f"aF{i}", [128, 3, 128], F32) for i in range(2)], "aF")
        uvb = Rot([sb(f"uvg{i}", [128, 2 * D], BF16) for i in range(16)], "uvg")
        dgb = Rot([sb(f"dg{i}", [128, 4, 128], BF16) for i in range(3)], "dg")
        x2b = Rot([sb(f"x2F{i}", [128, D], F32) for i in range(1)], "x2F")
        ptq = Rot(c.ps[0:2], "ptq")
        psS = Rot(c.ps[2:4], "psS")
        pvb = Rot([(c.ps[4], c.ps[5]), (c.ps[6], c.ps[7])], "pv")
        Wqk = [("Wq", kc) for kc in range(8)]

        def route(i, st):
            x1, x1k = x1b.next()
            DMA(P, "sp", x1[:], c.X1[i * 128:(i + 1) * 128, :], ["X1"], [x1k])
            ss, ssk = ssb.next()
            ACT(P, junk[:], x1[:], AF.Square, [x1k], [ssk], accum=ss[:, 0:1])
            rstd_chain(P, ss, ssk)
            h2, h2k = h2b.next()
            STT(P, h2[:], x1[:], ss[:, 3:4], g2b[:], ALU.mult, ALU.mult, [x1k, ssk, "g2b"], [h2k])
            h2h, h2hk = h2hb.next()
            CP(P, "act", h2h[:], h2[:], [h2k], [h2hk])
            yield
            h2T, h2Tk = h2Tb.next()
            for half in range(2):
                pt, ptk = ptq.next()
                for j in range(4):
                    cc = half * 4 + j
                    TR(P, pt[:, j * 128:(j + 1) * 128], h2[:, cc * 128:(cc + 1) * 128], c.ident[:], [h2k, "ident"], [ptk])
                CP(P, "act", h2T[:, half * 4:(half + 1) * 4, :], pt[:].rearrange("p (j t) -> p j t", j=4), [ptk], [(h2Tk, half)])
            h2Tks = [(h2Tk, 0), (h2Tk, 1)]
            yield
            for b4 in range(4):
                pq, pqk = ptq.next()
                for j in range(4):
                    blk = b4 * 4 + j
                    for kc in range(8):
                        MM(P, pq[:, j * 128:(j + 1) * 128], Wq[:, kc, blk * 128:(blk + 1) * 128], h2T[:, kc, :], kc == 0, kc == 7,
                           Wqk + h2Tks, [pqk])
                CP(P, "act", qTb[:, b4 * 4:(b4 + 1) * 4, :], pq[:].rearrange("p (j t) -> p j t", j=4), [pqk], [("qTb", b4)])
                yield
            for b4 in range(4):
                pS, pSk = psS.next()
                for j in range(4):
                    blk = b4 * 4 + j
                    MM(P, pS[:, j * 128:(j + 1) * 128], qTb[:, blk, :], subk[:, blk, :], True, True, [("qTb", b4), "subk"], [pSk])
                STT(P, Sc[:, b4 * 512:(b4 + 1) * 512].bitcast(I32), pS[:, :].bitcast(I32), cI[:, 0:1],
                    keyidx[:, b4 * 512:(b4 + 1) * 512], ALU.bitwise_and, ALU.bitwise_or, [pSk, "cI", "keyidx"], [("Sc", b4), "tmpB"])
                yield
            for blk in range(16):
                sblk = Sc[:, blk * 128:(blk + 1) * 128]
                sck = ("Sc", blk // 4)
                P.op("dve", lambda e, o=stop[:, blk, 0:8], s=sblk: e.max(out=o, in_=s), [sck], ["stop"])
                P.op("dve", lambda e, o=rep[:, 0:128], m=stop[:, blk, 0:8], s=sblk: e.match_replace(
                    out=o, in_to_replace=m, in_values=s, imm_value=-1e30), [sck, "stop"], ["repF"])
                P.op("dve", lambda e, o=stop[:, blk, 8:16], s=rep[:, 0:128]: e.max(out=o, in_=s), ["repF"], ["stop"])
                yield
            stop2 = stop[:].rearrange("p b k -> p (b k)")
            TS(P, "dve", itop_i[:], stop2.bitcast(I32), cI[:, 2:3], None, ALU.bitwise_and, None, ["stop", "cI"], ["itop_i"])
            CP(P, "dve", itop_f[:].rearrange("p b k -> p (b k)"), itop_i[:], ["itop_i"], ["itop_f"])
            yield
            sv = stop[:].rearrange("p (h q) k -> p h q k", q=2)
            iv = itop_f[:].rearrange("p (h q) k -> p h q k", q=2)
            cand = tmpA[:].rearrange("p (h a b) -> p h a b", h=8, a=16)
            TT(P, "dve", cand, sv[:, :, 0, :].unsqueeze(3).to_broadcast([128, 8, 16, 16]),
               sv[:, :, 1, :].unsqueeze(2).to_broadcast([128, 8, 16, 16]), ALU.add, ["stop"], ["tmpA"])
            yield
            STT(P, tmpB[:].bitcast(I32), tmpA[:].bitcast(I32), cI[:, 1:2], posidx[:], ALU.bitwise_and, ALU.bitwise_or,
                ["tmpA", "cI", "posidx"], ["tmpB"] + [("Sc", b_) for b_ in range(4)])
            yield
            for h in range(8):
                sblk = tmpB[:, h * 256:(h + 1) * 256]
                P.op("dve", lambda e, o=best[:, h, 0:8], s=sblk: e.max(out=o, in_=s), ["tmpB"], ["best"])
                P.op("dve", lambda e, o=rep[:], m=best[:, h, 0:8], s=sblk: e.match_replace(
                    out=o, in_to_replace=m, in_values=s, imm_value=-1e30), ["tmpB", "best"], ["repF"])
                P.op("dve", lambda e, o=best[:, h, 8:16], s=rep[:]: e.max(out=o, in_=s), ["repF"], ["best"])
                yield
            best2 = best[:].rearrange("p h k -> p (h k)")
            TS(P, "dve", pos_i[:, 0, :], best2.bitcast(I32), cI[:, 3:4], None, ALU.bitwise_and, None, ["best", "cI"], ["pos_i"])
            TS(P, "dve", pos_i[:, 1, :], pos_i[:, 0, :], cI[:, 5:6], None, ALU.logical_shift_right, None, ["pos_i", "cI"], ["pos_i"])
            TS(P, "dve", pos_i[:, 2, :], pos_i[:, 0, :], cI[:, 4:5], None, ALU.bitwise_and, None, ["pos_i", "cI"], ["pos_i"])
            CP(P, "dve", ab_f[:], pos_i[:, 1:3, :], ["pos_i"], ["ab_f"])
            yield
            for q in range(2):
                akv = ab_f[:, q, :].rearrange("p (h k) -> p h k", h=8)
                eq = tmpA[:].rearrange("p (h k a) -> p h k a", h=8, k=16)
                TT(P, "dve", eq, akv.unsqueeze(3).to_broadcast([128, 8, 16, 16]),
                   iotaA[:].rearrange("p (h k a) -> p h k a", h=8, k=16), ALU.is_equal, ["ab_f", "iotaA"], ["tmpA"])
                yield
                pr = tmpB[:].rearrange("p (h k a) -> p h k a", h=8, k=16)
                TT(P, "dve", pr, eq, iv[:, :, q, :].unsqueeze(2).to_broadcast([128, 8, 16, 16]), ALU.mult,
                   ["tmpA", "itop_f"], ["tmpB"] + [("Sc", b_) for b_ in range(4)])
                yield
                P.op("dve", lambda e, o=sel_f[:, q, :], s=tmpB[:].rearrange("p (x a) -> p x a", a=16): e.tensor_reduce(
                    out=o, in_=s, axis=AX.X, op=ALU.add), ["tmpB"], ["sel_f"])
                yield
            STT(P, sel_f[:, 2, :], sel_f[:, 0, :], 128.0, sel_f[:, 1, :], ALU.mult, ALU.add, ["sel_f"], ["sel_f"])
            TS(P, "dve", sel_f[:, 2, :], sel_f[:, 2, :], 0.0, 16383.0, ALU.max, ALU.min, ["sel_f"], ["sel_f"])
            idx, idxk = idxb.next()
            CP(P, "dve", idx[:], sel_f[:, 2, :], ["sel_f"], [idxk])
            yield
            gw, gwk = gwb.next()
            v3 = lambda ap: ap.rearrange("p (h k) -> p h k", h=8)
            TT(P, "dve", v3(gw[:, 0, :]), best[:], best[:, :, 0:1].to_broadcast([128, 8, 16]), ALU.subtract, ["best"], [gwk])
            ACT(P, gw[:, 1, :], gw[:, 0, :], AF.Exp, [gwk], [gwk])
            yield
            P.op("dve", lambda e, o=gsum[:, 0:8], s=v3(gw[:, 1, :]): e.tensor_reduce(out=o, in_=s, axis=AX.X, op=ALU.add),
                 [gwk], ["gsum"])
            RECIP(P, gsum[:, 8:16], gsum[:, 0:8], ["gsum"], ["gsum"])
            TT(P, "dve", v3(gw[:, 2, :]), v3(gw[:, 1, :]), gsum[:, 8:16].unsqueeze(2).to_broadcast([128, 8, 16]), ALU.mult,
               [gwk, "gsum"], [gwk])
            st.update(x1=x1, x1k=x1k, h2=h2h, h2k=h2hk, idx=idx, idxk=idxk, gw=gw, gwk=gwk)
            yield

        def slots(i, st, bg):
            x1, x1k, h2, h2k, idx, idxk, gw, gwk = (st[k_] for k_ in ("x1", "x1k", "h2", "h2k", "idx", "idxk", "gw", "gwk"))
            a, ak = ab.next()
            (pv0, pv1), pvk = pvb.next()
            LAG = 6
            GSZ = 4
            held = {}
            for s in range(128 + LAG):
                if s < 128:
                    uv, uvk = uvb.next()
                    held[s] = (uv, uvk)
                    P.dma("pool", lambda e, o=uv[:], ix=idx[:, s:s + 1]: e.indirect_dma_start(
                        out=o, out_offset=None, in_=c.UVB,
                        in_offset=bass.IndirectOffsetOnAxis(ap=ix.bitcast(U32), axis=0)), [idxk, "UVB"], [uvk])
                    pd, pdk = prodb.next()
                    TT(P, "dve", pd[:], uv[:, 0:D], h2[:], ALU.mult, [uvk, h2k], [pdk])
                    ACT(P, junk[:], pd[:], AF.Copy, [pdk], [(ak, s)], accum=a[:, 0, s:s + 1])
                    if s % GSZ == GSZ - 1:
                        gs_ = slice(s - GSZ + 1, s + 1)
                        ACT(P, a[:, 1, gs_], a[:, 0, gs_], AF.Gelu, [(ak, s_) for s_ in range(s - GSZ + 1, s + 1)],
                            [(ak, "g", s // GSZ)])
                r_ = s - LAG
                if r_ >= 0:
                    uv, uvk = held.pop(r_)
                    if r_ % GSZ == 0:
                        gq = slice(r_, r_ + GSZ)
                        TT(P, "dve", a[:, 2, gq], a[:, 1, gq], gw[:, 2, gq], ALU.mult, [(ak, "g", r_ // GSZ), gwk], [(ak, "w", r_ // GSZ)])
                        dg, dgk = dgb.next()
                        TT(P, "dve", dg[:], c.identb[:].unsqueeze(1).to_broadcast([128, GSZ, 128]),
                           a[:, 2, gq].unsqueeze(2).to_broadcast([128, GSZ, 128]), ALU.mult,
                           ["identb", (ak, "w", r_ // GSZ)], [dgk])
                    jg = r_ % GSZ
                    MM(P, pv0[:, :], dg[:, jg, :], uv[:, D:D + 512], r_ == 0, r_ == 127, [dgk, uvk], [(pvk, 0)])
                    MM(P, pv1[:, :], dg[:, jg, :], uv[:, D + 512:2 * D], r_ == 0, r_ == 127, [dgk, uvk], [(pvk, 1)])
                if bg is not None and s % 2 == 1:
                    next(bg, None)
            if bg is not None:
                for _ in bg:
                    pass
            x2, x2k = x2b.next()
            TT(P, "dve", x2[:, 0:512], x1[:, 0:512], pv0[:, :], ALU.add, [x1k, (pvk, 0)], [(x2k, 0)])
            TT(P, "dve", x2[:, 512:1024], x1[:, 512:1024], pv1[:, :], ALU.add, [x1k, (pvk, 1)], [(x2k, 1)])
            ss2, ss2k = ssb.next()
            ACT(P, junk[:], x2[:], AF.Square, [(x2k, 0), (x2k, 1)], [ss2k], accum=ss2[:, 0:1])
            rstd_chain(P, ss2, ss2k)
            STT(P, x2[:], x2[:], ss2[:, 3:4], gFb[:], ALU.mult, ALU.mult, [(x2k, 0), (x2k, 1), ss2k, "gFb"], [(x2k, 0), (x2k, 1)])
            DMA(P, "sp", c.out[i * 128:(i + 1) * 128, :], x2[:], [(x2k, 0), (x2k, 1)], ["out"])

        states = [dict() for _ in range(NT)]
        for _ in route(0, states[0]):
            pass
        for i in range(NT):
            bg = route(i + 1, states[i + 1]) if i + 1 < NT else None
            slots(i, states[i], bg)
        P.end_phase()
        P.alloc_sems(c.es0, c.sems)
        with nc.Block() as block:
            P.emit(block, c.sems)


def build(upto="F", debug=False):
    nc = bass.Bass("TRN2", target_bir_lowering=False)
    c = Ctx()
    c.nc = nc
    c.P = Prog(nc)
    c.sems = {}
    inp = {}

    def din(name, shape, dt=F32):
        inp[name] = nc.dram_tensor(name, list(shape), dt, kind="ExternalInput").ap()

    din("x", [T, D])
    din("mem", [256, D])
    din("mix_g", [128, 8])
    din("mem_g", [128, 8])
    din("w_in", [D, IN_DIM])
    din("b_merge", [128, 24])
    din("pe_k", [64, 32])
    din("pe_v", [64, 32])
    din("cw_k", [64, 32, 64])
    din("cw_v", [64, 32, 64])
    din("w_mem_kv", [D, 512])
    din("w_sb_br", [384, D])
    din("w_nsa_br", [384, D])
    din("w_mem_br", [256, D])
    din("w_out", [D, D])
    din("ffn_g", [D])
    din("peer_w_q", [D, 2048])
    din("subkT", [128, 16, 128])
    din("peer_uv", [16384, 2 * D])
    din("final_g", [D])
    c.inp = inp
    kind = "ExternalOutput" if debug else "Internal"

    def scr(name, shape, dt):
        return nc.dram_tensor(name, list(shape), dt, kind=kind).ap()

    c.FM = scr("FM", [FM_ROWS, T], BF16)
    c.HT = scr("HT", [D, T], BF16)
    c.TMV = scr("TMV", [T, 640], BF16)
    c.GATES = scr("GATES", [T, 18], F32)
    c.SBOT = scr("SBOT", [384, T], BF16)
    c.NSAOT = scr("NSAOT", [384, T], BF16)
    c.MEMOT = scr("MEMOT", [256, T], BF16)
    c.X1 = scr("X1", [T, D], F32)
    c.UVB = nc.dram_tensor("UVB", [16384, 2 * D], BF16, kind="Internal").ap()
    c.out = nc.dram_tensor("out", [T, D], F32, kind="ExternalOutput").ap()

    with ExitStack() as es0:
        c.es0 = es0
        c.ps = [es0.enter_context(nc.psum_tensor(f"ps{i}", [128, 512], F32)) for i in range(8)]
        c.ident = es0.enter_context(nc.sbuf_tensor("ident", [128, 128], F32))
        c.identb = es0.enter_context(nc.sbuf_tensor("identb", [128, 128], BF16))
        P = c.P
        MEMSET(P, "pool", c.ident[:], 1.0, [], ["ident"])
        ASEL(P, c.ident[:], c.ident[:], [[1, 128]], ALU.is_equal, 0.0, 0, -1, ["ident"], ["ident"])
        CP(P, "pool", c.identb[:], c.ident[:], ["ident"], ["identb"])
        phases = [("A", phase_A), ("B", phase_B), ("C", phase_C), ("D", phase_D), ("E", phase_E), ("F", phase_F)]
        for name, fn in phases:
            fn(c)
            if name == upto:
                break
    return nc


def make_inputs(inputs, b):
    f = lambda a: np.ascontiguousarray(a, dtype=np.float32)
    gcol = lambda g: f(np.asarray(g).reshape(8, 128).T)
    m = {
        "x": f(inputs["x"][b]),
        "mem": f(inputs["mem"][b]),
        "mix_g": gcol(inputs["mix_norm_g"][0]),
        "mem_g": gcol(inputs["mem_norm_g"][0]),
        "w_in": f(inputs["w_in"][0]),
        "b_merge": f(np.asarray(inputs["b_merge"][0]).reshape(24, 128).T),
        "pe_k": f(np.asarray(inputs["cmp_pe_k"][0]).T),
        "pe_v": f(np.asarray(inputs["cmp_pe_v"][0]).T),
        "cw_k": f(np.asarray(inputs["cmp_w_k"][0]).transpose(1, 0, 2)),
        "cw_v": f(np.asarray(inputs["cmp_w_v"][0]).transpose(1, 0, 2)),
        "w_mem_kv": f(inputs["w_mem_kv"][0]),
        "w_sb_br": f(inputs["w_sb_br"][0]),
        "w_nsa_br": f(inputs["w_nsa_br"][0]),
        "w_mem_br": f(inputs["w_mem_br"][0]),
        "w_out": f(inputs["w_out"][0]),
        "ffn_g": f(inputs["ffn_norm_g"][0]),
        "peer_w_q": f(inputs["peer_w_q"][0]),
        "subkT": f(np.asarray(inputs["peer_subkeys"][0]).transpose(3, 0, 1, 2).reshape(128, 16, 128)),
        "peer_uv": np.ascontiguousarray(np.concatenate([np.asarray(inputs["peer_u"][0], dtype=np.float32),
                                                        np.asarray(inputs["peer_v"][0], dtype=np.float32)], axis=1)),
        "final_g": f(inputs["final_norm_g"]),
    }
    return m


def kernel(**inputs):
    nc = build()
    shared = None
    in_maps = []
    for b in range(8):
        m = make_inputs(inputs, b)
        if shared is None:
            shared = m
        else:
            for k in m:
                if k not in ("x", "mem"):
                    m[k] = shared[k]
        in_maps.append(m)
    res = run_bass_kernel_spmd(nc, in_maps, core_ids=list(range(8)))
    return np.stack([np.asarray(r["out"]) for r in res.results], axis=0).astype(np.float32)
```
